# Optimizing a Trainium2 kernel written in Bass

```python
import jax, jax.numpy as jnp
from jax import lax
import numpy as np

D_MODEL = 1024
BATCH = 8
SEQ = 4096
DEPTH = 1

CHUNK = 64
Q_BLOCK = 128
HEAD_DIM = 64
FOX_HEADS = D_MODEL // 2 // HEAD_DIM
RWKV_HEADS = D_MODEL // 2 // HEAD_DIM
FOX_WIDTH = FOX_HEADS * HEAD_DIM
RWKV_WIDTH = RWKV_HEADS * HEAD_DIM
MIX_WIDTH = FOX_WIDTH + RWKV_WIDTH
DECAY_LORA = 64
AAA_LORA = 64
GATE_LORA = 160
D_FF = ((8 * D_MODEL // 3 + 255) // 256) * 256
NORM_EPS = 1e-6
LNX_EPS = 64e-5

FOX_COLS = 3 * FOX_WIDTH + FOX_HEADS
RWKV_COLS = 3 * RWKV_WIDTH + DECAY_LORA + AAA_LORA + GATE_LORA
W_IN_COLS = FOX_COLS + RWKV_COLS

kernel_name = "fox_rwkv7_hybrid_block"


def rmsnorm(x, g):
    x32 = x.astype(jnp.float32)
    y = x32 * lax.rsqrt(jnp.mean(x32 * x32, axis=-1, keepdims=True) + NORM_EPS)
    return y.astype(x.dtype) * g


def forgetting_attention(q, k, v, f_logit, f_bias):
    B, T, H, D = q.shape
    q = jnp.transpose(q, (0, 2, 1, 3))
    k = jnp.transpose(k, (0, 2, 1, 3))
    v = jnp.transpose(v, (0, 2, 1, 3))
    log_f = jax.nn.log_sigmoid(f_logit.astype(jnp.float32) + f_bias.astype(jnp.float32))
    c = jnp.cumsum(jnp.transpose(log_f, (0, 2, 1)), axis=-1)
    scale = D ** -0.5
    outs = []
    for i in range(T // Q_BLOCK):
        q0, q1 = i * Q_BLOCK, (i + 1) * Q_BLOCK
        s = jnp.einsum('bhqd,bhkd->bhqk', q[:, :, q0:q1], k[:, :, :q1]).astype(jnp.float32) * scale
        s = s + c[:, :, q0:q1, None] - c[:, :, None, :q1]
        mask = jnp.arange(q1)[None, :] <= (q0 + jnp.arange(Q_BLOCK))[:, None]
        s = jnp.where(mask, s, -jnp.inf)
        p = jax.nn.softmax(s, axis=-1).astype(v.dtype)
        outs.append(jnp.einsum('bhqk,bhkd->bhqd', p, v[:, :, :q1]))
    o = jnp.concatenate(outs, axis=2)
    return jnp.transpose(o, (0, 2, 1, 3)).reshape(B, T, H * D)


def rwkv7_scan(r, w, k, v, a, b):
    B, T, H, D = r.shape
    tm = lambda z: jnp.moveaxis(z.astype(jnp.float32), 1, 0)

    def step(S, inp):
        r_t, w_t, k_t, v_t, a_t, b_t = inp
        Sa = jnp.einsum('bhij,bhj->bhi', S, a_t)
        S = S * w_t[:, :, None, :] + Sa[..., None] * b_t[:, :, None, :] + v_t[..., None] * k_t[:, :, None, :]
        return S, jnp.einsum('bhij,bhj->bhi', S, r_t)

    S0 = jnp.zeros((B, H, D, D), jnp.float32)
    _, y = lax.scan(step, S0, (tm(r), tm(w), tm(k), tm(v), tm(a), tm(b)))
    return jnp.moveaxis(y, 0, 1).astype(r.dtype)


def rwkv7_time_mix(p, shift_mu, w0, w_up, a0, a_up, g_up, k_k, k_a, r_k, ln_w, ln_b):
    B, T, _ = p.shape
    H, D = RWKV_HEADS, HEAD_DIM
    p_prev = jnp.concatenate([jnp.zeros_like(p[:, :1]), p[:, :-1]], axis=1)
    p = p + shift_mu * (p_prev - p)
    r, k, v, w_lat, a_lat, g_lat = jnp.split(
        p, np.cumsum([RWKV_WIDTH, RWKV_WIDTH, RWKV_WIDTH, DECAY_LORA, AAA_LORA]).tolist(), axis=-1)
    w = -jax.nn.softplus(-(w0 + jnp.tanh(w_lat) @ w_up)) - 0.5
    decay = jnp.exp(-jnp.exp(w.astype(jnp.float32)))
    a = jax.nn.sigmoid(a0 + a_lat @ a_up)
    g = jax.nn.sigmoid(g_lat) @ g_up
    heads = lambda z: z.reshape(B, T, H, D)
    kk = heads(k * k_k).astype(jnp.float32)
    kk = kk / jnp.maximum(jnp.linalg.norm(kk, axis=-1, keepdims=True), 1e-12)
    k = k * (1.0 + (a - 1.0) * k_a)
    rh, kh, vh, ah = heads(r), heads(k), heads(v), heads(a)
    y = rwkv7_scan(rh, heads(decay), kh, vh, -kk, kk * ah.astype(jnp.float32))
    y32 = y.astype(jnp.float32)
    mu = jnp.mean(y32, axis=-1, keepdims=True)
    var = jnp.mean(jnp.square(y32 - mu), axis=-1, keepdims=True)
    y = ((y32 - mu) * lax.rsqrt(var + LNX_EPS)).astype(r.dtype).reshape(B, T, H * D) * ln_w + ln_b
    bonus = jnp.sum(rh * kh * r_k, axis=-1, keepdims=True) * vh
    return (y + bonus.reshape(B, T, H * D)) * g


def setup_inputs(seed: int = 0) -> dict:
    key = jax.random.key(seed)
    ks = jax.random.split(key, 24)
    L = DEPTH
    nrm = lambda k, shape, s: jax.random.normal(k, shape, jnp.float32) * s
    gain = lambda k, n: 1.0 + nrm(k, (L, n), 0.02)
    return {
        "x": jax.random.normal(ks[0], (BATCH, SEQ, D_MODEL), jnp.float32),
        "attn_norm_pre": gain(ks[1], D_MODEL),
        "attn_norm_post": gain(ks[2], D_MODEL),
        "w_in": nrm(ks[3], (L, D_MODEL, W_IN_COLS), D_MODEL ** -0.5),
        "fox_forget_bias": 2.0 + nrm(ks[4], (L, FOX_HEADS), 0.5),
        "shift_mu": jax.random.uniform(ks[5], (L, RWKV_COLS), jnp.float32),
        "rwkv_w0": -6.5 + 5.0 * jax.random.uniform(ks[6], (L, RWKV_WIDTH), jnp.float32),
        "rwkv_w_up": nrm(ks[7], (L, DECAY_LORA, RWKV_WIDTH), 0.1 * DECAY_LORA ** -0.5),
        "rwkv_a0": nrm(ks[8], (L, RWKV_WIDTH), 0.1),
        "rwkv_a_up": nrm(ks[9], (L, AAA_LORA, RWKV_WIDTH), AAA_LORA ** -0.5),
        "rwkv_g_up": nrm(ks[10], (L, GATE_LORA, RWKV_WIDTH), GATE_LORA ** -0.5),
        "rwkv_k_k": 0.85 + nrm(ks[11], (L, RWKV_WIDTH), 0.05),
        "rwkv_k_a": 1.0 + nrm(ks[12], (L, RWKV_WIDTH), 0.05),
        "rwkv_r_k": nrm(ks[13], (L, RWKV_HEADS, HEAD_DIM), 0.1),
        "rwkv_ln_w": 1.0 + nrm(ks[14], (L, RWKV_WIDTH), 0.02),
        "rwkv_ln_b": nrm(ks[15], (L, RWKV_WIDTH), 0.02),
        "w_out": nrm(ks[16], (L, MIX_WIDTH, D_MODEL), MIX_WIDTH ** -0.5),
        "ffn_norm_pre": gain(ks[17], D_MODEL),
        "ffn_norm_post": gain(ks[18], D_MODEL),
        "ffn_w_gate": nrm(ks[19], (L, D_MODEL, D_FF), D_MODEL ** -0.5),
        "ffn_w_up": nrm(ks[20], (L, D_MODEL, D_FF), D_MODEL ** -0.5),
        "ffn_w_down": nrm(ks[21], (L, D_FF, D_MODEL), D_FF ** -0.5),
    }


def reference(x, attn_norm_pre, attn_norm_post, w_in, fox_forget_bias, shift_mu,
              rwkv_w0, rwkv_w_up, rwkv_a0, rwkv_a_up, rwkv_g_up, rwkv_k_k, rwkv_k_a,
              rwkv_r_k, rwkv_ln_w, rwkv_ln_b, w_out, ffn_norm_pre, ffn_norm_post,
              ffn_w_gate, ffn_w_up, ffn_w_down):
    B, T, _ = x.shape
    h = x
    for l in range(DEPTH):
        u = rmsnorm(h, attn_norm_pre[l])
        proj = u @ w_in[l]
        p_fox, p_rwkv = proj[..., :FOX_COLS], proj[..., FOX_COLS:]
        fq, fk, fv, ff = jnp.split(p_fox, [FOX_WIDTH, 2 * FOX_WIDTH, 3 * FOX_WIDTH], axis=-1)
        fh = lambda z: z.reshape(B, T, FOX_HEADS, HEAD_DIM)
        o_fox = forgetting_attention(fh(fq), fh(fk), fh(fv), ff, fox_forget_bias[l])
        o_rwkv = rwkv7_time_mix(p_rwkv, shift_mu[l], rwkv_w0[l], rwkv_w_up[l], rwkv_a0[l],
                                rwkv_a_up[l], rwkv_g_up[l], rwkv_k_k[l], rwkv_k_a[l],
                                rwkv_r_k[l], rwkv_ln_w[l], rwkv_ln_b[l])
        mix = jnp.concatenate([o_fox, o_rwkv], axis=-1) @ w_out[l]
        h = h + rmsnorm(mix, attn_norm_post[l])
        z = rmsnorm(h, ffn_norm_pre[l])
        f = (jax.nn.silu(z @ ffn_w_gate[l]) * (z @ ffn_w_up[l])) @ ffn_w_down[l]
        h = h + rmsnorm(f, ffn_norm_post[l])
    return h
```

```python
import numpy as np
from contextlib import ExitStack
import concourse.bass as bass
import concourse.mybir as mybir
from concourse.bass_utils import run_bass_kernel_spmd

F32 = mybir.dt.float32
BF16 = mybir.dt.bfloat16
AF = mybir.ActivationFunctionType
ALU = mybir.AluOpType
AX = mybir.AxisListType

D = 1024
KC = 8
HD = 64
NH = 8
FOXW = 512
RW = 512
DFF = 2816
NFF = 22
WIN = 3368
RBASE = 1544
EPS = 1e-6
LNX_EPS = 64e-5
SB = 512
EPOCH = 12000


class Buf:
    __slots__ = ("w", "r")

    def __init__(self):
        self.w = None
        self.r = []


class _Rec:
    def __getattr__(self, name):
        def f(*a, **k):
            self.call = (name, a, k)
        return f


class Sched:
    ENGS = ("pe", "act", "dve", "pool", "sp")

    def __init__(self, nc, es):
        self.nc = nc
        self.es = es
        self.q = {e: [] for e in self.ENGS}
        self.cnt = {e: 0 for e in self.ENGS}
        self.run = {e: {} for e in self.ENGS}
        self.clk = {}
        self.sems = {}
        self.ndma = {"sp": 16, "act": 6, "pool": 6}
        self.dcnt = {e: 0 for e in self.ENGS}
        self.dtoks = {e: [] for e in self.ENGS}
        self.nwaits = 0

    def sem(self, key):
        s = self.sems.get(key)
        if s is None:
            s = self.es.enter_context(self.nc.semaphore("s_%s_%s" % key))
            self.sems[key] = s
        return s

    def _deps(self, eng, reads, writes, is_dma):
        deps = set()
        for b in reads:
            if b.w is not None:
                deps.add(b.w)
        for b in writes:
            if b.w is not None:
                deps.add(b.w)
            for t in b.r:
                deps.add(t)
        return deps

    def _waits(self, eng, deps):
        run = self.run[eng]
        waits = []
        for t in sorted(deps, key=lambda t: (str(t[0]), t[1])):
            key, val, isd = t
            if eng == "pe" and key[0] == "pe" and not isd:
                continue
            if run.get(key, 0) >= val:
                continue
            waits.append((key, val))
            for k2, v2 in self.clk[(key, val)].items():
                if run.get(k2, 0) < v2:
                    run[k2] = v2
        self.nwaits += len(waits)
        return waits

    def op(self, eng, fn, reads=(), writes=()):
        rec = _Rec()
        fn(rec)
        call = rec.call
        fn = lambda e, call=call: getattr(e, call[0])(*call[1], **call[2])
        deps = self._deps(eng, reads, writes, False)
        waits = self._waits(eng, deps)
        n = self.cnt[eng]
        self.cnt[eng] = n + 1
        key = (eng, n // EPOCH)
        val = n % EPOCH + 1
        tok = (key, val, False)
        c = dict(self.run[eng])
        c[key] = val
        self.clk[(key, val)] = c
        self.q[eng].append((waits, fn, key, 1))
        for b in reads:
            b.r.append(tok)
        for b in writes:
            b.w = tok
            b.r = []
        return tok

    def dma(self, eng, out, in_, reads=(), writes=(), **kw):
        deps = self._deps(eng, reads, writes, True)
        d = self.dcnt[eng]
        self.dcnt[eng] = d + 1
        nd = self.ndma[eng]
        if d >= nd:
            deps.add(self.dtoks[eng][d - nd])
        waits = self._waits(eng, deps)
        key = ("d" + eng, d % nd)
        val = 16 * (d // nd + 1)
        tok = (key, val, True)
        c = dict(self.run[eng])
        c[key] = val
        self.clk[(key, val)] = c
        self.dtoks[eng].append(tok)
        self.q[eng].append((waits, lambda e: e.dma_start(out=out, in_=in_, **kw), key, 16))
        for b in reads:
            b.r.append(tok)
        for b in writes:
            b.w = tok
            b.r = []
        return tok

    def barrier(self):
        best = {}
        for e in self.ENGS:
            n = self.cnt[e]
            if n > 0 and e != "sp":
                best[(e, (n - 1) // EPOCH)] = ((n - 1) % EPOCH + 1, False)
            for (k, v, isd) in self.dtoks[e][-self.ndma.get(e, 1):]:
                if best.get(k, (0, True))[0] < v:
                    best[k] = (v, True)
        toks = [(k, v, isd) for k, (v, isd) in best.items()]
        for e in self.ENGS:
            waits = []
            run = self.run[e]
            for (k, v, isd) in toks:
                if run.get(k, 0) < v:
                    waits.append((k, v))
                    for k2, v2 in self.clk[(k, v)].items():
                        if run.get(k2, 0) < v2:
                            run[k2] = v2
            self.q[e].append((waits, None, None, 0))

    def wait_tokens(self, eng, toks):
        waits = self._waits(eng, set(toks))
        self.q[eng].append((waits, None, None, 0))

    def emit(self):
        nc = self.nc
        for k in set(k for e in self.ENGS for (_, _, k, _) in self.q[e] if k is not None):
            self.sem(k)
        for e in self.ENGS:
            for (waits, _, _, _) in self.q[e]:
                for (k, v) in waits:
                    self.sem(k)
        with nc.Block() as block:
            def run(engname, engobj):
                for (waits, fn, key, inc) in self.q[engname]:
                    for (k, v) in waits:
                        engobj.wait_ge(self.sems[k], v)
                    if fn is not None:
                        ins = fn(engobj)
                        ins.then_inc(self.sems[key], inc)

            @block.tensor
            def _(e):
                run("pe", e)

            @block.scalar
            def _(e):
                run("act", e)

            @block.vector
            def _(e):
                run("dve", e)

            @block.gpsimd
            def _(e):
                run("pool", e)

            @block.sync
            def _(e):
                run("sp", e)


NMASK = 13
LEVELS = (2, 4, 8, 16, 32, 64)


def make_cmask():
    p = np.arange(128)[:, None]
    f = np.arange(128)[None, :]
    m = np.zeros((128, NMASK, 128), np.float32)
    m[:, 0, :] = (f > p)
    m[:, 1, :] = (f >= p)
    m[:, 2, :] = (f > p)
    m[:, 3, :] = (f >= p)
    m[:, 4, :] = ((p % 2 == 1) & (f == p - 1))
    m[:, 5, :] = ((f % 2 == 1) & (p == f - 1))
    for li, mm in enumerate(LEVELS):
        m[:, 6 + li, :] = (((p // mm) % 2 == 1) & ((f // mm) == (p // mm) - 1))
    m[:, 12, :] = ((p // 64) == (f // 64))
    return m


def build_nc(T, dbg=None, phases="FRO"):
    NSB = T // SB
    NBLK = T // 128
    nc = bass.Bass("TRN2", target_bir_lowering=False)
    es0 = ExitStack()
    S = Sched(nc, es0)

    def din(name, shape):
        return nc.dram_tensor(name, list(shape), F32, kind="ExternalInput").ap()

    x = din("x", [T, D])
    g_pre = din("attn_norm_pre", [D])
    g_post = din("attn_norm_post", [D])
    w_in = din("w_in", [D, WIN])
    din_fb = din("fox_forget_bias", [NH])
    shift_mu = din("shift_mu", [1824])
    rwkv_w0 = din("rwkv_w0", [RW])
    rwkv_w_up = din("rwkv_w_up", [64, RW])
    rwkv_a0 = din("rwkv_a0", [RW])
    rwkv_a_up = din("rwkv_a_up", [64, RW])
    rwkv_g_up = din("rwkv_g_up", [160, RW])
    rwkv_k_k = din("rwkv_k_k", [RW])
    rwkv_k_a = din("rwkv_k_a", [RW])
    rwkv_r_k = din("rwkv_r_k", [RW])
    rwkv_ln_w = din("rwkv_ln_w", [RW])
    rwkv_ln_b = din("rwkv_ln_b", [RW])
    w_out = din("w_out", [D, D])
    gf_pre = din("ffn_norm_pre", [D])
    gf_post = din("ffn_norm_post", [D])
    w_gate = din("ffn_w_gate", [D, DFF])
    w_up = din("ffn_w_up", [D, DFF])
    w_down = din("ffn_w_down", [DFF, D])
    cmask = din("cmask", [128, NMASK, 128])
    out = nc.dram_tensor("out", [T, D], F32, kind="ExternalOutput").ap()
    cat_scr = nc.dram_tensor("cat_scr", [D, T], BF16, kind="Internal").ap()
    scr_k = nc.dram_tensor("scr_k", [3, NH, T], BF16, kind="Internal").ap()
    scr_q = nc.dram_tensor("scr_q", [3, NH, T], BF16, kind="Internal").ap()
    dbg_aps = {}
    if dbg:
        for k, shp in dbg.items():
            if k == "stop":
                continue
            dbg_aps[k] = nc.dram_tensor("dbg_" + k, list(shp), F32, kind="ExternalOutput").ap()

    ucnt = [0]

    def uniq(name):
        ucnt[0] += 1
        return "%s_%d" % (name, ucnt[0])

    with es0:
        tp_ps = es0.enter_context(nc.psum_tensor("tp_ps", [128, KC, 128], BF16))
        b_tp = Buf()
        PB = [es0.enter_context(nc.psum_tensor("pb%d" % i, [128, SB], F32)) for i in range(7)]
        b_PB = [Buf() for _ in range(7)]
        ident_bf = es0.enter_context(nc.sbuf_tensor("ident_bf", [128, 128], BF16))
        ident_f = es0.enter_context(nc.sbuf_tensor("ident_f", [128, 128], F32))
        b_ident = Buf()
        S.op("pool", lambda e: e.memset(ident_f[:], 0.0), writes=[b_ident])
        S.op("pool", lambda e: e.affine_select(out=ident_f[:], in_=ident_f[:], pattern=[[-1, 128]],
                                               compare_op=ALU.not_equal, fill=1.0, base=0,
                                               channel_multiplier=1), reads=[b_ident], writes=[b_ident])
        S.op("dve", lambda e: e.tensor_copy(out=ident_bf[:], in_=ident_f[:]), reads=[b_ident], writes=[b_ident])

        def load_colvec(es, name, src, ncol):
            t = es.enter_context(nc.sbuf_tensor(uniq(name), [128, ncol], F32))
            b = Buf()
            S.dma("sp", t[:], src.rearrange("(k p) -> p k", p=128), writes=[b], allow_slow_non_contiguous=True)
            return t, b

        def make_front(es):
            def sbt(name, shape, dt=F32):
                return es.enter_context(nc.sbuf_tensor(uniq(name), list(shape), dt))
            st = {}
            st["xt"] = [sbt("xt%d" % i, [128, D], F32) for i in range(2)]
            st["b_xt"] = [Buf() for _ in range(2)]
            st["junk"] = sbt("junk", [128, D], BF16)
            st["b_junk"] = Buf()
            st["xn"] = [sbt("xn%d" % i, [128, D], BF16) for i in range(2)]
            st["b_xn"] = [Buf(), Buf()]
            st["ss"] = sbt("ss", [128, 8], F32)
            st["b_ss"] = [Buf() for _ in range(8)]
            st["uT"] = sbt("uT", [128, KC, SB], BF16)
            st["b_uT"] = Buf()
            st["tc"] = 0
            return st

        def front(st, s):
            xt, b_xt, xn, b_xn, ss, b_ss = st["xt"], st["b_xt"], st["xn"], st["b_xn"], st["ss"], st["b_ss"]
            junk, b_junk, uT, b_uT = st["junk"], st["b_junk"], st["uT"], st["b_uT"]
            for j in range(4):
                t0 = s * SB + j * 128
                xi = st["tc"] % 2
                ni = st["tc"] % 2
                si = st["tc"] % 8
                st["tc"] += 1
                S.dma("sp", xt[xi][:], x[t0:t0 + 128, :], writes=[b_xt[xi]])
                S.op("act", lambda e, xi=xi, si=si: e.activation(out=junk[:], in_=xt[xi][:], func=AF.Square,
                                                                 accum_out=ss[:, si:si + 1]),
                     reads=[b_xt[xi]], writes=[b_junk, b_ss[si]])
                S.op("dve", lambda e, si=si: e.tensor_scalar(out=ss[:, si:si + 1], in0=ss[:, si:si + 1],
                                                             scalar1=1.0 / D, scalar2=EPS, op0=ALU.mult, op1=ALU.add),
                     reads=[b_ss[si]], writes=[b_ss[si]])
                S.op("act", lambda e, si=si: e.sqrt(out=ss[:, si:si + 1], in_=ss[:, si:si + 1]),
                     reads=[b_ss[si]], writes=[b_ss[si]])
                S.op("dve", lambda e, si=si: e.reciprocal(out=ss[:, si:si + 1], in_=ss[:, si:si + 1]),
                     reads=[b_ss[si]], writes=[b_ss[si]])
                S.op("act", lambda e, xi=xi, ni=ni, si=si: e.activation(out=xn[ni][:], in_=xt[xi][:],
                                                                        func=AF.Copy, scale=ss[:, si:si + 1]),
                     reads=[b_xt[xi], b_ss[si]], writes=[b_xn[ni]])
                for kc in range(KC):
                    S.op("pe", lambda e, kc=kc, ni=ni: e.transpose(out=tp_ps[:, kc, :],
                                                                   in_=xn[ni][:, kc * 128:(kc + 1) * 128],
                                                                   identity=ident_bf[:]),
                         reads=[b_xn[ni], b_ident], writes=[b_tp])
                S.op("dve", lambda e, j=j: e.tensor_copy(out=uT[:, :, j * 128:(j + 1) * 128], in_=tp_ps[:, :, :]),
                     reads=[b_tp], writes=[b_uT])

        def load_weight_bf(st, dst, b_dst, src, c0, ncols, gvec, b_g, nk=KC):
            xt, b_xt = st["xt"], st["b_xt"]
            cnt = 0
            for kc in range(nk):
                for p0 in range(0, ncols, D):
                    n = min(D, ncols - p0)
                    i = cnt % 2
                    cnt += 1
                    S.dma("sp", xt[i][:, 0:n], src[kc * 128:(kc + 1) * 128, c0 + p0:c0 + p0 + n], writes=[b_xt[i]])
                    eng = "dve" if cnt % 2 == 0 else "pool"
                    if gvec is None:
                        S.op(eng, lambda e, kc=kc, i=i, p0=p0, n=n: e.tensor_copy(out=dst[:, kc, p0:p0 + n], in_=xt[i][:, 0:n]),
                             reads=[b_xt[i]], writes=[b_dst[kc]])
                    else:
                        S.op(eng, lambda e, kc=kc, i=i, p0=p0, n=n: e.tensor_scalar(
                            out=dst[:, kc, p0:p0 + n], in0=xt[i][:, 0:n], scalar1=gvec[:, kc:kc + 1], scalar2=None, op0=ALU.mult),
                             reads=[b_xt[i], b_g], writes=[b_dst[kc]])

        pjc = [0]

        def proj_fm(wt, b_w, st, c0, ncols, evac):
            pi = pjc[0] % 2
            pjc[0] += 1
            uT, b_uT = st["uT"], st["b_uT"]
            for kc in range(KC):
                S.op("pe", lambda e, kc=kc: e.matmul(out=PB[pi][0:ncols, :], lhsT=wt[:, kc, c0:c0 + ncols],
                                                     rhs=uT[:, kc, :], start=(kc == 0), stop=(kc == KC - 1)),
                     reads=[b_w[kc], b_uT], writes=[b_PB[pi]])
            evac(PB[pi], b_PB[pi])

        if "F" in phases:
            with ExitStack() as es:
                def sb(name, shape, dt=F32):
                    return es.enter_context(nc.sbuf_tensor(uniq(name), list(shape), dt))
                st = make_front(es)
                uT, b_uT = st["uT"], st["b_uT"]
                gT, b_gT = load_colvec(es, "gT", g_pre, KC)
                w_bf = sb("w_bf", [128, KC, RBASE], BF16)
                b_w = [Buf() for _ in range(KC)]
                load_weight_bf(st, w_bf, b_w, w_in, 0, RBASE, gT, b_gT)

                nfb = sb("nfb", [8, 1], F32)
                b_nfb = Buf()
                S.dma("sp", nfb[:], din_fb.rearrange("(h o) -> h o", o=1), writes=[b_nfb])
                S.op("dve", lambda e: e.tensor_scalar(out=nfb[:], in0=nfb[:], scalar1=-1.0, scalar2=None, op0=ALU.mult),
                     reads=[b_nfb], writes=[b_nfb])
                ones8 = sb("ones8", [8, SB], F32)
                b_ones8 = Buf()
                S.op("pool", lambda e: e.memset(ones8[:], 1.0), writes=[b_ones8])
                ones_f = sb("ones_f", [128, 64], F32)
                b_onesf = Buf()
                S.op("pool", lambda e: e.memset(ones_f[:], 1.0), writes=[b_onesf])
                maskneg_f = sb("maskneg_f", [128, 128], F32)
                maskneg = sb("maskneg", [128, 128], BF16)
                b_mask = Buf()
                S.op("pool", lambda e: e.memset(maskneg_f[:], 0.0), writes=[b_mask])
                S.op("pool", lambda e: e.affine_select(out=maskneg_f[:], in_=maskneg_f[:], pattern=[[1, 128]],
                                                       compare_op=ALU.is_ge, fill=-30000.0, base=0,
                                                       channel_multiplier=-1), reads=[b_mask], writes=[b_mask])
                S.op("dve", lambda e: e.tensor_copy(out=maskneg[:], in_=maskneg_f[:]), reads=[b_mask], writes=[b_mask])

                KT = sb("KT", [70, NH, T], BF16)
                b_KT = [Buf() for _ in range(NSB)]
                QT = sb("QT", [70, NH, SB], BF16)
                b_QT = Buf()
                VT = sb("VT", [128, NBLK, NH, 66], BF16)
                b_VT = [Buf() for _ in range(NSB)]
                S.op("pool", lambda e: e.memset(KT[64:70, :, :], 1.0), writes=b_KT)
                S.op("pool", lambda e: e.memset(QT[64:70, :, :], 1.0), writes=[b_QT])
                S.op("pool", lambda e: e.memset(VT[:, :, :, 64:66], 1.0), writes=b_VT)
                b_scrk = Buf()
                b_scrq = Buf()
                cneg = [sb("cneg%d" % i, [8, SB], F32) for i in range(2)]
                b_cneg = [Buf(), Buf()]
                fl = sb("fl", [8, SB], F32)
                b_fl = Buf()
                res1 = sb("res1", [8, SB], F32)
                b_res1 = Buf()
                ksp = sb("ksp", [8, 3, SB], BF16)
                qsp = sb("qsp", [8, 3, SB], BF16)
                b_ksp = Buf()
                b_qsp = Buf()
                catF = [sb("catF%d" % i, [64, SB], BF16) for i in range(2)]
                b_catF = [Buf(), Buf()]
                PT = [sb("PT%d" % i, [128, SB], BF16) for i in range(3)]
                b_PT = [Buf() for _ in range(3)]
                rs = sb("rs", [66, SB], F32)
                b_rs = Buf()
                bc_sb = sb("bc_sb", [64, SB], F32)
                b_bc = Buf()
                dbg_sb = sb("dbg_sb", [128, SB], F32)
                b_dbg = Buf()
                st_ps = [PB[2], PB[3]]
                b_st = [b_PB[2], b_PB[3]]
                o_ps, b_o = PB[4], b_PB[4]
                bc_ps, b_bcps = PB[5], b_PB[5]

                for s in range(NSB):
                    c_lo, c_hi = s * SB, (s + 1) * SB
                    front(st, s)
                    ci = s % 2

                    def ev_ff(pp, bp):
                        S.op("act", lambda e: e.activation(out=fl[:], in_=pp[0:8, :], func=AF.Exp, bias=nfb[:, 0:1], scale=-1.0),
                             reads=[bp, b_nfb], writes=[b_fl])
                        S.op("act", lambda e: e.activation(out=fl[:], in_=fl[:], func=AF.Ln, bias=1.0, scale=1.0),
                             reads=[b_fl], writes=[b_fl])
                        S.op("dve", lambda e: e.tensor_tensor_scan(out=cneg[ci][:], data0=ones8[:], data1=fl[:], initial=0.0,
                                                                   op0=ALU.mult, op1=ALU.add),
                             reads=[b_fl, b_ones8], writes=[b_cneg[ci]])
                        if s > 0:
                            S.op("dve", lambda e: e.tensor_scalar(out=cneg[ci][:], in0=cneg[ci][:],
                                                                  scalar1=cneg[1 - ci][:, SB - 1:SB], scalar2=None, op0=ALU.add),
                                 reads=[b_cneg[ci], b_cneg[1 - ci]], writes=[b_cneg[ci]])
                        S.op("dve", lambda e: e.tensor_copy(out=ksp[:, 0, :], in_=cneg[ci][:]), reads=[b_cneg[ci]], writes=[b_ksp])
                        S.op("dve", lambda e: e.tensor_tensor(out=res1[:], in0=cneg[ci][:], in1=ksp[:, 0, :], op=ALU.subtract),
                             reads=[b_cneg[ci], b_ksp], writes=[b_res1])
                        S.op("dve", lambda e: e.tensor_copy(out=ksp[:, 1, :], in_=res1[:]), reads=[b_res1], writes=[b_ksp])
                        S.op("dve", lambda e: e.tensor_tensor(out=res1[:], in0=res1[:], in1=ksp[:, 1, :], op=ALU.subtract),
                             reads=[b_res1, b_ksp], writes=[b_res1])
                        S.op("dve", lambda e: e.tensor_copy(out=ksp[:, 2, :], in_=res1[:]), reads=[b_res1], writes=[b_ksp])
                        S.op("dve", lambda e: e.tensor_scalar(out=qsp[:], in0=ksp[:], scalar1=-1.0, scalar2=None, op0=ALU.mult),
                             reads=[b_ksp], writes=[b_qsp])
                        S.dma("sp", scr_k[:, :, c_lo:c_hi].rearrange("r h t -> h r t"), ksp[:], reads=[b_ksp], writes=[b_scrk])
                        S.dma("sp", scr_q[:, :, c_lo:c_hi].rearrange("r h t -> h r t"), qsp[:], reads=[b_qsp], writes=[b_scrq])
                        S.dma("sp", KT[67:70, :, c_lo:c_hi], scr_k[:, :, c_lo:c_hi], reads=[b_scrk], writes=[b_KT[s]])
                        S.dma("sp", QT[64:67, :, :], scr_q[:, :, c_lo:c_hi], reads=[b_scrq], writes=[b_QT])

                    proj_fm(w_bf, b_w, st, 1536, 8, ev_ff)
                    for h in range(NH):
                        def ev_q(pp, bp, h=h):
                            S.op("act", lambda e: e.mul(out=QT[0:64, h, :], in_=pp[0:64, :], mul=0.125), reads=[bp], writes=[b_QT])
                        proj_fm(w_bf, b_w, st, h * 64, 64, ev_q)

                        def ev_k(pp, bp, h=h):
                            S.op("dve", lambda e: e.tensor_copy(out=KT[0:64, h, c_lo:c_hi], in_=pp[0:64, :]), reads=[bp], writes=[b_KT[s]])
                        proj_fm(w_bf, b_w, st, 512 + h * 64, 64, ev_k)
                    for j in range(4):
                        pi = pjc[0] % 2
                        pjc[0] += 1
                        for kc in range(KC):
                            S.op("pe", lambda e, kc=kc, j=j, pi=pi: e.matmul(out=PB[pi][:], lhsT=uT[:, kc, j * 128:(j + 1) * 128],
                                                                             rhs=w_bf[:, kc, 1024:1536], start=(kc == 0), stop=(kc == KC - 1)),
                                 reads=[b_w[kc], b_uT], writes=[b_PB[pi]])
                        S.op("act", lambda e, j=j, pi=pi: e.copy(out=VT[:, s * 4 + j, :, 0:64],
                                                                 in_=PB[pi][:].rearrange("p (h d) -> p h d", h=NH)),
                             reads=[b_PB[pi]], writes=[b_VT[s]])

                    stc = 0
                    for h in range(NH):
                        nkb = 4 * (s + 1)
                        for kb in range(nkb):
                            d = kb - 4 * s
                            q0 = 0 if d < 0 else d * 128
                            si_ = stc % 2
                            pi_ = stc % 3
                            stc += 1
                            ksb = kb // 4
                            diag = d >= 0
                            S.op("pe", lambda e, h=h, kb=kb, q0=q0, si_=si_, diag=diag: e.matmul(
                                out=st_ps[si_][:, q0:SB], lhsT=KT[0:70, h, kb * 128:(kb + 1) * 128], rhs=QT[0:70, h, q0:SB],
                                start=True, stop=(not diag)),
                                reads=[b_KT[ksb], b_QT], writes=[b_st[si_]])
                            if diag:
                                S.op("pe", lambda e, q0=q0, si_=si_: e.matmul(
                                    out=st_ps[si_][:, q0:q0 + 128], lhsT=ident_bf[:], rhs=maskneg[:], start=False, stop=True),
                                    reads=[b_ident, b_mask], writes=[b_st[si_]])
                            S.op("act", lambda e, q0=q0, si_=si_, pi_=pi_: e.activation(out=PT[pi_][:, q0:SB], in_=st_ps[si_][:, q0:SB],
                                                                                         func=AF.Exp),
                                 reads=[b_st[si_]], writes=[b_PT[pi_]])
                            S.op("pe", lambda e, h=h, kb=kb, q0=q0, pi_=pi_, nkb=nkb: e.matmul(
                                out=o_ps[0:66, q0:SB], lhsT=VT[:, kb, h, :], rhs=PT[pi_][:, q0:SB],
                                start=(kb == 0), stop=(kb == nkb - 1)),
                                reads=[b_VT[ksb], b_PT[pi_]], writes=[b_o])
                        S.op("dve", lambda e: e.reciprocal(out=rs[64:66, :], in_=o_ps[64:66, :]), reads=[b_o], writes=[b_rs])
                        S.op("pe", lambda e: e.matmul(out=bc_ps[0:64, :], lhsT=ones_f[64:65, 0:64], rhs=rs[64:65, :], start=True, stop=True),
                             reads=[b_rs, b_onesf], writes=[b_bcps])
                        S.op("act", lambda e: e.copy(out=bc_sb[:], in_=bc_ps[0:64, :]), reads=[b_bcps], writes=[b_bc])
                        fi = h % 2
                        S.op("dve", lambda e, fi=fi: e.tensor_tensor(out=catF[fi][:], in0=o_ps[0:64, :], in1=bc_sb[:], op=ALU.mult),
                             reads=[b_o, b_bc], writes=[b_catF[fi]])
                        S.dma("sp", cat_scr[h * 64:(h + 1) * 64, c_lo:c_hi], catF[fi][:], reads=[b_catF[fi]])
                        if dbg and "ofox" in dbg:
                            S.op("act", lambda e, fi=fi: e.copy(out=dbg_sb[0:64, :], in_=catF[fi][:]), reads=[b_catF[fi]], writes=[b_dbg])
                            S.dma("sp", dbg_aps["ofox"][h * 64:(h + 1) * 64, c_lo:c_hi], dbg_sb[0:64, :], reads=[b_dbg])
                S.barrier()
        if "R" in phases:
            with ExitStack() as es:
                def sb(name, shape, dt=F32):
                    return es.enter_context(nc.sbuf_tensor(uniq(name), list(shape), dt))
                st = make_front(es)
                uT, b_uT = st["uT"], st["b_uT"]
                gT, b_gT = load_colvec(es, "gTr", g_pre, KC)
                NRC = 1824
                wr_bf = sb("wr_bf", [128, KC, NRC], BF16)
                b_wr = [Buf() for _ in range(KC)]
                load_weight_bf(st, wr_bf, b_wr, w_in, RBASE, NRC, gT, b_gT)
                lo_bf = sb("lo_bf", [128, RW], BF16)
                b_lo = Buf()
                gup_bf = sb("gup_bf", [128, RW], BF16)
                gup1_bf = sb("gup1_bf", [32, RW], BF16)
                b_gup = Buf()
                xt, b_xt = st["xt"], st["b_xt"]
                S.dma("sp", xt[0][0:64, 0:RW], rwkv_w_up[:, :], writes=[b_xt[0]])
                S.dma("sp", xt[0][64:128, 0:RW], rwkv_a_up[:, :], writes=[b_xt[0]])
                S.op("dve", lambda e: e.tensor_copy(out=lo_bf[:], in_=xt[0][:, 0:RW]), reads=[b_xt[0]], writes=[b_lo])
                S.dma("sp", xt[1][:, 0:RW], rwkv_g_up[0:128, :], writes=[b_xt[1]])
                S.op("dve", lambda e: e.tensor_copy(out=gup_bf[:], in_=xt[1][:, 0:RW]), reads=[b_xt[1]], writes=[b_gup])
                S.dma("sp", xt[0][0:32, 0:RW], rwkv_g_up[128:160, :], reads=[b_lo], writes=[b_xt[0]])
                S.op("dve", lambda e: e.tensor_copy(out=gup1_bf[:], in_=xt[0][0:32, 0:RW]), reads=[b_xt[0]], writes=[b_gup])
                w0T, b_w0 = load_colvec(es, "w0T", rwkv_w0, 4)
                a0T, b_a0 = load_colvec(es, "a0T", rwkv_a0, 4)
                kkT, b_kkv = load_colvec(es, "kkT", rwkv_k_k, 4)
                kaT, b_kav = load_colvec(es, "kaT", rwkv_k_a, 4)
                rkT, b_rkv = load_colvec(es, "rkT", rwkv_r_k, 4)
                lnwT, b_lnw = load_colvec(es, "lnwT", rwkv_ln_w, 4)
                lnbT, b_lnb = load_colvec(es, "lnbT", rwkv_ln_b, 4)
                groups = {}
                gl = []
                for hp in range(4):
                    gl.append((("r", hp), hp * 128, 128))
                    gl.append((("k", hp), 512 + hp * 128, 128))
                    gl.append((("v", hp), 1024 + hp * 128, 128))
                gl.append(("wa", 1536, 128))
                gl.append(("g0", 1664, 128))
                gl.append(("g1", 1792, 32))
                muT = sb("muT", [128, len(gl)], F32)
                b_mu = Buf()
                carry = sb("carry", [128, len(gl)], F32)
                b_carry = [Buf() for _ in gl]
                S.op("pool", lambda e: e.memset(carry[:], 0.0), writes=b_carry)
                for gi, (nm, c0, n) in enumerate(gl):
                    groups[nm] = (gi, c0, n)
                    S.dma("sp", muT[0:n, gi:gi + 1], shift_mu[c0:c0 + n].rearrange("(p o) -> p o", o=1), writes=[b_mu])
                cm = sb("cm", [128, NMASK, 128], F32)
                b_cm = Buf()
                S.dma("sp", cm[:], cmask[:, :, :], writes=[b_cm])
                mk0T_bf = sb("mk0T_bf", [128, 128], BF16)
                S.op("dve", lambda e: e.tensor_copy(out=mk0T_bf[:], in_=cm[:, 5, :]), reads=[b_cm], writes=[b_cm])
                bd_mean = sb("bd_mean", [128, 128], F32)
                S.op("dve", lambda e: e.tensor_scalar(out=bd_mean[:], in0=cm[:, 12, :], scalar1=1.0 / 64, scalar2=None, op0=ALU.mult),
                     reads=[b_cm], writes=[b_cm])
                ones128 = sb("ones128", [128, 128], F32)
                S.op("pool", lambda e: e.memset(ones128[:], 1.0), writes=[b_cm])

                H32 = [sb("H32_%d" % i, [128, 128], F32) for i in range(4)]
                Hbf = [sb("Hbf_%d" % i, [128, 128], BF16) for i in range(4)]
                b_H32 = [Buf() for _ in range(4)]
                b_Hbf = [Buf() for _ in range(4)]
                for i in range(4):
                    S.op("pool", lambda e, i=i: e.memset(H32[i][:], 0.0), writes=[b_H32[i]])
                    S.op("pool", lambda e, i=i: e.memset(Hbf[i][:], 0.0), writes=[b_Hbf[i]])

                def wt(name, dt=F32, n=SB):
                    return sb(name, [128, n], dt), Buf()
                raw = [sb("raw%d" % i, [128, SB + 1], F32) for i in range(2)]
                b_raw = [Buf(), Buf()]
                rawc = [0]
                dlt, b_dlt = wt("dlt")
                wa_s, b_was = wt("wa_s")
                g0_s, b_g0s = wt("g0_s")
                g1_s, b_g1s = wt("g1_s")
                lat_bf, b_lat = wt("lat_bf", BF16)
                sg_bf, b_sg = wt("sg_bf", BF16)
                sg1_bf, b_sg1 = wt("sg1_bf", BF16)
                r_s, b_rs_ = wt("r_s")
                k_s, b_ks = wt("k_s")
                v_s, b_vs = wt("v_s")
                lw, b_lw = wt("lw")
                a_t, b_a = wt("a_t")
                g_t, b_g = wt("g_t")
                kq, b_kq = wt("kq")
                tmpA, b_tmpA = wt("tmpA")
                kk, b_kk = wt("kk")
                kmod, b_kmod = wt("kmod")
                bb, b_bb = wt("bb")
                bonus, b_bonus = wt("bonus")
                cum, b_cum = wt("cum")
                E_in, b_Ein = wt("E_in")
                E_neg, b_Eneg = wt("E_neg")
                E_ex, b_Eex = wt("E_ex")
                E_end, b_Eend = wt("E_end")
                y_sb, b_y = wt("y_sb")
                AR = sb("AR", [128, 4, 2, 128], BF16)
                b_AR = Buf()
                BT, b_BT = wt("BT", BF16)
                KTt, b_KTt = wt("KTt", BF16)
                BH, b_BH = wt("BH", BF16)
                KH, b_KH = wt("KH", BF16)
                Vb, b_Vb = wt("Vb", BF16)
                catR, b_catR = wt("catR", BF16)
                TM = [sb("TM%d" % i, [128, 4, 128], BF16) for i in range(4)]
                b_TM = [Buf() for _ in range(4)]
                NMt = [sb("NM%d" % i, [128, 4, 128], BF16) for i in range(4)]
                b_NM = [Buf() for _ in range(4)]
                Dm = [sb("Dm%d" % i, [128, 128], BF16) for i in range(4)]
                b_Dm = [Buf() for _ in range(4)]
                DTm = [sb("DTm%d" % i, [128, 128], BF16) for i in range(4)]
                b_DTm = [Buf() for _ in range(4)]
                Gm = [sb("Gm%d" % i, [128, 128], BF16) for i in range(4)]
                b_Gm = [Buf() for _ in range(4)]
                X2b = [sb("X2b%d" % i, [128, 64], BF16) for i in range(4)]
                b_X2b = [Buf() for _ in range(4)]
                U2s = [sb("U2s%d" % i, [128, 128], F32) for i in range(2)]
                b_U2s = [Buf(), Buf()]
                WTb = [sb("WTb%d" % i, [128, 128], BF16) for i in range(2)]
                b_WTb = [Buf(), Buf()]
                Ub = sb("Ub", [128, 128], BF16)
                b_Ub = Buf()
                dbg_sb = sb("dbg_sbr", [128, SB], F32)
                b_dbg = Buf()
                M1 = [PB[2], PB[3]]
                b_M1 = [b_PB[2], b_PB[3]]
                sA = [PB[4][:, i * 128:(i + 1) * 128] for i in range(4)]
                b_sA = [b_PB[4]] * 4
                sB = [PB[5][:, i * 128:(i + 1) * 128] for i in range(4)]
                b_sB = [b_PB[5]] * 4
                u2_ps, uh_ps = [PB[6][:, i * 128:(i + 1) * 128] for i in range(2)]
                y2_ps = [PB[6][:, (2 + i) * 128:(3 + i) * 128] for i in range(2)]
                b_u2 = b_wt = b_uh = b_yps = b_PB[6]

                def shifted(nm, dst, b_dst):
                    gi, c0, n = groups[nm]

                    def ev(pp, bp):
                        ri = rawc[0] % 2
                        rawc[0] += 1
                        rw_, brw = raw[ri], b_raw[ri]
                        S.op("act", lambda e: e.copy(out=rw_[0:n, 1:SB + 1], in_=pp[0:n, :]), reads=[bp], writes=[brw])
                        S.op("pool", lambda e: e.tensor_copy(out=rw_[0:n, 0:1], in_=carry[0:n, gi:gi + 1]),
                             reads=[b_carry[gi]], writes=[brw])
                        S.op("pool", lambda e: e.tensor_copy(out=carry[0:n, gi:gi + 1], in_=rw_[0:n, SB:SB + 1]),
                             reads=[brw], writes=[b_carry[gi]])
                        S.op("dve", lambda e: e.tensor_tensor(out=dlt[0:n, :], in0=rw_[0:n, 0:SB], in1=rw_[0:n, 1:SB + 1], op=ALU.subtract),
                             reads=[brw], writes=[b_dlt])
                        S.op("dve", lambda e: e.scalar_tensor_tensor(out=dst[0:n, :], in0=dlt[0:n, :], scalar=muT[0:n, gi:gi + 1],
                                                                     in1=rw_[0:n, 1:SB + 1], op0=ALU.mult, op1=ALU.add),
                             reads=[b_dlt, brw, b_mu], writes=[b_dst])
                    proj_fm(wr_bf, b_wr, st, c0, n, ev)

                def v4(ap):
                    return ap.rearrange("p (c t) -> p c t", c=4)

                for s in range(NSB):
                    c_lo, c_hi = s * SB, (s + 1) * SB
                    front(st, s)
                    shifted("wa", wa_s, b_was)
                    shifted("g0", g0_s, b_g0s)
                    shifted("g1", g1_s, b_g1s)
                    S.op("act", lambda e: e.activation(out=lat_bf[0:64, :], in_=wa_s[0:64, :], func=AF.Tanh), reads=[b_was], writes=[b_lat])
                    S.op("act", lambda e: e.copy(out=lat_bf[64:128, :], in_=wa_s[64:128, :]), reads=[b_was], writes=[b_lat])
                    S.op("act", lambda e: e.activation(out=sg_bf[:], in_=g0_s[:], func=AF.Sigmoid), reads=[b_g0s], writes=[b_sg])
                    S.op("act", lambda e: e.activation(out=sg1_bf[0:32, :], in_=g1_s[0:32, :], func=AF.Sigmoid), reads=[b_g1s], writes=[b_sg1])
                    for hp in range(4):
                        hc = slice(hp * 128, (hp + 1) * 128)
                        shifted(("r", hp), r_s, b_rs_)
                        shifted(("k", hp), k_s, b_ks)
                        shifted(("v", hp), v_s, b_vs)
                        p0, bp0 = PB[0], b_PB[0]
                        S.op("pe", lambda e: e.matmul(out=p0[:], lhsT=lo_bf[0:64, hc], rhs=lat_bf[0:64, :], start=True, stop=True),
                             reads=[b_lo, b_lat], writes=[bp0])
                        S.op("act", lambda e: e.activation(out=lw[:], in_=p0[:], func=AF.Sigmoid, bias=w0T[:, hp:hp + 1]),
                             reads=[bp0, b_w0], writes=[b_lw])
                        S.op("pool", lambda e: e.tensor_scalar(out=lw[:], in0=lw[:], scalar1=-0.6065306597126334, scalar2=None, op0=ALU.mult),
                             reads=[b_lw], writes=[b_lw])
                        p1, bp1 = PB[1], b_PB[1]
                        S.op("pe", lambda e: e.matmul(out=p1[:], lhsT=lo_bf[64:128, hc], rhs=lat_bf[64:128, :], start=True, stop=True),
                             reads=[b_lo, b_lat], writes=[bp1])
                        S.op("act", lambda e: e.activation(out=a_t[:], in_=p1[:], func=AF.Sigmoid, bias=a0T[:, hp:hp + 1]),
                             reads=[bp1, b_a0], writes=[b_a])
                        S.op("pe", lambda e: e.matmul(out=p0[:], lhsT=gup_bf[:, hc], rhs=sg_bf[:], start=True, stop=False),
                             reads=[b_gup, b_sg], writes=[bp0])
                        S.op("pe", lambda e: e.matmul(out=p0[:], lhsT=gup1_bf[0:32, hc], rhs=sg1_bf[0:32, :], start=False, stop=True),
                             reads=[b_gup, b_sg1], writes=[bp0])
                        S.op("act", lambda e: e.copy(out=g_t[:], in_=p0[:]), reads=[bp0], writes=[b_g])
                        S.op("dve", lambda e: e.tensor_scalar(out=kq[:], in0=k_s[:], scalar1=kkT[:, hp:hp + 1], scalar2=None, op0=ALU.mult),
                             reads=[b_ks, b_kkv], writes=[b_kq])
                        S.op("pool", lambda e: e.tensor_tensor(out=tmpA[:], in0=kq[:], in1=kq[:], op=ALU.mult), reads=[b_kq], writes=[b_tmpA])
                        S.op("pe", lambda e: e.matmul(out=p1[:], lhsT=cm[:, 12, :], rhs=tmpA[:], start=True, stop=True),
                             reads=[b_cm, b_tmpA], writes=[bp1])
                        S.op("act", lambda e: e.sqrt(out=tmpA[:], in_=p1[:]), reads=[bp1], writes=[b_tmpA])
                        S.op("dve", lambda e: e.tensor_scalar(out=tmpA[:], in0=tmpA[:], scalar1=1e-12, scalar2=None, op0=ALU.max),
                             reads=[b_tmpA], writes=[b_tmpA])
                        S.op("dve", lambda e: e.reciprocal(out=tmpA[:], in_=tmpA[:]), reads=[b_tmpA], writes=[b_tmpA])
                        S.op("pool", lambda e: e.tensor_tensor(out=kk[:], in0=kq[:], in1=tmpA[:], op=ALU.mult),
                             reads=[b_kq, b_tmpA], writes=[b_kk])
                        S.op("dve", lambda e: e.tensor_scalar(out=kmod[:], in0=a_t[:], scalar1=-1.0, scalar2=kaT[:, hp:hp + 1],
                                                              op0=ALU.add, op1=ALU.mult), reads=[b_a, b_kav], writes=[b_kmod])
                        S.op("dve", lambda e: e.scalar_tensor_tensor(out=kmod[:], in0=kmod[:], scalar=1.0, in1=k_s[:],
                                                                     op0=ALU.add, op1=ALU.mult), reads=[b_kmod, b_ks], writes=[b_kmod])
                        S.op("pool", lambda e: e.tensor_tensor(out=bb[:], in0=kk[:], in1=a_t[:], op=ALU.mult), reads=[b_kk, b_a], writes=[b_bb])
                        S.op("dve", lambda e: e.scalar_tensor_tensor(out=tmpA[:], in0=r_s[:], scalar=rkT[:, hp:hp + 1], in1=kmod[:],
                                                                     op0=ALU.mult, op1=ALU.mult), reads=[b_rs_, b_rkv, b_kmod], writes=[b_tmpA])
                        S.op("pe", lambda e: e.matmul(out=p1[:], lhsT=cm[:, 12, :], rhs=tmpA[:], start=True, stop=True),
                             reads=[b_cm, b_tmpA], writes=[bp1])
                        S.op("dve", lambda e: e.tensor_tensor(out=bonus[:], in0=p1[:], in1=v_s[:], op=ALU.mult), reads=[bp1, b_vs], writes=[b_bonus])
                        for c in range(4):
                            cc = slice(c * 128, (c + 1) * 128)
                            S.op("dve", lambda e, cc=cc: e.tensor_tensor_scan(out=cum[:, cc], data0=ones128[:], data1=lw[:, cc], initial=0.0,
                                                                              op0=ALU.mult, op1=ALU.add), reads=[b_lw, b_cm], writes=[b_cum])
                        S.op("act", lambda e: e.activation(out=E_in[:], in_=cum[:], func=AF.Exp), reads=[b_cum], writes=[b_Ein])
                        S.op("act", lambda e: e.activation(out=E_neg[:], in_=cum[:], func=AF.Exp, scale=-1.0), reads=[b_cum], writes=[b_Eneg])
                        for c in range(4):
                            cc = slice(c * 128, (c + 1) * 128)
                            S.op("act", lambda e, cc=cc, c=c: e.activation(out=E_end[:, cc], in_=cum[:, cc], func=AF.Exp, scale=-1.0,
                                                                           bias=cum[:, c * 128 + 127:c * 128 + 128]),
                                 reads=[b_cum], writes=[b_Eend])
                        S.op("pool", lambda e: e.tensor_tensor(out=tmpA[:], in0=cum[:], in1=lw[:], op=ALU.subtract), reads=[b_cum, b_lw], writes=[b_tmpA])
                        S.op("act", lambda e: e.activation(out=E_ex[:], in_=tmpA[:], func=AF.Exp), reads=[b_tmpA], writes=[b_Eex])
                        S.op("dve", lambda e: e.scalar_tensor_tensor(out=AR[:, :, 0, :], in0=v4(kk[:]), scalar=-1.0, in1=v4(E_ex[:]),
                                                                     op0=ALU.mult, op1=ALU.mult), reads=[b_kk, b_Eex], writes=[b_AR])
                        S.op("pool", lambda e: e.tensor_tensor(out=AR[:, :, 1, :], in0=v4(r_s[:]), in1=v4(E_in[:]), op=ALU.mult),
                             reads=[b_rs_, b_Ein], writes=[b_AR])
                        S.op("dve", lambda e: e.tensor_tensor(out=BT[:], in0=bb[:], in1=E_neg[:], op=ALU.mult), reads=[b_bb, b_Eneg], writes=[b_BT])
                        S.op("pool", lambda e: e.tensor_tensor(out=KTt[:], in0=kmod[:], in1=E_neg[:], op=ALU.mult), reads=[b_kmod, b_Eneg], writes=[b_KTt])
                        S.op("dve", lambda e: e.tensor_tensor(out=BH[:], in0=bb[:], in1=E_end[:], op=ALU.mult), reads=[b_bb, b_Eend], writes=[b_BH])
                        S.op("pool", lambda e: e.tensor_tensor(out=KH[:], in0=kmod[:], in1=E_end[:], op=ALU.mult), reads=[b_kmod, b_Eend], writes=[b_KH])
                        S.op("act", lambda e: e.copy(out=Vb[:], in_=v_s[:]), reads=[b_vs], writes=[b_Vb])

                        stop = (dbg or {}).get("stop", "")
                        if stop == "A":
                            continue
                        for cp in range(2):
                            chains = [(2 * cp + ci_, e_) for ci_ in range(2) for e_ in range(2)]
                            for ci_ in range(2):
                                c = 2 * cp + ci_
                                cc = slice(c * 128, (c + 1) * 128)
                                srcs = [(AR[:, c, 0, :], b_AR), (Vb[:, cc], b_Vb), (BH[:, cc], b_BH), (KH[:, cc], b_KH)]
                                for k_, (src, bsrc) in enumerate(srcs):
                                    S.op("pe", lambda e, k_=k_, src=src: e.transpose(out=tp_ps[:, k_, :], in_=src, identity=ident_bf[:]),
                                         reads=[bsrc, b_ident], writes=[b_tp])
                                S.op("act", lambda e, c=c: e.copy(out=TM[c][:], in_=tp_ps[:, 0:4, :]), reads=[b_tp], writes=[b_TM[c]])
                            for ch, (c, e_) in enumerate(chains):
                                pr = slice(e_ * 64, (e_ + 1) * 64)
                                cc = slice(c * 128, (c + 1) * 128)
                                m1, bm1 = M1[ch % 2], b_M1[ch % 2]
                                S.op("pe", lambda e, pr=pr, cc=cc, c=c, m1=m1: e.matmul(out=m1[:, 0:256], lhsT=BT[pr, cc],
                                                                                     rhs=AR[pr, c, :, :], start=True, stop=True),
                                     reads=[b_BT, b_AR], writes=[bm1])
                                S.op("pe", lambda e, pr=pr, cc=cc, c=c, m1=m1: e.matmul(out=m1[:, 256:512], lhsT=KTt[pr, cc],
                                                                                     rhs=AR[pr, c, :, :], start=True, stop=True),
                                     reads=[b_KTt, b_AR], writes=[bm1])
                                S.op("dve", lambda e, ch=ch, m1=m1: e.tensor_tensor(
                                    out=NMt[ch][:], in0=m1[:].rearrange("p (a t) -> p a t", a=4),
                                    in1=cm[:, 0:4, :],
                                    op=ALU.mult), reads=[bm1, b_cm], writes=[b_NM[ch]])
                                S.op("pe", lambda e, pr=pr, cc=cc, c=c, ch=ch: e.matmul(out=sA[ch], lhsT=AR[pr, c, 0, :], rhs=BT[pr, cc],
                                                                                     start=True, stop=True),
                                     reads=[b_AR, b_BT], writes=[b_sA[ch]])
                                S.op("dve", lambda e, ch=ch: e.tensor_tensor(out=Dm[ch][:], in0=sA[ch], in1=cm[:, 4, :], op=ALU.mult),
                                     reads=[b_sA[ch], b_cm], writes=[b_Dm[ch]])
                                S.op("pool", lambda e, ch=ch: e.tensor_tensor(out=Dm[ch][:], in0=Dm[ch][:], in1=ident_bf[:], op=ALU.add),
                                     reads=[b_Dm[ch], b_ident], writes=[b_Dm[ch]])
                                S.op("pool", lambda e, ch=ch: e.tensor_tensor(out=DTm[ch][:], in0=NMt[ch][:, 0, :], in1=mk0T_bf[:], op=ALU.mult),
                                     reads=[b_NM[ch], b_cm], writes=[b_DTm[ch]])
                                S.op("pool", lambda e, ch=ch: e.tensor_tensor(out=DTm[ch][:], in0=DTm[ch][:], in1=ident_bf[:], op=ALU.add),
                                     reads=[b_DTm[ch], b_ident], writes=[b_DTm[ch]])
                            if stop == "B":
                                continue
                            for li, mm in enumerate(LEVELS):
                                last = (li == len(LEVELS) - 1)
                                for ch in range(4):
                                    S.op("pe", lambda e, ch=ch: e.matmul(out=sA[ch], lhsT=NMt[ch][:, 0, :], rhs=Dm[ch][:], start=True, stop=True),
                                         reads=[b_NM[ch], b_Dm[ch]], writes=[b_sA[ch]])
                                    S.op("dve", lambda e, ch=ch, li=li: e.tensor_tensor(out=Gm[ch][:], in0=sA[ch], in1=cm[:, 6 + li, :], op=ALU.mult),
                                         reads=[b_sA[ch], b_cm], writes=[b_Gm[ch]])
                                for ch in range(4):
                                    if not last:
                                        S.op("pe", lambda e, ch=ch: e.matmul(out=sA[ch], lhsT=DTm[ch][:], rhs=Gm[ch][:], start=True, stop=False),
                                             reads=[b_DTm[ch], b_Gm[ch]], writes=[b_sA[ch]])
                                        S.op("pe", lambda e, ch=ch: e.matmul(out=sA[ch], lhsT=ident_bf[:], rhs=Dm[ch][:], start=False, stop=True),
                                             reads=[b_ident, b_Dm[ch]], writes=[b_sA[ch]])
                                    S.op("pe", lambda e, ch=ch: e.matmul(out=sB[ch], lhsT=Gm[ch][:], rhs=DTm[ch][:], start=True, stop=False),
                                         reads=[b_DTm[ch], b_Gm[ch]], writes=[b_sB[ch]])
                                    S.op("pe", lambda e, ch=ch: e.matmul(out=sB[ch], lhsT=ident_bf[:], rhs=DTm[ch][:], start=False, stop=True),
                                         reads=[b_ident, b_DTm[ch]], writes=[b_sB[ch]])
                                    if not last:
                                        S.op("act", lambda e, ch=ch: e.copy(out=Dm[ch][:], in_=sA[ch]), reads=[b_sA[ch]], writes=[b_Dm[ch]])
                                    S.op("act", lambda e, ch=ch: e.copy(out=DTm[ch][:], in_=sB[ch]), reads=[b_sB[ch]], writes=[b_DTm[ch]])
                            if stop == "C":
                                continue
                            for ch, (c, e_) in enumerate(chains):
                                pr = slice(e_ * 64, (e_ + 1) * 64)
                                S.op("pe", lambda e, ch=ch, c=c, pr=pr: e.matmul(out=sA[ch][:, 0:64], lhsT=NMt[ch][:, 2, :], rhs=TM[c][:, 1, pr],
                                                                              start=True, stop=True),
                                     reads=[b_NM[ch], b_TM[c]], writes=[b_sA[ch]])
                                S.op("act", lambda e, ch=ch: e.copy(out=X2b[ch][:], in_=sA[ch][:, 0:64]), reads=[b_sA[ch]], writes=[b_X2b[ch]])
                            for ci_ in range(2):
                                c = 2 * cp + ci_
                                for e_ in range(2):
                                    ch = ci_ * 2 + e_
                                    pr = slice(e_ * 64, (e_ + 1) * 64)
                                    S.op("pe", lambda e, ch=ch, pr=pr: e.matmul(out=u2_ps[:, pr], lhsT=DTm[ch][:], rhs=X2b[ch][:], start=True, stop=True),
                                         reads=[b_DTm[ch], b_X2b[ch]], writes=[b_u2])
                                    S.op("pe", lambda e, ch=ch, c=c: e.matmul(out=sA[ch], lhsT=TM[c][:, 0, :], rhs=DTm[ch][:], start=True, stop=True),
                                         reads=[b_DTm[ch], b_TM[c]], writes=[b_sA[ch]])
                                    S.op("dve", lambda e, ci_=ci_, ch=ch, pr=pr: e.tensor_copy(out=WTb[ci_][pr, :], in_=sA[ch][pr, :]),
                                         reads=[b_sA[ch]], writes=[b_WTb[ci_]])
                                S.op("act", lambda e, ci_=ci_: e.copy(out=U2s[ci_][:], in_=u2_ps), reads=[b_u2], writes=[b_U2s[ci_]])
                            if stop == "D":
                                continue
                            for ci_ in range(2):
                                c = 2 * cp + ci_
                                cc = slice(c * 128, (c + 1) * 128)
                                S.op("pe", lambda e, ci_=ci_: e.matmul(out=uh_ps, lhsT=WTb[ci_][:], rhs=Hbf[hp][:], start=True, stop=True),
                                     reads=[b_WTb[ci_], b_Hbf[hp]], writes=[b_uh])
                                S.op("dve", lambda e, ci_=ci_: e.tensor_tensor(out=Ub[:], in0=uh_ps, in1=U2s[ci_][:], op=ALU.add),
                                     reads=[b_uh, b_U2s[ci_]], writes=[b_Ub])
                                for e_ in range(2):
                                    ch = ci_ * 2 + e_
                                    pr = slice(e_ * 64, (e_ + 1) * 64)
                                    yp = y2_ps[e_]
                                    S.op("pe", lambda e, c=c, yp=yp: e.matmul(out=yp, lhsT=Hbf[hp][:], rhs=AR[:, c, 1, :], start=True, stop=False),
                                         reads=[b_Hbf[hp], b_AR], writes=[b_yps])
                                    S.op("pe", lambda e, ch=ch, yp=yp: e.matmul(out=yp, lhsT=Ub[:], rhs=NMt[ch][:, 1, :], start=False, stop=False),
                                         reads=[b_Ub, b_NM[ch]], writes=[b_yps])
                                    S.op("pe", lambda e, ch=ch, c=c, yp=yp: e.matmul(out=yp, lhsT=TM[c][:, 1, :], rhs=NMt[ch][:, 3, :], start=False, stop=True),
                                         reads=[b_TM[c], b_NM[ch]], writes=[b_yps])
                                for e_ in range(2):
                                    pr = slice(e_ * 64, (e_ + 1) * 64)
                                    S.op("act", lambda e, cc=cc, pr=pr, e_=e_: e.copy(out=y_sb[pr, cc], in_=y2_ps[e_][pr, :]), reads=[b_yps], writes=[b_y])
                                S.op("pe", lambda e, c=c: e.matmul(out=uh_ps, lhsT=TM[c][:, 2, :], rhs=Ub[:], start=True, stop=False),
                                     reads=[b_TM[c], b_Ub], writes=[b_uh])
                                S.op("pe", lambda e, c=c: e.matmul(out=uh_ps, lhsT=TM[c][:, 3, :], rhs=TM[c][:, 1, :], start=False, stop=True),
                                     reads=[b_TM[c]], writes=[b_uh])
                                for e_ in range(2):
                                    pr = slice(e_ * 64, (e_ + 1) * 64)
                                    S.op("dve", lambda e, pr=pr, c=c: e.scalar_tensor_tensor(
                                        out=H32[hp][pr, pr], in0=H32[hp][pr, pr], scalar=E_in[pr, c * 128 + 127:c * 128 + 128],
                                        in1=uh_ps[pr, pr], op0=ALU.mult, op1=ALU.add),
                                        reads=[b_H32[hp], b_Ein, b_uh], writes=[b_H32[hp]])
                                S.op("pool", lambda e: e.tensor_copy(out=Hbf[hp][:], in_=H32[hp][:]), reads=[b_H32[hp]], writes=[b_Hbf[hp]])
                        if dbg and "yraw" in dbg:
                            S.op("act", lambda e: e.copy(out=dbg_sb[:], in_=y_sb[:]), reads=[b_y], writes=[b_dbg])
                            S.dma("sp", dbg_aps["yraw"][hp * 128:(hp + 1) * 128, c_lo:c_hi], dbg_sb[:], reads=[b_dbg])
                        S.op("pe", lambda e: e.matmul(out=p0[:], lhsT=bd_mean[:], rhs=y_sb[:], start=True, stop=True),
                             reads=[b_cm, b_y], writes=[bp0])
                        S.op("dve", lambda e: e.tensor_tensor(out=y_sb[:], in0=y_sb[:], in1=p0[:], op=ALU.subtract), reads=[b_y, bp0], writes=[b_y])
                        S.op("pool", lambda e: e.tensor_tensor(out=tmpA[:], in0=y_sb[:], in1=y_sb[:], op=ALU.mult), reads=[b_y], writes=[b_tmpA])
                        S.op("pe", lambda e: e.matmul(out=p1[:], lhsT=bd_mean[:], rhs=tmpA[:], start=True, stop=True),
                             reads=[b_cm, b_tmpA], writes=[bp1])
                        S.op("dve", lambda e: e.tensor_scalar(out=tmpA[:], in0=p1[:], scalar1=LNX_EPS, scalar2=None, op0=ALU.add),
                             reads=[bp1], writes=[b_tmpA])
                        S.op("act", lambda e: e.sqrt(out=tmpA[:], in_=tmpA[:]), reads=[b_tmpA], writes=[b_tmpA])
                        S.op("dve", lambda e: e.reciprocal(out=tmpA[:], in_=tmpA[:]), reads=[b_tmpA], writes=[b_tmpA])
                        S.op("pool", lambda e: e.tensor_tensor(out=y_sb[:], in0=y_sb[:], in1=tmpA[:], op=ALU.mult), reads=[b_y, b_tmpA], writes=[b_y])
                        S.op("dve", lambda e: e.tensor_scalar(out=y_sb[:], in0=y_sb[:], scalar1=lnwT[:, hp:hp + 1], scalar2=lnbT[:, hp:hp + 1],
                                                              op0=ALU.mult, op1=ALU.add), reads=[b_y, b_lnw, b_lnb], writes=[b_y])
                        S.op("pool", lambda e: e.tensor_tensor(out=y_sb[:], in0=y_sb[:], in1=bonus[:], op=ALU.add), reads=[b_y, b_bonus], writes=[b_y])
                        S.op("dve", lambda e: e.tensor_tensor(out=catR[:], in0=y_sb[:], in1=g_t[:], op=ALU.mult), reads=[b_y, b_g], writes=[b_catR])
                        S.dma("sp", cat_scr[512 + hp * 128:512 + (hp + 1) * 128, c_lo:c_hi], catR[:], reads=[b_catR])
                        if dbg and "orwkv" in dbg:
                            S.op("act", lambda e: e.copy(out=dbg_sb[:], in_=catR[:]), reads=[b_catR], writes=[b_dbg])
                            S.dma("sp", dbg_aps["orwkv"][hp * 128:(hp + 1) * 128, c_lo:c_hi], dbg_sb[:], reads=[b_dbg])
                S.barrier()
        if "O" in phases:
            with ExitStack() as es:
                def sb(name, shape, dt=F32):
                    return es.enter_context(nc.sbuf_tensor(uniq(name), list(shape), dt))
                UT = 256
                NU = T // UT
                stg = [sb("stg%d" % i, [128, D], F32) for i in range(2)]
                b_stg = [Buf(), Buf()]
                st = {"xt": stg, "b_xt": b_stg}
                gfT, b_gfT = load_colvec(es, "gfT", gf_pre, KC)
                wout_bf = sb("wout_bf", [128, KC, D], BF16)
                b_wout = [Buf() for _ in range(KC)]
                wg_bf = sb("wg_bf", [128, KC, DFF], BF16)
                b_wg = [Buf() for _ in range(KC)]
                wu_bf = sb("wu_bf", [128, KC, DFF], BF16)
                b_wu = [Buf() for _ in range(KC)]
                wd_bf = sb("wd_bf", [128, NFF, D], BF16)
                b_wd = [Buf() for _ in range(NFF)]
                load_weight_bf(st, wout_bf, b_wout, w_out, 0, D, None, None)
                load_weight_bf(st, wg_bf, b_wg, w_gate, 0, DFF, gfT, b_gfT)
                load_weight_bf(st, wu_bf, b_wu, w_up, 0, DFF, gfT, b_gfT)
                load_weight_bf(st, wd_bf, b_wd, w_down, 0, D, None, None, nk=NFF)
                gpost_bc = sb("gpost_bc", [128, D], F32)
                gfpost_bc = sb("gfpost_bc", [128, D], F32)
                b_gbc = Buf()
                S.dma("sp", gpost_bc[:], g_post.partition_broadcast(128), writes=[b_gbc])
                S.dma("sp", gfpost_bc[:], gf_post.partition_broadcast(128), writes=[b_gbc])
                catT = sb("catT", [128, KC, UT], BF16)
                b_catT = Buf()
                zn = sb("zn", [128, D], BF16)
                b_zn = Buf()
                zT = sb("zT", [128, KC, UT], BF16)
                b_zT = Buf()
                aT = sb("aT", [128, NFF, UT], BF16)
                b_aT = Buf()
                junk = sb("junkO", [128, D], BF16)
                b_junk = Buf()
                sgt = [sb("sgt%d" % i, [128, UT], F32) for i in range(2)]
                b_sgt = [Buf(), Buf()]
                t1 = sb("t1", [128, D], F32)
                b_t1 = Buf()
                ssO = sb("ssO", [128, 4], F32)
                b_ssO = Buf()
                cat_v = cat_scr.rearrange("(k p) t -> p k t", p=128)

                def rstd_from(srcs, bsrcs):
                    for i, (ap_, b_) in enumerate(zip(srcs, bsrcs)):
                        n = ap_.shape[1]
                        S.op("act", lambda e, ap_=ap_, i=i, n=n: e.activation(out=junk[:, 0:n], in_=ap_, func=AF.Square, accum_out=ssO[:, i:i + 1]),
                             reads=[b_], writes=[b_junk, b_ssO])
                    if len(srcs) == 2:
                        S.op("dve", lambda e: e.tensor_tensor(out=ssO[:, 2:3], in0=ssO[:, 0:1], in1=ssO[:, 1:2], op=ALU.add),
                             reads=[b_ssO], writes=[b_ssO])
                        src = ssO[:, 2:3]
                    else:
                        src = ssO[:, 0:1]
                    S.op("dve", lambda e: e.tensor_scalar(out=ssO[:, 2:3], in0=src, scalar1=1.0 / D, scalar2=EPS, op0=ALU.mult, op1=ALU.add),
                         reads=[b_ssO], writes=[b_ssO])
                    S.op("act", lambda e: e.sqrt(out=ssO[:, 2:3], in_=ssO[:, 2:3]), reads=[b_ssO], writes=[b_ssO])
                    S.op("dve", lambda e: e.reciprocal(out=ssO[:, 2:3], in_=ssO[:, 2:3]), reads=[b_ssO], writes=[b_ssO])

                for u in range(NU):
                    t0 = u * UT
                    S.dma("sp", catT[:], cat_v[:, :, t0:t0 + UT], writes=[b_catT])
                    for j in range(2):
                        tj = t0 + j * 128
                        S.dma("sp", stg[j][:], x[tj:tj + 128, :], writes=[b_stg[j]])
                        for half in range(2):
                            for kc in range(KC):
                                S.op("pe", lambda e, kc=kc, half=half, j=j: e.matmul(
                                    out=PB[half][:], lhsT=catT[:, kc, j * 128:(j + 1) * 128], rhs=wout_bf[:, kc, half * 512:(half + 1) * 512],
                                    start=(kc == 0), stop=(kc == KC - 1)), reads=[b_catT, b_wout[kc]], writes=[b_PB[half]])
                        rstd_from([PB[0][:], PB[1][:]], [b_PB[0], b_PB[1]])
                        for half in range(2):
                            S.op("act", lambda e, half=half: e.activation(out=t1[:, half * 512:(half + 1) * 512], in_=PB[half][:], func=AF.Copy,
                                                                          scale=ssO[:, 2:3]), reads=[b_PB[half], b_ssO], writes=[b_t1])
                        S.op("pool", lambda e: e.tensor_tensor(out=t1[:], in0=t1[:], in1=gpost_bc[:], op=ALU.mult), reads=[b_t1, b_gbc], writes=[b_t1])
                        S.op("dve", lambda e, j=j: e.tensor_tensor(out=stg[j][:], in0=stg[j][:], in1=t1[:], op=ALU.add),
                             reads=[b_stg[j], b_t1], writes=[b_stg[j]])
                        if dbg and "h" in dbg:
                            S.dma("sp", dbg_aps["h"][tj:tj + 128, :], stg[j][:], reads=[b_stg[j]])
                        rstd_from([stg[j][:]], [b_stg[j]])
                        S.op("act", lambda e, j=j: e.activation(out=zn[:], in_=stg[j][:], func=AF.Copy, scale=ssO[:, 2:3]),
                             reads=[b_stg[j], b_ssO], writes=[b_zn])
                        for kc in range(KC):
                            S.op("pe", lambda e, kc=kc: e.transpose(out=tp_ps[:, kc, :], in_=zn[:, kc * 128:(kc + 1) * 128], identity=ident_bf[:]),
                                 reads=[b_zn, b_ident], writes=[b_tp])
                        S.op("dve", lambda e, j=j: e.tensor_copy(out=zT[:, :, j * 128:(j + 1) * 128], in_=tp_ps[:, :, :]), reads=[b_tp], writes=[b_zT])
                    for ffc in range(NFF):
                        gi_ = 2 + 2 * (ffc % 2)
                        ui_ = 3 + 2 * (ffc % 2)
                        fc = slice(ffc * 128, (ffc + 1) * 128)
                        for kc in range(KC):
                            S.op("pe", lambda e, kc=kc, fc=fc, gi_=gi_: e.matmul(out=PB[gi_][:, 0:UT], lhsT=wg_bf[:, kc, fc], rhs=zT[:, kc, :],
                                                                                 start=(kc == 0), stop=(kc == KC - 1)),
                                 reads=[b_wg[kc], b_zT], writes=[b_PB[gi_]])
                        for kc in range(KC):
                            S.op("pe", lambda e, kc=kc, fc=fc, ui_=ui_: e.matmul(out=PB[ui_][:, 0:UT], lhsT=wu_bf[:, kc, fc], rhs=zT[:, kc, :],
                                                                                 start=(kc == 0), stop=(kc == KC - 1)),
                                 reads=[b_wu[kc], b_zT], writes=[b_PB[ui_]])
                        si_ = ffc % 2
                        S.op("act", lambda e, gi_=gi_, si_=si_: e.activation(out=sgt[si_][:], in_=PB[gi_][:, 0:UT], func=AF.Silu),
                             reads=[b_PB[gi_]], writes=[b_sgt[si_]])
                        S.op("dve", lambda e, ui_=ui_, si_=si_, ffc=ffc: e.tensor_tensor(out=aT[:, ffc, :], in0=PB[ui_][:, 0:UT], in1=sgt[si_][:], op=ALU.mult),
                             reads=[b_PB[ui_], b_sgt[si_]], writes=[b_aT])
                    for j in range(2):
                        tj = t0 + j * 128
                        for half in range(2):
                            for ffc in range(NFF):
                                S.op("pe", lambda e, ffc=ffc, half=half, j=j: e.matmul(
                                    out=PB[half][:], lhsT=aT[:, ffc, j * 128:(j + 1) * 128], rhs=wd_bf[:, ffc, half * 512:(half + 1) * 512],
                                    start=(ffc == 0), stop=(ffc == NFF - 1)), reads=[b_aT, b_wd[ffc]], writes=[b_PB[half]])
                        rstd_from([PB[0][:], PB[1][:]], [b_PB[0], b_PB[1]])
                        for half in range(2):
                            S.op("act", lambda e, half=half: e.activation(out=t1[:, half * 512:(half + 1) * 512], in_=PB[half][:], func=AF.Copy,
                                                                          scale=ssO[:, 2:3]), reads=[b_PB[half], b_ssO], writes=[b_t1])
                        S.op("pool", lambda e: e.tensor_tensor(out=t1[:], in0=t1[:], in1=gfpost_bc[:], op=ALU.mult), reads=[b_t1, b_gbc], writes=[b_t1])
                        S.op("dve", lambda e, j=j: e.tensor_tensor(out=t1[:], in0=t1[:], in1=stg[j][:], op=ALU.add),
                             reads=[b_stg[j], b_t1], writes=[b_t1])
                        S.dma("sp", out[tj:tj + 128, :], t1[:], reads=[b_t1])

        S.wait_tokens("sp", [t for e in S.ENGS for t in S.dtoks[e]])
        S.emit()
    return nc


WNAMES = ["attn_norm_pre", "attn_norm_post", "w_in", "fox_forget_bias", "shift_mu", "rwkv_w0", "rwkv_w_up", "rwkv_a0",
          "rwkv_a_up", "rwkv_g_up", "rwkv_k_k", "rwkv_k_a", "rwkv_r_k", "rwkv_ln_w", "rwkv_ln_b", "w_out",
          "ffn_norm_pre", "ffn_norm_post", "ffn_w_gate", "ffn_w_up", "ffn_w_down"]


def make_in_map(inputs, b, T):
    m = {"x": np.ascontiguousarray(np.asarray(inputs["x"], dtype=np.float32)[b, :T])}
    for k in WNAMES:
        a = np.asarray(inputs[k], dtype=np.float32)[0]
        if k == "rwkv_r_k":
            a = a.reshape(-1)
        m[k] = np.ascontiguousarray(a)
    m["cmask"] = make_cmask()
    return m


def kernel(**inputs):
    x = np.asarray(inputs["x"])
    B, T, _ = x.shape
    nc = build_nc(T)
    in_maps = [make_in_map(inputs, b, T) for b in range(B)]
    res = run_bass_kernel_spmd(nc, in_maps, core_ids=list(range(B)))
    return np.stack([np.asarray(r["out"], dtype=np.float32) for r in res.results], axis=0)
```

```python
import numpy as np
from contextlib import ExitStack
import concourse.bass as bass
import concourse.mybir as mybir
from concourse.bass_utils import run_bass_kernel_spmd

F32 = mybir.dt.float32
BF16 = mybir.dt.bfloat16
AF = mybir.ActivationFunctionType
ALU = mybir.AluOpType
AX = mybir.AxisListType

D = 1024
KC = 8
HD = 64
NH = 8
FOXW = 512
RW = 512
DFF = 2816
NFF = 22
WIN = 3368
RBASE = 1544
EPS = 1e-6
LNX_EPS = 64e-5
SB = 512
EPOCH = 12000


class Buf:
    __slots__ = ("w", "r")

    def __init__(self):
        self.w = None
        self.r = []


class _Rec:
    def __getattr__(self, name):
        def f(*a, **k):
            self.call = (name, a, k)
        return f


class Sched:
    ENGS = ("pe", "act", "dve", "pool", "sp")

    def __init__(self, nc, es):
        self.nc = nc
        self.es = es
        self.q = {e: [] for e in self.ENGS}
        self.cnt = {e: 0 for e in self.ENGS}
        self.run = {e: {} for e in self.ENGS}
        self.clk = {}
        self.sems = {}
        self.ndma = {"sp": 16, "act": 6, "pool": 6}
        self.dcnt = {e: 0 for e in self.ENGS}
        self.dtoks = {e: [] for e in self.ENGS}
        self.nwaits = 0

    def sem(self, key):
        s = self.sems.get(key)
        if s is None:
            s = self.es.enter_context(self.nc.semaphore("s_%s_%s" % key))
            self.sems[key] = s
        return s

    def _deps(self, eng, reads, writes, is_dma):
        deps = set()
        for b in reads:
            if b.w is not None:
                deps.add(b.w)
        for b in writes:
            if b.w is not None:
                deps.add(b.w)
            for t in b.r:
                deps.add(t)
        return deps

    def _waits(self, eng, deps):
        run = self.run[eng]
        waits = []
        for t in sorted(deps, key=lambda t: (str(t[0]), t[1])):
            key, val, isd = t
            if eng == "pe" and key[0] == "pe" and not isd:
                continue
            if run.get(key, 0) >= val:
                continue
            waits.append((key, val))
            for k2, v2 in self.clk[(key, val)].items():
                if run.get(k2, 0) < v2:
                    run[k2] = v2
        self.nwaits += len(waits)
        return waits

    def op(self, eng, fn, reads=(), writes=()):
        rec = _Rec()
        fn(rec)
        call = rec.call
        fn = lambda e, call=call: getattr(e, call[0])(*call[1], **call[2])
        deps = self._deps(eng, reads, writes, False)
        waits = self._waits(eng, deps)
        n = self.cnt[eng]
        self.cnt[eng] = n + 1
        key = (eng, n // EPOCH)
        val = n % EPOCH + 1
        tok = (key, val, False)
        c = dict(self.run[eng])
        c[key] = val
        self.clk[(key, val)] = c
        self.q[eng].append((waits, fn, key, 1))
        for b in reads:
            b.r.append(tok)
        for b in writes:
            b.w = tok
            b.r = []
        return tok

    def dma(self, eng, out, in_, reads=(), writes=(), **kw):
        deps = self._deps(eng, reads, writes, True)
        d = self.dcnt[eng]
        self.dcnt[eng] = d + 1
        nd = self.ndma[eng]
        if d >= nd:
            deps.add(self.dtoks[eng][d - nd])
        waits = self._waits(eng, deps)
        key = ("d" + eng, d % nd)
        val = 16 * (d // nd + 1)
        tok = (key, val, True)
        c = dict(self.run[eng])
        c[key] = val
        self.clk[(key, val)] = c
        self.dtoks[eng].append(tok)
        self.q[eng].append((waits, lambda e: e.dma_start(out=out, in_=in_, **kw), key, 16))
        for b in reads:
            b.r.append(tok)
        for b in writes:
            b.w = tok
            b.r = []
        return tok

    def barrier(self):
        best = {}
        for e in self.ENGS:
            n = self.cnt[e]
            if n > 0 and e != "sp":
                best[(e, (n - 1) // EPOCH)] = ((n - 1) % EPOCH + 1, False)
            for (k, v, isd) in self.dtoks[e][-self.ndma.get(e, 1):]:
                if best.get(k, (0, True))[0] < v:
                    best[k] = (v, True)
        toks = [(k, v, isd) for k, (v, isd) in best.items()]
        for e in self.ENGS:
            waits = []
            run = self.run[e]
            for (k, v, isd) in toks:
                if run.get(k, 0) < v:
                    waits.append((k, v))
                    for k2, v2 in self.clk[(k, v)].items():
                        if run.get(k2, 0) < v2:
                            run[k2] = v2
            self.q[e].append((waits, None, None, 0))

    def wait_tokens(self, eng, toks):
        waits = self._waits(eng, set(toks))
        self.q[eng].append((waits, None, None, 0))

    def emit(self):
        nc = self.nc
        for k in set(k for e in self.ENGS for (_, _, k, _) in self.q[e] if k is not None):
            self.sem(k)
        for e in self.ENGS:
            for (waits, _, _, _) in self.q[e]:
                for (k, v) in waits:
                    self.sem(k)
        with nc.Block() as block:
            def run(engname, engobj):
                for (waits, fn, key, inc) in self.q[engname]:
                    for (k, v) in waits:
                        engobj.wait_ge(self.sems[k], v)
                    if fn is not None:
                        ins = fn(engobj)
                        ins.then_inc(self.sems[key], inc)

            @block.tensor
            def _(e):
                run("pe", e)

            @block.scalar
            def _(e):
                run("act", e)

            @block.vector
            def _(e):
                run("dve", e)

            @block.gpsimd
            def _(e):
                run("pool", e)

            @block.sync
            def _(e):
                run("sp", e)


NMASK = 13
LEVELS = (2, 4, 8, 16, 32, 64)


def make_cmask():
    p = np.arange(128)[:, None]
    f = np.arange(128)[None, :]
    m = np.zeros((128, NMASK, 128), np.float32)
    m[:, 0, :] = (f > p)
    m[:, 1, :] = (f >= p)
    m[:, 2, :] = (f > p)
    m[:, 3, :] = (f >= p)
    m[:, 4, :] = ((p % 2 == 1) & (f == p - 1))
    m[:, 5, :] = ((f % 2 == 1) & (p == f - 1))
    for li, mm in enumerate(LEVELS):
        m[:, 6 + li, :] = (((p // mm) % 2 == 1) & ((f // mm) == (p // mm) - 1))
    m[:, 12, :] = ((p // 64) == (f // 64))
    return m


def build_nc(T, dbg=None, phases="FRO"):
    NSB = T // SB
    NBLK = T // 128
    nc = bass.Bass("TRN2", target_bir_lowering=False)
    es0 = ExitStack()
    S = Sched(nc, es0)

    def din(name, shape):
        return nc.dram_tensor(name, list(shape), F32, kind="ExternalInput").ap()

    x = din("x", [T, D])
    g_pre = din("attn_norm_pre", [D])
    g_post = din("attn_norm_post", [D])
    w_in = din("w_in", [D, WIN])
    din_fb = din("fox_forget_bias", [NH])
    shift_mu = din("shift_mu", [1824])
    rwkv_w0 = din("rwkv_w0", [RW])
    rwkv_w_up = din("rwkv_w_up", [64, RW])
    rwkv_a0 = din("rwkv_a0", [RW])
    rwkv_a_up = din("rwkv_a_up", [64, RW])
    rwkv_g_up = din("rwkv_g_up", [160, RW])
    rwkv_k_k = din("rwkv_k_k", [RW])
    rwkv_k_a = din("rwkv_k_a", [RW])
    rwkv_r_k = din("rwkv_r_k", [RW])
    rwkv_ln_w = din("rwkv_ln_w", [RW])
    rwkv_ln_b = din("rwkv_ln_b", [RW])
    w_out = din("w_out", [D, D])
    gf_pre = din("ffn_norm_pre", [D])
    gf_post = din("ffn_norm_post", [D])
    w_gate = din("ffn_w_gate", [D, DFF])
    w_up = din("ffn_w_up", [D, DFF])
    w_down = din("ffn_w_down", [DFF, D])
    cmask = din("cmask", [128, NMASK, 128])
    out = nc.dram_tensor("out", [T, D], F32, kind="ExternalOutput").ap()
    cat_scr = nc.dram_tensor("cat_scr", [D, T], BF16, kind="Internal").ap()
    scr_k = nc.dram_tensor("scr_k", [3, NH, T], BF16, kind="Internal").ap()
    scr_q = nc.dram_tensor("scr_q", [3, NH, T], BF16, kind="Internal").ap()
    dbg_aps = {}
    if dbg:
        for k, shp in dbg.items():
            if k == "stop":
                continue
            dbg_aps[k] = nc.dram_tensor("dbg_" + k, list(shp), F32, kind="ExternalOutput").ap()

    ucnt = [0]

    def uniq(name):
        ucnt[0] += 1
        return "%s_%d" % (name, ucnt[0])

    with es0:
        tp_ps = es0.enter_context(nc.psum_tensor("tp_ps", [128, KC, 128], BF16))
        b_tp = Buf()
        PB = [es0.enter_context(nc.psum_tensor("pb%d" % i, [128, SB], F32)) for i in range(7)]
        b_PB = [Buf() for _ in range(7)]
        ident_bf = es0.enter_context(nc.sbuf_tensor("ident_bf", [128, 128], BF16))
        ident_f = es0.enter_context(nc.sbuf_tensor("ident_f", [128, 128], F32))
        b_ident = Buf()
        S.op("pool", lambda e: e.memset(ident_f[:], 0.0), writes=[b_ident])
        S.op("pool", lambda e: e.affine_select(out=ident_f[:], in_=ident_f[:], pattern=[[-1, 128]],
                                               compare_op=ALU.not_equal, fill=1.0, base=0,
                                               channel_multiplier=1), reads=[b_ident], writes=[b_ident])
        S.op("dve", lambda e: e.tensor_copy(out=ident_bf[:], in_=ident_f[:]), reads=[b_ident], writes=[b_ident])

        def load_colvec(es, name, src, ncol):
            t = es.enter_context(nc.sbuf_tensor(uniq(name), [128, ncol], F32))
            b = Buf()
            S.dma("sp", t[:], src.rearrange("(k p) -> p k", p=128), writes=[b], allow_slow_non_contiguous=True)
            return t, b

        def make_front(es):
            def sbt(name, shape, dt=F32):
                return es.enter_context(nc.sbuf_tensor(uniq(name), list(shape), dt))
            st = {}
            st["xt"] = [sbt("xt%d" % i, [128, D], F32) for i in range(2)]
            st["b_xt"] = [Buf() for _ in range(2)]
            st["junk"] = sbt("junk", [128, D], BF16)
            st["b_junk"] = Buf()
            st["xn"] = [sbt("xn%d" % i, [128, D], BF16) for i in range(2)]
            st["b_xn"] = [Buf(), Buf()]
            st["ss"] = sbt("ss", [128, 8], F32)
            st["b_ss"] = [Buf() for _ in range(8)]
            st["uT"] = sbt("uT", [128, KC, SB], BF16)
            st["b_uT"] = Buf()
            st["tc"] = 0
            return st

        def front(st, s):
            xt, b_xt, xn, b_xn, ss, b_ss = st["xt"], st["b_xt"], st["xn"], st["b_xn"], st["ss"], st["b_ss"]
            junk, b_junk, uT, b_uT = st["junk"], st["b_junk"], st["uT"], st["b_uT"]
            for j in range(4):
                t0 = s * SB + j * 128
                xi = st["tc"] % 2
                ni = st["tc"] % 2
                si = st["tc"] % 8
                st["tc"] += 1
                S.dma("sp", xt[xi][:], x[t0:t0 + 128, :], writes=[b_xt[xi]])
                S.op("act", lambda e, xi=xi, si=si: e.activation(out=junk[:], in_=xt[xi][:], func=AF.Square,
                                                                 accum_out=ss[:, si:si + 1]),
                     reads=[b_xt[xi]], writes=[b_junk, b_ss[si]])
                S.op("dve", lambda e, si=si: e.tensor_scalar(out=ss[:, si:si + 1], in0=ss[:, si:si + 1],
                                                             scalar1=1.0 / D, scalar2=EPS, op0=ALU.mult, op1=ALU.add),
                     reads=[b_ss[si]], writes=[b_ss[si]])
                S.op("act", lambda e, si=si: e.sqrt(out=ss[:, si:si + 1], in_=ss[:, si:si + 1]),
                     reads=[b_ss[si]], writes=[b_ss[si]])
                S.op("dve", lambda e, si=si: e.reciprocal(out=ss[:, si:si + 1], in_=ss[:, si:si + 1]),
                     reads=[b_ss[si]], writes=[b_ss[si]])
                S.op("act", lambda e, xi=xi, ni=ni, si=si: e.activation(out=xn[ni][:], in_=xt[xi][:],
                                                                        func=AF.Copy, scale=ss[:, si:si + 1]),
                     reads=[b_xt[xi], b_ss[si]], writes=[b_xn[ni]])
                for kc in range(KC):
                    S.op("pe", lambda e, kc=kc, ni=ni: e.transpose(out=tp_ps[:, kc, :],
                                                                   in_=xn[ni][:, kc * 128:(kc + 1) * 128],
                                                                   identity=ident_bf[:]),
                         reads=[b_xn[ni], b_ident], writes=[b_tp])
                S.op("dve", lambda e, j=j: e.tensor_copy(out=uT[:, :, j * 128:(j + 1) * 128], in_=tp_ps[:, :, :]),
                     reads=[b_tp], writes=[b_uT])

        def load_weight_bf(st, dst, b_dst, src, c0, ncols, gvec, b_g, nk=KC):
            xt, b_xt = st["xt"], st["b_xt"]
            cnt = 0
            for kc in range(nk):
                for p0 in range(0, ncols, D):
                    n = min(D, ncols - p0)
                    i = cnt % 2
                    cnt += 1
                    S.dma("sp", xt[i][:, 0:n], src[kc * 128:(kc + 1) * 128, c0 + p0:c0 + p0 + n], writes=[b_xt[i]])
                    eng = "dve" if cnt % 2 == 0 else "pool"
                    if gvec is None:
                        S.op(eng, lambda e, kc=kc, i=i, p0=p0, n=n: e.tensor_copy(out=dst[:, kc, p0:p0 + n], in_=xt[i][:, 0:n]),
                             reads=[b_xt[i]], writes=[b_dst[kc]])
                    else:
                        S.op(eng, lambda e, kc=kc, i=i, p0=p0, n=n: e.tensor_scalar(
                            out=dst[:, kc, p0:p0 + n], in0=xt[i][:, 0:n], scalar1=gvec[:, kc:kc + 1], scalar2=None, op0=ALU.mult),
                             reads=[b_xt[i], b_g], writes=[b_dst[kc]])

        pjc = [0]

        def proj_fm(wt, b_w, st, c0, ncols, evac):
            pi = pjc[0] % 2
            pjc[0] += 1
            uT, b_uT = st["uT"], st["b_uT"]
            for kc in range(KC):
                S.op("pe", lambda e, kc=kc: e.matmul(out=PB[pi][0:ncols, :], lhsT=wt[:, kc, c0:c0 + ncols],
                                                     rhs=uT[:, kc, :], start=(kc == 0), stop=(kc == KC - 1)),
                     reads=[b_w[kc], b_uT], writes=[b_PB[pi]])
            evac(PB[pi], b_PB[pi])

        if "F" in phases:
            with ExitStack() as es:
                def sb(name, shape, dt=F32):
                    return es.enter_context(nc.sbuf_tensor(uniq(name), list(shape), dt))
                st = make_front(es)
                uT, b_uT = st["uT"], st["b_uT"]
                gT, b_gT = load_colvec(es, "gT", g_pre, KC)
                w_bf = sb("w_bf", [128, KC, RBASE], BF16)
                b_w = [Buf() for _ in range(KC)]
                load_weight_bf(st, w_bf, b_w, w_in, 0, RBASE, gT, b_gT)

                nfb = sb("nfb", [8, 1], F32)
                b_nfb = Buf()
                S.dma("sp", nfb[:], din_fb.rearrange("(h o) -> h o", o=1), writes=[b_nfb])
                S.op("dve", lambda e: e.tensor_scalar(out=nfb[:], in0=nfb[:], scalar1=-1.0, scalar2=None, op0=ALU.mult),
                     reads=[b_nfb], writes=[b_nfb])
                ones8 = sb("ones8", [8, SB], F32)
                b_ones8 = Buf()
                S.op("pool", lambda e: e.memset(ones8[:], 1.0), writes=[b_ones8])
                ones_f = sb("ones_f", [128, 64], F32)
                b_onesf = Buf()
                S.op("pool", lambda e: e.memset(ones_f[:], 1.0), writes=[b_onesf])
                maskneg_f = sb("maskneg_f", [128, 128], F32)
                maskneg = sb("maskneg", [128, 128], BF16)
                b_mask = Buf()
                S.op("pool", lambda e: e.memset(maskneg_f[:], 0.0), writes=[b_mask])
                S.op("pool", lambda e: e.affine_select(out=maskneg_f[:], in_=maskneg_f[:], pattern=[[1, 128]],
                                                       compare_op=ALU.is_ge, fill=-30000.0, base=0,
                                                       channel_multiplier=-1), reads=[b_mask], writes=[b_mask])
                S.op("dve", lambda e: e.tensor_copy(out=maskneg[:], in_=maskneg_f[:]), reads=[b_mask], writes=[b_mask])

                KT = sb("KT", [70, NH, T], BF16)
                b_KT = [Buf() for _ in range(NSB)]
                QT = sb("QT", [70, NH, SB], BF16)
                b_QT = Buf()
                VT = sb("VT", [128, NBLK, NH, 66], BF16)
                b_VT = [Buf() for _ in range(NSB)]
                S.op("pool", lambda e: e.memset(KT[64:70, :, :], 1.0), writes=b_KT)
                S.op("pool", lambda e: e.memset(QT[64:70, :, :], 1.0), writes=[b_QT])
                S.op("pool", lambda e: e.memset(VT[:, :, :, 64:66], 1.0), writes=b_VT)
                b_scrk = Buf()
                b_scrq = Buf()
                cneg = [sb("cneg%d" % i, [8, SB], F32) for i in range(2)]
                b_cneg = [Buf(), Buf()]
                fl = sb("fl", [8, SB], F32)
                b_fl = Buf()
                res1 = sb("res1", [8, SB], F32)
                b_res1 = Buf()
                ksp = sb("ksp", [8, 3, SB], BF16)
                qsp = sb("qsp", [8, 3, SB], BF16)
                b_ksp = Buf()
                b_qsp = Buf()
                catF = [sb("catF%d" % i, [64, SB], BF16) for i in range(2)]
                b_catF = [Buf(), Buf()]
                PT = [sb("PT%d" % i, [128, SB], BF16) for i in range(3)]
                b_PT = [Buf() for _ in range(3)]
                rs = sb("rs", [66, SB], F32)
                b_rs = Buf()
                bc_sb = sb("bc_sb", [64, SB], F32)
                b_bc = Buf()
                dbg_sb = sb("dbg_sb", [128, SB], F32)
                b_dbg = Buf()
                st_ps = [PB[2], PB[3]]
                b_st = [b_PB[2], b_PB[3]]
                o_ps2 = [PB[4], PB[5]]
                b_o2 = [b_PB[4], b_PB[5]]
                bc_ps, b_bcps = PB[6], b_PB[6]

                for s in range(NSB):
                    c_lo, c_hi = s * SB, (s + 1) * SB
                    front(st, s)
                    ci = s % 2

                    def ev_ff(pp, bp):
                        S.op("act", lambda e: e.activation(out=fl[:], in_=pp[0:8, :], func=AF.Exp, bias=nfb[:, 0:1], scale=-1.0),
                             reads=[bp, b_nfb], writes=[b_fl])
                        S.op("act", lambda e: e.activation(out=fl[:], in_=fl[:], func=AF.Ln, bias=1.0, scale=1.0),
                             reads=[b_fl], writes=[b_fl])
                        S.op("dve", lambda e: e.tensor_tensor_scan(out=cneg[ci][:], data0=ones8[:], data1=fl[:], initial=0.0,
                                                                   op0=ALU.mult, op1=ALU.add),
                             reads=[b_fl, b_ones8], writes=[b_cneg[ci]])
                        if s > 0:
                            S.op("dve", lambda e: e.tensor_scalar(out=cneg[ci][:], in0=cneg[ci][:],
                                                                  scalar1=cneg[1 - ci][:, SB - 1:SB], scalar2=None, op0=ALU.add),
                                 reads=[b_cneg[ci], b_cneg[1 - ci]], writes=[b_cneg[ci]])
                        S.op("dve", lambda e: e.tensor_copy(out=ksp[:, 0, :], in_=cneg[ci][:]), reads=[b_cneg[ci]], writes=[b_ksp])
                        S.op("dve", lambda e: e.tensor_tensor(out=res1[:], in0=cneg[ci][:], in1=ksp[:, 0, :], op=ALU.subtract),
                             reads=[b_cneg[ci], b_ksp], writes=[b_res1])
                        S.op("dve", lambda e: e.tensor_copy(out=ksp[:, 1, :], in_=res1[:]), reads=[b_res1], writes=[b_ksp])
                        S.op("dve", lambda e: e.tensor_tensor(out=res1[:], in0=res1[:], in1=ksp[:, 1, :], op=ALU.subtract),
                             reads=[b_res1, b_ksp], writes=[b_res1])
                        S.op("dve", lambda e: e.tensor_copy(out=ksp[:, 2, :], in_=res1[:]), reads=[b_res1], writes=[b_ksp])
                        S.op("dve", lambda e: e.tensor_scalar(out=qsp[:], in0=ksp[:], scalar1=-1.0, scalar2=None, op0=ALU.mult),
                             reads=[b_ksp], writes=[b_qsp])
                        S.dma("sp", scr_k[:, :, c_lo:c_hi].rearrange("r h t -> h r t"), ksp[:], reads=[b_ksp], writes=[b_scrk])
                        S.dma("sp", scr_q[:, :, c_lo:c_hi].rearrange("r h t -> h r t"), qsp[:], reads=[b_qsp], writes=[b_scrq])
                        S.dma("sp", KT[67:70, :, c_lo:c_hi], scr_k[:, :, c_lo:c_hi], reads=[b_scrk], writes=[b_KT[s]])
                        S.dma("sp", QT[64:67, :, :], scr_q[:, :, c_lo:c_hi], reads=[b_scrq], writes=[b_QT])

                    proj_fm(w_bf, b_w, st, 1536, 8, ev_ff)
                    for h in range(NH):
                        def ev_q(pp, bp, h=h):
                            S.op("act", lambda e: e.mul(out=QT[0:64, h, :], in_=pp[0:64, :], mul=0.125), reads=[bp], writes=[b_QT])
                        proj_fm(w_bf, b_w, st, h * 64, 64, ev_q)

                        def ev_k(pp, bp, h=h):
                            S.op("dve", lambda e: e.tensor_copy(out=KT[0:64, h, c_lo:c_hi], in_=pp[0:64, :]), reads=[bp], writes=[b_KT[s]])
                        proj_fm(w_bf, b_w, st, 512 + h * 64, 64, ev_k)
                    for j in range(4):
                        pi = pjc[0] % 2
                        pjc[0] += 1
                        for kc in range(KC):
                            S.op("pe", lambda e, kc=kc, j=j, pi=pi: e.matmul(out=PB[pi][:], lhsT=uT[:, kc, j * 128:(j + 1) * 128],
                                                                             rhs=w_bf[:, kc, 1024:1536], start=(kc == 0), stop=(kc == KC - 1)),
                                 reads=[b_w[kc], b_uT], writes=[b_PB[pi]])
                        S.op("act", lambda e, j=j, pi=pi: e.copy(out=VT[:, s * 4 + j, :, 0:64],
                                                                 in_=PB[pi][:].rearrange("p (h d) -> p h d", h=NH)),
                             reads=[b_PB[pi]], writes=[b_VT[s]])

                    tiles = []
                    nkb = 4 * (s + 1)
                    for h in range(NH):
                        for kb in range(nkb):
                            d = kb - 4 * s
                            tiles.append((h, kb, 0 if d < 0 else d * 128, d >= 0, len(tiles)))

                    def emit_qk(tl):
                        h, kb, q0, diag, idx = tl
                        si_ = idx % 2
                        S.op("pe", lambda e: e.matmul(out=st_ps[si_][:, q0:SB], lhsT=KT[0:70, h, kb * 128:(kb + 1) * 128],
                                                      rhs=QT[0:70, h, q0:SB], start=True, stop=(not diag)),
                             reads=[b_KT[kb // 4], b_QT], writes=[b_st[si_]])
                        if diag:
                            S.op("pe", lambda e: e.matmul(out=st_ps[si_][:, q0:q0 + 128], lhsT=ident_bf[:], rhs=maskneg[:],
                                                          start=False, stop=True),
                                 reads=[b_ident, b_mask], writes=[b_st[si_]])

                    emit_qk(tiles[0])
                    for ti, tl in enumerate(tiles):
                        h, kb, q0, diag, idx = tl
                        si_ = idx % 2
                        pi_ = idx % 3
                        oi_ = h % 2
                        if ti + 1 < len(tiles):
                            emit_qk(tiles[ti + 1])
                        S.op("act", lambda e: e.activation(out=PT[pi_][:, q0:SB], in_=st_ps[si_][:, q0:SB], func=AF.Exp),
                             reads=[b_st[si_]], writes=[b_PT[pi_]])
                        S.op("pe", lambda e: e.matmul(out=o_ps2[oi_][0:66, q0:SB], lhsT=VT[:, kb, h, :], rhs=PT[pi_][:, q0:SB],
                                                      start=(kb == 0), stop=(kb == nkb - 1)),
                             reads=[b_VT[kb // 4], b_PT[pi_]], writes=[b_o2[oi_]])
                        if kb != nkb - 1:
                            continue
                        o_ps, b_o = o_ps2[oi_], b_o2[oi_]
                        S.op("dve", lambda e: e.reciprocal(out=rs[64:66, :], in_=o_ps[64:66, :]), reads=[b_o], writes=[b_rs])
                        S.op("pe", lambda e: e.matmul(out=bc_ps[0:64, :], lhsT=ones_f[64:65, 0:64], rhs=rs[64:65, :], start=True, stop=True),
                             reads=[b_rs, b_onesf], writes=[b_bcps])
                        S.op("act", lambda e: e.copy(out=bc_sb[:], in_=bc_ps[0:64, :]), reads=[b_bcps], writes=[b_bc])
                        fi = h % 2
                        S.op("dve", lambda e: e.tensor_tensor(out=catF[fi][:], in0=o_ps[0:64, :], in1=bc_sb[:], op=ALU.mult),
                             reads=[b_o, b_bc], writes=[b_catF[fi]])
                        S.dma("sp", cat_scr[h * 64:(h + 1) * 64, c_lo:c_hi], catF[fi][:], reads=[b_catF[fi]])
                        if dbg and "ofox" in dbg:
                            S.op("act", lambda e, fi=fi: e.copy(out=dbg_sb[0:64, :], in_=catF[fi][:]), reads=[b_catF[fi]], writes=[b_dbg])
                            S.dma("sp", dbg_aps["ofox"][h * 64:(h + 1) * 64, c_lo:c_hi], dbg_sb[0:64, :], reads=[b_dbg])
                S.barrier()
        if "R" in phases:
            with ExitStack() as es:
                def sb(name, shape, dt=F32):
                    return es.enter_context(nc.sbuf_tensor(uniq(name), list(shape), dt))
                st = make_front(es)
                uT, b_uT = st["uT"], st["b_uT"]
                gT, b_gT = load_colvec(es, "gTr", g_pre, KC)
                NRC = 1824
                wr_bf = sb("wr_bf", [128, KC, NRC], BF16)
                b_wr = [Buf() for _ in range(KC)]
                load_weight_bf(st, wr_bf, b_wr, w_in, RBASE, NRC, gT, b_gT)
                lo_bf = sb("lo_bf", [128, RW], BF16)
                b_lo = Buf()
                gup_bf = sb("gup_bf", [128, RW], BF16)
                gup1_bf = sb("gup1_bf", [32, RW], BF16)
                b_gup = Buf()
                xt, b_xt = st["xt"], st["b_xt"]
                S.dma("sp", xt[0][0:64, 0:RW], rwkv_w_up[:, :], writes=[b_xt[0]])
                S.dma("sp", xt[0][64:128, 0:RW], rwkv_a_up[:, :], writes=[b_xt[0]])
                S.op("dve", lambda e: e.tensor_copy(out=lo_bf[:], in_=xt[0][:, 0:RW]), reads=[b_xt[0]], writes=[b_lo])
                S.dma("sp", xt[1][:, 0:RW], rwkv_g_up[0:128, :], writes=[b_xt[1]])
                S.op("dve", lambda e: e.tensor_copy(out=gup_bf[:], in_=xt[1][:, 0:RW]), reads=[b_xt[1]], writes=[b_gup])
                S.dma("sp", xt[0][0:32, 0:RW], rwkv_g_up[128:160, :], reads=[b_lo], writes=[b_xt[0]])
                S.op("dve", lambda e: e.tensor_copy(out=gup1_bf[:], in_=xt[0][0:32, 0:RW]), reads=[b_xt[0]], writes=[b_gup])
                w0T, b_w0 = load_colvec(es, "w0T", rwkv_w0, 4)
                a0T, b_a0 = load_colvec(es, "a0T", rwkv_a0, 4)
                kkT, b_kkv = load_colvec(es, "kkT", rwkv_k_k, 4)
                kaT, b_kav = load_colvec(es, "kaT", rwkv_k_a, 4)
                rkT, b_rkv = load_colvec(es, "rkT", rwkv_r_k, 4)
                lnwT, b_lnw = load_colvec(es, "lnwT", rwkv_ln_w, 4)
                lnbT, b_lnb = load_colvec(es, "lnbT", rwkv_ln_b, 4)
                groups = {}
                gl = []
                for hp in range(4):
                    gl.append((("r", hp), hp * 128, 128))
                    gl.append((("k", hp), 512 + hp * 128, 128))
                    gl.append((("v", hp), 1024 + hp * 128, 128))
                gl.append(("wa", 1536, 128))
                gl.append(("g0", 1664, 128))
                gl.append(("g1", 1792, 32))
                muT = sb("muT", [128, len(gl)], F32)
                b_mu = Buf()
                carry = sb("carry", [128, len(gl)], F32)
                b_carry = [Buf() for _ in gl]
                S.op("pool", lambda e: e.memset(carry[:], 0.0), writes=b_carry)
                for gi, (nm, c0, n) in enumerate(gl):
                    groups[nm] = (gi, c0, n)
                    S.dma("sp", muT[0:n, gi:gi + 1], shift_mu[c0:c0 + n].rearrange("(p o) -> p o", o=1), writes=[b_mu])
                cm = sb("cm", [128, NMASK, 128], F32)
                b_cm = Buf()
                S.dma("sp", cm[:], cmask[:, :, :], writes=[b_cm])
                mk0T_bf = sb("mk0T_bf", [128, 128], BF16)
                S.op("dve", lambda e: e.tensor_copy(out=mk0T_bf[:], in_=cm[:, 5, :]), reads=[b_cm], writes=[b_cm])
                bd_mean = sb("bd_mean", [128, 128], F32)
                S.op("dve", lambda e: e.tensor_scalar(out=bd_mean[:], in0=cm[:, 12, :], scalar1=1.0 / 64, scalar2=None, op0=ALU.mult),
                     reads=[b_cm], writes=[b_cm])
                ones128 = sb("ones128", [128, 128], F32)
                S.op("pool", lambda e: e.memset(ones128[:], 1.0), writes=[b_cm])

                H32 = [sb("H32_%d" % i, [128, 128], F32) for i in range(4)]
                Hbf = [sb("Hbf_%d" % i, [128, 128], BF16) for i in range(4)]
                b_H32 = [Buf() for _ in range(4)]
                b_Hbf = [Buf() for _ in range(4)]
                for i in range(4):
                    S.op("pool", lambda e, i=i: e.memset(H32[i][:], 0.0), writes=[b_H32[i]])
                    S.op("pool", lambda e, i=i: e.memset(Hbf[i][:], 0.0), writes=[b_Hbf[i]])

                def wt(name, dt=F32, n=SB):
                    return sb(name, [128, n], dt), Buf()
                raw = [sb("raw%d" % i, [128, SB + 1], F32) for i in range(2)]
                b_raw = [Buf(), Buf()]
                rawc = [0]
                dlt, b_dlt = wt("dlt")
                wa_s, b_was = wt("wa_s")
                g0_s, b_g0s = wt("g0_s")
                g1_s, b_g1s = wt("g1_s")
                lat_bf, b_lat = wt("lat_bf", BF16)
                sg_bf, b_sg = wt("sg_bf", BF16)
                sg1_bf, b_sg1 = wt("sg1_bf", BF16)
                r_s, b_rs_ = wt("r_s")
                k_s, b_ks = wt("k_s")
                v_s, b_vs = wt("v_s")
                lw, b_lw = wt("lw")
                a_t, b_a = wt("a_t")
                g_t, b_g = wt("g_t")
                kq, b_kq = wt("kq")
                tmpA, b_tmpA = wt("tmpA")
                kk, b_kk = wt("kk")
                kmod, b_kmod = wt("kmod")
                bb, b_bb = wt("bb")
                bonus, b_bonus = wt("bonus")
                cum, b_cum = wt("cum")
                E_in, b_Ein = wt("E_in")
                E_neg, b_Eneg = wt("E_neg")
                E_ex, b_Eex = wt("E_ex")
                E_end, b_Eend = wt("E_end")
                y_sb, b_y = wt("y_sb")
                AR = sb("AR", [128, 4, 2, 128], BF16)
                b_AR = Buf()
                BT, b_BT = wt("BT", BF16)
                KTt, b_KTt = wt("KTt", BF16)
                BH, b_BH = wt("BH", BF16)
                KH, b_KH = wt("KH", BF16)
                Vb, b_Vb = wt("Vb", BF16)
                catR, b_catR = wt("catR", BF16)
                TM = [sb("TM%d" % i, [128, 4, 128], BF16) for i in range(4)]
                b_TM = [Buf() for _ in range(4)]
                NMt = [sb("NM%d" % i, [128, 4, 128], BF16) for i in range(4)]
                b_NM = [Buf() for _ in range(4)]
                Dm = [sb("Dm%d" % i, [128, 128], BF16) for i in range(4)]
                b_Dm = [Buf() for _ in range(4)]
                DTm = [sb("DTm%d" % i, [128, 128], BF16) for i in range(4)]
                b_DTm = [Buf() for _ in range(4)]
                Gm = [sb("Gm%d" % i, [128, 128], BF16) for i in range(4)]
                b_Gm = [Buf() for _ in range(4)]
                X2b = [sb("X2b%d" % i, [128, 64], BF16) for i in range(4)]
                b_X2b = [Buf() for _ in range(4)]
                U2s = [sb("U2s%d" % i, [128, 128], F32) for i in range(2)]
                b_U2s = [Buf(), Buf()]
                WTb = [sb("WTb%d" % i, [128, 128], BF16) for i in range(2)]
                b_WTb = [Buf(), Buf()]
                Ub = sb("Ub", [128, 128], BF16)
                b_Ub = Buf()
                dbg_sb = sb("dbg_sbr", [128, SB], F32)
                b_dbg = Buf()
                M1 = [PB[4], PB[5]]
                b_M1 = [b_PB[4], b_PB[5]]
                sA = [PB[i][:, 0:128] for i in range(4)]
                b_sA = [b_PB[i] for i in range(4)]
                sB = [PB[i][:, 128:256] for i in range(4)]
                b_sB = [b_PB[i] for i in range(4)]
                u2_ps, uh_ps = [PB[6][:, i * 128:(i + 1) * 128] for i in range(2)]
                y2_ps = [PB[6][:, (2 + i) * 128:(3 + i) * 128] for i in range(2)]
                b_u2 = b_wt = b_uh = b_yps = b_PB[6]

                def shifted(nm, dst, b_dst):
                    gi, c0, n = groups[nm]

                    def ev(pp, bp):
                        ri = rawc[0] % 2
                        rawc[0] += 1
                        rw_, brw = raw[ri], b_raw[ri]
                        S.op("act", lambda e: e.copy(out=rw_[0:n, 1:SB + 1], in_=pp[0:n, :]), reads=[bp], writes=[brw])
                        S.op("pool", lambda e: e.tensor_copy(out=rw_[0:n, 0:1], in_=carry[0:n, gi:gi + 1]),
                             reads=[b_carry[gi]], writes=[brw])
                        S.op("pool", lambda e: e.tensor_copy(out=carry[0:n, gi:gi + 1], in_=rw_[0:n, SB:SB + 1]),
                             reads=[brw], writes=[b_carry[gi]])
                        S.op("dve", lambda e: e.tensor_tensor(out=dlt[0:n, :], in0=rw_[0:n, 0:SB], in1=rw_[0:n, 1:SB + 1], op=ALU.subtract),
                             reads=[brw], writes=[b_dlt])
                        S.op("dve", lambda e: e.scalar_tensor_tensor(out=dst[0:n, :], in0=dlt[0:n, :], scalar=muT[0:n, gi:gi + 1],
                                                                     in1=rw_[0:n, 1:SB + 1], op0=ALU.mult, op1=ALU.add),
                             reads=[b_dlt, brw, b_mu], writes=[b_dst])
                    proj_fm(wr_bf, b_wr, st, c0, n, ev)

                def v4(ap):
                    return ap.rearrange("p (c t) -> p c t", c=4)

                for s in range(NSB):
                    c_lo, c_hi = s * SB, (s + 1) * SB
                    front(st, s)
                    shifted("wa", wa_s, b_was)
                    shifted("g0", g0_s, b_g0s)
                    shifted("g1", g1_s, b_g1s)
                    S.op("act", lambda e: e.activation(out=lat_bf[0:64, :], in_=wa_s[0:64, :], func=AF.Tanh), reads=[b_was], writes=[b_lat])
                    S.op("act", lambda e: e.copy(out=lat_bf[64:128, :], in_=wa_s[64:128, :]), reads=[b_was], writes=[b_lat])
                    S.op("act", lambda e: e.activation(out=sg_bf[:], in_=g0_s[:], func=AF.Sigmoid), reads=[b_g0s], writes=[b_sg])
                    S.op("act", lambda e: e.activation(out=sg1_bf[0:32, :], in_=g1_s[0:32, :], func=AF.Sigmoid), reads=[b_g1s], writes=[b_sg1])
                    for hp in range(4):
                        hc = slice(hp * 128, (hp + 1) * 128)
                        shifted(("r", hp), r_s, b_rs_)
                        shifted(("k", hp), k_s, b_ks)
                        shifted(("v", hp), v_s, b_vs)
                        p0, bp0 = PB[0], b_PB[0]
                        S.op("pe", lambda e: e.matmul(out=p0[:], lhsT=lo_bf[0:64, hc], rhs=lat_bf[0:64, :], start=True, stop=True),
                             reads=[b_lo, b_lat], writes=[bp0])
                        S.op("act", lambda e: e.activation(out=lw[:], in_=p0[:], func=AF.Sigmoid, bias=w0T[:, hp:hp + 1]),
                             reads=[bp0, b_w0], writes=[b_lw])
                        S.op("pool", lambda e: e.tensor_scalar(out=lw[:], in0=lw[:], scalar1=-0.6065306597126334, scalar2=None, op0=ALU.mult),
                             reads=[b_lw], writes=[b_lw])
                        p1, bp1 = PB[1], b_PB[1]
                        S.op("pe", lambda e: e.matmul(out=p1[:], lhsT=lo_bf[64:128, hc], rhs=lat_bf[64:128, :], start=True, stop=True),
                             reads=[b_lo, b_lat], writes=[bp1])
                        S.op("act", lambda e: e.activation(out=a_t[:], in_=p1[:], func=AF.Sigmoid, bias=a0T[:, hp:hp + 1]),
                             reads=[bp1, b_a0], writes=[b_a])
                        S.op("pe", lambda e: e.matmul(out=p0[:], lhsT=gup_bf[:, hc], rhs=sg_bf[:], start=True, stop=False),
                             reads=[b_gup, b_sg], writes=[bp0])
                        S.op("pe", lambda e: e.matmul(out=p0[:], lhsT=gup1_bf[0:32, hc], rhs=sg1_bf[0:32, :], start=False, stop=True),
                             reads=[b_gup, b_sg1], writes=[bp0])
                        S.op("act", lambda e: e.copy(out=g_t[:], in_=p0[:]), reads=[bp0], writes=[b_g])
                        S.op("dve", lambda e: e.tensor_scalar(out=kq[:], in0=k_s[:], scalar1=kkT[:, hp:hp + 1], scalar2=None, op0=ALU.mult),
                             reads=[b_ks, b_kkv], writes=[b_kq])
                        S.op("pool", lambda e: e.tensor_tensor(out=tmpA[:], in0=kq[:], in1=kq[:], op=ALU.mult), reads=[b_kq], writes=[b_tmpA])
                        S.op("pe", lambda e: e.matmul(out=p1[:], lhsT=cm[:, 12, :], rhs=tmpA[:], start=True, stop=True),
                             reads=[b_cm, b_tmpA], writes=[bp1])
                        S.op("act", lambda e: e.sqrt(out=tmpA[:], in_=p1[:]), reads=[bp1], writes=[b_tmpA])
                        S.op("dve", lambda e: e.tensor_scalar(out=tmpA[:], in0=tmpA[:], scalar1=1e-12, scalar2=None, op0=ALU.max),
                             reads=[b_tmpA], writes=[b_tmpA])
                        S.op("dve", lambda e: e.reciprocal(out=tmpA[:], in_=tmpA[:]), reads=[b_tmpA], writes=[b_tmpA])
                        S.op("pool", lambda e: e.tensor_tensor(out=kk[:], in0=kq[:], in1=tmpA[:], op=ALU.mult),
                             reads=[b_kq, b_tmpA], writes=[b_kk])
                        S.op("dve", lambda e: e.tensor_scalar(out=kmod[:], in0=a_t[:], scalar1=-1.0, scalar2=kaT[:, hp:hp + 1],
                                                              op0=ALU.add, op1=ALU.mult), reads=[b_a, b_kav], writes=[b_kmod])
                        S.op("dve", lambda e: e.scalar_tensor_tensor(out=kmod[:], in0=kmod[:], scalar=1.0, in1=k_s[:],
                                                                     op0=ALU.add, op1=ALU.mult), reads=[b_kmod, b_ks], writes=[b_kmod])
                        S.op("pool", lambda e: e.tensor_tensor(out=bb[:], in0=kk[:], in1=a_t[:], op=ALU.mult), reads=[b_kk, b_a], writes=[b_bb])
                        S.op("dve", lambda e: e.scalar_tensor_tensor(out=tmpA[:], in0=r_s[:], scalar=rkT[:, hp:hp + 1], in1=kmod[:],
                                                                     op0=ALU.mult, op1=ALU.mult), reads=[b_rs_, b_rkv, b_kmod], writes=[b_tmpA])
                        S.op("pe", lambda e: e.matmul(out=p1[:], lhsT=cm[:, 12, :], rhs=tmpA[:], start=True, stop=True),
                             reads=[b_cm, b_tmpA], writes=[bp1])
                        S.op("dve", lambda e: e.tensor_tensor(out=bonus[:], in0=p1[:], in1=v_s[:], op=ALU.mult), reads=[bp1, b_vs], writes=[b_bonus])
                        for c in range(4):
                            cc = slice(c * 128, (c + 1) * 128)
                            S.op("dve", lambda e, cc=cc: e.tensor_tensor_scan(out=cum[:, cc], data0=ones128[:], data1=lw[:, cc], initial=0.0,
                                                                              op0=ALU.mult, op1=ALU.add), reads=[b_lw, b_cm], writes=[b_cum])
                        S.op("act", lambda e: e.activation(out=E_in[:], in_=cum[:], func=AF.Exp), reads=[b_cum], writes=[b_Ein])
                        S.op("act", lambda e: e.activation(out=E_neg[:], in_=cum[:], func=AF.Exp, scale=-1.0), reads=[b_cum], writes=[b_Eneg])
                        for c in range(4):
                            cc = slice(c * 128, (c + 1) * 128)
                            S.op("act", lambda e, cc=cc, c=c: e.activation(out=E_end[:, cc], in_=cum[:, cc], func=AF.Exp, scale=-1.0,
                                                                           bias=cum[:, c * 128 + 127:c * 128 + 128]),
                                 reads=[b_cum], writes=[b_Eend])
                        S.op("pool", lambda e: e.tensor_tensor(out=tmpA[:], in0=cum[:], in1=lw[:], op=ALU.subtract), reads=[b_cum, b_lw], writes=[b_tmpA])
                        S.op("act", lambda e: e.activation(out=E_ex[:], in_=tmpA[:], func=AF.Exp), reads=[b_tmpA], writes=[b_Eex])
                        S.op("dve", lambda e: e.scalar_tensor_tensor(out=AR[:, :, 0, :], in0=v4(kk[:]), scalar=-1.0, in1=v4(E_ex[:]),
                                                                     op0=ALU.mult, op1=ALU.mult), reads=[b_kk, b_Eex], writes=[b_AR])
                        S.op("pool", lambda e: e.tensor_tensor(out=AR[:, :, 1, :], in0=v4(r_s[:]), in1=v4(E_in[:]), op=ALU.mult),
                             reads=[b_rs_, b_Ein], writes=[b_AR])
                        S.op("dve", lambda e: e.tensor_tensor(out=BT[:], in0=bb[:], in1=E_neg[:], op=ALU.mult), reads=[b_bb, b_Eneg], writes=[b_BT])
                        S.op("pool", lambda e: e.tensor_tensor(out=KTt[:], in0=kmod[:], in1=E_neg[:], op=ALU.mult), reads=[b_kmod, b_Eneg], writes=[b_KTt])
                        S.op("dve", lambda e: e.tensor_tensor(out=BH[:], in0=bb[:], in1=E_end[:], op=ALU.mult), reads=[b_bb, b_Eend], writes=[b_BH])
                        S.op("pool", lambda e: e.tensor_tensor(out=KH[:], in0=kmod[:], in1=E_end[:], op=ALU.mult), reads=[b_kmod, b_Eend], writes=[b_KH])
                        S.op("act", lambda e: e.copy(out=Vb[:], in_=v_s[:]), reads=[b_vs], writes=[b_Vb])

                        stop = (dbg or {}).get("stop", "")
                        if stop == "A":
                            continue
                        for cp in range(2):
                            chains = [(2 * cp + ci_, e_) for ci_ in range(2) for e_ in range(2)]
                            for ci_ in range(2):
                                c = 2 * cp + ci_
                                cc = slice(c * 128, (c + 1) * 128)
                                srcs = [(AR[:, c, 0, :], b_AR), (Vb[:, cc], b_Vb), (BH[:, cc], b_BH), (KH[:, cc], b_KH)]
                                for k_, (src, bsrc) in enumerate(srcs):
                                    S.op("pe", lambda e, k_=k_, src=src: e.transpose(out=tp_ps[:, k_, :], in_=src, identity=ident_bf[:]),
                                         reads=[bsrc, b_ident], writes=[b_tp])
                                S.op("act", lambda e, c=c: e.copy(out=TM[c][:], in_=tp_ps[:, 0:4, :]), reads=[b_tp], writes=[b_TM[c]])
                            for ch, (c, e_) in enumerate(chains):
                                pr = slice(e_ * 64, (e_ + 1) * 64)
                                cc = slice(c * 128, (c + 1) * 128)
                                m1, bm1 = M1[ch % 2], b_M1[ch % 2]
                                S.op("pe", lambda e, pr=pr, cc=cc, c=c, m1=m1: e.matmul(out=m1[:, 0:256], lhsT=BT[pr, cc],
                                                                                     rhs=AR[pr, c, :, :], start=True, stop=True),
                                     reads=[b_BT, b_AR], writes=[bm1])
                                S.op("pe", lambda e, pr=pr, cc=cc, c=c, m1=m1: e.matmul(out=m1[:, 256:512], lhsT=KTt[pr, cc],
                                                                                     rhs=AR[pr, c, :, :], start=True, stop=True),
                                     reads=[b_KTt, b_AR], writes=[bm1])
                                S.op("dve", lambda e, ch=ch, m1=m1: e.tensor_tensor(
                                    out=NMt[ch][:], in0=m1[:].rearrange("p (a t) -> p a t", a=4),
                                    in1=cm[:, 0:4, :],
                                    op=ALU.mult), reads=[bm1, b_cm], writes=[b_NM[ch]])
                                S.op("pe", lambda e, pr=pr, cc=cc, c=c, ch=ch: e.matmul(out=sA[ch], lhsT=AR[pr, c, 0, :], rhs=BT[pr, cc],
                                                                                     start=True, stop=True),
                                     reads=[b_AR, b_BT], writes=[b_sA[ch]])
                                S.op("dve", lambda e, ch=ch: e.tensor_tensor(out=Dm[ch][:], in0=sA[ch], in1=cm[:, 4, :], op=ALU.mult),
                                     reads=[b_sA[ch], b_cm], writes=[b_Dm[ch]])
                                S.op("pool", lambda e, ch=ch: e.tensor_tensor(out=Dm[ch][:], in0=Dm[ch][:], in1=ident_bf[:], op=ALU.add),
                                     reads=[b_Dm[ch], b_ident], writes=[b_Dm[ch]])
                                S.op("pool", lambda e, ch=ch: e.tensor_tensor(out=DTm[ch][:], in0=NMt[ch][:, 0, :], in1=mk0T_bf[:], op=ALU.mult),
                                     reads=[b_NM[ch], b_cm], writes=[b_DTm[ch]])
                                S.op("pool", lambda e, ch=ch: e.tensor_tensor(out=DTm[ch][:], in0=DTm[ch][:], in1=ident_bf[:], op=ALU.add),
                                     reads=[b_DTm[ch], b_ident], writes=[b_DTm[ch]])
                            if stop == "B":
                                continue
                            for li, mm in enumerate(LEVELS):
                                last = (li == len(LEVELS) - 1)
                                for ch in range(4):
                                    S.op("pe", lambda e, ch=ch: e.matmul(out=sA[ch], lhsT=NMt[ch][:, 0, :], rhs=Dm[ch][:], start=True, stop=True),
                                         reads=[b_NM[ch], b_Dm[ch]], writes=[b_sA[ch]])
                                    S.op("dve", lambda e, ch=ch, li=li: e.tensor_tensor(out=Gm[ch][:], in0=sA[ch], in1=cm[:, 6 + li, :], op=ALU.mult),
                                         reads=[b_sA[ch], b_cm], writes=[b_Gm[ch]])
                                for ch in range(4):
                                    if not last:
                                        S.op("pe", lambda e, ch=ch: e.matmul(out=sA[ch], lhsT=DTm[ch][:], rhs=Gm[ch][:], start=True, stop=False),
                                             reads=[b_DTm[ch], b_Gm[ch]], writes=[b_sA[ch]])
                                        S.op("pe", lambda e, ch=ch: e.matmul(out=sA[ch], lhsT=ident_bf[:], rhs=Dm[ch][:], start=False, stop=True),
                                             reads=[b_ident, b_Dm[ch]], writes=[b_sA[ch]])
                                    S.op("pe", lambda e, ch=ch: e.matmul(out=sB[ch], lhsT=Gm[ch][:], rhs=DTm[ch][:], start=True, stop=False),
                                         reads=[b_DTm[ch], b_Gm[ch]], writes=[b_sB[ch]])
                                    S.op("pe", lambda e, ch=ch: e.matmul(out=sB[ch], lhsT=ident_bf[:], rhs=DTm[ch][:], start=False, stop=True),
                                         reads=[b_ident, b_DTm[ch]], writes=[b_sB[ch]])
                                    if not last:
                                        S.op("act", lambda e, ch=ch: e.copy(out=Dm[ch][:], in_=sA[ch]), reads=[b_sA[ch]], writes=[b_Dm[ch]])
                                    S.op("act", lambda e, ch=ch: e.copy(out=DTm[ch][:], in_=sB[ch]), reads=[b_sB[ch]], writes=[b_DTm[ch]])
                            if stop == "C":
                                continue
                            for ch, (c, e_) in enumerate(chains):
                                pr = slice(e_ * 64, (e_ + 1) * 64)
                                S.op("pe", lambda e, ch=ch, c=c, pr=pr: e.matmul(out=sA[ch][:, 0:64], lhsT=NMt[ch][:, 2, :], rhs=TM[c][:, 1, pr],
                                                                              start=True, stop=True),
                                     reads=[b_NM[ch], b_TM[c]], writes=[b_sA[ch]])
                                S.op("act", lambda e, ch=ch: e.copy(out=X2b[ch][:], in_=sA[ch][:, 0:64]), reads=[b_sA[ch]], writes=[b_X2b[ch]])
                            for ci_ in range(2):
                                c = 2 * cp + ci_
                                for e_ in range(2):
                                    ch = ci_ * 2 + e_
                                    pr = slice(e_ * 64, (e_ + 1) * 64)
                                    S.op("pe", lambda e, ch=ch, pr=pr: e.matmul(out=u2_ps[:, pr], lhsT=DTm[ch][:], rhs=X2b[ch][:], start=True, stop=True),
                                         reads=[b_DTm[ch], b_X2b[ch]], writes=[b_u2])
                                    S.op("pe", lambda e, ch=ch, c=c: e.matmul(out=sA[ch], lhsT=TM[c][:, 0, :], rhs=DTm[ch][:], start=True, stop=True),
                                         reads=[b_DTm[ch], b_TM[c]], writes=[b_sA[ch]])
                                    S.op("dve", lambda e, ci_=ci_, ch=ch, pr=pr: e.tensor_copy(out=WTb[ci_][pr, :], in_=sA[ch][pr, :]),
                                         reads=[b_sA[ch]], writes=[b_WTb[ci_]])
                                S.op("act", lambda e, ci_=ci_: e.copy(out=U2s[ci_][:], in_=u2_ps), reads=[b_u2], writes=[b_U2s[ci_]])
                            if stop == "D":
                                continue
                            for ci_ in range(2):
                                c = 2 * cp + ci_
                                cc = slice(c * 128, (c + 1) * 128)
                                S.op("pe", lambda e, ci_=ci_: e.matmul(out=uh_ps, lhsT=WTb[ci_][:], rhs=Hbf[hp][:], start=True, stop=True),
                                     reads=[b_WTb[ci_], b_Hbf[hp]], writes=[b_uh])
                                S.op("dve", lambda e, ci_=ci_: e.tensor_tensor(out=Ub[:], in0=uh_ps, in1=U2s[ci_][:], op=ALU.add),
                                     reads=[b_uh, b_U2s[ci_]], writes=[b_Ub])
                                for e_ in range(2):
                                    ch = ci_ * 2 + e_
                                    pr = slice(e_ * 64, (e_ + 1) * 64)
                                    yp = y2_ps[e_]
                                    S.op("pe", lambda e, c=c, yp=yp: e.matmul(out=yp, lhsT=Hbf[hp][:], rhs=AR[:, c, 1, :], start=True, stop=False),
                                         reads=[b_Hbf[hp], b_AR], writes=[b_yps])
                                    S.op("pe", lambda e, ch=ch, yp=yp: e.matmul(out=yp, lhsT=Ub[:], rhs=NMt[ch][:, 1, :], start=False, stop=False),
                                         reads=[b_Ub, b_NM[ch]], writes=[b_yps])
                                    S.op("pe", lambda e, ch=ch, c=c, yp=yp: e.matmul(out=yp, lhsT=TM[c][:, 1, :], rhs=NMt[ch][:, 3, :], start=False, stop=True),
                                         reads=[b_TM[c], b_NM[ch]], writes=[b_yps])
                                for e_ in range(2):
                                    pr = slice(e_ * 64, (e_ + 1) * 64)
                                    S.op("act", lambda e, cc=cc, pr=pr, e_=e_: e.copy(out=y_sb[pr, cc], in_=y2_ps[e_][pr, :]), reads=[b_yps], writes=[b_y])
                                S.op("pe", lambda e, c=c: e.matmul(out=uh_ps, lhsT=TM[c][:, 2, :], rhs=Ub[:], start=True, stop=False),
                                     reads=[b_TM[c], b_Ub], writes=[b_uh])
                                S.op("pe", lambda e, c=c: e.matmul(out=uh_ps, lhsT=TM[c][:, 3, :], rhs=TM[c][:, 1, :], start=False, stop=True),
                                     reads=[b_TM[c]], writes=[b_uh])
                                for e_ in range(2):
                                    pr = slice(e_ * 64, (e_ + 1) * 64)
                                    S.op("dve", lambda e, pr=pr, c=c: e.scalar_tensor_tensor(
                                        out=H32[hp][pr, pr], in0=H32[hp][pr, pr], scalar=E_in[pr, c * 128 + 127:c * 128 + 128],
                                        in1=uh_ps[pr, pr], op0=ALU.mult, op1=ALU.add),
                                        reads=[b_H32[hp], b_Ein, b_uh], writes=[b_H32[hp]])
                                S.op("pool", lambda e: e.tensor_copy(out=Hbf[hp][:], in_=H32[hp][:]), reads=[b_H32[hp]], writes=[b_Hbf[hp]])
                        if dbg and "yraw" in dbg:
                            S.op("act", lambda e: e.copy(out=dbg_sb[:], in_=y_sb[:]), reads=[b_y], writes=[b_dbg])
                            S.dma("sp", dbg_aps["yraw"][hp * 128:(hp + 1) * 128, c_lo:c_hi], dbg_sb[:], reads=[b_dbg])
                        S.op("pe", lambda e: e.matmul(out=p0[:], lhsT=bd_mean[:], rhs=y_sb[:], start=True, stop=True),
                             reads=[b_cm, b_y], writes=[bp0])
                        S.op("dve", lambda e: e.tensor_tensor(out=y_sb[:], in0=y_sb[:], in1=p0[:], op=ALU.subtract), reads=[b_y, bp0], writes=[b_y])
                        S.op("pool", lambda e: e.tensor_tensor(out=tmpA[:], in0=y_sb[:], in1=y_sb[:], op=ALU.mult), reads=[b_y], writes=[b_tmpA])
                        S.op("pe", lambda e: e.matmul(out=p1[:], lhsT=bd_mean[:], rhs=tmpA[:], start=True, stop=True),
                             reads=[b_cm, b_tmpA], writes=[bp1])
                        S.op("dve", lambda e: e.tensor_scalar(out=tmpA[:], in0=p1[:], scalar1=LNX_EPS, scalar2=None, op0=ALU.add),
                             reads=[bp1], writes=[b_tmpA])
                        S.op("act", lambda e: e.sqrt(out=tmpA[:], in_=tmpA[:]), reads=[b_tmpA], writes=[b_tmpA])
                        S.op("dve", lambda e: e.reciprocal(out=tmpA[:], in_=tmpA[:]), reads=[b_tmpA], writes=[b_tmpA])
                        S.op("pool", lambda e: e.tensor_tensor(out=y_sb[:], in0=y_sb[:], in1=tmpA[:], op=ALU.mult), reads=[b_y, b_tmpA], writes=[b_y])
                        S.op("dve", lambda e: e.tensor_scalar(out=y_sb[:], in0=y_sb[:], scalar1=lnwT[:, hp:hp + 1], scalar2=lnbT[:, hp:hp + 1],
                                                              op0=ALU.mult, op1=ALU.add), reads=[b_y, b_lnw, b_lnb], writes=[b_y])
                        S.op("pool", lambda e: e.tensor_tensor(out=y_sb[:], in0=y_sb[:], in1=bonus[:], op=ALU.add), reads=[b_y, b_bonus], writes=[b_y])
                        S.op("dve", lambda e: e.tensor_tensor(out=catR[:], in0=y_sb[:], in1=g_t[:], op=ALU.mult), reads=[b_y, b_g], writes=[b_catR])
                        S.dma("sp", cat_scr[512 + hp * 128:512 + (hp + 1) * 128, c_lo:c_hi], catR[:], reads=[b_catR])
                        if dbg and "orwkv" in dbg:
                            S.op("act", lambda e: e.copy(out=dbg_sb[:], in_=catR[:]), reads=[b_catR], writes=[b_dbg])
                            S.dma("sp", dbg_aps["orwkv"][hp * 128:(hp + 1) * 128, c_lo:c_hi], dbg_sb[:], reads=[b_dbg])
                S.barrier()
        if "O" in phases:
            with ExitStack() as es:
                def sb(name, shape, dt=F32):
                    return es.enter_context(nc.sbuf_tensor(uniq(name), list(shape), dt))
                UT = 256
                NU = T // UT
                stg = [sb("stg%d" % i, [128, D], F32) for i in range(2)]
                b_stg = [Buf(), Buf()]
                st = {"xt": stg, "b_xt": b_stg}
                gfT, b_gfT = load_colvec(es, "gfT", gf_pre, KC)
                wout_bf = sb("wout_bf", [128, KC, D], BF16)
                b_wout = [Buf() for _ in range(KC)]
                wg_bf = sb("wg_bf", [128, KC, DFF], BF16)
                b_wg = [Buf() for _ in range(KC)]
                wu_bf = sb("wu_bf", [128, KC, DFF], BF16)
                b_wu = [Buf() for _ in range(KC)]
                wd_bf = sb("wd_bf", [128, NFF, D], BF16)
                b_wd = [Buf() for _ in range(NFF)]
                load_weight_bf(st, wout_bf, b_wout, w_out, 0, D, None, None)
                load_weight_bf(st, wg_bf, b_wg, w_gate, 0, DFF, gfT, b_gfT)
                load_weight_bf(st, wu_bf, b_wu, w_up, 0, DFF, gfT, b_gfT)
                load_weight_bf(st, wd_bf, b_wd, w_down, 0, D, None, None, nk=NFF)
                gpost_bc = sb("gpost_bc", [128, D], F32)
                gfpost_bc = sb("gfpost_bc", [128, D], F32)
                b_gbc = Buf()
                S.dma("sp", gpost_bc[:], g_post.partition_broadcast(128), writes=[b_gbc])
                S.dma("sp", gfpost_bc[:], gf_post.partition_broadcast(128), writes=[b_gbc])
                catT = sb("catT", [128, KC, UT], BF16)
                b_catT = Buf()
                zn = sb("zn", [128, D], BF16)
                b_zn = Buf()
                zT = sb("zT", [128, KC, UT], BF16)
                b_zT = Buf()
                aT = sb("aT", [128, NFF, UT], BF16)
                b_aT = Buf()
                junk = sb("junkO", [128, D], BF16)
                b_junk = Buf()
                sgt = [sb("sgt%d" % i, [128, UT], F32) for i in range(2)]
                b_sgt = [Buf(), Buf()]
                t1 = sb("t1", [128, D], F32)
                b_t1 = Buf()
                ssO = sb("ssO", [128, 4], F32)
                b_ssO = Buf()
                cat_v = cat_scr.rearrange("(k p) t -> p k t", p=128)

                def rstd_from(srcs, bsrcs):
                    for i, (ap_, b_) in enumerate(zip(srcs, bsrcs)):
                        n = ap_.shape[1]
                        S.op("act", lambda e, ap_=ap_, i=i, n=n: e.activation(out=junk[:, 0:n], in_=ap_, func=AF.Square, accum_out=ssO[:, i:i + 1]),
                             reads=[b_], writes=[b_junk, b_ssO])
                    if len(srcs) == 2:
                        S.op("dve", lambda e: e.tensor_tensor(out=ssO[:, 2:3], in0=ssO[:, 0:1], in1=ssO[:, 1:2], op=ALU.add),
                             reads=[b_ssO], writes=[b_ssO])
                        src = ssO[:, 2:3]
                    else:
                        src = ssO[:, 0:1]
                    S.op("dve", lambda e: e.tensor_scalar(out=ssO[:, 2:3], in0=src, scalar1=1.0 / D, scalar2=EPS, op0=ALU.mult, op1=ALU.add),
                         reads=[b_ssO], writes=[b_ssO])
                    S.op("act", lambda e: e.sqrt(out=ssO[:, 2:3], in_=ssO[:, 2:3]), reads=[b_ssO], writes=[b_ssO])
                    S.op("dve", lambda e: e.reciprocal(out=ssO[:, 2:3], in_=ssO[:, 2:3]), reads=[b_ssO], writes=[b_ssO])

                for u in range(NU):
                    t0 = u * UT
                    S.dma("sp", catT[:], cat_v[:, :, t0:t0 + UT], writes=[b_catT])
                    for j in range(2):
                        tj = t0 + j * 128
                        S.dma("sp", stg[j][:], x[tj:tj + 128, :], writes=[b_stg[j]])
                        for half in range(2):
                            for kc in range(KC):
                                S.op("pe", lambda e, kc=kc, half=half, j=j: e.matmul(
                                    out=PB[half][:], lhsT=catT[:, kc, j * 128:(j + 1) * 128], rhs=wout_bf[:, kc, half * 512:(half + 1) * 512],
                                    start=(kc == 0), stop=(kc == KC - 1)), reads=[b_catT, b_wout[kc]], writes=[b_PB[half]])
                        rstd_from([PB[0][:], PB[1][:]], [b_PB[0], b_PB[1]])
                        for half in range(2):
                            S.op("act", lambda e, half=half: e.activation(out=t1[:, half * 512:(half + 1) * 512], in_=PB[half][:], func=AF.Copy,
                                                                          scale=ssO[:, 2:3]), reads=[b_PB[half], b_ssO], writes=[b_t1])
                        S.op("pool", lambda e: e.tensor_tensor(out=t1[:], in0=t1[:], in1=gpost_bc[:], op=ALU.mult), reads=[b_t1, b_gbc], writes=[b_t1])
                        S.op("dve", lambda e, j=j: e.tensor_tensor(out=stg[j][:], in0=stg[j][:], in1=t1[:], op=ALU.add),
                             reads=[b_stg[j], b_t1], writes=[b_stg[j]])
                        if dbg and "h" in dbg:
                            S.dma("sp", dbg_aps["h"][tj:tj + 128, :], stg[j][:], reads=[b_stg[j]])
                        rstd_from([stg[j][:]], [b_stg[j]])
                        S.op("act", lambda e, j=j: e.activation(out=zn[:], in_=stg[j][:], func=AF.Copy, scale=ssO[:, 2:3]),
                             reads=[b_stg[j], b_ssO], writes=[b_zn])
                        for kc in range(KC):
                            S.op("pe", lambda e, kc=kc: e.transpose(out=tp_ps[:, kc, :], in_=zn[:, kc * 128:(kc + 1) * 128], identity=ident_bf[:]),
                                 reads=[b_zn, b_ident], writes=[b_tp])
                        S.op("dve", lambda e, j=j: e.tensor_copy(out=zT[:, :, j * 128:(j + 1) * 128], in_=tp_ps[:, :, :]), reads=[b_tp], writes=[b_zT])
                    for ffc in range(NFF):
                        gi_ = 2 + 2 * (ffc % 2)
                        ui_ = 3 + 2 * (ffc % 2)
                        fc = slice(ffc * 128, (ffc + 1) * 128)
                        for kc in range(KC):
                            S.op("pe", lambda e, kc=kc, fc=fc, gi_=gi_: e.matmul(out=PB[gi_][:, 0:UT], lhsT=wg_bf[:, kc, fc], rhs=zT[:, kc, :],
                                                                                 start=(kc == 0), stop=(kc == KC - 1)),
                                 reads=[b_wg[kc], b_zT], writes=[b_PB[gi_]])
                        for kc in range(KC):
                            S.op("pe", lambda e, kc=kc, fc=fc, ui_=ui_: e.matmul(out=PB[ui_][:, 0:UT], lhsT=wu_bf[:, kc, fc], rhs=zT[:, kc, :],
                                                                                 start=(kc == 0), stop=(kc == KC - 1)),
                                 reads=[b_wu[kc], b_zT], writes=[b_PB[ui_]])
                        si_ = ffc % 2
                        S.op("act", lambda e, gi_=gi_, si_=si_: e.activation(out=sgt[si_][:], in_=PB[gi_][:, 0:UT], func=AF.Silu),
                             reads=[b_PB[gi_]], writes=[b_sgt[si_]])
                        S.op("dve", lambda e, ui_=ui_, si_=si_, ffc=ffc: e.tensor_tensor(out=aT[:, ffc, :], in0=PB[ui_][:, 0:UT], in1=sgt[si_][:], op=ALU.mult),
                             reads=[b_PB[ui_], b_sgt[si_]], writes=[b_aT])
                    for j in range(2):
                        tj = t0 + j * 128
                        for half in range(2):
                            for ffc in range(NFF):
                                S.op("pe", lambda e, ffc=ffc, half=half, j=j: e.matmul(
                                    out=PB[half][:], lhsT=aT[:, ffc, j * 128:(j + 1) * 128], rhs=wd_bf[:, ffc, half * 512:(half + 1) * 512],
                                    start=(ffc == 0), stop=(ffc == NFF - 1)), reads=[b_aT, b_wd[ffc]], writes=[b_PB[half]])
                        rstd_from([PB[0][:], PB[1][:]], [b_PB[0], b_PB[1]])
                        for half in range(2):
                            S.op("act", lambda e, half=half: e.activation(out=t1[:, half * 512:(half + 1) * 512], in_=PB[half][:], func=AF.Copy,
                                                                          scale=ssO[:, 2:3]), reads=[b_PB[half], b_ssO], writes=[b_t1])
                        S.op("pool", lambda e: e.tensor_tensor(out=t1[:], in0=t1[:], in1=gfpost_bc[:], op=ALU.mult), reads=[b_t1, b_gbc], writes=[b_t1])
                        S.op("dve", lambda e, j=j: e.tensor_tensor(out=t1[:], in0=t1[:], in1=stg[j][:], op=ALU.add),
                             reads=[b_stg[j], b_t1], writes=[b_t1])
                        S.dma("sp", out[tj:tj + 128, :], t1[:], reads=[b_t1])

        S.wait_tokens("sp", [t for e in S.ENGS for t in S.dtoks[e]])
        S.emit()
    return nc


WNAMES = ["attn_norm_pre", "attn_norm_post", "w_in", "fox_forget_bias", "shift_mu", "rwkv_w0", "rwkv_w_up", "rwkv_a0",
          "rwkv_a_up", "rwkv_g_up", "rwkv_k_k", "rwkv_k_a", "rwkv_r_k", "rwkv_ln_w", "rwkv_ln_b", "w_out",
          "ffn_norm_pre", "ffn_norm_post", "ffn_w_gate", "ffn_w_up", "ffn_w_down"]


def make_in_map(inputs, b, T):
    m = {"x": np.ascontiguousarray(np.asarray(inputs["x"], dtype=np.float32)[b, :T])}
    for k in WNAMES:
        a = np.asarray(inputs[k], dtype=np.float32)[0]
        if k == "rwkv_r_k":
            a = a.reshape(-1)
        m[k] = np.ascontiguousarray(a)
    m["cmask"] = make_cmask()
    return m


def kernel(**inputs):
    x = np.asarray(inputs["x"])
    B, T, _ = x.shape
    nc = build_nc(T)
    in_maps = [make_in_map(inputs, b, T) for b in range(B)]
    res = run_bass_kernel_spmd(nc, in_maps, core_ids=list(range(B)))
    return np.stack([np.asarray(r["out"], dtype=np.float32) for r in res.results], axis=0)
```

```python
import numpy as np
from contextlib import ExitStack
import concourse.bass as bass
import concourse.mybir as mybir
from concourse.bass_utils import run_bass_kernel_spmd

F32 = mybir.dt.float32
BF16 = mybir.dt.bfloat16
AF = mybir.ActivationFunctionType
ALU = mybir.AluOpType
AX = mybir.AxisListType

D = 1024
KC = 8
HD = 64
NH = 8
FOXW = 512
RW = 512
DFF = 2816
NFF = 22
WIN = 3368
RBASE = 1544
EPS = 1e-6
LNX_EPS = 64e-5
SB = 512
EPOCH = 12000


class Buf:
    __slots__ = ("w", "r")

    def __init__(self):
        self.w = None
        self.r = []


class _Rec:
    def __getattr__(self, name):
        def f(*a, **k):
            self.call = (name, a, k)
        return f


class Sched:
    ENGS = ("pe", "act", "dve", "pool", "sp")

    def __init__(self, nc, es):
        self.nc = nc
        self.es = es
        self.q = {e: [] for e in self.ENGS}
        self.cnt = {e: 0 for e in self.ENGS}
        self.run = {e: {} for e in self.ENGS}
        self.clk = {}
        self.sems = {}
        self.ndma = {"sp": 16, "act": 6, "pool": 6}
        self.dcnt = {e: 0 for e in self.ENGS}
        self.dtoks = {e: [] for e in self.ENGS}
        self.nwaits = 0

    def sem(self, key):
        s = self.sems.get(key)
        if s is None:
            s = self.es.enter_context(self.nc.semaphore("s_%s_%s" % key))
            self.sems[key] = s
        return s

    def _deps(self, eng, reads, writes, is_dma):
        deps = set()
        for b in reads:
            if b.w is not None:
                deps.add(b.w)
        for b in writes:
            if b.w is not None:
                deps.add(b.w)
            for t in b.r:
                deps.add(t)
        return deps

    def _waits(self, eng, deps):
        run = self.run[eng]
        waits = []
        for t in sorted(deps, key=lambda t: (str(t[0]), t[1])):
            key, val, isd = t
            if eng == "pe" and key[0] == "pe" and not isd:
                continue
            if run.get(key, 0) >= val:
                continue
            waits.append((key, val))
            for k2, v2 in self.clk[(key, val)].items():
                if run.get(k2, 0) < v2:
                    run[k2] = v2
        self.nwaits += len(waits)
        return waits

    def op(self, eng, fn, reads=(), writes=()):
        rec = _Rec()
        fn(rec)
        call = rec.call
        fn = lambda e, call=call: getattr(e, call[0])(*call[1], **call[2])
        deps = self._deps(eng, reads, writes, False)
        waits = self._waits(eng, deps)
        n = self.cnt[eng]
        self.cnt[eng] = n + 1
        key = (eng, n // EPOCH)
        val = n % EPOCH + 1
        tok = (key, val, False)
        c = dict(self.run[eng])
        c[key] = val
        self.clk[(key, val)] = c
        self.q[eng].append((waits, fn, key, 1))
        for b in reads:
            b.r.append(tok)
        for b in writes:
            b.w = tok
            b.r = []
        return tok

    def dma(self, eng, out, in_, reads=(), writes=(), **kw):
        deps = self._deps(eng, reads, writes, True)
        d = self.dcnt[eng]
        self.dcnt[eng] = d + 1
        nd = self.ndma[eng]
        if d >= nd:
            deps.add(self.dtoks[eng][d - nd])
        waits = self._waits(eng, deps)
        key = ("d" + eng, d % nd)
        val = 16 * (d // nd + 1)
        tok = (key, val, True)
        c = dict(self.run[eng])
        c[key] = val
        self.clk[(key, val)] = c
        self.dtoks[eng].append(tok)
        self.q[eng].append((waits, lambda e: e.dma_start(out=out, in_=in_, **kw), key, 16))
        for b in reads:
            b.r.append(tok)
        for b in writes:
            b.w = tok
            b.r = []
        return tok

    def barrier(self):
        best = {}
        for e in self.ENGS:
            n = self.cnt[e]
            if n > 0 and e != "sp":
                best[(e, (n - 1) // EPOCH)] = ((n - 1) % EPOCH + 1, False)
            for (k, v, isd) in self.dtoks[e][-self.ndma.get(e, 1):]:
                if best.get(k, (0, True))[0] < v:
                    best[k] = (v, True)
        toks = [(k, v, isd) for k, (v, isd) in best.items()]
        for e in self.ENGS:
            waits = []
            run = self.run[e]
            for (k, v, isd) in toks:
                if run.get(k, 0) < v:
                    waits.append((k, v))
                    for k2, v2 in self.clk[(k, v)].items():
                        if run.get(k2, 0) < v2:
                            run[k2] = v2
            self.q[e].append((waits, None, None, 0))

    def wait_tokens(self, eng, toks):
        waits = self._waits(eng, set(toks))
        self.q[eng].append((waits, None, None, 0))

    def emit(self):
        nc = self.nc
        for k in set(k for e in self.ENGS for (_, _, k, _) in self.q[e] if k is not None):
            self.sem(k)
        for e in self.ENGS:
            for (waits, _, _, _) in self.q[e]:
                for (k, v) in waits:
                    self.sem(k)
        with nc.Block() as block:
            def run(engname, engobj):
                for (waits, fn, key, inc) in self.q[engname]:
                    for (k, v) in waits:
                        engobj.wait_ge(self.sems[k], v)
                    if fn is not None:
                        ins = fn(engobj)
                        ins.then_inc(self.sems[key], inc)

            @block.tensor
            def _(e):
                run("pe", e)

            @block.scalar
            def _(e):
                run("act", e)

            @block.vector
            def _(e):
                run("dve", e)

            @block.gpsimd
            def _(e):
                run("pool", e)

            @block.sync
            def _(e):
                run("sp", e)


NMASK = 13
LEVELS = (2, 4, 8, 16, 32, 64)


def make_cmask():
    p = np.arange(128)[:, None]
    f = np.arange(128)[None, :]
    m = np.zeros((128, NMASK, 128), np.float32)
    m[:, 0, :] = (f > p)
    m[:, 1, :] = (f >= p)
    m[:, 2, :] = (f > p)
    m[:, 3, :] = (f >= p)
    m[:, 4, :] = ((p % 2 == 1) & (f == p - 1))
    m[:, 5, :] = ((f % 2 == 1) & (p == f - 1))
    for li, mm in enumerate(LEVELS):
        m[:, 6 + li, :] = (((p // mm) % 2 == 1) & ((f // mm) == (p // mm) - 1))
    m[:, 12, :] = ((p // 64) == (f // 64))
    return m


def build_nc(T, dbg=None, phases="FRO"):
    NSB = T // SB
    NBLK = T // 128
    nc = bass.Bass("TRN2", target_bir_lowering=False)
    es0 = ExitStack()
    S = Sched(nc, es0)

    def din(name, shape):
        return nc.dram_tensor(name, list(shape), F32, kind="ExternalInput").ap()

    x = din("x", [T, D])
    g_pre = din("attn_norm_pre", [D])
    g_post = din("attn_norm_post", [D])
    w_in = din("w_in", [D, WIN])
    din_fb = din("fox_forget_bias", [NH])
    shift_mu = din("shift_mu", [1824])
    rwkv_w0 = din("rwkv_w0", [RW])
    rwkv_w_up = din("rwkv_w_up", [64, RW])
    rwkv_a0 = din("rwkv_a0", [RW])
    rwkv_a_up = din("rwkv_a_up", [64, RW])
    rwkv_g_up = din("rwkv_g_up", [160, RW])
    rwkv_k_k = din("rwkv_k_k", [RW])
    rwkv_k_a = din("rwkv_k_a", [RW])
    rwkv_r_k = din("rwkv_r_k", [RW])
    rwkv_ln_w = din("rwkv_ln_w", [RW])
    rwkv_ln_b = din("rwkv_ln_b", [RW])
    w_out = din("w_out", [D, D])
    gf_pre = din("ffn_norm_pre", [D])
    gf_post = din("ffn_norm_post", [D])
    w_gate = din("ffn_w_gate", [D, DFF])
    w_up = din("ffn_w_up", [D, DFF])
    w_down = din("ffn_w_down", [DFF, D])
    cmask = din("cmask", [128, NMASK, 128])
    out = nc.dram_tensor("out", [T, D], F32, kind="ExternalOutput").ap()
    cat_scr = nc.dram_tensor("cat_scr", [D, T], BF16, kind="Internal").ap()
    scr_k = nc.dram_tensor("scr_k", [3, NH, T], BF16, kind="Internal").ap()
    scr_q = nc.dram_tensor("scr_q", [3, NH, T], BF16, kind="Internal").ap()
    dbg_aps = {}
    if dbg:
        for k, shp in dbg.items():
            if k == "stop":
                continue
            dbg_aps[k] = nc.dram_tensor("dbg_" + k, list(shp), F32, kind="ExternalOutput").ap()

    ucnt = [0]

    def uniq(name):
        ucnt[0] += 1
        return "%s_%d" % (name, ucnt[0])

    with es0:
        tp_ps = es0.enter_context(nc.psum_tensor("tp_ps", [128, KC, 128], BF16))
        b_tp = Buf()
        PB = [es0.enter_context(nc.psum_tensor("pb%d" % i, [128, SB], F32)) for i in range(7)]
        b_PB = [Buf() for _ in range(7)]
        ident_bf = es0.enter_context(nc.sbuf_tensor("ident_bf", [128, 128], BF16))
        ident_f = es0.enter_context(nc.sbuf_tensor("ident_f", [128, 128], F32))
        b_ident = Buf()
        S.op("pool", lambda e: e.memset(ident_f[:], 0.0), writes=[b_ident])
        S.op("pool", lambda e: e.affine_select(out=ident_f[:], in_=ident_f[:], pattern=[[-1, 128]],
                                               compare_op=ALU.not_equal, fill=1.0, base=0,
                                               channel_multiplier=1), reads=[b_ident], writes=[b_ident])
        S.op("dve", lambda e: e.tensor_copy(out=ident_bf[:], in_=ident_f[:]), reads=[b_ident], writes=[b_ident])

        def load_colvec(es, name, src, ncol):
            t = es.enter_context(nc.sbuf_tensor(uniq(name), [128, ncol], F32))
            b = Buf()
            S.dma("sp", t[:], src.rearrange("(k p) -> p k", p=128), writes=[b], allow_slow_non_contiguous=True)
            return t, b

        def make_front(es):
            def sbt(name, shape, dt=F32):
                return es.enter_context(nc.sbuf_tensor(uniq(name), list(shape), dt))
            st = {}
            st["xt"] = [sbt("xt%d" % i, [128, D], F32) for i in range(2)]
            st["b_xt"] = [Buf() for _ in range(2)]
            st["junk"] = sbt("junk", [128, D], BF16)
            st["b_junk"] = Buf()
            st["xn"] = [sbt("xn%d" % i, [128, D], BF16) for i in range(2)]
            st["b_xn"] = [Buf(), Buf()]
            st["ss"] = sbt("ss", [128, 8], F32)
            st["b_ss"] = [Buf() for _ in range(8)]
            st["uT"] = sbt("uT", [128, KC, SB], BF16)
            st["b_uT"] = Buf()
            st["tc"] = 0
            return st

        def front(st, s):
            xt, b_xt, xn, b_xn, ss, b_ss = st["xt"], st["b_xt"], st["xn"], st["b_xn"], st["ss"], st["b_ss"]
            junk, b_junk, uT, b_uT = st["junk"], st["b_junk"], st["uT"], st["b_uT"]
            for j in range(4):
                t0 = s * SB + j * 128
                xi = st["tc"] % 2
                ni = st["tc"] % 2
                si = st["tc"] % 8
                st["tc"] += 1
                S.dma("sp", xt[xi][:], x[t0:t0 + 128, :], writes=[b_xt[xi]])
                S.op("act", lambda e, xi=xi, si=si: e.activation(out=junk[:], in_=xt[xi][:], func=AF.Square,
                                                                 accum_out=ss[:, si:si + 1]),
                     reads=[b_xt[xi]], writes=[b_junk, b_ss[si]])
                S.op("dve", lambda e, si=si: e.tensor_scalar(out=ss[:, si:si + 1], in0=ss[:, si:si + 1],
                                                             scalar1=1.0 / D, scalar2=EPS, op0=ALU.mult, op1=ALU.add),
                     reads=[b_ss[si]], writes=[b_ss[si]])
                S.op("act", lambda e, si=si: e.sqrt(out=ss[:, si:si + 1], in_=ss[:, si:si + 1]),
                     reads=[b_ss[si]], writes=[b_ss[si]])
                S.op("dve", lambda e, si=si: e.reciprocal(out=ss[:, si:si + 1], in_=ss[:, si:si + 1]),
                     reads=[b_ss[si]], writes=[b_ss[si]])
                S.op("act", lambda e, xi=xi, ni=ni, si=si: e.activation(out=xn[ni][:], in_=xt[xi][:],
                                                                        func=AF.Copy, scale=ss[:, si:si + 1]),
                     reads=[b_xt[xi], b_ss[si]], writes=[b_xn[ni]])
                for kc in range(KC):
                    S.op("pe", lambda e, kc=kc, ni=ni: e.transpose(out=tp_ps[:, kc, :],
                                                                   in_=xn[ni][:, kc * 128:(kc + 1) * 128],
                                                                   identity=ident_bf[:]),
                         reads=[b_xn[ni], b_ident], writes=[b_tp])
                S.op("dve", lambda e, j=j: e.tensor_copy(out=uT[:, :, j * 128:(j + 1) * 128], in_=tp_ps[:, :, :]),
                     reads=[b_tp], writes=[b_uT])

        def load_weight_bf(st, dst, b_dst, src, c0, ncols, gvec, b_g, nk=KC):
            xt, b_xt = st["xt"], st["b_xt"]
            cnt = 0
            for kc in range(nk):
                for p0 in range(0, ncols, D):
                    n = min(D, ncols - p0)
                    i = cnt % 2
                    cnt += 1
                    S.dma("sp", xt[i][:, 0:n], src[kc * 128:(kc + 1) * 128, c0 + p0:c0 + p0 + n], writes=[b_xt[i]])
                    if cnt % 2 == 0:
                        if gvec is None:
                            S.op("dve", lambda e: e.tensor_copy(out=dst[:, kc, p0:p0 + n], in_=xt[i][:, 0:n]),
                                 reads=[b_xt[i]], writes=[b_dst[kc]])
                        else:
                            S.op("dve", lambda e: e.tensor_scalar(
                                out=dst[:, kc, p0:p0 + n], in0=xt[i][:, 0:n], scalar1=gvec[:, kc:kc + 1], scalar2=None, op0=ALU.mult),
                                 reads=[b_xt[i], b_g], writes=[b_dst[kc]])
                    else:
                        if gvec is None:
                            S.op("act", lambda e: e.copy(out=dst[:, kc, p0:p0 + n], in_=xt[i][:, 0:n]),
                                 reads=[b_xt[i]], writes=[b_dst[kc]])
                        else:
                            S.op("act", lambda e: e.activation(out=dst[:, kc, p0:p0 + n], in_=xt[i][:, 0:n], func=AF.Copy,
                                                               scale=gvec[:, kc:kc + 1]),
                                 reads=[b_xt[i], b_g], writes=[b_dst[kc]])

        pjc = [0]

        def proj_fm(wt, b_w, st, c0, ncols, evac):
            pi = pjc[0] % 2
            pjc[0] += 1
            uT, b_uT = st["uT"], st["b_uT"]
            for kc in range(KC):
                S.op("pe", lambda e, kc=kc: e.matmul(out=PB[pi][0:ncols, :], lhsT=wt[:, kc, c0:c0 + ncols],
                                                     rhs=uT[:, kc, :], start=(kc == 0), stop=(kc == KC - 1)),
                     reads=[b_w[kc], b_uT], writes=[b_PB[pi]])
            evac(PB[pi], b_PB[pi])

        if "F" in phases:
            with ExitStack() as es:
                def sb(name, shape, dt=F32):
                    return es.enter_context(nc.sbuf_tensor(uniq(name), list(shape), dt))
                st = make_front(es)
                uT, b_uT = st["uT"], st["b_uT"]
                gT, b_gT = load_colvec(es, "gT", g_pre, KC)
                w_bf = sb("w_bf", [128, KC, RBASE], BF16)
                b_w = [Buf() for _ in range(KC)]
                load_weight_bf(st, w_bf, b_w, w_in, 0, RBASE, gT, b_gT)

                nfb = sb("nfb", [8, 1], F32)
                b_nfb = Buf()
                S.dma("sp", nfb[:], din_fb.rearrange("(h o) -> h o", o=1), writes=[b_nfb])
                S.op("dve", lambda e: e.tensor_scalar(out=nfb[:], in0=nfb[:], scalar1=-1.0, scalar2=None, op0=ALU.mult),
                     reads=[b_nfb], writes=[b_nfb])
                ones8 = sb("ones8", [8, SB], F32)
                b_ones8 = Buf()
                S.op("pool", lambda e: e.memset(ones8[:], 1.0), writes=[b_ones8])
                ones_f = sb("ones_f", [128, 64], F32)
                b_onesf = Buf()
                S.op("pool", lambda e: e.memset(ones_f[:], 1.0), writes=[b_onesf])
                maskneg_f = sb("maskneg_f", [128, 128], F32)
                maskneg = sb("maskneg", [128, 128], BF16)
                b_mask = Buf()
                S.op("pool", lambda e: e.memset(maskneg_f[:], 0.0), writes=[b_mask])
                S.op("pool", lambda e: e.affine_select(out=maskneg_f[:], in_=maskneg_f[:], pattern=[[1, 128]],
                                                       compare_op=ALU.is_ge, fill=-30000.0, base=0,
                                                       channel_multiplier=-1), reads=[b_mask], writes=[b_mask])
                S.op("dve", lambda e: e.tensor_copy(out=maskneg[:], in_=maskneg_f[:]), reads=[b_mask], writes=[b_mask])

                KT = sb("KT", [70, NH, T], BF16)
                b_KT = [Buf() for _ in range(NSB)]
                QT = sb("QT", [70, NH, SB], BF16)
                b_QT = Buf()
                VT = sb("VT", [128, NBLK, NH, 66], BF16)
                b_VT = [Buf() for _ in range(NSB)]
                S.op("pool", lambda e: e.memset(KT[64:70, :, :], 1.0), writes=b_KT)
                S.op("pool", lambda e: e.memset(QT[64:70, :, :], 1.0), writes=[b_QT])
                S.op("pool", lambda e: e.memset(VT[:, :, :, 64:66], 1.0), writes=b_VT)
                b_scrk = Buf()
                b_scrq = Buf()
                cneg = [sb("cneg%d" % i, [8, SB], F32) for i in range(2)]
                b_cneg = [Buf(), Buf()]
                fl = sb("fl", [8, SB], F32)
                b_fl = Buf()
                res1 = sb("res1", [8, SB], F32)
                b_res1 = Buf()
                ksp = sb("ksp", [8, 3, SB], BF16)
                qsp = sb("qsp", [8, 3, SB], BF16)
                b_ksp = Buf()
                b_qsp = Buf()
                catF = [sb("catF%d" % i, [64, SB], BF16) for i in range(2)]
                b_catF = [Buf(), Buf()]
                PT = [sb("PT%d" % i, [128, SB], BF16) for i in range(3)]
                b_PT = [Buf() for _ in range(3)]
                rs = sb("rs", [66, SB], F32)
                b_rs = Buf()
                bc_sb = sb("bc_sb", [64, SB], F32)
                b_bc = Buf()
                dbg_sb = sb("dbg_sb", [128, SB], F32)
                b_dbg = Buf()
                st_ps = [PB[2], PB[3]]
                b_st = [b_PB[2], b_PB[3]]
                o_ps2 = [PB[4], PB[5]]
                b_o2 = [b_PB[4], b_PB[5]]
                bc_ps, b_bcps = PB[6], b_PB[6]

                for s in range(NSB):
                    c_lo, c_hi = s * SB, (s + 1) * SB
                    front(st, s)
                    ci = s % 2

                    def ev_ff(pp, bp):
                        S.op("act", lambda e: e.activation(out=fl[:], in_=pp[0:8, :], func=AF.Exp, bias=nfb[:, 0:1], scale=-1.0),
                             reads=[bp, b_nfb], writes=[b_fl])
                        S.op("act", lambda e: e.activation(out=fl[:], in_=fl[:], func=AF.Ln, bias=1.0, scale=1.0),
                             reads=[b_fl], writes=[b_fl])
                        S.op("dve", lambda e: e.tensor_tensor_scan(out=cneg[ci][:], data0=ones8[:], data1=fl[:], initial=0.0,
                                                                   op0=ALU.mult, op1=ALU.add),
                             reads=[b_fl, b_ones8], writes=[b_cneg[ci]])
                        if s > 0:
                            S.op("dve", lambda e: e.tensor_scalar(out=cneg[ci][:], in0=cneg[ci][:],
                                                                  scalar1=cneg[1 - ci][:, SB - 1:SB], scalar2=None, op0=ALU.add),
                                 reads=[b_cneg[ci], b_cneg[1 - ci]], writes=[b_cneg[ci]])
                        S.op("dve", lambda e: e.tensor_copy(out=ksp[:, 0, :], in_=cneg[ci][:]), reads=[b_cneg[ci]], writes=[b_ksp])
                        S.op("dve", lambda e: e.tensor_tensor(out=res1[:], in0=cneg[ci][:], in1=ksp[:, 0, :], op=ALU.subtract),
                             reads=[b_cneg[ci], b_ksp], writes=[b_res1])
                        S.op("dve", lambda e: e.tensor_copy(out=ksp[:, 1, :], in_=res1[:]), reads=[b_res1], writes=[b_ksp])
                        S.op("dve", lambda e: e.tensor_tensor(out=res1[:], in0=res1[:], in1=ksp[:, 1, :], op=ALU.subtract),
                             reads=[b_res1, b_ksp], writes=[b_res1])
                        S.op("dve", lambda e: e.tensor_copy(out=ksp[:, 2, :], in_=res1[:]), reads=[b_res1], writes=[b_ksp])
                        S.op("dve", lambda e: e.tensor_scalar(out=qsp[:], in0=ksp[:], scalar1=-1.0, scalar2=None, op0=ALU.mult),
                             reads=[b_ksp], writes=[b_qsp])
                        S.dma("sp", scr_k[:, :, c_lo:c_hi].rearrange("r h t -> h r t"), ksp[:], reads=[b_ksp], writes=[b_scrk])
                        S.dma("sp", scr_q[:, :, c_lo:c_hi].rearrange("r h t -> h r t"), qsp[:], reads=[b_qsp], writes=[b_scrq])
                        S.dma("sp", KT[67:70, :, c_lo:c_hi], scr_k[:, :, c_lo:c_hi], reads=[b_scrk], writes=[b_KT[s]])
                        S.dma("sp", QT[64:67, :, :], scr_q[:, :, c_lo:c_hi], reads=[b_scrq], writes=[b_QT])

                    proj_fm(w_bf, b_w, st, 1536, 8, ev_ff)
                    for h in range(NH):
                        def ev_q(pp, bp, h=h):
                            S.op("act", lambda e: e.mul(out=QT[0:64, h, :], in_=pp[0:64, :], mul=0.125), reads=[bp], writes=[b_QT])
                        proj_fm(w_bf, b_w, st, h * 64, 64, ev_q)

                        def ev_k(pp, bp, h=h):
                            S.op("dve", lambda e: e.tensor_copy(out=KT[0:64, h, c_lo:c_hi], in_=pp[0:64, :]), reads=[bp], writes=[b_KT[s]])
                        proj_fm(w_bf, b_w, st, 512 + h * 64, 64, ev_k)
                    for j in range(4):
                        pi = pjc[0] % 2
                        pjc[0] += 1
                        for kc in range(KC):
                            S.op("pe", lambda e, kc=kc, j=j, pi=pi: e.matmul(out=PB[pi][:], lhsT=uT[:, kc, j * 128:(j + 1) * 128],
                                                                             rhs=w_bf[:, kc, 1024:1536], start=(kc == 0), stop=(kc == KC - 1)),
                                 reads=[b_w[kc], b_uT], writes=[b_PB[pi]])
                        S.op("act", lambda e, j=j, pi=pi: e.copy(out=VT[:, s * 4 + j, :, 0:64],
                                                                 in_=PB[pi][:].rearrange("p (h d) -> p h d", h=NH)),
                             reads=[b_PB[pi]], writes=[b_VT[s]])

                    tiles = []
                    nkb = 4 * (s + 1)
                    for h in range(NH):
                        for kb in range(nkb):
                            d = kb - 4 * s
                            tiles.append((h, kb, 0 if d < 0 else d * 128, d >= 0, len(tiles)))

                    def emit_qk(tl):
                        h, kb, q0, diag, idx = tl
                        si_ = idx % 2
                        S.op("pe", lambda e: e.matmul(out=st_ps[si_][:, q0:SB], lhsT=KT[0:70, h, kb * 128:(kb + 1) * 128],
                                                      rhs=QT[0:70, h, q0:SB], start=True, stop=(not diag)),
                             reads=[b_KT[kb // 4], b_QT], writes=[b_st[si_]])
                        if diag:
                            S.op("pe", lambda e: e.matmul(out=st_ps[si_][:, q0:q0 + 128], lhsT=ident_bf[:], rhs=maskneg[:],
                                                          start=False, stop=True),
                                 reads=[b_ident, b_mask], writes=[b_st[si_]])

                    emit_qk(tiles[0])
                    for ti, tl in enumerate(tiles):
                        h, kb, q0, diag, idx = tl
                        si_ = idx % 2
                        pi_ = idx % 3
                        oi_ = h % 2
                        if ti + 1 < len(tiles):
                            emit_qk(tiles[ti + 1])
                        S.op("act", lambda e: e.activation(out=PT[pi_][:, q0:SB], in_=st_ps[si_][:, q0:SB], func=AF.Exp),
                             reads=[b_st[si_]], writes=[b_PT[pi_]])
                        S.op("pe", lambda e: e.matmul(out=o_ps2[oi_][0:66, q0:SB], lhsT=VT[:, kb, h, :], rhs=PT[pi_][:, q0:SB],
                                                      start=(kb == 0), stop=(kb == nkb - 1)),
                             reads=[b_VT[kb // 4], b_PT[pi_]], writes=[b_o2[oi_]])
                        if kb != nkb - 1:
                            continue
                        o_ps, b_o = o_ps2[oi_], b_o2[oi_]
                        S.op("dve", lambda e: e.reciprocal(out=rs[64:66, :], in_=o_ps[64:66, :]), reads=[b_o], writes=[b_rs])
                        S.op("pe", lambda e: e.matmul(out=bc_ps[0:64, :], lhsT=ones_f[64:65, 0:64], rhs=rs[64:65, :], start=True, stop=True),
                             reads=[b_rs, b_onesf], writes=[b_bcps])
                        S.op("act", lambda e: e.copy(out=bc_sb[:], in_=bc_ps[0:64, :]), reads=[b_bcps], writes=[b_bc])
                        fi = h % 2
                        S.op("dve", lambda e: e.tensor_tensor(out=catF[fi][:], in0=o_ps[0:64, :], in1=bc_sb[:], op=ALU.mult),
                             reads=[b_o, b_bc], writes=[b_catF[fi]])
                        S.dma("sp", cat_scr[h * 64:(h + 1) * 64, c_lo:c_hi], catF[fi][:], reads=[b_catF[fi]])
                        if dbg and "ofox" in dbg:
                            S.op("act", lambda e, fi=fi: e.copy(out=dbg_sb[0:64, :], in_=catF[fi][:]), reads=[b_catF[fi]], writes=[b_dbg])
                            S.dma("sp", dbg_aps["ofox"][h * 64:(h + 1) * 64, c_lo:c_hi], dbg_sb[0:64, :], reads=[b_dbg])
                S.barrier()
        if "R" in phases:
            with ExitStack() as es:
                def sb(name, shape, dt=F32):
                    return es.enter_context(nc.sbuf_tensor(uniq(name), list(shape), dt))
                st = make_front(es)
                uT, b_uT = st["uT"], st["b_uT"]
                gT, b_gT = load_colvec(es, "gTr", g_pre, KC)
                NRC = 1824
                wr_bf = sb("wr_bf", [128, KC, NRC], BF16)
                b_wr = [Buf() for _ in range(KC)]
                load_weight_bf(st, wr_bf, b_wr, w_in, RBASE, NRC, gT, b_gT)
                lo_bf = sb("lo_bf", [128, RW], BF16)
                b_lo = Buf()
                gup_bf = sb("gup_bf", [128, RW], BF16)
                gup1_bf = sb("gup1_bf", [32, RW], BF16)
                b_gup = Buf()
                xt, b_xt = st["xt"], st["b_xt"]
                S.dma("sp", xt[0][0:64, 0:RW], rwkv_w_up[:, :], writes=[b_xt[0]])
                S.dma("sp", xt[0][64:128, 0:RW], rwkv_a_up[:, :], writes=[b_xt[0]])
                S.op("dve", lambda e: e.tensor_copy(out=lo_bf[:], in_=xt[0][:, 0:RW]), reads=[b_xt[0]], writes=[b_lo])
                S.dma("sp", xt[1][:, 0:RW], rwkv_g_up[0:128, :], writes=[b_xt[1]])
                S.op("dve", lambda e: e.tensor_copy(out=gup_bf[:], in_=xt[1][:, 0:RW]), reads=[b_xt[1]], writes=[b_gup])
                S.dma("sp", xt[0][0:32, 0:RW], rwkv_g_up[128:160, :], reads=[b_lo], writes=[b_xt[0]])
                S.op("dve", lambda e: e.tensor_copy(out=gup1_bf[:], in_=xt[0][0:32, 0:RW]), reads=[b_xt[0]], writes=[b_gup])
                w0T, b_w0 = load_colvec(es, "w0T", rwkv_w0, 4)
                a0T, b_a0 = load_colvec(es, "a0T", rwkv_a0, 4)
                kkT, b_kkv = load_colvec(es, "kkT", rwkv_k_k, 4)
                kaT, b_kav = load_colvec(es, "kaT", rwkv_k_a, 4)
                rkT, b_rkv = load_colvec(es, "rkT", rwkv_r_k, 4)
                lnwT, b_lnw = load_colvec(es, "lnwT", rwkv_ln_w, 4)
                lnbT, b_lnb = load_colvec(es, "lnbT", rwkv_ln_b, 4)
                groups = {}
                gl = []
                for hp in range(4):
                    gl.append((("r", hp), hp * 128, 128))
                    gl.append((("k", hp), 512 + hp * 128, 128))
                    gl.append((("v", hp), 1024 + hp * 128, 128))
                gl.append(("wa", 1536, 128))
                gl.append(("g0", 1664, 128))
                gl.append(("g1", 1792, 32))
                muT = sb("muT", [128, len(gl)], F32)
                b_mu = Buf()
                carry = sb("carry", [128, len(gl)], F32)
                b_carry = [Buf() for _ in gl]
                S.op("pool", lambda e: e.memset(carry[:], 0.0), writes=b_carry)
                for gi, (nm, c0, n) in enumerate(gl):
                    groups[nm] = (gi, c0, n)
                    S.dma("sp", muT[0:n, gi:gi + 1], shift_mu[c0:c0 + n].rearrange("(p o) -> p o", o=1), writes=[b_mu])
                cm = sb("cm", [128, NMASK, 128], F32)
                b_cm = Buf()
                S.dma("sp", cm[:], cmask[:, :, :], writes=[b_cm])
                mk0T_bf = sb("mk0T_bf", [128, 128], BF16)
                S.op("dve", lambda e: e.tensor_copy(out=mk0T_bf[:], in_=cm[:, 5, :]), reads=[b_cm], writes=[b_cm])
                bd_mean = sb("bd_mean", [128, 128], F32)
                S.op("dve", lambda e: e.tensor_scalar(out=bd_mean[:], in0=cm[:, 12, :], scalar1=1.0 / 64, scalar2=None, op0=ALU.mult),
                     reads=[b_cm], writes=[b_cm])
                ones128 = sb("ones128", [128, 128], F32)
                S.op("pool", lambda e: e.memset(ones128[:], 1.0), writes=[b_cm])

                H32 = [sb("H32_%d" % i, [128, 128], F32) for i in range(4)]
                Hbf = [sb("Hbf_%d" % i, [128, 128], BF16) for i in range(4)]
                b_H32 = [Buf() for _ in range(4)]
                b_Hbf = [Buf() for _ in range(4)]
                for i in range(4):
                    S.op("pool", lambda e, i=i: e.memset(H32[i][:], 0.0), writes=[b_H32[i]])
                    S.op("pool", lambda e, i=i: e.memset(Hbf[i][:], 0.0), writes=[b_Hbf[i]])

                def wt(name, dt=F32, n=SB):
                    return sb(name, [128, n], dt), Buf()
                raw = [sb("raw%d" % i, [128, SB + 1], F32) for i in range(2)]
                b_raw = [Buf(), Buf()]
                rawc = [0]
                dlt, b_dlt = wt("dlt")
                wa_s, b_was = wt("wa_s")
                g0_s, b_g0s = wt("g0_s")
                g1_s, b_g1s = wt("g1_s")
                lat_bf, b_lat = wt("lat_bf", BF16)
                sg_bf, b_sg = wt("sg_bf", BF16)
                sg1_bf, b_sg1 = wt("sg1_bf", BF16)
                r_s, b_rs_ = wt("r_s")
                k_s, b_ks = wt("k_s")
                v_s, b_vs = wt("v_s")
                lw, b_lw = wt("lw")
                a_t, b_a = wt("a_t")
                g_t, b_g = wt("g_t")
                kq, b_kq = wt("kq")
                tmpA, b_tmpA = wt("tmpA")
                kk, b_kk = wt("kk")
                kmod, b_kmod = wt("kmod")
                bb, b_bb = wt("bb")
                bonus, b_bonus = wt("bonus")
                cum, b_cum = wt("cum")
                E_in, b_Ein = wt("E_in")
                E_neg, b_Eneg = wt("E_neg")
                E_ex, b_Eex = wt("E_ex")
                E_end, b_Eend = wt("E_end")
                y_sb, b_y = wt("y_sb")
                AR = sb("AR", [128, 4, 2, 128], BF16)
                b_AR = Buf()
                BT, b_BT = wt("BT", BF16)
                KTt, b_KTt = wt("KTt", BF16)
                BH, b_BH = wt("BH", BF16)
                KH, b_KH = wt("KH", BF16)
                Vb, b_Vb = wt("Vb", BF16)
                catR, b_catR = wt("catR", BF16)
                TM = [sb("TM%d" % i, [128, 4, 128], BF16) for i in range(4)]
                b_TM = [Buf() for _ in range(4)]
                NMt = [sb("NM%d" % i, [128, 4, 128], BF16) for i in range(4)]
                b_NM = [Buf() for _ in range(4)]
                Dm = [sb("Dm%d" % i, [128, 128], BF16) for i in range(4)]
                b_Dm = [Buf() for _ in range(4)]
                DTm = [sb("DTm%d" % i, [128, 128], BF16) for i in range(4)]
                b_DTm = [Buf() for _ in range(4)]
                Gm = [sb("Gm%d" % i, [128, 128], BF16) for i in range(4)]
                b_Gm = [Buf() for _ in range(4)]
                X2b = [sb("X2b%d" % i, [128, 64], BF16) for i in range(4)]
                b_X2b = [Buf() for _ in range(4)]
                U2s = [sb("U2s%d" % i, [128, 128], F32) for i in range(2)]
                b_U2s = [Buf(), Buf()]
                WTb = [sb("WTb%d" % i, [128, 128], BF16) for i in range(2)]
                b_WTb = [Buf(), Buf()]
                Ub = sb("Ub", [128, 128], BF16)
                b_Ub = Buf()
                dbg_sb = sb("dbg_sbr", [128, SB], F32)
                b_dbg = Buf()
                M1 = [PB[4], PB[5]]
                b_M1 = [b_PB[4], b_PB[5]]
                sA = [PB[i][:, 0:128] for i in range(4)]
                b_sA = [b_PB[i] for i in range(4)]
                sB = [PB[i][:, 128:256] for i in range(4)]
                b_sB = [b_PB[i] for i in range(4)]
                u2_ps, uh_ps = [PB[6][:, i * 128:(i + 1) * 128] for i in range(2)]
                y2_ps = [PB[6][:, (2 + i) * 128:(3 + i) * 128] for i in range(2)]
                b_u2 = b_wt = b_uh = b_yps = b_PB[6]

                def shifted(nm, dst, b_dst):
                    gi, c0, n = groups[nm]

                    def ev(pp, bp):
                        ri = rawc[0] % 2
                        rawc[0] += 1
                        rw_, brw = raw[ri], b_raw[ri]
                        S.op("act", lambda e: e.copy(out=rw_[0:n, 1:SB + 1], in_=pp[0:n, :]), reads=[bp], writes=[brw])
                        S.op("pool", lambda e: e.tensor_copy(out=rw_[0:n, 0:1], in_=carry[0:n, gi:gi + 1]),
                             reads=[b_carry[gi]], writes=[brw])
                        S.op("pool", lambda e: e.tensor_copy(out=carry[0:n, gi:gi + 1], in_=rw_[0:n, SB:SB + 1]),
                             reads=[brw], writes=[b_carry[gi]])
                        S.op("dve", lambda e: e.tensor_tensor(out=dlt[0:n, :], in0=rw_[0:n, 0:SB], in1=rw_[0:n, 1:SB + 1], op=ALU.subtract),
                             reads=[brw], writes=[b_dlt])
                        S.op("dve", lambda e: e.scalar_tensor_tensor(out=dst[0:n, :], in0=dlt[0:n, :], scalar=muT[0:n, gi:gi + 1],
                                                                     in1=rw_[0:n, 1:SB + 1], op0=ALU.mult, op1=ALU.add),
                             reads=[b_dlt, brw, b_mu], writes=[b_dst])
                    proj_fm(wr_bf, b_wr, st, c0, n, ev)

                def v4(ap):
                    return ap.rearrange("p (c t) -> p c t", c=4)

                for s in range(NSB):
                    c_lo, c_hi = s * SB, (s + 1) * SB
                    front(st, s)
                    shifted("wa", wa_s, b_was)
                    shifted("g0", g0_s, b_g0s)
                    shifted("g1", g1_s, b_g1s)
                    S.op("act", lambda e: e.activation(out=lat_bf[0:64, :], in_=wa_s[0:64, :], func=AF.Tanh), reads=[b_was], writes=[b_lat])
                    S.op("act", lambda e: e.copy(out=lat_bf[64:128, :], in_=wa_s[64:128, :]), reads=[b_was], writes=[b_lat])
                    S.op("act", lambda e: e.activation(out=sg_bf[:], in_=g0_s[:], func=AF.Sigmoid), reads=[b_g0s], writes=[b_sg])
                    S.op("act", lambda e: e.activation(out=sg1_bf[0:32, :], in_=g1_s[0:32, :], func=AF.Sigmoid), reads=[b_g1s], writes=[b_sg1])
                    for hp in range(4):
                        hc = slice(hp * 128, (hp + 1) * 128)
                        shifted(("r", hp), r_s, b_rs_)
                        shifted(("k", hp), k_s, b_ks)
                        shifted(("v", hp), v_s, b_vs)
                        p0, bp0 = PB[0], b_PB[0]
                        S.op("pe", lambda e: e.matmul(out=p0[:], lhsT=lo_bf[0:64, hc], rhs=lat_bf[0:64, :], start=True, stop=True),
                             reads=[b_lo, b_lat], writes=[bp0])
                        S.op("act", lambda e: e.activation(out=lw[:], in_=p0[:], func=AF.Sigmoid, bias=w0T[:, hp:hp + 1]),
                             reads=[bp0, b_w0], writes=[b_lw])
                        S.op("pool", lambda e: e.tensor_scalar(out=lw[:], in0=lw[:], scalar1=-0.6065306597126334, scalar2=None, op0=ALU.mult),
                             reads=[b_lw], writes=[b_lw])
                        p1, bp1 = PB[1], b_PB[1]
                        S.op("pe", lambda e: e.matmul(out=p1[:], lhsT=lo_bf[64:128, hc], rhs=lat_bf[64:128, :], start=True, stop=True),
                             reads=[b_lo, b_lat], writes=[bp1])
                        S.op("act", lambda e: e.activation(out=a_t[:], in_=p1[:], func=AF.Sigmoid, bias=a0T[:, hp:hp + 1]),
                             reads=[bp1, b_a0], writes=[b_a])
                        S.op("pe", lambda e: e.matmul(out=p0[:], lhsT=gup_bf[:, hc], rhs=sg_bf[:], start=True, stop=False),
                             reads=[b_gup, b_sg], writes=[bp0])
                        S.op("pe", lambda e: e.matmul(out=p0[:], lhsT=gup1_bf[0:32, hc], rhs=sg1_bf[0:32, :], start=False, stop=True),
                             reads=[b_gup, b_sg1], writes=[bp0])
                        S.op("act", lambda e: e.copy(out=g_t[:], in_=p0[:]), reads=[bp0], writes=[b_g])
                        S.op("dve", lambda e: e.tensor_scalar(out=kq[:], in0=k_s[:], scalar1=kkT[:, hp:hp + 1], scalar2=None, op0=ALU.mult),
                             reads=[b_ks, b_kkv], writes=[b_kq])
                        S.op("pool", lambda e: e.tensor_tensor(out=tmpA[:], in0=kq[:], in1=kq[:], op=ALU.mult), reads=[b_kq], writes=[b_tmpA])
                        S.op("pe", lambda e: e.matmul(out=p1[:], lhsT=cm[:, 12, :], rhs=tmpA[:], start=True, stop=True),
                             reads=[b_cm, b_tmpA], writes=[bp1])
                        S.op("act", lambda e: e.sqrt(out=tmpA[:], in_=p1[:]), reads=[bp1], writes=[b_tmpA])
                        S.op("dve", lambda e: e.tensor_scalar(out=tmpA[:], in0=tmpA[:], scalar1=1e-12, scalar2=None, op0=ALU.max),
                             reads=[b_tmpA], writes=[b_tmpA])
                        S.op("dve", lambda e: e.reciprocal(out=tmpA[:], in_=tmpA[:]), reads=[b_tmpA], writes=[b_tmpA])
                        S.op("pool", lambda e: e.tensor_tensor(out=kk[:], in0=kq[:], in1=tmpA[:], op=ALU.mult),
                             reads=[b_kq, b_tmpA], writes=[b_kk])
                        S.op("dve", lambda e: e.tensor_scalar(out=kmod[:], in0=a_t[:], scalar1=-1.0, scalar2=kaT[:, hp:hp + 1],
                                                              op0=ALU.add, op1=ALU.mult), reads=[b_a, b_kav], writes=[b_kmod])
                        S.op("dve", lambda e: e.scalar_tensor_tensor(out=kmod[:], in0=kmod[:], scalar=1.0, in1=k_s[:],
                                                                     op0=ALU.add, op1=ALU.mult), reads=[b_kmod, b_ks], writes=[b_kmod])
                        S.op("pool", lambda e: e.tensor_tensor(out=bb[:], in0=kk[:], in1=a_t[:], op=ALU.mult), reads=[b_kk, b_a], writes=[b_bb])
                        S.op("dve", lambda e: e.scalar_tensor_tensor(out=tmpA[:], in0=r_s[:], scalar=rkT[:, hp:hp + 1], in1=kmod[:],
                                                                     op0=ALU.mult, op1=ALU.mult), reads=[b_rs_, b_rkv, b_kmod], writes=[b_tmpA])
                        S.op("pe", lambda e: e.matmul(out=p1[:], lhsT=cm[:, 12, :], rhs=tmpA[:], start=True, stop=True),
                             reads=[b_cm, b_tmpA], writes=[bp1])
                        S.op("dve", lambda e: e.tensor_tensor(out=bonus[:], in0=p1[:], in1=v_s[:], op=ALU.mult), reads=[bp1, b_vs], writes=[b_bonus])
                        for c in range(4):
                            cc = slice(c * 128, (c + 1) * 128)
                            S.op("dve", lambda e, cc=cc: e.tensor_tensor_scan(out=cum[:, cc], data0=ones128[:], data1=lw[:, cc], initial=0.0,
                                                                              op0=ALU.mult, op1=ALU.add), reads=[b_lw, b_cm], writes=[b_cum])
                        S.op("act", lambda e: e.activation(out=E_in[:], in_=cum[:], func=AF.Exp), reads=[b_cum], writes=[b_Ein])
                        S.op("act", lambda e: e.activation(out=E_neg[:], in_=cum[:], func=AF.Exp, scale=-1.0), reads=[b_cum], writes=[b_Eneg])
                        for c in range(4):
                            cc = slice(c * 128, (c + 1) * 128)
                            S.op("act", lambda e, cc=cc, c=c: e.activation(out=E_end[:, cc], in_=cum[:, cc], func=AF.Exp, scale=-1.0,
                                                                           bias=cum[:, c * 128 + 127:c * 128 + 128]),
                                 reads=[b_cum], writes=[b_Eend])
                        S.op("pool", lambda e: e.tensor_tensor(out=tmpA[:], in0=cum[:], in1=lw[:], op=ALU.subtract), reads=[b_cum, b_lw], writes=[b_tmpA])
                        S.op("act", lambda e: e.activation(out=E_ex[:], in_=tmpA[:], func=AF.Exp), reads=[b_tmpA], writes=[b_Eex])
                        S.op("dve", lambda e: e.scalar_tensor_tensor(out=AR[:, :, 0, :], in0=v4(kk[:]), scalar=-1.0, in1=v4(E_ex[:]),
                                                                     op0=ALU.mult, op1=ALU.mult), reads=[b_kk, b_Eex], writes=[b_AR])
                        S.op("pool", lambda e: e.tensor_tensor(out=AR[:, :, 1, :], in0=v4(r_s[:]), in1=v4(E_in[:]), op=ALU.mult),
                             reads=[b_rs_, b_Ein], writes=[b_AR])
                        S.op("dve", lambda e: e.tensor_tensor(out=BT[:], in0=bb[:], in1=E_neg[:], op=ALU.mult), reads=[b_bb, b_Eneg], writes=[b_BT])
                        S.op("pool", lambda e: e.tensor_tensor(out=KTt[:], in0=kmod[:], in1=E_neg[:], op=ALU.mult), reads=[b_kmod, b_Eneg], writes=[b_KTt])
                        S.op("dve", lambda e: e.tensor_tensor(out=BH[:], in0=bb[:], in1=E_end[:], op=ALU.mult), reads=[b_bb, b_Eend], writes=[b_BH])
                        S.op("pool", lambda e: e.tensor_tensor(out=KH[:], in0=kmod[:], in1=E_end[:], op=ALU.mult), reads=[b_kmod, b_Eend], writes=[b_KH])
                        S.op("act", lambda e: e.copy(out=Vb[:], in_=v_s[:]), reads=[b_vs], writes=[b_Vb])

                        stop = (dbg or {}).get("stop", "")
                        if stop == "A":
                            continue
                        for cp in range(2):
                            chains = [(2 * cp + ci_, e_) for ci_ in range(2) for e_ in range(2)]
                            for ci_ in range(2):
                                c = 2 * cp + ci_
                                cc = slice(c * 128, (c + 1) * 128)
                                srcs = [(AR[:, c, 0, :], b_AR), (Vb[:, cc], b_Vb), (BH[:, cc], b_BH), (KH[:, cc], b_KH)]
                                for k_, (src, bsrc) in enumerate(srcs):
                                    S.op("pe", lambda e, k_=k_, src=src: e.transpose(out=tp_ps[:, k_, :], in_=src, identity=ident_bf[:]),
                                         reads=[bsrc, b_ident], writes=[b_tp])
                                S.op("act", lambda e, c=c: e.copy(out=TM[c][:], in_=tp_ps[:, 0:4, :]), reads=[b_tp], writes=[b_TM[c]])
                            for ch, (c, e_) in enumerate(chains):
                                pr = slice(e_ * 64, (e_ + 1) * 64)
                                cc = slice(c * 128, (c + 1) * 128)
                                m1, bm1 = M1[ch % 2], b_M1[ch % 2]
                                S.op("pe", lambda e, pr=pr, cc=cc, c=c, m1=m1: e.matmul(out=m1[:, 0:256], lhsT=BT[pr, cc],
                                                                                     rhs=AR[pr, c, :, :], start=True, stop=True),
                                     reads=[b_BT, b_AR], writes=[bm1])
                                S.op("pe", lambda e, pr=pr, cc=cc, c=c, m1=m1: e.matmul(out=m1[:, 256:512], lhsT=KTt[pr, cc],
                                                                                     rhs=AR[pr, c, :, :], start=True, stop=True),
                                     reads=[b_KTt, b_AR], writes=[bm1])
                                S.op("dve", lambda e, ch=ch, m1=m1: e.tensor_tensor(
                                    out=NMt[ch][:], in0=m1[:].rearrange("p (a t) -> p a t", a=4),
                                    in1=cm[:, 0:4, :],
                                    op=ALU.mult), reads=[bm1, b_cm], writes=[b_NM[ch]])
                                S.op("pe", lambda e, pr=pr, cc=cc, c=c, ch=ch: e.matmul(out=sA[ch], lhsT=AR[pr, c, 0, :], rhs=BT[pr, cc],
                                                                                     start=True, stop=True),
                                     reads=[b_AR, b_BT], writes=[b_sA[ch]])
                                S.op("dve", lambda e, ch=ch: e.tensor_tensor(out=Dm[ch][:], in0=sA[ch], in1=cm[:, 4, :], op=ALU.mult),
                                     reads=[b_sA[ch], b_cm], writes=[b_Dm[ch]])
                                S.op("pool", lambda e, ch=ch: e.tensor_tensor(out=Dm[ch][:], in0=Dm[ch][:], in1=ident_bf[:], op=ALU.add),
                                     reads=[b_Dm[ch], b_ident], writes=[b_Dm[ch]])
                                S.op("pool", lambda e, ch=ch: e.tensor_tensor(out=DTm[ch][:], in0=NMt[ch][:, 0, :], in1=mk0T_bf[:], op=ALU.mult),
                                     reads=[b_NM[ch], b_cm], writes=[b_DTm[ch]])
                                S.op("pool", lambda e, ch=ch: e.tensor_tensor(out=DTm[ch][:], in0=DTm[ch][:], in1=ident_bf[:], op=ALU.add),
                                     reads=[b_DTm[ch], b_ident], writes=[b_DTm[ch]])
                            if stop == "B":
                                continue
                            for li, mm in enumerate(LEVELS):
                                last = (li == len(LEVELS) - 1)
                                for ch in range(4):
                                    S.op("pe", lambda e, ch=ch: e.matmul(out=sA[ch], lhsT=NMt[ch][:, 0, :], rhs=Dm[ch][:], start=True, stop=True),
                                         reads=[b_NM[ch], b_Dm[ch]], writes=[b_sA[ch]])
                                    S.op("dve", lambda e, ch=ch, li=li: e.tensor_tensor(out=Gm[ch][:], in0=sA[ch], in1=cm[:, 6 + li, :], op=ALU.mult),
                                         reads=[b_sA[ch], b_cm], writes=[b_Gm[ch]])
                                    S.op("pool", lambda e, ch=ch: e.tensor_tensor(out=Gm[ch][:], in0=Gm[ch][:], in1=ident_bf[:], op=ALU.add),
                                         reads=[b_Gm[ch], b_ident], writes=[b_Gm[ch]])
                                for ch in range(4):
                                    if not last:
                                        S.op("pe", lambda e, ch=ch: e.matmul(out=sA[ch], lhsT=DTm[ch][:], rhs=Gm[ch][:], start=True, stop=True),
                                             reads=[b_DTm[ch], b_Gm[ch]], writes=[b_sA[ch]])
                                    S.op("pe", lambda e, ch=ch: e.matmul(out=sB[ch], lhsT=Gm[ch][:], rhs=DTm[ch][:], start=True, stop=True),
                                         reads=[b_DTm[ch], b_Gm[ch]], writes=[b_sB[ch]])
                                    if not last:
                                        S.op("act", lambda e, ch=ch: e.copy(out=Dm[ch][:], in_=sA[ch]), reads=[b_sA[ch]], writes=[b_Dm[ch]])
                                    S.op("act", lambda e, ch=ch: e.copy(out=DTm[ch][:], in_=sB[ch]), reads=[b_sB[ch]], writes=[b_DTm[ch]])
                            if stop == "C":
                                continue
                            for ch, (c, e_) in enumerate(chains):
                                pr = slice(e_ * 64, (e_ + 1) * 64)
                                S.op("pe", lambda e, ch=ch, c=c, pr=pr: e.matmul(out=sA[ch][:, 0:64], lhsT=NMt[ch][:, 2, :], rhs=TM[c][:, 1, pr],
                                                                              start=True, stop=True),
                                     reads=[b_NM[ch], b_TM[c]], writes=[b_sA[ch]])
                                S.op("act", lambda e, ch=ch: e.copy(out=X2b[ch][:], in_=sA[ch][:, 0:64]), reads=[b_sA[ch]], writes=[b_X2b[ch]])
                            for ci_ in range(2):
                                c = 2 * cp + ci_
                                for e_ in range(2):
                                    ch = ci_ * 2 + e_
                                    pr = slice(e_ * 64, (e_ + 1) * 64)
                                    S.op("pe", lambda e, ch=ch, pr=pr: e.matmul(out=u2_ps[:, pr], lhsT=DTm[ch][:], rhs=X2b[ch][:], start=True, stop=True),
                                         reads=[b_DTm[ch], b_X2b[ch]], writes=[b_u2])
                                    S.op("pe", lambda e, ch=ch, c=c: e.matmul(out=sA[ch], lhsT=TM[c][:, 0, :], rhs=DTm[ch][:], start=True, stop=True),
                                         reads=[b_DTm[ch], b_TM[c]], writes=[b_sA[ch]])
                                    S.op("dve", lambda e, ci_=ci_, ch=ch, pr=pr: e.tensor_copy(out=WTb[ci_][pr, :], in_=sA[ch][pr, :]),
                                         reads=[b_sA[ch]], writes=[b_WTb[ci_]])
                                S.op("act", lambda e, ci_=ci_: e.copy(out=U2s[ci_][:], in_=u2_ps), reads=[b_u2], writes=[b_U2s[ci_]])
                            if stop == "D":
                                continue
                            for ci_ in range(2):
                                c = 2 * cp + ci_
                                cc = slice(c * 128, (c + 1) * 128)
                                S.op("pe", lambda e, ci_=ci_: e.matmul(out=uh_ps, lhsT=WTb[ci_][:], rhs=Hbf[hp][:], start=True, stop=True),
                                     reads=[b_WTb[ci_], b_Hbf[hp]], writes=[b_uh])
                                S.op("dve", lambda e, ci_=ci_: e.tensor_tensor(out=Ub[:], in0=uh_ps, in1=U2s[ci_][:], op=ALU.add),
                                     reads=[b_uh, b_U2s[ci_]], writes=[b_Ub])
                                for e_ in range(2):
                                    ch = ci_ * 2 + e_
                                    pr = slice(e_ * 64, (e_ + 1) * 64)
                                    yp = y2_ps[e_]
                                    S.op("pe", lambda e, c=c, yp=yp: e.matmul(out=yp, lhsT=Hbf[hp][:], rhs=AR[:, c, 1, :], start=True, stop=False),
                                         reads=[b_Hbf[hp], b_AR], writes=[b_yps])
                                    S.op("pe", lambda e, ch=ch, yp=yp: e.matmul(out=yp, lhsT=Ub[:], rhs=NMt[ch][:, 1, :], start=False, stop=False),
                                         reads=[b_Ub, b_NM[ch]], writes=[b_yps])
                                    S.op("pe", lambda e, ch=ch, c=c, yp=yp: e.matmul(out=yp, lhsT=TM[c][:, 1, :], rhs=NMt[ch][:, 3, :], start=False, stop=True),
                                         reads=[b_TM[c], b_NM[ch]], writes=[b_yps])
                                for e_ in range(2):
                                    pr = slice(e_ * 64, (e_ + 1) * 64)
                                    S.op("act", lambda e, cc=cc, pr=pr, e_=e_: e.copy(out=y_sb[pr, cc], in_=y2_ps[e_][pr, :]), reads=[b_yps], writes=[b_y])
                                S.op("pe", lambda e, c=c: e.matmul(out=uh_ps, lhsT=TM[c][:, 2, :], rhs=Ub[:], start=True, stop=False),
                                     reads=[b_TM[c], b_Ub], writes=[b_uh])
                                S.op("pe", lambda e, c=c: e.matmul(out=uh_ps, lhsT=TM[c][:, 3, :], rhs=TM[c][:, 1, :], start=False, stop=True),
                                     reads=[b_TM[c]], writes=[b_uh])
                                for e_ in range(2):
                                    pr = slice(e_ * 64, (e_ + 1) * 64)
                                    S.op("dve", lambda e, pr=pr, c=c: e.scalar_tensor_tensor(
                                        out=H32[hp][pr, pr], in0=H32[hp][pr, pr], scalar=E_in[pr, c * 128 + 127:c * 128 + 128],
                                        in1=uh_ps[pr, pr], op0=ALU.mult, op1=ALU.add),
                                        reads=[b_H32[hp], b_Ein, b_uh], writes=[b_H32[hp]])
                                S.op("pool", lambda e: e.tensor_copy(out=Hbf[hp][:], in_=H32[hp][:]), reads=[b_H32[hp]], writes=[b_Hbf[hp]])
                        if dbg and "yraw" in dbg:
                            S.op("act", lambda e: e.copy(out=dbg_sb[:], in_=y_sb[:]), reads=[b_y], writes=[b_dbg])
                            S.dma("sp", dbg_aps["yraw"][hp * 128:(hp + 1) * 128, c_lo:c_hi], dbg_sb[:], reads=[b_dbg])
                        S.op("pe", lambda e: e.matmul(out=p0[:], lhsT=bd_mean[:], rhs=y_sb[:], start=True, stop=True),
                             reads=[b_cm, b_y], writes=[bp0])
                        S.op("dve", lambda e: e.tensor_tensor(out=y_sb[:], in0=y_sb[:], in1=p0[:], op=ALU.subtract), reads=[b_y, bp0], writes=[b_y])
                        S.op("pool", lambda e: e.tensor_tensor(out=tmpA[:], in0=y_sb[:], in1=y_sb[:], op=ALU.mult), reads=[b_y], writes=[b_tmpA])
                        S.op("pe", lambda e: e.matmul(out=p1[:], lhsT=bd_mean[:], rhs=tmpA[:], start=True, stop=True),
                             reads=[b_cm, b_tmpA], writes=[bp1])
                        S.op("dve", lambda e: e.tensor_scalar(out=tmpA[:], in0=p1[:], scalar1=LNX_EPS, scalar2=None, op0=ALU.add),
                             reads=[bp1], writes=[b_tmpA])
                        S.op("act", lambda e: e.sqrt(out=tmpA[:], in_=tmpA[:]), reads=[b_tmpA], writes=[b_tmpA])
                        S.op("dve", lambda e: e.reciprocal(out=tmpA[:], in_=tmpA[:]), reads=[b_tmpA], writes=[b_tmpA])
                        S.op("pool", lambda e: e.tensor_tensor(out=y_sb[:], in0=y_sb[:], in1=tmpA[:], op=ALU.mult), reads=[b_y, b_tmpA], writes=[b_y])
                        S.op("dve", lambda e: e.tensor_scalar(out=y_sb[:], in0=y_sb[:], scalar1=lnwT[:, hp:hp + 1], scalar2=lnbT[:, hp:hp + 1],
                                                              op0=ALU.mult, op1=ALU.add), reads=[b_y, b_lnw, b_lnb], writes=[b_y])
                        S.op("pool", lambda e: e.tensor_tensor(out=y_sb[:], in0=y_sb[:], in1=bonus[:], op=ALU.add), reads=[b_y, b_bonus], writes=[b_y])
                        S.op("dve", lambda e: e.tensor_tensor(out=catR[:], in0=y_sb[:], in1=g_t[:], op=ALU.mult), reads=[b_y, b_g], writes=[b_catR])
                        S.dma("sp", cat_scr[512 + hp * 128:512 + (hp + 1) * 128, c_lo:c_hi], catR[:], reads=[b_catR])
                        if dbg and "orwkv" in dbg:
                            S.op("act", lambda e: e.copy(out=dbg_sb[:], in_=catR[:]), reads=[b_catR], writes=[b_dbg])
                            S.dma("sp", dbg_aps["orwkv"][hp * 128:(hp + 1) * 128, c_lo:c_hi], dbg_sb[:], reads=[b_dbg])
                S.barrier()
        if "O" in phases:
            with ExitStack() as es:
                def sb(name, shape, dt=F32):
                    return es.enter_context(nc.sbuf_tensor(uniq(name), list(shape), dt))
                UT = 256
                NU = T // UT
                stg = [sb("stg%d" % i, [128, D], F32) for i in range(2)]
                b_stg = [Buf(), Buf()]
                st = {"xt": stg, "b_xt": b_stg}
                gfT, b_gfT = load_colvec(es, "gfT", gf_pre, KC)
                wout_bf = sb("wout_bf", [128, KC, D], BF16)
                b_wout = [Buf() for _ in range(KC)]
                wg_bf = sb("wg_bf", [128, KC, DFF], BF16)
                b_wg = [Buf() for _ in range(KC)]
                wu_bf = sb("wu_bf", [128, KC, DFF], BF16)
                b_wu = [Buf() for _ in range(KC)]
                wd_bf = sb("wd_bf", [128, NFF, D], BF16)
                b_wd = [Buf() for _ in range(NFF)]
                load_weight_bf(st, wout_bf, b_wout, w_out, 0, D, None, None)
                load_weight_bf(st, wg_bf, b_wg, w_gate, 0, DFF, gfT, b_gfT)
                load_weight_bf(st, wu_bf, b_wu, w_up, 0, DFF, gfT, b_gfT)
                load_weight_bf(st, wd_bf, b_wd, w_down, 0, D, None, None, nk=NFF)
                gpost_bc = sb("gpost_bc", [128, D], F32)
                gfpost_bc = sb("gfpost_bc", [128, D], F32)
                b_gbc = Buf()
                S.dma("sp", gpost_bc[:], g_post.partition_broadcast(128), writes=[b_gbc])
                S.dma("sp", gfpost_bc[:], gf_post.partition_broadcast(128), writes=[b_gbc])
                catT = sb("catT", [128, KC, UT], BF16)
                b_catT = Buf()
                zn = sb("zn", [128, D], BF16)
                b_zn = Buf()
                zT = sb("zT", [128, KC, UT], BF16)
                b_zT = Buf()
                aT = sb("aT", [128, NFF, UT], BF16)
                b_aT = Buf()
                junk = sb("junkO", [128, D], BF16)
                b_junk = Buf()
                sgt = [sb("sgt%d" % i, [128, UT], F32) for i in range(2)]
                b_sgt = [Buf(), Buf()]
                t1 = sb("t1", [128, D], F32)
                b_t1 = Buf()
                ssO = sb("ssO", [128, 4], F32)
                b_ssO = Buf()
                cat_v = cat_scr.rearrange("(k p) t -> p k t", p=128)

                def rstd_from(srcs, bsrcs):
                    for i, (ap_, b_) in enumerate(zip(srcs, bsrcs)):
                        n = ap_.shape[1]
                        S.op("act", lambda e, ap_=ap_, i=i, n=n: e.activation(out=junk[:, 0:n], in_=ap_, func=AF.Square, accum_out=ssO[:, i:i + 1]),
                             reads=[b_], writes=[b_junk, b_ssO])
                    if len(srcs) == 2:
                        S.op("dve", lambda e: e.tensor_tensor(out=ssO[:, 2:3], in0=ssO[:, 0:1], in1=ssO[:, 1:2], op=ALU.add),
                             reads=[b_ssO], writes=[b_ssO])
                        src = ssO[:, 2:3]
                    else:
                        src = ssO[:, 0:1]
                    S.op("dve", lambda e: e.tensor_scalar(out=ssO[:, 2:3], in0=src, scalar1=1.0 / D, scalar2=EPS, op0=ALU.mult, op1=ALU.add),
                         reads=[b_ssO], writes=[b_ssO])
                    S.op("act", lambda e: e.sqrt(out=ssO[:, 2:3], in_=ssO[:, 2:3]), reads=[b_ssO], writes=[b_ssO])
                    S.op("dve", lambda e: e.reciprocal(out=ssO[:, 2:3], in_=ssO[:, 2:3]), reads=[b_ssO], writes=[b_ssO])

                for u in range(NU):
                    t0 = u * UT
                    S.dma("sp", catT[:], cat_v[:, :, t0:t0 + UT], writes=[b_catT])
                    for j in range(2):
                        tj = t0 + j * 128
                        S.dma("sp", stg[j][:], x[tj:tj + 128, :], writes=[b_stg[j]])
                        for half in range(2):
                            for kc in range(KC):
                                S.op("pe", lambda e, kc=kc, half=half, j=j: e.matmul(
                                    out=PB[half][:], lhsT=catT[:, kc, j * 128:(j + 1) * 128], rhs=wout_bf[:, kc, half * 512:(half + 1) * 512],
                                    start=(kc == 0), stop=(kc == KC - 1)), reads=[b_catT, b_wout[kc]], writes=[b_PB[half]])
                        rstd_from([PB[0][:], PB[1][:]], [b_PB[0], b_PB[1]])
                        for half in range(2):
                            S.op("act", lambda e, half=half: e.activation(out=t1[:, half * 512:(half + 1) * 512], in_=PB[half][:], func=AF.Copy,
                                                                          scale=ssO[:, 2:3]), reads=[b_PB[half], b_ssO], writes=[b_t1])
                        S.op("pool", lambda e: e.tensor_tensor(out=t1[:], in0=t1[:], in1=gpost_bc[:], op=ALU.mult), reads=[b_t1, b_gbc], writes=[b_t1])
                        S.op("dve", lambda e, j=j: e.tensor_tensor(out=stg[j][:], in0=stg[j][:], in1=t1[:], op=ALU.add),
                             reads=[b_stg[j], b_t1], writes=[b_stg[j]])
                        if dbg and "h" in dbg:
                            S.dma("sp", dbg_aps["h"][tj:tj + 128, :], stg[j][:], reads=[b_stg[j]])
                        rstd_from([stg[j][:]], [b_stg[j]])
                        S.op("act", lambda e, j=j: e.activation(out=zn[:], in_=stg[j][:], func=AF.Copy, scale=ssO[:, 2:3]),
                             reads=[b_stg[j], b_ssO], writes=[b_zn])
                        for kc in range(KC):
                            S.op("pe", lambda e, kc=kc: e.transpose(out=tp_ps[:, kc, :], in_=zn[:, kc * 128:(kc + 1) * 128], identity=ident_bf[:]),
                                 reads=[b_zn, b_ident], writes=[b_tp])
                        S.op("dve", lambda e, j=j: e.tensor_copy(out=zT[:, :, j * 128:(j + 1) * 128], in_=tp_ps[:, :, :]), reads=[b_tp], writes=[b_zT])
                    for ffc in range(NFF):
                        gi_ = 2 + 2 * (ffc % 2)
                        ui_ = 3 + 2 * (ffc % 2)
                        fc = slice(ffc * 128, (ffc + 1) * 128)
                        for kc in range(KC):
                            S.op("pe", lambda e, kc=kc, fc=fc, gi_=gi_: e.matmul(out=PB[gi_][:, 0:UT], lhsT=wg_bf[:, kc, fc], rhs=zT[:, kc, :],
                                                                                 start=(kc == 0), stop=(kc == KC - 1)),
                                 reads=[b_wg[kc], b_zT], writes=[b_PB[gi_]])
                        for kc in range(KC):
                            S.op("pe", lambda e, kc=kc, fc=fc, ui_=ui_: e.matmul(out=PB[ui_][:, 0:UT], lhsT=wu_bf[:, kc, fc], rhs=zT[:, kc, :],
                                                                                 start=(kc == 0), stop=(kc == KC - 1)),
                                 reads=[b_wu[kc], b_zT], writes=[b_PB[ui_]])
                        si_ = ffc % 2
                        S.op("act", lambda e, gi_=gi_, si_=si_: e.activation(out=sgt[si_][:], in_=PB[gi_][:, 0:UT], func=AF.Silu),
                             reads=[b_PB[gi_]], writes=[b_sgt[si_]])
                        S.op("dve", lambda e, ui_=ui_, si_=si_, ffc=ffc: e.tensor_tensor(out=aT[:, ffc, :], in0=PB[ui_][:, 0:UT], in1=sgt[si_][:], op=ALU.mult),
                             reads=[b_PB[ui_], b_sgt[si_]], writes=[b_aT])
                    for j in range(2):
                        tj = t0 + j * 128
                        for half in range(2):
                            for ffc in range(NFF):
                                S.op("pe", lambda e, ffc=ffc, half=half, j=j: e.matmul(
                                    out=PB[half][:], lhsT=aT[:, ffc, j * 128:(j + 1) * 128], rhs=wd_bf[:, ffc, half * 512:(half + 1) * 512],
                                    start=(ffc == 0), stop=(ffc == NFF - 1)), reads=[b_aT, b_wd[ffc]], writes=[b_PB[half]])
                        rstd_from([PB[0][:], PB[1][:]], [b_PB[0], b_PB[1]])
                        for half in range(2):
                            S.op("act", lambda e, half=half: e.activation(out=t1[:, half * 512:(half + 1) * 512], in_=PB[half][:], func=AF.Copy,
                                                                          scale=ssO[:, 2:3]), reads=[b_PB[half], b_ssO], writes=[b_t1])
                        S.op("pool", lambda e: e.tensor_tensor(out=t1[:], in0=t1[:], in1=gfpost_bc[:], op=ALU.mult), reads=[b_t1, b_gbc], writes=[b_t1])
                        S.op("dve", lambda e, j=j: e.tensor_tensor(out=t1[:], in0=t1[:], in1=stg[j][:], op=ALU.add),
                             reads=[b_stg[j], b_t1], writes=[b_t1])
                        S.dma("sp", out[tj:tj + 128, :], t1[:], reads=[b_t1])

        S.wait_tokens("sp", [t for e in S.ENGS for t in S.dtoks[e]])
        S.emit()
    return nc


WNAMES = ["attn_norm_pre", "attn_norm_post", "w_in", "fox_forget_bias", "shift_mu", "rwkv_w0", "rwkv_w_up", "rwkv_a0",
          "rwkv_a_up", "rwkv_g_up", "rwkv_k_k", "rwkv_k_a", "rwkv_r_k", "rwkv_ln_w", "rwkv_ln_b", "w_out",
          "ffn_norm_pre", "ffn_norm_post", "ffn_w_gate", "ffn_w_up", "ffn_w_down"]


def make_in_map(inputs, b, T):
    m = {"x": np.ascontiguousarray(np.asarray(inputs["x"], dtype=np.float32)[b, :T])}
    for k in WNAMES:
        a = np.asarray(inputs[k], dtype=np.float32)[0]
        if k == "rwkv_r_k":
            a = a.reshape(-1)
        m[k] = np.ascontiguousarray(a)
    m["cmask"] = make_cmask()
    return m


def kernel(**inputs):
    x = np.asarray(inputs["x"])
    B, T, _ = x.shape
    nc = build_nc(T)
    in_maps = [make_in_map(inputs, b, T) for b in range(B)]
    res = run_bass_kernel_spmd(nc, in_maps, core_ids=list(range(B)))
    return np.stack([np.asarray(r["out"], dtype=np.float32) for r in res.results], axis=0)
```

```python
import numpy as np
from contextlib import ExitStack
import concourse.bass as bass
import concourse.mybir as mybir
from concourse.bass_utils import run_bass_kernel_spmd

F32 = mybir.dt.float32
BF16 = mybir.dt.bfloat16
AF = mybir.ActivationFunctionType
ALU = mybir.AluOpType
AX = mybir.AxisListType

D = 1024
KC = 8
HD = 64
NH = 8
FOXW = 512
RW = 512
DFF = 2816
NFF = 22
WIN = 3368
RBASE = 1544
EPS = 1e-6
LNX_EPS = 64e-5
SB = 512
EPOCH = 12000


class Buf:
    __slots__ = ("w", "r")

    def __init__(self):
        self.w = None
        self.r = []


class _Rec:
    def __getattr__(self, name):
        def f(*a, **k):
            self.call = (name, a, k)
        return f


class Sched:
    ENGS = ("pe", "act", "dve", "pool", "sp")

    def __init__(self, nc, es):
        self.nc = nc
        self.es = es
        self.q = {e: [] for e in self.ENGS}
        self.cnt = {e: 0 for e in self.ENGS}
        self.run = {e: {} for e in self.ENGS}
        self.clk = {}
        self.sems = {}
        self.ndma = {"sp": 16, "act": 6, "pool": 6}
        self.dcnt = {e: 0 for e in self.ENGS}
        self.dtoks = {e: [] for e in self.ENGS}
        self.nwaits = 0

    def sem(self, key):
        s = self.sems.get(key)
        if s is None:
            s = self.es.enter_context(self.nc.semaphore("s_%s_%s" % key))
            self.sems[key] = s
        return s

    def _deps(self, eng, reads, writes, is_dma):
        deps = set()
        for b in reads:
            if b.w is not None:
                deps.add(b.w)
        for b in writes:
            if b.w is not None:
                deps.add(b.w)
            for t in b.r:
                deps.add(t)
        return deps

    def _waits(self, eng, deps):
        run = self.run[eng]
        waits = []
        for t in sorted(deps, key=lambda t: (str(t[0]), t[1])):
            key, val, isd = t
            if eng == "pe" and key[0] == "pe" and not isd:
                continue
            if run.get(key, 0) >= val:
                continue
            waits.append((key, val))
            for k2, v2 in self.clk[(key, val)].items():
                if run.get(k2, 0) < v2:
                    run[k2] = v2
        self.nwaits += len(waits)
        return waits

    def op(self, eng, fn, reads=(), writes=()):
        rec = _Rec()
        fn(rec)
        call = rec.call
        fn = lambda e, call=call: getattr(e, call[0])(*call[1], **call[2])
        deps = self._deps(eng, reads, writes, False)
        waits = self._waits(eng, deps)
        n = self.cnt[eng]
        self.cnt[eng] = n + 1
        key = (eng, n // EPOCH)
        val = n % EPOCH + 1
        tok = (key, val, False)
        c = dict(self.run[eng])
        c[key] = val
        self.clk[(key, val)] = c
        self.q[eng].append((waits, fn, key, 1))
        for b in reads:
            b.r.append(tok)
        for b in writes:
            b.w = tok
            b.r = []
        return tok

    def dma(self, eng, out, in_, reads=(), writes=(), **kw):
        deps = self._deps(eng, reads, writes, True)
        d = self.dcnt[eng]
        self.dcnt[eng] = d + 1
        nd = self.ndma[eng]
        if d >= nd:
            deps.add(self.dtoks[eng][d - nd])
        waits = self._waits(eng, deps)
        key = ("d" + eng, d % nd)
        val = 16 * (d // nd + 1)
        tok = (key, val, True)
        c = dict(self.run[eng])
        c[key] = val
        self.clk[(key, val)] = c
        self.dtoks[eng].append(tok)
        self.q[eng].append((waits, lambda e: e.dma_start(out=out, in_=in_, **kw), key, 16))
        for b in reads:
            b.r.append(tok)
        for b in writes:
            b.w = tok
            b.r = []
        return tok

    def barrier(self):
        best = {}
        for e in self.ENGS:
            n = self.cnt[e]
            if n > 0 and e != "sp":
                best[(e, (n - 1) // EPOCH)] = ((n - 1) % EPOCH + 1, False)
            for (k, v, isd) in self.dtoks[e][-self.ndma.get(e, 1):]:
                if best.get(k, (0, True))[0] < v:
                    best[k] = (v, True)
        toks = [(k, v, isd) for k, (v, isd) in best.items()]
        for e in self.ENGS:
            waits = []
            run = self.run[e]
            for (k, v, isd) in toks:
                if run.get(k, 0) < v:
                    waits.append((k, v))
                    for k2, v2 in self.clk[(k, v)].items():
                        if run.get(k2, 0) < v2:
                            run[k2] = v2
            self.q[e].append((waits, None, None, 0))

    def wait_tokens(self, eng, toks):
        waits = self._waits(eng, set(toks))
        self.q[eng].append((waits, None, None, 0))

    def emit(self):
        nc = self.nc
        for k in set(k for e in self.ENGS for (_, _, k, _) in self.q[e] if k is not None):
            self.sem(k)
        for e in self.ENGS:
            for (waits, _, _, _) in self.q[e]:
                for (k, v) in waits:
                    self.sem(k)
        with nc.Block() as block:
            def run(engname, engobj):
                for (waits, fn, key, inc) in self.q[engname]:
                    for (k, v) in waits:
                        engobj.wait_ge(self.sems[k], v)
                    if fn is not None:
                        ins = fn(engobj)
                        ins.then_inc(self.sems[key], inc)

            @block.tensor
            def _(e):
                run("pe", e)

            @block.scalar
            def _(e):
                run("act", e)

            @block.vector
            def _(e):
                run("dve", e)

            @block.gpsimd
            def _(e):
                run("pool", e)

            @block.sync
            def _(e):
                run("sp", e)


NMASK = 13
LEVELS = (2, 4, 8, 16, 32, 64)


def make_cmask():
    p = np.arange(128)[:, None]
    f = np.arange(128)[None, :]
    m = np.zeros((128, NMASK, 128), np.float32)
    m[:, 0, :] = (f > p)
    m[:, 1, :] = (f >= p)
    m[:, 2, :] = (f > p)
    m[:, 3, :] = (f >= p)
    m[:, 4, :] = ((p % 2 == 1) & (f == p - 1))
    m[:, 5, :] = ((f % 2 == 1) & (p == f - 1))
    for li, mm in enumerate(LEVELS):
        m[:, 6 + li, :] = (((p // mm) % 2 == 1) & ((f // mm) == (p // mm) - 1))
    m[:, 12, :] = ((p // 64) == (f // 64))
    return m


def build_nc(T, dbg=None, phases="FRO"):
    NSB = T // SB
    NBLK = T // 128
    nc = bass.Bass("TRN2", target_bir_lowering=False)
    es0 = ExitStack()
    S = Sched(nc, es0)

    def din(name, shape):
        return nc.dram_tensor(name, list(shape), F32, kind="ExternalInput").ap()

    x = din("x", [T, D])
    g_pre = din("attn_norm_pre", [D])
    g_post = din("attn_norm_post", [D])
    w_in = din("w_in", [D, WIN])
    din_fb = din("fox_forget_bias", [NH])
    shift_mu = din("shift_mu", [1824])
    rwkv_w0 = din("rwkv_w0", [RW])
    rwkv_w_up = din("rwkv_w_up", [64, RW])
    rwkv_a0 = din("rwkv_a0", [RW])
    rwkv_a_up = din("rwkv_a_up", [64, RW])
    rwkv_g_up = din("rwkv_g_up", [160, RW])
    rwkv_k_k = din("rwkv_k_k", [RW])
    rwkv_k_a = din("rwkv_k_a", [RW])
    rwkv_r_k = din("rwkv_r_k", [RW])
    rwkv_ln_w = din("rwkv_ln_w", [RW])
    rwkv_ln_b = din("rwkv_ln_b", [RW])
    w_out = din("w_out", [D, D])
    gf_pre = din("ffn_norm_pre", [D])
    gf_post = din("ffn_norm_post", [D])
    w_gate = din("ffn_w_gate", [D, DFF])
    w_up = din("ffn_w_up", [D, DFF])
    w_down = din("ffn_w_down", [DFF, D])
    cmask = din("cmask", [128, NMASK, 128])
    out = nc.dram_tensor("out", [T, D], F32, kind="ExternalOutput").ap()
    cat_scr = nc.dram_tensor("cat_scr", [D, T], BF16, kind="Internal").ap()
    scr_k = nc.dram_tensor("scr_k", [3, NH, T], BF16, kind="Internal").ap()
    scr_q = nc.dram_tensor("scr_q", [3, NH, T], BF16, kind="Internal").ap()
    dbg_aps = {}
    if dbg:
        for k, shp in dbg.items():
            if k == "stop":
                continue
            dbg_aps[k] = nc.dram_tensor("dbg_" + k, list(shp), F32, kind="ExternalOutput").ap()

    ucnt = [0]

    def uniq(name):
        ucnt[0] += 1
        return "%s_%d" % (name, ucnt[0])

    with es0:
        tp_ps = es0.enter_context(nc.psum_tensor("tp_ps", [128, KC, 128], BF16))
        b_tp = Buf()
        PB = [es0.enter_context(nc.psum_tensor("pb%d" % i, [128, SB], F32)) for i in range(7)]
        b_PB = [Buf() for _ in range(7)]
        ident_bf = es0.enter_context(nc.sbuf_tensor("ident_bf", [128, 128], BF16))
        ident_f = es0.enter_context(nc.sbuf_tensor("ident_f", [128, 128], F32))
        b_ident = Buf()
        S.op("pool", lambda e: e.memset(ident_f[:], 0.0), writes=[b_ident])
        S.op("pool", lambda e: e.affine_select(out=ident_f[:], in_=ident_f[:], pattern=[[-1, 128]],
                                               compare_op=ALU.not_equal, fill=1.0, base=0,
                                               channel_multiplier=1), reads=[b_ident], writes=[b_ident])
        S.op("dve", lambda e: e.tensor_copy(out=ident_bf[:], in_=ident_f[:]), reads=[b_ident], writes=[b_ident])

        def load_colvec(es, name, src, ncol):
            t = es.enter_context(nc.sbuf_tensor(uniq(name), [128, ncol], F32))
            b = Buf()
            S.dma("sp", t[:], src.rearrange("(k p) -> p k", p=128), writes=[b], allow_slow_non_contiguous=True)
            return t, b

        def make_front(es):
            def sbt(name, shape, dt=F32):
                return es.enter_context(nc.sbuf_tensor(uniq(name), list(shape), dt))
            st = {}
            st["xt"] = [sbt("xt%d" % i, [128, D], F32) for i in range(2)]
            st["b_xt"] = [Buf() for _ in range(2)]
            st["junk"] = sbt("junk", [128, D], BF16)
            st["b_junk"] = Buf()
            st["xn"] = [sbt("xn%d" % i, [128, D], BF16) for i in range(2)]
            st["b_xn"] = [Buf(), Buf()]
            st["ss"] = sbt("ss", [128, 8], F32)
            st["b_ss"] = [Buf() for _ in range(8)]
            st["uT"] = sbt("uT", [128, KC, SB], BF16)
            st["b_uT"] = Buf()
            st["tc"] = 0
            return st

        def front(st, s):
            xt, b_xt, xn, b_xn, ss, b_ss = st["xt"], st["b_xt"], st["xn"], st["b_xn"], st["ss"], st["b_ss"]
            junk, b_junk, uT, b_uT = st["junk"], st["b_junk"], st["uT"], st["b_uT"]
            for j in range(4):
                t0 = s * SB + j * 128
                xi = st["tc"] % 2
                ni = st["tc"] % 2
                si = st["tc"] % 8
                st["tc"] += 1
                S.dma("sp", xt[xi][:], x[t0:t0 + 128, :], writes=[b_xt[xi]])
                S.op("act", lambda e, xi=xi, si=si: e.activation(out=junk[:], in_=xt[xi][:], func=AF.Square,
                                                                 accum_out=ss[:, si:si + 1]),
                     reads=[b_xt[xi]], writes=[b_junk, b_ss[si]])
                S.op("dve", lambda e, si=si: e.tensor_scalar(out=ss[:, si:si + 1], in0=ss[:, si:si + 1],
                                                             scalar1=1.0 / D, scalar2=EPS, op0=ALU.mult, op1=ALU.add),
                     reads=[b_ss[si]], writes=[b_ss[si]])
                S.op("act", lambda e, si=si: e.sqrt(out=ss[:, si:si + 1], in_=ss[:, si:si + 1]),
                     reads=[b_ss[si]], writes=[b_ss[si]])
                S.op("dve", lambda e, si=si: e.reciprocal(out=ss[:, si:si + 1], in_=ss[:, si:si + 1]),
                     reads=[b_ss[si]], writes=[b_ss[si]])
                S.op("act", lambda e, xi=xi, ni=ni, si=si: e.activation(out=xn[ni][:], in_=xt[xi][:],
                                                                        func=AF.Copy, scale=ss[:, si:si + 1]),
                     reads=[b_xt[xi], b_ss[si]], writes=[b_xn[ni]])
                for kc in range(KC):
                    S.op("pe", lambda e, kc=kc, ni=ni: e.transpose(out=tp_ps[:, kc, :],
                                                                   in_=xn[ni][:, kc * 128:(kc + 1) * 128],
                                                                   identity=ident_bf[:]),
                         reads=[b_xn[ni], b_ident], writes=[b_tp])
                S.op("dve", lambda e, j=j: e.tensor_copy(out=uT[:, :, j * 128:(j + 1) * 128], in_=tp_ps[:, :, :]),
                     reads=[b_tp], writes=[b_uT])

        def load_weight_bf(st, dst, b_dst, src, c0, ncols, gvec, b_g, nk=KC):
            xt, b_xt = st["xt"], st["b_xt"]
            cnt = 0
            for kc in range(nk):
                for p0 in range(0, ncols, D):
                    n = min(D, ncols - p0)
                    i = cnt % 2
                    cnt += 1
                    S.dma("sp", xt[i][:, 0:n], src[kc * 128:(kc + 1) * 128, c0 + p0:c0 + p0 + n], writes=[b_xt[i]])
                    if cnt % 2 == 0:
                        if gvec is None:
                            S.op("dve", lambda e: e.tensor_copy(out=dst[:, kc, p0:p0 + n], in_=xt[i][:, 0:n]),
                                 reads=[b_xt[i]], writes=[b_dst[kc]])
                        else:
                            S.op("dve", lambda e: e.tensor_scalar(
                                out=dst[:, kc, p0:p0 + n], in0=xt[i][:, 0:n], scalar1=gvec[:, kc:kc + 1], scalar2=None, op0=ALU.mult),
                                 reads=[b_xt[i], b_g], writes=[b_dst[kc]])
                    else:
                        if gvec is None:
                            S.op("act", lambda e: e.copy(out=dst[:, kc, p0:p0 + n], in_=xt[i][:, 0:n]),
                                 reads=[b_xt[i]], writes=[b_dst[kc]])
                        else:
                            S.op("act", lambda e: e.activation(out=dst[:, kc, p0:p0 + n], in_=xt[i][:, 0:n], func=AF.Copy,
                                                               scale=gvec[:, kc:kc + 1]),
                                 reads=[b_xt[i], b_g], writes=[b_dst[kc]])

        pjc = [0]

        def proj_fm(wt, b_w, st, c0, ncols, evac):
            pi = pjc[0] % 2
            pjc[0] += 1
            uT, b_uT = st["uT"], st["b_uT"]
            for kc in range(KC):
                S.op("pe", lambda e, kc=kc: e.matmul(out=PB[pi][0:ncols, :], lhsT=wt[:, kc, c0:c0 + ncols],
                                                     rhs=uT[:, kc, :], start=(kc == 0), stop=(kc == KC - 1)),
                     reads=[b_w[kc], b_uT], writes=[b_PB[pi]])
            evac(PB[pi], b_PB[pi])

        if "F" in phases:
            with ExitStack() as es:
                def sb(name, shape, dt=F32):
                    return es.enter_context(nc.sbuf_tensor(uniq(name), list(shape), dt))
                st = make_front(es)
                uT, b_uT = st["uT"], st["b_uT"]
                gT, b_gT = load_colvec(es, "gT", g_pre, KC)
                w_bf = sb("w_bf", [128, KC, RBASE], BF16)
                b_w = [Buf() for _ in range(KC)]
                load_weight_bf(st, w_bf, b_w, w_in, 0, RBASE, gT, b_gT)

                nfb = sb("nfb", [8, 1], F32)
                b_nfb = Buf()
                S.dma("sp", nfb[:], din_fb.rearrange("(h o) -> h o", o=1), writes=[b_nfb])
                S.op("dve", lambda e: e.tensor_scalar(out=nfb[:], in0=nfb[:], scalar1=-1.0, scalar2=None, op0=ALU.mult),
                     reads=[b_nfb], writes=[b_nfb])
                ones8 = sb("ones8", [8, SB], F32)
                b_ones8 = Buf()
                S.op("pool", lambda e: e.memset(ones8[:], 1.0), writes=[b_ones8])
                ones_f = sb("ones_f", [128, 64], F32)
                b_onesf = Buf()
                S.op("pool", lambda e: e.memset(ones_f[:], 1.0), writes=[b_onesf])
                maskneg_f = sb("maskneg_f", [128, 128], F32)
                maskneg = sb("maskneg", [128, 128], BF16)
                b_mask = Buf()
                S.op("pool", lambda e: e.memset(maskneg_f[:], 0.0), writes=[b_mask])
                S.op("pool", lambda e: e.affine_select(out=maskneg_f[:], in_=maskneg_f[:], pattern=[[1, 128]],
                                                       compare_op=ALU.is_ge, fill=-30000.0, base=0,
                                                       channel_multiplier=-1), reads=[b_mask], writes=[b_mask])
                S.op("dve", lambda e: e.tensor_copy(out=maskneg[:], in_=maskneg_f[:]), reads=[b_mask], writes=[b_mask])

                KT = sb("KT", [70, NH, T], BF16)
                b_KT = [Buf() for _ in range(NSB)]
                QT = sb("QT", [70, NH, SB], BF16)
                b_QT = Buf()
                VT = sb("VT", [128, NBLK, NH, 66], BF16)
                b_VT = [Buf() for _ in range(NSB)]
                S.op("pool", lambda e: e.memset(KT[64:70, :, :], 1.0), writes=b_KT)
                S.op("pool", lambda e: e.memset(QT[64:70, :, :], 1.0), writes=[b_QT])
                S.op("pool", lambda e: e.memset(VT[:, :, :, 64:66], 1.0), writes=b_VT)
                b_scrk = Buf()
                b_scrq = Buf()
                cneg = [sb("cneg%d" % i, [8, SB], F32) for i in range(2)]
                b_cneg = [Buf(), Buf()]
                fl = sb("fl", [8, SB], F32)
                b_fl = Buf()
                res1 = sb("res1", [8, SB], F32)
                b_res1 = Buf()
                ksp = sb("ksp", [8, 3, SB], BF16)
                qsp = sb("qsp", [8, 3, SB], BF16)
                b_ksp = Buf()
                b_qsp = Buf()
                catF = [sb("catF%d" % i, [64, SB], BF16) for i in range(2)]
                b_catF = [Buf(), Buf()]
                PT = [sb("PT%d" % i, [128, SB], BF16) for i in range(3)]
                b_PT = [Buf() for _ in range(3)]
                rs = sb("rs", [66, SB], F32)
                b_rs = Buf()
                bc_sb = sb("bc_sb", [64, SB], F32)
                b_bc = Buf()
                dbg_sb = sb("dbg_sb", [128, SB], F32)
                b_dbg = Buf()
                st_ps = [PB[2], PB[3]]
                b_st = [b_PB[2], b_PB[3]]
                o_ps2 = [PB[4], PB[5]]
                b_o2 = [b_PB[4], b_PB[5]]
                bc_ps, b_bcps = PB[6], b_PB[6]

                for s in range(NSB):
                    c_lo, c_hi = s * SB, (s + 1) * SB
                    front(st, s)
                    ci = s % 2

                    def ev_ff(pp, bp):
                        S.op("act", lambda e: e.activation(out=fl[:], in_=pp[0:8, :], func=AF.Exp, bias=nfb[:, 0:1], scale=-1.0),
                             reads=[bp, b_nfb], writes=[b_fl])
                        S.op("act", lambda e: e.activation(out=fl[:], in_=fl[:], func=AF.Ln, bias=1.0, scale=1.0),
                             reads=[b_fl], writes=[b_fl])
                        S.op("dve", lambda e: e.tensor_tensor_scan(out=cneg[ci][:], data0=ones8[:], data1=fl[:], initial=0.0,
                                                                   op0=ALU.mult, op1=ALU.add),
                             reads=[b_fl, b_ones8], writes=[b_cneg[ci]])
                        if s > 0:
                            S.op("dve", lambda e: e.tensor_scalar(out=cneg[ci][:], in0=cneg[ci][:],
                                                                  scalar1=cneg[1 - ci][:, SB - 1:SB], scalar2=None, op0=ALU.add),
                                 reads=[b_cneg[ci], b_cneg[1 - ci]], writes=[b_cneg[ci]])
                        S.op("dve", lambda e: e.tensor_copy(out=ksp[:, 0, :], in_=cneg[ci][:]), reads=[b_cneg[ci]], writes=[b_ksp])
                        S.op("dve", lambda e: e.tensor_tensor(out=res1[:], in0=cneg[ci][:], in1=ksp[:, 0, :], op=ALU.subtract),
                             reads=[b_cneg[ci], b_ksp], writes=[b_res1])
                        S.op("dve", lambda e: e.tensor_copy(out=ksp[:, 1, :], in_=res1[:]), reads=[b_res1], writes=[b_ksp])
                        S.op("dve", lambda e: e.tensor_tensor(out=res1[:], in0=res1[:], in1=ksp[:, 1, :], op=ALU.subtract),
                             reads=[b_res1, b_ksp], writes=[b_res1])
                        S.op("dve", lambda e: e.tensor_copy(out=ksp[:, 2, :], in_=res1[:]), reads=[b_res1], writes=[b_ksp])
                        S.op("dve", lambda e: e.tensor_scalar(out=qsp[:], in0=ksp[:], scalar1=-1.0, scalar2=None, op0=ALU.mult),
                             reads=[b_ksp], writes=[b_qsp])
                        S.dma("sp", scr_k[:, :, c_lo:c_hi].rearrange("r h t -> h r t"), ksp[:], reads=[b_ksp], writes=[b_scrk])
                        S.dma("sp", scr_q[:, :, c_lo:c_hi].rearrange("r h t -> h r t"), qsp[:], reads=[b_qsp], writes=[b_scrq])
                        S.dma("sp", KT[67:70, :, c_lo:c_hi], scr_k[:, :, c_lo:c_hi], reads=[b_scrk], writes=[b_KT[s]])
                        S.dma("sp", QT[64:67, :, :], scr_q[:, :, c_lo:c_hi], reads=[b_scrq], writes=[b_QT])

                    proj_fm(w_bf, b_w, st, 1536, 8, ev_ff)
                    for h in range(NH):
                        def ev_q(pp, bp, h=h):
                            S.op("act", lambda e: e.mul(out=QT[0:64, h, :], in_=pp[0:64, :], mul=0.125), reads=[bp], writes=[b_QT])
                        proj_fm(w_bf, b_w, st, h * 64, 64, ev_q)

                        def ev_k(pp, bp, h=h):
                            S.op("dve", lambda e: e.tensor_copy(out=KT[0:64, h, c_lo:c_hi], in_=pp[0:64, :]), reads=[bp], writes=[b_KT[s]])
                        proj_fm(w_bf, b_w, st, 512 + h * 64, 64, ev_k)
                    for j in range(4):
                        pi = pjc[0] % 2
                        pjc[0] += 1
                        for kc in range(KC):
                            S.op("pe", lambda e, kc=kc, j=j, pi=pi: e.matmul(out=PB[pi][:], lhsT=uT[:, kc, j * 128:(j + 1) * 128],
                                                                             rhs=w_bf[:, kc, 1024:1536], start=(kc == 0), stop=(kc == KC - 1)),
                                 reads=[b_w[kc], b_uT], writes=[b_PB[pi]])
                        S.op("act", lambda e, j=j, pi=pi: e.copy(out=VT[:, s * 4 + j, :, 0:64],
                                                                 in_=PB[pi][:].rearrange("p (h d) -> p h d", h=NH)),
                             reads=[b_PB[pi]], writes=[b_VT[s]])

                    tiles = []
                    nkb = 4 * (s + 1)
                    for h in range(NH):
                        for kb in range(nkb):
                            d = kb - 4 * s
                            tiles.append((h, kb, 0 if d < 0 else d * 128, d >= 0, len(tiles)))

                    def emit_qk(tl):
                        h, kb, q0, diag, idx = tl
                        si_ = idx % 2
                        S.op("pe", lambda e: e.matmul(out=st_ps[si_][:, q0:SB], lhsT=KT[0:70, h, kb * 128:(kb + 1) * 128],
                                                      rhs=QT[0:70, h, q0:SB], start=True, stop=(not diag)),
                             reads=[b_KT[kb // 4], b_QT], writes=[b_st[si_]])
                        if diag:
                            S.op("pe", lambda e: e.matmul(out=st_ps[si_][:, q0:q0 + 128], lhsT=ident_bf[:], rhs=maskneg[:],
                                                          start=False, stop=True),
                                 reads=[b_ident, b_mask], writes=[b_st[si_]])

                    emit_qk(tiles[0])
                    for ti, tl in enumerate(tiles):
                        h, kb, q0, diag, idx = tl
                        si_ = idx % 2
                        pi_ = idx % 3
                        oi_ = h % 2
                        if ti + 1 < len(tiles):
                            emit_qk(tiles[ti + 1])
                        S.op("act", lambda e: e.activation(out=PT[pi_][:, q0:SB], in_=st_ps[si_][:, q0:SB], func=AF.Exp),
                             reads=[b_st[si_]], writes=[b_PT[pi_]])
                        S.op("pe", lambda e: e.matmul(out=o_ps2[oi_][0:66, q0:SB], lhsT=VT[:, kb, h, :], rhs=PT[pi_][:, q0:SB],
                                                      start=(kb == 0), stop=(kb == nkb - 1)),
                             reads=[b_VT[kb // 4], b_PT[pi_]], writes=[b_o2[oi_]])
                        if kb != nkb - 1:
                            continue
                        o_ps, b_o = o_ps2[oi_], b_o2[oi_]
                        S.op("dve", lambda e: e.reciprocal(out=rs[64:66, :], in_=o_ps[64:66, :]), reads=[b_o], writes=[b_rs])
                        S.op("pe", lambda e: e.matmul(out=bc_ps[0:64, :], lhsT=ones_f[64:65, 0:64], rhs=rs[64:65, :], start=True, stop=True),
                             reads=[b_rs, b_onesf], writes=[b_bcps])
                        S.op("act", lambda e: e.copy(out=bc_sb[:], in_=bc_ps[0:64, :]), reads=[b_bcps], writes=[b_bc])
                        fi = h % 2
                        S.op("dve", lambda e: e.tensor_tensor(out=catF[fi][:], in0=o_ps[0:64, :], in1=bc_sb[:], op=ALU.mult),
                             reads=[b_o, b_bc], writes=[b_catF[fi]])
                        S.dma("sp", cat_scr[h * 64:(h + 1) * 64, c_lo:c_hi], catF[fi][:], reads=[b_catF[fi]])
                        if dbg and "ofox" in dbg:
                            S.op("act", lambda e, fi=fi: e.copy(out=dbg_sb[0:64, :], in_=catF[fi][:]), reads=[b_catF[fi]], writes=[b_dbg])
                            S.dma("sp", dbg_aps["ofox"][h * 64:(h + 1) * 64, c_lo:c_hi], dbg_sb[0:64, :], reads=[b_dbg])
                S.barrier()
        if "R" in phases:
            with ExitStack() as es:
                def sb(name, shape, dt=F32):
                    return es.enter_context(nc.sbuf_tensor(uniq(name), list(shape), dt))
                st = make_front(es)
                uT, b_uT = st["uT"], st["b_uT"]
                gT, b_gT = load_colvec(es, "gTr", g_pre, KC)
                NRC = 1824
                wr_bf = sb("wr_bf", [128, KC, NRC], BF16)
                b_wr = [Buf() for _ in range(KC)]
                load_weight_bf(st, wr_bf, b_wr, w_in, RBASE, NRC, gT, b_gT)
                lo_bf = sb("lo_bf", [128, RW], BF16)
                b_lo = Buf()
                gup_bf = sb("gup_bf", [128, RW], BF16)
                gup1_bf = sb("gup1_bf", [32, RW], BF16)
                b_gup = Buf()
                xt, b_xt = st["xt"], st["b_xt"]
                S.dma("sp", xt[0][0:64, 0:RW], rwkv_w_up[:, :], writes=[b_xt[0]])
                S.dma("sp", xt[0][64:128, 0:RW], rwkv_a_up[:, :], writes=[b_xt[0]])
                S.op("dve", lambda e: e.tensor_copy(out=lo_bf[:], in_=xt[0][:, 0:RW]), reads=[b_xt[0]], writes=[b_lo])
                S.dma("sp", xt[1][:, 0:RW], rwkv_g_up[0:128, :], writes=[b_xt[1]])
                S.op("dve", lambda e: e.tensor_copy(out=gup_bf[:], in_=xt[1][:, 0:RW]), reads=[b_xt[1]], writes=[b_gup])
                S.dma("sp", xt[0][0:32, 0:RW], rwkv_g_up[128:160, :], reads=[b_lo], writes=[b_xt[0]])
                S.op("dve", lambda e: e.tensor_copy(out=gup1_bf[:], in_=xt[0][0:32, 0:RW]), reads=[b_xt[0]], writes=[b_gup])
                w0T, b_w0 = load_colvec(es, "w0T", rwkv_w0, 4)
                a0T, b_a0 = load_colvec(es, "a0T", rwkv_a0, 4)
                kkT, b_kkv = load_colvec(es, "kkT", rwkv_k_k, 4)
                kaT, b_kav = load_colvec(es, "kaT", rwkv_k_a, 4)
                rkT, b_rkv = load_colvec(es, "rkT", rwkv_r_k, 4)
                lnwT, b_lnw = load_colvec(es, "lnwT", rwkv_ln_w, 4)
                lnbT, b_lnb = load_colvec(es, "lnbT", rwkv_ln_b, 4)
                groups = {}
                gl = []
                for hp in range(4):
                    gl.append((("r", hp), hp * 128, 128))
                    gl.append((("k", hp), 512 + hp * 128, 128))
                    gl.append((("v", hp), 1024 + hp * 128, 128))
                gl.append(("wa", 1536, 128))
                gl.append(("g0", 1664, 128))
                gl.append(("g1", 1792, 32))
                muT = sb("muT", [128, len(gl)], F32)
                b_mu = Buf()
                carry = sb("carry", [128, len(gl)], F32)
                b_carry = [Buf() for _ in gl]
                S.op("pool", lambda e: e.memset(carry[:], 0.0), writes=b_carry)
                for gi, (nm, c0, n) in enumerate(gl):
                    groups[nm] = (gi, c0, n)
                    S.dma("sp", muT[0:n, gi:gi + 1], shift_mu[c0:c0 + n].rearrange("(p o) -> p o", o=1), writes=[b_mu])
                cm = sb("cm", [128, NMASK, 128], F32)
                b_cm = Buf()
                S.dma("sp", cm[:], cmask[:, :, :], writes=[b_cm])
                mk0T_bf = sb("mk0T_bf", [128, 128], BF16)
                S.op("dve", lambda e: e.tensor_copy(out=mk0T_bf[:], in_=cm[:, 5, :]), reads=[b_cm], writes=[b_cm])
                bd_mean = sb("bd_mean", [128, 128], F32)
                S.op("dve", lambda e: e.tensor_scalar(out=bd_mean[:], in0=cm[:, 12, :], scalar1=1.0 / 64, scalar2=None, op0=ALU.mult),
                     reads=[b_cm], writes=[b_cm])
                ones128 = sb("ones128", [128, 128], F32)
                S.op("pool", lambda e: e.memset(ones128[:], 1.0), writes=[b_cm])

                H32 = [sb("H32_%d" % i, [128, 128], F32) for i in range(4)]
                Hbf = [sb("Hbf_%d" % i, [128, 128], BF16) for i in range(4)]
                b_H32 = [Buf() for _ in range(4)]
                b_Hbf = [Buf() for _ in range(4)]
                for i in range(4):
                    S.op("pool", lambda e, i=i: e.memset(H32[i][:], 0.0), writes=[b_H32[i]])
                    S.op("pool", lambda e, i=i: e.memset(Hbf[i][:], 0.0), writes=[b_Hbf[i]])

                def wt(name, dt=F32, n=SB):
                    return sb(name, [128, n], dt), Buf()
                raw = [sb("raw%d" % i, [128, SB + 1], F32) for i in range(2)]
                b_raw = [Buf(), Buf()]
                rawc = [0]
                dlt, b_dlt = wt("dlt")
                wa_s, b_was = wt("wa_s")
                g0_s, b_g0s = wt("g0_s")
                g1_s, b_g1s = wt("g1_s")
                lat_bf, b_lat = wt("lat_bf", BF16)
                sg_bf, b_sg = wt("sg_bf", BF16)
                sg1_bf, b_sg1 = wt("sg1_bf", BF16)
                r_s, b_rs_ = wt("r_s")
                k_s, b_ks = wt("k_s")
                v_s, b_vs = wt("v_s")
                lw, b_lw = wt("lw")
                a_t, b_a = wt("a_t")
                g_t, b_g = wt("g_t")
                kq, b_kq = wt("kq")
                tmpA, b_tmpA = wt("tmpA")
                kk, b_kk = wt("kk")
                kmod, b_kmod = wt("kmod")
                bb, b_bb = wt("bb")
                bonus, b_bonus = wt("bonus")
                cum, b_cum = wt("cum")
                E_in, b_Ein = wt("E_in")
                E_neg, b_Eneg = wt("E_neg")
                E_ex, b_Eex = wt("E_ex")
                E_end, b_Eend = wt("E_end")
                y_sb, b_y = wt("y_sb")
                AR = sb("AR", [128, 4, 2, 128], BF16)
                b_AR = Buf()
                BT, b_BT = wt("BT", BF16)
                KTt, b_KTt = wt("KTt", BF16)
                BH, b_BH = wt("BH", BF16)
                KH, b_KH = wt("KH", BF16)
                Vb, b_Vb = wt("Vb", BF16)
                catR, b_catR = wt("catR", BF16)
                TM = [sb("TM%d" % i, [128, 4, 128], BF16) for i in range(4)]
                b_TM = [Buf() for _ in range(4)]
                NMt = [sb("NM%d" % i, [128, 4, 128], BF16) for i in range(4)]
                b_NM = [Buf() for _ in range(4)]
                Dm = [sb("Dm%d" % i, [128, 128], BF16) for i in range(4)]
                b_Dm = [Buf() for _ in range(4)]
                DTm = [sb("DTm%d" % i, [128, 128], BF16) for i in range(4)]
                b_DTm = [Buf() for _ in range(4)]
                Gm = [sb("Gm%d" % i, [128, 128], BF16) for i in range(4)]
                b_Gm = [Buf() for _ in range(4)]
                X2b = [sb("X2b%d" % i, [128, 64], BF16) for i in range(4)]
                b_X2b = [Buf() for _ in range(4)]
                U2s = [sb("U2s%d" % i, [128, 128], F32) for i in range(2)]
                b_U2s = [Buf(), Buf()]
                WTb = [sb("WTb%d" % i, [128, 128], BF16) for i in range(2)]
                b_WTb = [Buf(), Buf()]
                Ub = sb("Ub", [128, 128], BF16)
                b_Ub = Buf()
                dbg_sb = sb("dbg_sbr", [128, SB], F32)
                b_dbg = Buf()
                M1 = [PB[4], PB[5]]
                b_M1 = [b_PB[4], b_PB[5]]
                sA = [PB[i][:, 0:128] for i in range(4)]
                b_sA = [b_PB[i] for i in range(4)]
                sB = [PB[i][:, 128:256] for i in range(4)]
                b_sB = [b_PB[i] for i in range(4)]
                u2_ps, uh_ps = [PB[6][:, i * 128:(i + 1) * 128] for i in range(2)]
                y2_ps = [PB[6][:, (2 + i) * 128:(3 + i) * 128] for i in range(2)]
                b_u2 = b_wt = b_uh = b_yps = b_PB[6]

                def shifted(nm, dst, b_dst):
                    gi, c0, n = groups[nm]

                    def ev(pp, bp):
                        ri = rawc[0] % 2
                        rawc[0] += 1
                        rw_, brw = raw[ri], b_raw[ri]
                        S.op("act", lambda e: e.copy(out=rw_[0:n, 1:SB + 1], in_=pp[0:n, :]), reads=[bp], writes=[brw])
                        S.op("pool", lambda e: e.tensor_copy(out=rw_[0:n, 0:1], in_=carry[0:n, gi:gi + 1]),
                             reads=[b_carry[gi]], writes=[brw])
                        S.op("pool", lambda e: e.tensor_copy(out=carry[0:n, gi:gi + 1], in_=rw_[0:n, SB:SB + 1]),
                             reads=[brw], writes=[b_carry[gi]])
                        S.op("dve", lambda e: e.tensor_tensor(out=dlt[0:n, :], in0=rw_[0:n, 0:SB], in1=rw_[0:n, 1:SB + 1], op=ALU.subtract),
                             reads=[brw], writes=[b_dlt])
                        S.op("dve", lambda e: e.scalar_tensor_tensor(out=dst[0:n, :], in0=dlt[0:n, :], scalar=muT[0:n, gi:gi + 1],
                                                                     in1=rw_[0:n, 1:SB + 1], op0=ALU.mult, op1=ALU.add),
                             reads=[b_dlt, brw, b_mu], writes=[b_dst])
                    proj_fm(wr_bf, b_wr, st, c0, n, ev)

                def v4(ap):
                    return ap.rearrange("p (c t) -> p c t", c=4)

                AR_b = sb("AR_b", [128, 4, 2, 128], BF16)
                BT_b = sb("BT_b", [128, SB], BF16)
                KTt_b = sb("KTt_b", [128, SB], BF16)
                BH_b = sb("BH_b", [128, SB], BF16)
                KH_b = sb("KH_b", [128, SB], BF16)
                Vb_b = sb("Vb_b", [128, SB], BF16)
                E_in_b = sb("E_in_b", [128, SB], F32)
                bonus_b = sb("bonus_b", [128, SB], F32)
                g_t_b = sb("g_t_b", [128, SB], F32)
                AR_2 = [AR, AR_b]
                b_AR_2 = [b_AR, Buf()]
                BT_2 = [BT, BT_b]
                b_BT_2 = [b_BT, Buf()]
                KTt_2 = [KTt, KTt_b]
                b_KTt_2 = [b_KTt, Buf()]
                BH_2 = [BH, BH_b]
                b_BH_2 = [b_BH, Buf()]
                KH_2 = [KH, KH_b]
                b_KH_2 = [b_KH, Buf()]
                Vb_2 = [Vb, Vb_b]
                b_Vb_2 = [b_Vb, Buf()]
                E_in_2 = [E_in, E_in_b]
                b_Ein_2 = [b_Ein, Buf()]
                bonus_2 = [bonus, bonus_b]
                b_bonus_2 = [b_bonus, Buf()]
                g_t_2 = [g_t, g_t_b]
                b_g_2 = [b_g, Buf()]
                tmpB, b_tmpB = wt("tmpB")
                M1 = [PB[2], PB[3], PB[4], PB[5]]
                b_M1 = [b_PB[2], b_PB[3], b_PB[4], b_PB[5]]
                sA = [PB[2 + i][:, 0:128] for i in range(4)]
                b_sA = [b_PB[2 + i] for i in range(4)]
                sB = [PB[2 + i][:, 128:256] for i in range(4)]
                b_sB = [b_PB[2 + i] for i in range(4)]

                def gen_front(s, hp, pb):
                    c_lo, c_hi = s * SB, (s + 1) * SB
                    AR, b_AR = AR_2[pb], b_AR_2[pb]
                    BT, b_BT = BT_2[pb], b_BT_2[pb]
                    KTt, b_KTt = KTt_2[pb], b_KTt_2[pb]
                    BH, b_BH = BH_2[pb], b_BH_2[pb]
                    KH, b_KH = KH_2[pb], b_KH_2[pb]
                    Vb, b_Vb = Vb_2[pb], b_Vb_2[pb]
                    E_in, b_Ein = E_in_2[pb], b_Ein_2[pb]
                    bonus, b_bonus = bonus_2[pb], b_bonus_2[pb]
                    g_t, b_g = g_t_2[pb], b_g_2[pb]
                    if hp == 0:
                        front(st, s)
                        shifted("wa", wa_s, b_was)
                        shifted("g0", g0_s, b_g0s)
                        shifted("g1", g1_s, b_g1s)
                        S.op("act", lambda e: e.activation(out=lat_bf[0:64, :], in_=wa_s[0:64, :], func=AF.Tanh), reads=[b_was], writes=[b_lat])
                        S.op("act", lambda e: e.copy(out=lat_bf[64:128, :], in_=wa_s[64:128, :]), reads=[b_was], writes=[b_lat])
                        S.op("act", lambda e: e.activation(out=sg_bf[:], in_=g0_s[:], func=AF.Sigmoid), reads=[b_g0s], writes=[b_sg])
                        S.op("act", lambda e: e.activation(out=sg1_bf[0:32, :], in_=g1_s[0:32, :], func=AF.Sigmoid), reads=[b_g1s], writes=[b_sg1])
                        yield
                    hc = slice(hp * 128, (hp + 1) * 128)
                    shifted(("r", hp), r_s, b_rs_)
                    shifted(("k", hp), k_s, b_ks)
                    yield
                    shifted(("v", hp), v_s, b_vs)
                    p0, bp0 = PB[0], b_PB[0]
                    S.op("pe", lambda e: e.matmul(out=p0[:], lhsT=lo_bf[0:64, hc], rhs=lat_bf[0:64, :], start=True, stop=True),
                         reads=[b_lo, b_lat], writes=[bp0])
                    yield
                    S.op("act", lambda e: e.activation(out=lw[:], in_=p0[:], func=AF.Sigmoid, bias=w0T[:, hp:hp + 1]),
                         reads=[bp0, b_w0], writes=[b_lw])
                    S.op("pool", lambda e: e.tensor_scalar(out=lw[:], in0=lw[:], scalar1=-0.6065306597126334, scalar2=None, op0=ALU.mult),
                         reads=[b_lw], writes=[b_lw])
                    p1, bp1 = PB[1], b_PB[1]
                    yield
                    S.op("pe", lambda e: e.matmul(out=p1[:], lhsT=lo_bf[64:128, hc], rhs=lat_bf[64:128, :], start=True, stop=True),
                         reads=[b_lo, b_lat], writes=[bp1])
                    S.op("act", lambda e: e.activation(out=a_t[:], in_=p1[:], func=AF.Sigmoid, bias=a0T[:, hp:hp + 1]),
                         reads=[bp1, b_a0], writes=[b_a])
                    S.op("pe", lambda e: e.matmul(out=p0[:], lhsT=gup_bf[:, hc], rhs=sg_bf[:], start=True, stop=False),
                         reads=[b_gup, b_sg], writes=[bp0])
                    yield
                    S.op("pe", lambda e: e.matmul(out=p0[:], lhsT=gup1_bf[0:32, hc], rhs=sg1_bf[0:32, :], start=False, stop=True),
                         reads=[b_gup, b_sg1], writes=[bp0])
                    S.op("act", lambda e: e.copy(out=g_t[:], in_=p0[:]), reads=[bp0], writes=[b_g])
                    S.op("dve", lambda e: e.tensor_scalar(out=kq[:], in0=k_s[:], scalar1=kkT[:, hp:hp + 1], scalar2=None, op0=ALU.mult),
                         reads=[b_ks, b_kkv], writes=[b_kq])
                    yield
                    S.op("pool", lambda e: e.tensor_tensor(out=tmpA[:], in0=kq[:], in1=kq[:], op=ALU.mult), reads=[b_kq], writes=[b_tmpA])
                    S.op("pe", lambda e: e.matmul(out=p1[:], lhsT=cm[:, 12, :], rhs=tmpA[:], start=True, stop=True),
                         reads=[b_cm, b_tmpA], writes=[bp1])
                    S.op("act", lambda e: e.sqrt(out=tmpA[:], in_=p1[:]), reads=[bp1], writes=[b_tmpA])
                    yield
                    S.op("dve", lambda e: e.tensor_scalar(out=tmpA[:], in0=tmpA[:], scalar1=1e-12, scalar2=None, op0=ALU.max),
                         reads=[b_tmpA], writes=[b_tmpA])
                    S.op("dve", lambda e: e.reciprocal(out=tmpA[:], in_=tmpA[:]), reads=[b_tmpA], writes=[b_tmpA])
                    S.op("pool", lambda e: e.tensor_tensor(out=kk[:], in0=kq[:], in1=tmpA[:], op=ALU.mult),
                         reads=[b_kq, b_tmpA], writes=[b_kk])
                    yield
                    S.op("dve", lambda e: e.tensor_scalar(out=kmod[:], in0=a_t[:], scalar1=-1.0, scalar2=kaT[:, hp:hp + 1],
                                                          op0=ALU.add, op1=ALU.mult), reads=[b_a, b_kav], writes=[b_kmod])
                    S.op("dve", lambda e: e.scalar_tensor_tensor(out=kmod[:], in0=kmod[:], scalar=1.0, in1=k_s[:],
                                                                 op0=ALU.add, op1=ALU.mult), reads=[b_kmod, b_ks], writes=[b_kmod])
                    S.op("pool", lambda e: e.tensor_tensor(out=bb[:], in0=kk[:], in1=a_t[:], op=ALU.mult), reads=[b_kk, b_a], writes=[b_bb])
                    yield
                    S.op("dve", lambda e: e.scalar_tensor_tensor(out=tmpA[:], in0=r_s[:], scalar=rkT[:, hp:hp + 1], in1=kmod[:],
                                                                 op0=ALU.mult, op1=ALU.mult), reads=[b_rs_, b_rkv, b_kmod], writes=[b_tmpA])
                    S.op("pe", lambda e: e.matmul(out=p1[:], lhsT=cm[:, 12, :], rhs=tmpA[:], start=True, stop=True),
                         reads=[b_cm, b_tmpA], writes=[bp1])
                    S.op("dve", lambda e: e.tensor_tensor(out=bonus[:], in0=p1[:], in1=v_s[:], op=ALU.mult), reads=[bp1, b_vs], writes=[b_bonus])
                    yield
                    for c in range(4):
                        cc = slice(c * 128, (c + 1) * 128)
                        S.op("dve", lambda e, cc=cc: e.tensor_tensor_scan(out=cum[:, cc], data0=ones128[:], data1=lw[:, cc], initial=0.0,
                                                                          op0=ALU.mult, op1=ALU.add), reads=[b_lw, b_cm], writes=[b_cum])
                    S.op("act", lambda e: e.activation(out=E_in[:], in_=cum[:], func=AF.Exp), reads=[b_cum], writes=[b_Ein])
                    S.op("act", lambda e: e.activation(out=E_neg[:], in_=cum[:], func=AF.Exp, scale=-1.0), reads=[b_cum], writes=[b_Eneg])
                    yield
                    for c in range(4):
                        cc = slice(c * 128, (c + 1) * 128)
                        S.op("act", lambda e, cc=cc, c=c: e.activation(out=E_end[:, cc], in_=cum[:, cc], func=AF.Exp, scale=-1.0,
                                                                       bias=cum[:, c * 128 + 127:c * 128 + 128]),
                             reads=[b_cum], writes=[b_Eend])
                    S.op("pool", lambda e: e.tensor_tensor(out=tmpA[:], in0=cum[:], in1=lw[:], op=ALU.subtract), reads=[b_cum, b_lw], writes=[b_tmpA])
                    S.op("act", lambda e: e.activation(out=E_ex[:], in_=tmpA[:], func=AF.Exp), reads=[b_tmpA], writes=[b_Eex])
                    yield
                    S.op("dve", lambda e: e.scalar_tensor_tensor(out=AR[:, :, 0, :], in0=v4(kk[:]), scalar=-1.0, in1=v4(E_ex[:]),
                                                                 op0=ALU.mult, op1=ALU.mult), reads=[b_kk, b_Eex], writes=[b_AR])
                    S.op("pool", lambda e: e.tensor_tensor(out=AR[:, :, 1, :], in0=v4(r_s[:]), in1=v4(E_in[:]), op=ALU.mult),
                         reads=[b_rs_, b_Ein], writes=[b_AR])
                    S.op("dve", lambda e: e.tensor_tensor(out=BT[:], in0=bb[:], in1=E_neg[:], op=ALU.mult), reads=[b_bb, b_Eneg], writes=[b_BT])
                    yield
                    S.op("pool", lambda e: e.tensor_tensor(out=KTt[:], in0=kmod[:], in1=E_neg[:], op=ALU.mult), reads=[b_kmod, b_Eneg], writes=[b_KTt])
                    S.op("dve", lambda e: e.tensor_tensor(out=BH[:], in0=bb[:], in1=E_end[:], op=ALU.mult), reads=[b_bb, b_Eend], writes=[b_BH])
                    S.op("pool", lambda e: e.tensor_tensor(out=KH[:], in0=kmod[:], in1=E_end[:], op=ALU.mult), reads=[b_kmod, b_Eend], writes=[b_KH])
                    yield
                    S.op("act", lambda e: e.copy(out=Vb[:], in_=v_s[:]), reads=[b_vs], writes=[b_Vb])

                    yield

                def gen_chunks(s, hp, pb):
                    c_lo, c_hi = s * SB, (s + 1) * SB
                    AR, b_AR = AR_2[pb], b_AR_2[pb]
                    BT, b_BT = BT_2[pb], b_BT_2[pb]
                    KTt, b_KTt = KTt_2[pb], b_KTt_2[pb]
                    BH, b_BH = BH_2[pb], b_BH_2[pb]
                    KH, b_KH = KH_2[pb], b_KH_2[pb]
                    Vb, b_Vb = Vb_2[pb], b_Vb_2[pb]
                    E_in, b_Ein = E_in_2[pb], b_Ein_2[pb]
                    bonus, b_bonus = bonus_2[pb], b_bonus_2[pb]
                    g_t, b_g = g_t_2[pb], b_g_2[pb]
                    hc = slice(hp * 128, (hp + 1) * 128)
                    p0, bp0 = PB[0], b_PB[0]
                    p1, bp1 = PB[1], b_PB[1]
                    stop = (dbg or {}).get("stop", "")
                    if stop == "A":
                        return
                    for cp in range(2):
                        chains = [(2 * cp + ci_, e_) for ci_ in range(2) for e_ in range(2)]
                        for ci_ in range(2):
                            c = 2 * cp + ci_
                            cc = slice(c * 128, (c + 1) * 128)
                            srcs = [(AR[:, c, 0, :], b_AR), (Vb[:, cc], b_Vb), (BH[:, cc], b_BH), (KH[:, cc], b_KH)]
                            for k_, (src, bsrc) in enumerate(srcs):
                                S.op("pe", lambda e, k_=k_, src=src: e.transpose(out=tp_ps[:, k_, :], in_=src, identity=ident_bf[:]),
                                     reads=[bsrc, b_ident], writes=[b_tp])
                            S.op("act", lambda e, c=c: e.copy(out=TM[c][:], in_=tp_ps[:, 0:4, :]), reads=[b_tp], writes=[b_TM[c]])
                        yield
                        for ch, (c, e_) in enumerate(chains):
                            pr = slice(e_ * 64, (e_ + 1) * 64)
                            cc = slice(c * 128, (c + 1) * 128)
                            m1, bm1 = M1[ch], b_M1[ch]
                            S.op("pe", lambda e, pr=pr, cc=cc, c=c, m1=m1: e.matmul(out=m1[:, 0:256], lhsT=BT[pr, cc],
                                                                                 rhs=AR[pr, c, :, :], start=True, stop=True),
                                 reads=[b_BT, b_AR], writes=[bm1])
                            S.op("pe", lambda e, pr=pr, cc=cc, c=c, m1=m1: e.matmul(out=m1[:, 256:512], lhsT=KTt[pr, cc],
                                                                                 rhs=AR[pr, c, :, :], start=True, stop=True),
                                 reads=[b_KTt, b_AR], writes=[bm1])
                            S.op("dve", lambda e, ch=ch, m1=m1: e.tensor_tensor(
                                out=NMt[ch][:], in0=m1[:].rearrange("p (a t) -> p a t", a=4),
                                in1=cm[:, 0:4, :],
                                op=ALU.mult), reads=[bm1, b_cm], writes=[b_NM[ch]])
                            S.op("pe", lambda e, pr=pr, cc=cc, c=c, ch=ch: e.matmul(out=sA[ch], lhsT=AR[pr, c, 0, :], rhs=BT[pr, cc],
                                                                                 start=True, stop=True),
                                 reads=[b_AR, b_BT], writes=[b_sA[ch]])
                            S.op("dve", lambda e, ch=ch: e.tensor_tensor(out=Dm[ch][:], in0=sA[ch], in1=cm[:, 4, :], op=ALU.mult),
                                 reads=[b_sA[ch], b_cm], writes=[b_Dm[ch]])
                            S.op("pool", lambda e, ch=ch: e.tensor_tensor(out=Dm[ch][:], in0=Dm[ch][:], in1=ident_bf[:], op=ALU.add),
                                 reads=[b_Dm[ch], b_ident], writes=[b_Dm[ch]])
                            S.op("pool", lambda e, ch=ch: e.tensor_tensor(out=DTm[ch][:], in0=NMt[ch][:, 0, :], in1=mk0T_bf[:], op=ALU.mult),
                                 reads=[b_NM[ch], b_cm], writes=[b_DTm[ch]])
                            S.op("pool", lambda e, ch=ch: e.tensor_tensor(out=DTm[ch][:], in0=DTm[ch][:], in1=ident_bf[:], op=ALU.add),
                                 reads=[b_DTm[ch], b_ident], writes=[b_DTm[ch]])
                        if stop == "B":
                            continue
                        yield
                        for li, mm in enumerate(LEVELS):
                            last = (li == len(LEVELS) - 1)
                            yield
                            for ch in range(4):
                                S.op("pe", lambda e, ch=ch: e.matmul(out=sA[ch], lhsT=NMt[ch][:, 0, :], rhs=Dm[ch][:], start=True, stop=True),
                                     reads=[b_NM[ch], b_Dm[ch]], writes=[b_sA[ch]])
                                S.op("dve", lambda e, ch=ch, li=li: e.tensor_tensor(out=Gm[ch][:], in0=sA[ch], in1=cm[:, 6 + li, :], op=ALU.mult),
                                     reads=[b_sA[ch], b_cm], writes=[b_Gm[ch]])
                                S.op("pool", lambda e, ch=ch: e.tensor_tensor(out=Gm[ch][:], in0=Gm[ch][:], in1=ident_bf[:], op=ALU.add),
                                     reads=[b_Gm[ch], b_ident], writes=[b_Gm[ch]])
                            yield
                            for ch in range(4):
                                if not last:
                                    S.op("pe", lambda e, ch=ch: e.matmul(out=sA[ch], lhsT=DTm[ch][:], rhs=Gm[ch][:], start=True, stop=True),
                                         reads=[b_DTm[ch], b_Gm[ch]], writes=[b_sA[ch]])
                                S.op("pe", lambda e, ch=ch: e.matmul(out=sB[ch], lhsT=Gm[ch][:], rhs=DTm[ch][:], start=True, stop=True),
                                     reads=[b_DTm[ch], b_Gm[ch]], writes=[b_sB[ch]])
                                if not last:
                                    S.op("act", lambda e, ch=ch: e.copy(out=Dm[ch][:], in_=sA[ch]), reads=[b_sA[ch]], writes=[b_Dm[ch]])
                                S.op("act", lambda e, ch=ch: e.copy(out=DTm[ch][:], in_=sB[ch]), reads=[b_sB[ch]], writes=[b_DTm[ch]])
                        if stop == "C":
                            continue
                        yield
                        for ch, (c, e_) in enumerate(chains):
                            pr = slice(e_ * 64, (e_ + 1) * 64)
                            S.op("pe", lambda e, ch=ch, c=c, pr=pr: e.matmul(out=sA[ch][:, 0:64], lhsT=NMt[ch][:, 2, :], rhs=TM[c][:, 1, pr],
                                                                          start=True, stop=True),
                                 reads=[b_NM[ch], b_TM[c]], writes=[b_sA[ch]])
                            S.op("act", lambda e, ch=ch: e.copy(out=X2b[ch][:], in_=sA[ch][:, 0:64]), reads=[b_sA[ch]], writes=[b_X2b[ch]])
                        for ci_ in range(2):
                            c = 2 * cp + ci_
                            for e_ in range(2):
                                ch = ci_ * 2 + e_
                                pr = slice(e_ * 64, (e_ + 1) * 64)
                                S.op("pe", lambda e, ch=ch, pr=pr: e.matmul(out=u2_ps[:, pr], lhsT=DTm[ch][:], rhs=X2b[ch][:], start=True, stop=True),
                                     reads=[b_DTm[ch], b_X2b[ch]], writes=[b_u2])
                                S.op("pe", lambda e, ch=ch, c=c: e.matmul(out=sA[ch], lhsT=TM[c][:, 0, :], rhs=DTm[ch][:], start=True, stop=True),
                                     reads=[b_DTm[ch], b_TM[c]], writes=[b_sA[ch]])
                                S.op("dve", lambda e, ci_=ci_, ch=ch, pr=pr: e.tensor_copy(out=WTb[ci_][pr, :], in_=sA[ch][pr, :]),
                                     reads=[b_sA[ch]], writes=[b_WTb[ci_]])
                            S.op("act", lambda e, ci_=ci_: e.copy(out=U2s[ci_][:], in_=u2_ps), reads=[b_u2], writes=[b_U2s[ci_]])
                        if stop == "D":
                            continue
                        yield
                        for ci_ in range(2):
                            c = 2 * cp + ci_
                            cc = slice(c * 128, (c + 1) * 128)
                            yield
                            S.op("pe", lambda e, ci_=ci_: e.matmul(out=uh_ps, lhsT=WTb[ci_][:], rhs=Hbf[hp][:], start=True, stop=True),
                                 reads=[b_WTb[ci_], b_Hbf[hp]], writes=[b_uh])
                            S.op("dve", lambda e, ci_=ci_: e.tensor_tensor(out=Ub[:], in0=uh_ps, in1=U2s[ci_][:], op=ALU.add),
                                 reads=[b_uh, b_U2s[ci_]], writes=[b_Ub])
                            for e_ in range(2):
                                ch = ci_ * 2 + e_
                                pr = slice(e_ * 64, (e_ + 1) * 64)
                                yp = y2_ps[e_]
                                S.op("pe", lambda e, c=c, yp=yp: e.matmul(out=yp, lhsT=Hbf[hp][:], rhs=AR[:, c, 1, :], start=True, stop=False),
                                     reads=[b_Hbf[hp], b_AR], writes=[b_yps])
                                S.op("pe", lambda e, ch=ch, yp=yp: e.matmul(out=yp, lhsT=Ub[:], rhs=NMt[ch][:, 1, :], start=False, stop=False),
                                     reads=[b_Ub, b_NM[ch]], writes=[b_yps])
                                S.op("pe", lambda e, ch=ch, c=c, yp=yp: e.matmul(out=yp, lhsT=TM[c][:, 1, :], rhs=NMt[ch][:, 3, :], start=False, stop=True),
                                     reads=[b_TM[c], b_NM[ch]], writes=[b_yps])
                            for e_ in range(2):
                                pr = slice(e_ * 64, (e_ + 1) * 64)
                                S.op("act", lambda e, cc=cc, pr=pr, e_=e_: e.copy(out=y_sb[pr, cc], in_=y2_ps[e_][pr, :]), reads=[b_yps], writes=[b_y])
                            S.op("pe", lambda e, c=c: e.matmul(out=uh_ps, lhsT=TM[c][:, 2, :], rhs=Ub[:], start=True, stop=False),
                                 reads=[b_TM[c], b_Ub], writes=[b_uh])
                            S.op("pe", lambda e, c=c: e.matmul(out=uh_ps, lhsT=TM[c][:, 3, :], rhs=TM[c][:, 1, :], start=False, stop=True),
                                 reads=[b_TM[c]], writes=[b_uh])
                            for e_ in range(2):
                                pr = slice(e_ * 64, (e_ + 1) * 64)
                                S.op("dve", lambda e, pr=pr, c=c: e.scalar_tensor_tensor(
                                    out=H32[hp][pr, pr], in0=H32[hp][pr, pr], scalar=E_in[pr, c * 128 + 127:c * 128 + 128],
                                    in1=uh_ps[pr, pr], op0=ALU.mult, op1=ALU.add),
                                    reads=[b_H32[hp], b_Ein, b_uh], writes=[b_H32[hp]])
                            S.op("pool", lambda e: e.tensor_copy(out=Hbf[hp][:], in_=H32[hp][:]), reads=[b_H32[hp]], writes=[b_Hbf[hp]])
                    if dbg and "yraw" in dbg:
                        S.op("act", lambda e: e.copy(out=dbg_sb[:], in_=y_sb[:]), reads=[b_y], writes=[b_dbg])
                        S.dma("sp", dbg_aps["yraw"][hp * 128:(hp + 1) * 128, c_lo:c_hi], dbg_sb[:], reads=[b_dbg])
                    yield
                    S.op("pe", lambda e: e.matmul(out=p0[:], lhsT=bd_mean[:], rhs=y_sb[:], start=True, stop=True),
                         reads=[b_cm, b_y], writes=[bp0])
                    S.op("dve", lambda e: e.tensor_tensor(out=y_sb[:], in0=y_sb[:], in1=p0[:], op=ALU.subtract), reads=[b_y, bp0], writes=[b_y])
                    S.op("pool", lambda e: e.tensor_tensor(out=tmpB[:], in0=y_sb[:], in1=y_sb[:], op=ALU.mult), reads=[b_y], writes=[b_tmpB])
                    S.op("pe", lambda e: e.matmul(out=p1[:], lhsT=bd_mean[:], rhs=tmpB[:], start=True, stop=True),
                         reads=[b_cm, b_tmpB], writes=[bp1])
                    S.op("dve", lambda e: e.tensor_scalar(out=tmpB[:], in0=p1[:], scalar1=LNX_EPS, scalar2=None, op0=ALU.add),
                         reads=[bp1], writes=[b_tmpB])
                    S.op("act", lambda e: e.sqrt(out=tmpB[:], in_=tmpB[:]), reads=[b_tmpB], writes=[b_tmpB])
                    S.op("dve", lambda e: e.reciprocal(out=tmpB[:], in_=tmpB[:]), reads=[b_tmpB], writes=[b_tmpB])
                    S.op("pool", lambda e: e.tensor_tensor(out=y_sb[:], in0=y_sb[:], in1=tmpB[:], op=ALU.mult), reads=[b_y, b_tmpB], writes=[b_y])
                    S.op("dve", lambda e: e.tensor_scalar(out=y_sb[:], in0=y_sb[:], scalar1=lnwT[:, hp:hp + 1], scalar2=lnbT[:, hp:hp + 1],
                                                          op0=ALU.mult, op1=ALU.add), reads=[b_y, b_lnw, b_lnb], writes=[b_y])
                    S.op("pool", lambda e: e.tensor_tensor(out=y_sb[:], in0=y_sb[:], in1=bonus[:], op=ALU.add), reads=[b_y, b_bonus], writes=[b_y])
                    S.op("dve", lambda e: e.tensor_tensor(out=catR[:], in0=y_sb[:], in1=g_t[:], op=ALU.mult), reads=[b_y, b_g], writes=[b_catR])
                    S.dma("sp", cat_scr[512 + hp * 128:512 + (hp + 1) * 128, c_lo:c_hi], catR[:], reads=[b_catR])
                    if dbg and "orwkv" in dbg:
                        S.op("act", lambda e: e.copy(out=dbg_sb[:], in_=catR[:]), reads=[b_catR], writes=[b_dbg])
                        S.dma("sp", dbg_aps["orwkv"][hp * 128:(hp + 1) * 128, c_lo:c_hi], dbg_sb[:], reads=[b_dbg])
                    yield

                units = [(s_, hp_) for s_ in range(NSB) for hp_ in range(4)]
                for _ in gen_front(units[0][0], units[0][1], 0):
                    pass
                for ui, (s_, hp_) in enumerate(units):
                    gc = gen_chunks(s_, hp_, ui % 2)
                    gf = gen_front(units[ui + 1][0], units[ui + 1][1], (ui + 1) % 2) if ui + 1 < len(units) else iter(())
                    live = [gc, gf]
                    while live:
                        for g in list(live):
                            try:
                                next(g)
                            except StopIteration:
                                live.remove(g)
                S.barrier()
        if "O" in phases:
            with ExitStack() as es:
                def sb(name, shape, dt=F32):
                    return es.enter_context(nc.sbuf_tensor(uniq(name), list(shape), dt))
                UT = 256
                NU = T // UT
                stg = [sb("stg%d" % i, [128, D], F32) for i in range(2)]
                b_stg = [Buf(), Buf()]
                st = {"xt": stg, "b_xt": b_stg}
                gfT, b_gfT = load_colvec(es, "gfT", gf_pre, KC)
                wout_bf = sb("wout_bf", [128, KC, D], BF16)
                b_wout = [Buf() for _ in range(KC)]
                wg_bf = sb("wg_bf", [128, KC, DFF], BF16)
                b_wg = [Buf() for _ in range(KC)]
                wu_bf = sb("wu_bf", [128, KC, DFF], BF16)
                b_wu = [Buf() for _ in range(KC)]
                wd_bf = sb("wd_bf", [128, NFF, D], BF16)
                b_wd = [Buf() for _ in range(NFF)]
                load_weight_bf(st, wout_bf, b_wout, w_out, 0, D, None, None)
                load_weight_bf(st, wg_bf, b_wg, w_gate, 0, DFF, gfT, b_gfT)
                load_weight_bf(st, wu_bf, b_wu, w_up, 0, DFF, gfT, b_gfT)
                load_weight_bf(st, wd_bf, b_wd, w_down, 0, D, None, None, nk=NFF)
                gpost_bc = sb("gpost_bc", [128, D], F32)
                gfpost_bc = sb("gfpost_bc", [128, D], F32)
                b_gbc = Buf()
                S.dma("sp", gpost_bc[:], g_post.partition_broadcast(128), writes=[b_gbc])
                S.dma("sp", gfpost_bc[:], gf_post.partition_broadcast(128), writes=[b_gbc])
                catT = sb("catT", [128, KC, UT], BF16)
                b_catT = Buf()
                zn = sb("zn", [128, D], BF16)
                b_zn = Buf()
                zT = sb("zT", [128, KC, UT], BF16)
                b_zT = Buf()
                aT = sb("aT", [128, NFF, UT], BF16)
                b_aT = Buf()
                junk = sb("junkO", [128, D], BF16)
                b_junk = Buf()
                sgt = [sb("sgt%d" % i, [128, UT], F32) for i in range(2)]
                b_sgt = [Buf(), Buf()]
                t1 = sb("t1", [128, D], F32)
                b_t1 = Buf()
                ssO = sb("ssO", [128, 4], F32)
                b_ssO = Buf()
                cat_v = cat_scr.rearrange("(k p) t -> p k t", p=128)

                def rstd_from(srcs, bsrcs):
                    for i, (ap_, b_) in enumerate(zip(srcs, bsrcs)):
                        n = ap_.shape[1]
                        S.op("act", lambda e, ap_=ap_, i=i, n=n: e.activation(out=junk[:, 0:n], in_=ap_, func=AF.Square, accum_out=ssO[:, i:i + 1]),
                             reads=[b_], writes=[b_junk, b_ssO])
                    if len(srcs) == 2:
                        S.op("dve", lambda e: e.tensor_tensor(out=ssO[:, 2:3], in0=ssO[:, 0:1], in1=ssO[:, 1:2], op=ALU.add),
                             reads=[b_ssO], writes=[b_ssO])
                        src = ssO[:, 2:3]
                    else:
                        src = ssO[:, 0:1]
                    S.op("dve", lambda e: e.tensor_scalar(out=ssO[:, 2:3], in0=src, scalar1=1.0 / D, scalar2=EPS, op0=ALU.mult, op1=ALU.add),
                         reads=[b_ssO], writes=[b_ssO])
                    S.op("act", lambda e: e.sqrt(out=ssO[:, 2:3], in_=ssO[:, 2:3]), reads=[b_ssO], writes=[b_ssO])
                    S.op("dve", lambda e: e.reciprocal(out=ssO[:, 2:3], in_=ssO[:, 2:3]), reads=[b_ssO], writes=[b_ssO])

                for u in range(NU):
                    t0 = u * UT
                    S.dma("sp", catT[:], cat_v[:, :, t0:t0 + UT], writes=[b_catT])
                    for j in range(2):
                        tj = t0 + j * 128
                        S.dma("sp", stg[j][:], x[tj:tj + 128, :], writes=[b_stg[j]])
                        for half in range(2):
                            for kc in range(KC):
                                S.op("pe", lambda e, kc=kc, half=half, j=j: e.matmul(
                                    out=PB[half][:], lhsT=catT[:, kc, j * 128:(j + 1) * 128], rhs=wout_bf[:, kc, half * 512:(half + 1) * 512],
                                    start=(kc == 0), stop=(kc == KC - 1)), reads=[b_catT, b_wout[kc]], writes=[b_PB[half]])
                        rstd_from([PB[0][:], PB[1][:]], [b_PB[0], b_PB[1]])
                        for half in range(2):
                            S.op("act", lambda e, half=half: e.activation(out=t1[:, half * 512:(half + 1) * 512], in_=PB[half][:], func=AF.Copy,
                                                                          scale=ssO[:, 2:3]), reads=[b_PB[half], b_ssO], writes=[b_t1])
                        S.op("pool", lambda e: e.tensor_tensor(out=t1[:], in0=t1[:], in1=gpost_bc[:], op=ALU.mult), reads=[b_t1, b_gbc], writes=[b_t1])
                        S.op("dve", lambda e, j=j: e.tensor_tensor(out=stg[j][:], in0=stg[j][:], in1=t1[:], op=ALU.add),
                             reads=[b_stg[j], b_t1], writes=[b_stg[j]])
                        if dbg and "h" in dbg:
                            S.dma("sp", dbg_aps["h"][tj:tj + 128, :], stg[j][:], reads=[b_stg[j]])
                        rstd_from([stg[j][:]], [b_stg[j]])
                        S.op("act", lambda e, j=j: e.activation(out=zn[:], in_=stg[j][:], func=AF.Copy, scale=ssO[:, 2:3]),
                             reads=[b_stg[j], b_ssO], writes=[b_zn])
                        for kc in range(KC):
                            S.op("pe", lambda e, kc=kc: e.transpose(out=tp_ps[:, kc, :], in_=zn[:, kc * 128:(kc + 1) * 128], identity=ident_bf[:]),
                                 reads=[b_zn, b_ident], writes=[b_tp])
                        S.op("dve", lambda e, j=j: e.tensor_copy(out=zT[:, :, j * 128:(j + 1) * 128], in_=tp_ps[:, :, :]), reads=[b_tp], writes=[b_zT])
                    for ffc in range(NFF):
                        gi_ = 2 + 2 * (ffc % 2)
                        ui_ = 3 + 2 * (ffc % 2)
                        fc = slice(ffc * 128, (ffc + 1) * 128)
                        for kc in range(KC):
                            S.op("pe", lambda e, kc=kc, fc=fc, gi_=gi_: e.matmul(out=PB[gi_][:, 0:UT], lhsT=wg_bf[:, kc, fc], rhs=zT[:, kc, :],
                                                                                 start=(kc == 0), stop=(kc == KC - 1)),
                                 reads=[b_wg[kc], b_zT], writes=[b_PB[gi_]])
                        for kc in range(KC):
                            S.op("pe", lambda e, kc=kc, fc=fc, ui_=ui_: e.matmul(out=PB[ui_][:, 0:UT], lhsT=wu_bf[:, kc, fc], rhs=zT[:, kc, :],
                                                                                 start=(kc == 0), stop=(kc == KC - 1)),
                                 reads=[b_wu[kc], b_zT], writes=[b_PB[ui_]])
                        si_ = ffc % 2
                        S.op("act", lambda e, gi_=gi_, si_=si_: e.activation(out=sgt[si_][:], in_=PB[gi_][:, 0:UT], func=AF.Silu),
                             reads=[b_PB[gi_]], writes=[b_sgt[si_]])
                        S.op("dve", lambda e, ui_=ui_, si_=si_, ffc=ffc: e.tensor_tensor(out=aT[:, ffc, :], in0=PB[ui_][:, 0:UT], in1=sgt[si_][:], op=ALU.mult),
                             reads=[b_PB[ui_], b_sgt[si_]], writes=[b_aT])
                    for j in range(2):
                        tj = t0 + j * 128
                        for half in range(2):
                            for ffc in range(NFF):
                                S.op("pe", lambda e, ffc=ffc, half=half, j=j: e.matmul(
                                    out=PB[half][:], lhsT=aT[:, ffc, j * 128:(j + 1) * 128], rhs=wd_bf[:, ffc, half * 512:(half + 1) * 512],
                                    start=(ffc == 0), stop=(ffc == NFF - 1)), reads=[b_aT, b_wd[ffc]], writes=[b_PB[half]])
                        rstd_from([PB[0][:], PB[1][:]], [b_PB[0], b_PB[1]])
                        for half in range(2):
                            S.op("act", lambda e, half=half: e.activation(out=t1[:, half * 512:(half + 1) * 512], in_=PB[half][:], func=AF.Copy,
                                                                          scale=ssO[:, 2:3]), reads=[b_PB[half], b_ssO], writes=[b_t1])
                        S.op("pool", lambda e: e.tensor_tensor(out=t1[:], in0=t1[:], in1=gfpost_bc[:], op=ALU.mult), reads=[b_t1, b_gbc], writes=[b_t1])
                        S.op("dve", lambda e, j=j: e.tensor_tensor(out=t1[:], in0=t1[:], in1=stg[j][:], op=ALU.add),
                             reads=[b_stg[j], b_t1], writes=[b_t1])
                        S.dma("sp", out[tj:tj + 128, :], t1[:], reads=[b_t1])

        S.wait_tokens("sp", [t for e in S.ENGS for t in S.dtoks[e]])
        S.emit()
    return nc


WNAMES = ["attn_norm_pre", "attn_norm_post", "w_in", "fox_forget_bias", "shift_mu", "rwkv_w0", "rwkv_w_up", "rwkv_a0",
          "rwkv_a_up", "rwkv_g_up", "rwkv_k_k", "rwkv_k_a", "rwkv_r_k", "rwkv_ln_w", "rwkv_ln_b", "w_out",
          "ffn_norm_pre", "ffn_norm_post", "ffn_w_gate", "ffn_w_up", "ffn_w_down"]


def make_in_map(inputs, b, T):
    m = {"x": np.ascontiguousarray(np.asarray(inputs["x"], dtype=np.float32)[b, :T])}
    for k in WNAMES:
        a = np.asarray(inputs[k], dtype=np.float32)[0]
        if k == "rwkv_r_k":
            a = a.reshape(-1)
        m[k] = np.ascontiguousarray(a)
    m["cmask"] = make_cmask()
    return m


def kernel(**inputs):
    x = np.asarray(inputs["x"])
    B, T, _ = x.shape
    nc = build_nc(T)
    in_maps = [make_in_map(inputs, b, T) for b in range(B)]
    res = run_bass_kernel_spmd(nc, in_maps, core_ids=list(range(B)))
    return np.stack([np.asarray(r["out"], dtype=np.float32) for r in res.results], axis=0)
```

```python
import numpy as np
from contextlib import ExitStack
import concourse.bass as bass
import concourse.mybir as mybir
from concourse.bass_utils import run_bass_kernel_spmd

F32 = mybir.dt.float32
BF16 = mybir.dt.bfloat16
AF = mybir.ActivationFunctionType
ALU = mybir.AluOpType
AX = mybir.AxisListType

D = 1024
KC = 8
HD = 64
NH = 8
FOXW = 512
RW = 512
DFF = 2816
NFF = 22
WIN = 3368
RBASE = 1544
EPS = 1e-6
LNX_EPS = 64e-5
SB = 512
EPOCH = 12000


class Buf:
    __slots__ = ("w", "r")

    def __init__(self):
        self.w = None
        self.r = []


class _Rec:
    def __getattr__(self, name):
        def f(*a, **k):
            self.call = (name, a, k)
        return f


class Sched:
    ENGS = ("pe", "act", "dve", "pool", "sp")

    def __init__(self, nc, es):
        self.nc = nc
        self.es = es
        self.q = {e: [] for e in self.ENGS}
        self.cnt = {e: 0 for e in self.ENGS}
        self.run = {e: {} for e in self.ENGS}
        self.clk = {}
        self.sems = {}
        self.ndma = {"sp": 16, "act": 6, "pool": 6}
        self.dcnt = {e: 0 for e in self.ENGS}
        self.dtoks = {e: [] for e in self.ENGS}
        self.nwaits = 0

    def sem(self, key):
        s = self.sems.get(key)
        if s is None:
            s = self.es.enter_context(self.nc.semaphore("s_%s_%s" % key))
            self.sems[key] = s
        return s

    def _deps(self, eng, reads, writes, is_dma):
        deps = set()
        for b in reads:
            if b.w is not None:
                deps.add(b.w)
        for b in writes:
            if b.w is not None:
                deps.add(b.w)
            for t in b.r:
                deps.add(t)
        return deps

    def _waits(self, eng, deps):
        run = self.run[eng]
        waits = []
        for t in sorted(deps, key=lambda t: (str(t[0]), t[1])):
            key, val, isd = t
            if eng == "pe" and key[0] == "pe" and not isd:
                continue
            if run.get(key, 0) >= val:
                continue
            waits.append((key, val))
            for k2, v2 in self.clk[(key, val)].items():
                if run.get(k2, 0) < v2:
                    run[k2] = v2
        self.nwaits += len(waits)
        return waits

    def op(self, eng, fn, reads=(), writes=()):
        rec = _Rec()
        fn(rec)
        call = rec.call
        fn = lambda e, call=call: getattr(e, call[0])(*call[1], **call[2])
        deps = self._deps(eng, reads, writes, False)
        waits = self._waits(eng, deps)
        n = self.cnt[eng]
        self.cnt[eng] = n + 1
        key = (eng, n // EPOCH)
        val = n % EPOCH + 1
        tok = (key, val, False)
        c = dict(self.run[eng])
        c[key] = val
        self.clk[(key, val)] = c
        self.q[eng].append((waits, fn, key, 1))
        for b in reads:
            b.r.append(tok)
        for b in writes:
            b.w = tok
            b.r = []
        return tok

    def dma(self, eng, out, in_, reads=(), writes=(), **kw):
        deps = self._deps(eng, reads, writes, True)
        d = self.dcnt[eng]
        self.dcnt[eng] = d + 1
        nd = self.ndma[eng]
        if d >= nd:
            deps.add(self.dtoks[eng][d - nd])
        waits = self._waits(eng, deps)
        key = ("d" + eng, d % nd)
        val = 16 * (d // nd + 1)
        tok = (key, val, True)
        c = dict(self.run[eng])
        c[key] = val
        self.clk[(key, val)] = c
        self.dtoks[eng].append(tok)
        self.q[eng].append((waits, lambda e: e.dma_start(out=out, in_=in_, **kw), key, 16))
        for b in reads:
            b.r.append(tok)
        for b in writes:
            b.w = tok
            b.r = []
        return tok

    def barrier(self):
        best = {}
        for e in self.ENGS:
            n = self.cnt[e]
            if n > 0 and e != "sp":
                best[(e, (n - 1) // EPOCH)] = ((n - 1) % EPOCH + 1, False)
            for (k, v, isd) in self.dtoks[e][-self.ndma.get(e, 1):]:
                if best.get(k, (0, True))[0] < v:
                    best[k] = (v, True)
        toks = [(k, v, isd) for k, (v, isd) in best.items()]
        for e in self.ENGS:
            waits = []
            run = self.run[e]
            for (k, v, isd) in toks:
                if run.get(k, 0) < v:
                    waits.append((k, v))
                    for k2, v2 in self.clk[(k, v)].items():
                        if run.get(k2, 0) < v2:
                            run[k2] = v2
            self.q[e].append((waits, None, None, 0))

    def wait_tokens(self, eng, toks):
        waits = self._waits(eng, set(toks))
        self.q[eng].append((waits, None, None, 0))

    def emit(self):
        nc = self.nc
        for k in set(k for e in self.ENGS for (_, _, k, _) in self.q[e] if k is not None):
            self.sem(k)
        for e in self.ENGS:
            for (waits, _, _, _) in self.q[e]:
                for (k, v) in waits:
                    self.sem(k)
        with nc.Block() as block:
            def run(engname, engobj):
                for (waits, fn, key, inc) in self.q[engname]:
                    for (k, v) in waits:
                        engobj.wait_ge(self.sems[k], v)
                    if fn is not None:
                        ins = fn(engobj)
                        ins.then_inc(self.sems[key], inc)

            @block.tensor
            def _(e):
                run("pe", e)

            @block.scalar
            def _(e):
                run("act", e)

            @block.vector
            def _(e):
                run("dve", e)

            @block.gpsimd
            def _(e):
                run("pool", e)

            @block.sync
            def _(e):
                run("sp", e)


NMASK = 13
LEVELS = (2, 4, 8, 16, 32, 64)


def make_cmask():
    p = np.arange(128)[:, None]
    f = np.arange(128)[None, :]
    m = np.zeros((128, NMASK, 128), np.float32)
    m[:, 0, :] = (f > p)
    m[:, 1, :] = (f >= p)
    m[:, 2, :] = (f > p)
    m[:, 3, :] = (f >= p)
    m[:, 4, :] = ((p % 2 == 1) & (f == p - 1))
    m[:, 5, :] = ((f % 2 == 1) & (p == f - 1))
    for li, mm in enumerate(LEVELS):
        m[:, 6 + li, :] = (((p // mm) % 2 == 1) & ((f // mm) == (p // mm) - 1))
    m[:, 12, :] = ((p // 64) == (f // 64))
    return m


def build_nc(T, dbg=None, phases="FRO"):
    NSB = T // SB
    NBLK = T // 128
    nc = bass.Bass("TRN2", target_bir_lowering=False)
    es0 = ExitStack()
    S = Sched(nc, es0)

    def din(name, shape):
        return nc.dram_tensor(name, list(shape), F32, kind="ExternalInput").ap()

    x = din("x", [T, D])
    g_pre = din("attn_norm_pre", [D])
    g_post = din("attn_norm_post", [D])
    w_in = din("w_in", [D, WIN])
    din_fb = din("fox_forget_bias", [NH])
    shift_mu = din("shift_mu", [1824])
    rwkv_w0 = din("rwkv_w0", [RW])
    rwkv_w_up = din("rwkv_w_up", [64, RW])
    rwkv_a0 = din("rwkv_a0", [RW])
    rwkv_a_up = din("rwkv_a_up", [64, RW])
    rwkv_g_up = din("rwkv_g_up", [160, RW])
    rwkv_k_k = din("rwkv_k_k", [RW])
    rwkv_k_a = din("rwkv_k_a", [RW])
    rwkv_r_k = din("rwkv_r_k", [RW])
    rwkv_ln_w = din("rwkv_ln_w", [RW])
    rwkv_ln_b = din("rwkv_ln_b", [RW])
    w_out = din("w_out", [D, D])
    gf_pre = din("ffn_norm_pre", [D])
    gf_post = din("ffn_norm_post", [D])
    w_gate = din("ffn_w_gate", [D, DFF])
    w_up = din("ffn_w_up", [D, DFF])
    w_down = din("ffn_w_down", [DFF, D])
    cmask = din("cmask", [128, NMASK, 128])
    out = nc.dram_tensor("out", [T, D], F32, kind="ExternalOutput").ap()
    cat_scr = nc.dram_tensor("cat_scr", [D, T], BF16, kind="Internal").ap()
    scr_k = nc.dram_tensor("scr_k", [3, NH, T], BF16, kind="Internal").ap()
    scr_q = nc.dram_tensor("scr_q", [3, NH, T], BF16, kind="Internal").ap()
    dbg_aps = {}
    if dbg:
        for k, shp in dbg.items():
            if k == "stop":
                continue
            dbg_aps[k] = nc.dram_tensor("dbg_" + k, list(shp), F32, kind="ExternalOutput").ap()

    ucnt = [0]

    def uniq(name):
        ucnt[0] += 1
        return "%s_%d" % (name, ucnt[0])

    with es0:
        tp_ps = es0.enter_context(nc.psum_tensor("tp_ps", [128, KC, 128], BF16))
        b_tp = Buf()
        PB = [es0.enter_context(nc.psum_tensor("pb%d" % i, [128, SB], F32)) for i in range(7)]
        b_PB = [Buf() for _ in range(7)]
        ident_bf = es0.enter_context(nc.sbuf_tensor("ident_bf", [128, 128], BF16))
        ident_f = es0.enter_context(nc.sbuf_tensor("ident_f", [128, 128], F32))
        b_ident = Buf()
        S.op("pool", lambda e: e.memset(ident_f[:], 0.0), writes=[b_ident])
        S.op("pool", lambda e: e.affine_select(out=ident_f[:], in_=ident_f[:], pattern=[[-1, 128]],
                                               compare_op=ALU.not_equal, fill=1.0, base=0,
                                               channel_multiplier=1), reads=[b_ident], writes=[b_ident])
        S.op("dve", lambda e: e.tensor_copy(out=ident_bf[:], in_=ident_f[:]), reads=[b_ident], writes=[b_ident])

        def load_colvec(es, name, src, ncol):
            t = es.enter_context(nc.sbuf_tensor(uniq(name), [128, ncol], F32))
            b = Buf()
            S.dma("sp", t[:], src.rearrange("(k p) -> p k", p=128), writes=[b], allow_slow_non_contiguous=True)
            return t, b

        def make_front(es):
            def sbt(name, shape, dt=F32):
                return es.enter_context(nc.sbuf_tensor(uniq(name), list(shape), dt))
            st = {}
            st["xt"] = [sbt("xt%d" % i, [128, D], F32) for i in range(2)]
            st["b_xt"] = [Buf() for _ in range(2)]
            st["junk"] = sbt("junk", [128, D], BF16)
            st["b_junk"] = Buf()
            st["xn"] = [sbt("xn%d" % i, [128, D], BF16) for i in range(2)]
            st["b_xn"] = [Buf(), Buf()]
            st["ss"] = sbt("ss", [128, 8], F32)
            st["b_ss"] = [Buf() for _ in range(8)]
            st["uT"] = sbt("uT", [128, KC, SB], BF16)
            st["b_uT"] = Buf()
            st["tc"] = 0
            return st

        def front(st, s):
            xt, b_xt, xn, b_xn, ss, b_ss = st["xt"], st["b_xt"], st["xn"], st["b_xn"], st["ss"], st["b_ss"]
            junk, b_junk, uT, b_uT = st["junk"], st["b_junk"], st["uT"], st["b_uT"]
            for j in range(4):
                t0 = s * SB + j * 128
                xi = st["tc"] % 2
                ni = st["tc"] % 2
                si = st["tc"] % 8
                st["tc"] += 1
                S.dma("sp", xt[xi][:], x[t0:t0 + 128, :], writes=[b_xt[xi]])
                S.op("act", lambda e, xi=xi, si=si: e.activation(out=junk[:], in_=xt[xi][:], func=AF.Square,
                                                                 accum_out=ss[:, si:si + 1]),
                     reads=[b_xt[xi]], writes=[b_junk, b_ss[si]])
                S.op("dve", lambda e, si=si: e.tensor_scalar(out=ss[:, si:si + 1], in0=ss[:, si:si + 1],
                                                             scalar1=1.0 / D, scalar2=EPS, op0=ALU.mult, op1=ALU.add),
                     reads=[b_ss[si]], writes=[b_ss[si]])
                S.op("act", lambda e, si=si: e.sqrt(out=ss[:, si:si + 1], in_=ss[:, si:si + 1]),
                     reads=[b_ss[si]], writes=[b_ss[si]])
                S.op("dve", lambda e, si=si: e.reciprocal(out=ss[:, si:si + 1], in_=ss[:, si:si + 1]),
                     reads=[b_ss[si]], writes=[b_ss[si]])
                S.op("act", lambda e, xi=xi, ni=ni, si=si: e.activation(out=xn[ni][:], in_=xt[xi][:],
                                                                        func=AF.Copy, scale=ss[:, si:si + 1]),
                     reads=[b_xt[xi], b_ss[si]], writes=[b_xn[ni]])
                for kc in range(KC):
                    S.op("pe", lambda e, kc=kc, ni=ni: e.transpose(out=tp_ps[:, kc, :],
                                                                   in_=xn[ni][:, kc * 128:(kc + 1) * 128],
                                                                   identity=ident_bf[:]),
                         reads=[b_xn[ni], b_ident], writes=[b_tp])
                S.op("dve", lambda e, j=j: e.tensor_copy(out=uT[:, :, j * 128:(j + 1) * 128], in_=tp_ps[:, :, :]),
                     reads=[b_tp], writes=[b_uT])

        def load_weight_bf(st, dst, b_dst, src, c0, ncols, gvec, b_g, nk=KC):
            xt, b_xt = st["xt"], st["b_xt"]
            cnt = 0
            for kc in range(nk):
                for p0 in range(0, ncols, D):
                    n = min(D, ncols - p0)
                    i = cnt % 2
                    cnt += 1
                    S.dma("sp", xt[i][:, 0:n], src[kc * 128:(kc + 1) * 128, c0 + p0:c0 + p0 + n], writes=[b_xt[i]])
                    if cnt % 2 == 0:
                        if gvec is None:
                            S.op("dve", lambda e: e.tensor_copy(out=dst[:, kc, p0:p0 + n], in_=xt[i][:, 0:n]),
                                 reads=[b_xt[i]], writes=[b_dst[kc]])
                        else:
                            S.op("dve", lambda e: e.tensor_scalar(
                                out=dst[:, kc, p0:p0 + n], in0=xt[i][:, 0:n], scalar1=gvec[:, kc:kc + 1], scalar2=None, op0=ALU.mult),
                                 reads=[b_xt[i], b_g], writes=[b_dst[kc]])
                    else:
                        if gvec is None:
                            S.op("act", lambda e: e.copy(out=dst[:, kc, p0:p0 + n], in_=xt[i][:, 0:n]),
                                 reads=[b_xt[i]], writes=[b_dst[kc]])
                        else:
                            S.op("act", lambda e: e.activation(out=dst[:, kc, p0:p0 + n], in_=xt[i][:, 0:n], func=AF.Copy,
                                                               scale=gvec[:, kc:kc + 1]),
                                 reads=[b_xt[i], b_g], writes=[b_dst[kc]])

        pjc = [0]

        def proj_fm(wt, b_w, st, c0, ncols, evac):
            pi = pjc[0] % 2
            pjc[0] += 1
            uT, b_uT = st["uT"], st["b_uT"]
            for kc in range(KC):
                S.op("pe", lambda e, kc=kc: e.matmul(out=PB[pi][0:ncols, :], lhsT=wt[:, kc, c0:c0 + ncols],
                                                     rhs=uT[:, kc, :], start=(kc == 0), stop=(kc == KC - 1)),
                     reads=[b_w[kc], b_uT], writes=[b_PB[pi]])
            evac(PB[pi], b_PB[pi])

        esR = ExitStack()
        NRC = 1824
        wr_bf = esR.enter_context(nc.sbuf_tensor("wr_bf_g", [128, KC, NRC], BF16))
        b_wr = [Buf() for _ in range(KC)]
        PSTW = 256
        pst = [esR.enter_context(nc.sbuf_tensor("pst%d" % i, [128, PSTW], F32)) for i in range(2)]
        b_pst = [Buf(), Buf()]
        gTr, b_gTr = load_colvec(esR, "gTr0", g_pre, KC)

        def prefetch_R():
            cnt = 0
            for kc in range(KC):
                for p0 in range(0, NRC, PSTW):
                    n = min(PSTW, NRC - p0)
                    i = cnt % 2
                    cnt += 1
                    S.dma("pool", pst[i][:, 0:n], w_in[kc * 128:(kc + 1) * 128, RBASE + p0:RBASE + p0 + n], writes=[b_pst[i]])
                    S.op("pool", lambda e: e.tensor_scalar(out=wr_bf[:, kc, p0:p0 + n], in0=pst[i][:, 0:n],
                                                           scalar1=gTr[:, kc:kc + 1], scalar2=None, op0=ALU.mult),
                         reads=[b_pst[i], b_gTr], writes=[b_wr[kc]])

        if "F" not in phases:
            prefetch_R()
        if "F" in phases:
            with ExitStack() as es:
                def sb(name, shape, dt=F32):
                    return es.enter_context(nc.sbuf_tensor(uniq(name), list(shape), dt))
                st = make_front(es)
                uT, b_uT = st["uT"], st["b_uT"]
                gT, b_gT = load_colvec(es, "gT", g_pre, KC)
                w_bf = sb("w_bf", [128, KC, RBASE], BF16)
                b_w = [Buf() for _ in range(KC)]
                load_weight_bf(st, w_bf, b_w, w_in, 0, RBASE, gT, b_gT)

                nfb = sb("nfb", [8, 1], F32)
                b_nfb = Buf()
                S.dma("sp", nfb[:], din_fb.rearrange("(h o) -> h o", o=1), writes=[b_nfb])
                S.op("dve", lambda e: e.tensor_scalar(out=nfb[:], in0=nfb[:], scalar1=-1.0, scalar2=None, op0=ALU.mult),
                     reads=[b_nfb], writes=[b_nfb])
                ones8 = sb("ones8", [8, SB], F32)
                b_ones8 = Buf()
                S.op("pool", lambda e: e.memset(ones8[:], 1.0), writes=[b_ones8])
                ones_f = sb("ones_f", [128, 64], F32)
                b_onesf = Buf()
                S.op("pool", lambda e: e.memset(ones_f[:], 1.0), writes=[b_onesf])
                maskneg_f = sb("maskneg_f", [128, 128], F32)
                maskneg = sb("maskneg", [128, 128], BF16)
                b_mask = Buf()
                S.op("pool", lambda e: e.memset(maskneg_f[:], 0.0), writes=[b_mask])
                S.op("pool", lambda e: e.affine_select(out=maskneg_f[:], in_=maskneg_f[:], pattern=[[1, 128]],
                                                       compare_op=ALU.is_ge, fill=-30000.0, base=0,
                                                       channel_multiplier=-1), reads=[b_mask], writes=[b_mask])
                S.op("dve", lambda e: e.tensor_copy(out=maskneg[:], in_=maskneg_f[:]), reads=[b_mask], writes=[b_mask])

                KT = sb("KT", [70, NH, T], BF16)
                b_KT = [Buf() for _ in range(NSB)]
                QT = sb("QT", [70, NH, SB], BF16)
                b_QT = Buf()
                VT = sb("VT", [128, NBLK, NH, 66], BF16)
                b_VT = [Buf() for _ in range(NSB)]
                S.op("pool", lambda e: e.memset(KT[64:70, :, :], 1.0), writes=b_KT)
                S.op("pool", lambda e: e.memset(QT[64:70, :, :], 1.0), writes=[b_QT])
                S.op("pool", lambda e: e.memset(VT[:, :, :, 64:66], 1.0), writes=b_VT)
                b_scrk = Buf()
                b_scrq = Buf()
                cneg = [sb("cneg%d" % i, [8, SB], F32) for i in range(2)]
                b_cneg = [Buf(), Buf()]
                fl = sb("fl", [8, SB], F32)
                b_fl = Buf()
                res1, b_res1 = fl, b_fl
                ksp = sb("ksp", [8, 3, SB], BF16)
                qsp = sb("qsp", [8, 3, SB], BF16)
                b_ksp = Buf()
                b_qsp = Buf()
                catF = [sb("catF%d" % i, [64, SB], BF16) for i in range(2)]
                b_catF = [Buf(), Buf()]
                PT = [sb("PT%d" % i, [128, SB], BF16) for i in range(3)]
                b_PT = [Buf() for _ in range(3)]
                rs = sb("rs", [66, SB], F32)
                b_rs = Buf()
                bc_sb = sb("bc_sb", [64, SB], F32)
                b_bc = Buf()
                dbg_sb = sb("dbg_sb", [128, SB if dbg else 2], F32)
                b_dbg = Buf()
                st_ps = [PB[2], PB[3]]
                b_st = [b_PB[2], b_PB[3]]
                o_ps2 = [PB[4], PB[5]]
                b_o2 = [b_PB[4], b_PB[5]]
                bc_ps, b_bcps = PB[6], b_PB[6]

                prefetch_R()
                for s in range(NSB):
                    c_lo, c_hi = s * SB, (s + 1) * SB
                    front(st, s)
                    ci = s % 2

                    def ev_ff(pp, bp):
                        S.op("act", lambda e: e.activation(out=fl[:], in_=pp[0:8, :], func=AF.Exp, bias=nfb[:, 0:1], scale=-1.0),
                             reads=[bp, b_nfb], writes=[b_fl])
                        S.op("act", lambda e: e.activation(out=fl[:], in_=fl[:], func=AF.Ln, bias=1.0, scale=1.0),
                             reads=[b_fl], writes=[b_fl])
                        S.op("dve", lambda e: e.tensor_tensor_scan(out=cneg[ci][:], data0=ones8[:], data1=fl[:], initial=0.0,
                                                                   op0=ALU.mult, op1=ALU.add),
                             reads=[b_fl, b_ones8], writes=[b_cneg[ci]])
                        if s > 0:
                            S.op("dve", lambda e: e.tensor_scalar(out=cneg[ci][:], in0=cneg[ci][:],
                                                                  scalar1=cneg[1 - ci][:, SB - 1:SB], scalar2=None, op0=ALU.add),
                                 reads=[b_cneg[ci], b_cneg[1 - ci]], writes=[b_cneg[ci]])
                        S.op("dve", lambda e: e.tensor_copy(out=ksp[:, 0, :], in_=cneg[ci][:]), reads=[b_cneg[ci]], writes=[b_ksp])
                        S.op("dve", lambda e: e.tensor_tensor(out=res1[:], in0=cneg[ci][:], in1=ksp[:, 0, :], op=ALU.subtract),
                             reads=[b_cneg[ci], b_ksp], writes=[b_res1])
                        S.op("dve", lambda e: e.tensor_copy(out=ksp[:, 1, :], in_=res1[:]), reads=[b_res1], writes=[b_ksp])
                        S.op("dve", lambda e: e.tensor_tensor(out=res1[:], in0=res1[:], in1=ksp[:, 1, :], op=ALU.subtract),
                             reads=[b_res1, b_ksp], writes=[b_res1])
                        S.op("dve", lambda e: e.tensor_copy(out=ksp[:, 2, :], in_=res1[:]), reads=[b_res1], writes=[b_ksp])
                        S.op("dve", lambda e: e.tensor_scalar(out=qsp[:], in0=ksp[:], scalar1=-1.0, scalar2=None, op0=ALU.mult),
                             reads=[b_ksp], writes=[b_qsp])
                        S.dma("sp", scr_k[:, :, c_lo:c_hi].rearrange("r h t -> h r t"), ksp[:], reads=[b_ksp], writes=[b_scrk])
                        S.dma("sp", scr_q[:, :, c_lo:c_hi].rearrange("r h t -> h r t"), qsp[:], reads=[b_qsp], writes=[b_scrq])
                        S.dma("sp", KT[67:70, :, c_lo:c_hi], scr_k[:, :, c_lo:c_hi], reads=[b_scrk], writes=[b_KT[s]])
                        S.dma("sp", QT[64:67, :, :], scr_q[:, :, c_lo:c_hi], reads=[b_scrq], writes=[b_QT])

                    proj_fm(w_bf, b_w, st, 1536, 8, ev_ff)
                    for h in range(NH):
                        def ev_q(pp, bp, h=h):
                            S.op("act", lambda e: e.mul(out=QT[0:64, h, :], in_=pp[0:64, :], mul=0.125), reads=[bp], writes=[b_QT])
                        proj_fm(w_bf, b_w, st, h * 64, 64, ev_q)

                        def ev_k(pp, bp, h=h):
                            S.op("dve", lambda e: e.tensor_copy(out=KT[0:64, h, c_lo:c_hi], in_=pp[0:64, :]), reads=[bp], writes=[b_KT[s]])
                        proj_fm(w_bf, b_w, st, 512 + h * 64, 64, ev_k)
                    for j in range(4):
                        pi = pjc[0] % 2
                        pjc[0] += 1
                        for kc in range(KC):
                            S.op("pe", lambda e, kc=kc, j=j, pi=pi: e.matmul(out=PB[pi][:], lhsT=uT[:, kc, j * 128:(j + 1) * 128],
                                                                             rhs=w_bf[:, kc, 1024:1536], start=(kc == 0), stop=(kc == KC - 1)),
                                 reads=[b_w[kc], b_uT], writes=[b_PB[pi]])
                        S.op("act", lambda e, j=j, pi=pi: e.copy(out=VT[:, s * 4 + j, :, 0:64],
                                                                 in_=PB[pi][:].rearrange("p (h d) -> p h d", h=NH)),
                             reads=[b_PB[pi]], writes=[b_VT[s]])

                    tiles = []
                    nkb = 4 * (s + 1)
                    for h in range(NH):
                        for kb in range(nkb):
                            d = kb - 4 * s
                            tiles.append((h, kb, 0 if d < 0 else d * 128, d >= 0, len(tiles)))

                    def emit_qk(tl):
                        h, kb, q0, diag, idx = tl
                        si_ = idx % 2
                        S.op("pe", lambda e: e.matmul(out=st_ps[si_][:, q0:SB], lhsT=KT[0:70, h, kb * 128:(kb + 1) * 128],
                                                      rhs=QT[0:70, h, q0:SB], start=True, stop=(not diag)),
                             reads=[b_KT[kb // 4], b_QT], writes=[b_st[si_]])
                        if diag:
                            S.op("pe", lambda e: e.matmul(out=st_ps[si_][:, q0:q0 + 128], lhsT=ident_bf[:], rhs=maskneg[:],
                                                          start=False, stop=True),
                                 reads=[b_ident, b_mask], writes=[b_st[si_]])

                    emit_qk(tiles[0])
                    for ti, tl in enumerate(tiles):
                        h, kb, q0, diag, idx = tl
                        si_ = idx % 2
                        pi_ = idx % 3
                        oi_ = h % 2
                        if ti + 1 < len(tiles):
                            emit_qk(tiles[ti + 1])
                        S.op("act", lambda e: e.activation(out=PT[pi_][:, q0:SB], in_=st_ps[si_][:, q0:SB], func=AF.Exp),
                             reads=[b_st[si_]], writes=[b_PT[pi_]])
                        S.op("pe", lambda e: e.matmul(out=o_ps2[oi_][0:66, q0:SB], lhsT=VT[:, kb, h, :], rhs=PT[pi_][:, q0:SB],
                                                      start=(kb == 0), stop=(kb == nkb - 1)),
                             reads=[b_VT[kb // 4], b_PT[pi_]], writes=[b_o2[oi_]])
                        if kb != nkb - 1:
                            continue
                        o_ps, b_o = o_ps2[oi_], b_o2[oi_]
                        S.op("dve", lambda e: e.reciprocal(out=rs[64:66, :], in_=o_ps[64:66, :]), reads=[b_o], writes=[b_rs])
                        S.op("pe", lambda e: e.matmul(out=bc_ps[0:64, :], lhsT=ones_f[64:65, 0:64], rhs=rs[64:65, :], start=True, stop=True),
                             reads=[b_rs, b_onesf], writes=[b_bcps])
                        S.op("act", lambda e: e.copy(out=bc_sb[:], in_=bc_ps[0:64, :]), reads=[b_bcps], writes=[b_bc])
                        fi = h % 2
                        S.op("dve", lambda e: e.tensor_tensor(out=catF[fi][:], in0=o_ps[0:64, :], in1=bc_sb[:], op=ALU.mult),
                             reads=[b_o, b_bc], writes=[b_catF[fi]])
                        S.dma("sp", cat_scr[h * 64:(h + 1) * 64, c_lo:c_hi], catF[fi][:], reads=[b_catF[fi]])
                        if dbg and "ofox" in dbg:
                            S.op("act", lambda e, fi=fi: e.copy(out=dbg_sb[0:64, :], in_=catF[fi][:]), reads=[b_catF[fi]], writes=[b_dbg])
                            S.dma("sp", dbg_aps["ofox"][h * 64:(h + 1) * 64, c_lo:c_hi], dbg_sb[0:64, :], reads=[b_dbg])
                S.barrier()
        if "R" in phases:
            with ExitStack() as es:
                def sb(name, shape, dt=F32):
                    return es.enter_context(nc.sbuf_tensor(uniq(name), list(shape), dt))
                st = make_front(es)
                uT, b_uT = st["uT"], st["b_uT"]
                lo_bf = sb("lo_bf", [128, RW], BF16)
                b_lo = Buf()
                gup_bf = sb("gup_bf", [128, RW], BF16)
                gup1_bf = sb("gup1_bf", [32, RW], BF16)
                b_gup = Buf()
                xt, b_xt = st["xt"], st["b_xt"]
                S.dma("sp", xt[0][0:64, 0:RW], rwkv_w_up[:, :], writes=[b_xt[0]])
                S.dma("sp", xt[0][64:128, 0:RW], rwkv_a_up[:, :], writes=[b_xt[0]])
                S.op("dve", lambda e: e.tensor_copy(out=lo_bf[:], in_=xt[0][:, 0:RW]), reads=[b_xt[0]], writes=[b_lo])
                S.dma("sp", xt[1][:, 0:RW], rwkv_g_up[0:128, :], writes=[b_xt[1]])
                S.op("dve", lambda e: e.tensor_copy(out=gup_bf[:], in_=xt[1][:, 0:RW]), reads=[b_xt[1]], writes=[b_gup])
                S.dma("sp", xt[0][0:32, 0:RW], rwkv_g_up[128:160, :], reads=[b_lo], writes=[b_xt[0]])
                S.op("dve", lambda e: e.tensor_copy(out=gup1_bf[:], in_=xt[0][0:32, 0:RW]), reads=[b_xt[0]], writes=[b_gup])
                w0T, b_w0 = load_colvec(es, "w0T", rwkv_w0, 4)
                a0T, b_a0 = load_colvec(es, "a0T", rwkv_a0, 4)
                kkT, b_kkv = load_colvec(es, "kkT", rwkv_k_k, 4)
                kaT, b_kav = load_colvec(es, "kaT", rwkv_k_a, 4)
                rkT, b_rkv = load_colvec(es, "rkT", rwkv_r_k, 4)
                lnwT, b_lnw = load_colvec(es, "lnwT", rwkv_ln_w, 4)
                lnbT, b_lnb = load_colvec(es, "lnbT", rwkv_ln_b, 4)
                groups = {}
                gl = []
                for hp in range(4):
                    gl.append((("r", hp), hp * 128, 128))
                    gl.append((("k", hp), 512 + hp * 128, 128))
                    gl.append((("v", hp), 1024 + hp * 128, 128))
                gl.append(("wa", 1536, 128))
                gl.append(("g0", 1664, 128))
                gl.append(("g1", 1792, 32))
                muT = sb("muT", [128, len(gl)], F32)
                b_mu = Buf()
                carry = sb("carry", [128, len(gl)], F32)
                b_carry = [Buf() for _ in gl]
                S.op("pool", lambda e: e.memset(carry[:], 0.0), writes=b_carry)
                for gi, (nm, c0, n) in enumerate(gl):
                    groups[nm] = (gi, c0, n)
                    S.dma("sp", muT[0:n, gi:gi + 1], shift_mu[c0:c0 + n].rearrange("(p o) -> p o", o=1), writes=[b_mu])
                cm = sb("cm", [128, NMASK, 128], F32)
                b_cm = Buf()
                S.dma("sp", cm[:], cmask[:, :, :], writes=[b_cm])
                mk0T_bf = sb("mk0T_bf", [128, 128], BF16)
                S.op("dve", lambda e: e.tensor_copy(out=mk0T_bf[:], in_=cm[:, 5, :]), reads=[b_cm], writes=[b_cm])
                bd_mean = sb("bd_mean", [128, 128], F32)
                S.op("dve", lambda e: e.tensor_scalar(out=bd_mean[:], in0=cm[:, 12, :], scalar1=1.0 / 64, scalar2=None, op0=ALU.mult),
                     reads=[b_cm], writes=[b_cm])
                ones128 = sb("ones128", [128, 128], F32)
                S.op("pool", lambda e: e.memset(ones128[:], 1.0), writes=[b_cm])

                H32 = [sb("H32_%d" % i, [128, 128], F32) for i in range(4)]
                Hbf = [sb("Hbf_%d" % i, [128, 128], BF16) for i in range(4)]
                b_H32 = [Buf() for _ in range(4)]
                b_Hbf = [Buf() for _ in range(4)]
                HbfB = [sb("HbfB_%d" % i, [128, 128], BF16) for i in range(4)]
                b_HbfB = [Buf() for _ in range(4)]
                Hbf2 = [[Hbf[i], HbfB[i]] for i in range(4)]
                b_Hbf2 = [[b_Hbf[i], b_HbfB[i]] for i in range(4)]
                for i in range(4):
                    S.op("pool", lambda e, i=i: e.memset(H32[i][:], 0.0), writes=[b_H32[i]])
                    S.op("pool", lambda e, i=i: e.memset(Hbf[i][:], 0.0), writes=[b_Hbf[i]])
                    S.op("pool", lambda e, i=i: e.memset(HbfB[i][:], 0.0), writes=[b_HbfB[i]])

                def wt(name, dt=F32, n=SB):
                    return sb(name, [128, n], dt), Buf()
                raw = [sb("raw%d" % i, [128, SB + 1], F32) for i in range(2)]
                b_raw = [Buf(), Buf()]
                rawc = [0]
                dlt, b_dlt = wt("dlt")
                wa_s, b_was = wt("wa_s")
                g0_s, b_g0s = wt("g0_s")
                g1_s, b_g1s = wt("g1_s")
                lat_bf, b_lat = wt("lat_bf", BF16)
                sg_bf, b_sg = wt("sg_bf", BF16)
                sg1_bf, b_sg1 = wt("sg1_bf", BF16)
                r_s, b_rs_ = wt("r_s")
                k_s, b_ks = wt("k_s")
                v_s, b_vs = wt("v_s")
                lw, b_lw = wt("lw")
                a_t, b_a = wt("a_t")
                g_t, b_g = wt("g_t")
                kq, b_kq = wt("kq")
                tmpA, b_tmpA = wt("tmpA")
                kk, b_kk = wt("kk")
                kmod, b_kmod = wt("kmod")
                bb, b_bb = wt("bb")
                bonus, b_bonus = wt("bonus")
                cum, b_cum = wt("cum")
                E_in, b_Ein = wt("E_in")
                E_neg, b_Eneg = wt("E_neg")
                E_ex, b_Eex = wt("E_ex")
                E_end, b_Eend = wt("E_end")
                y_sb, b_y = wt("y_sb")
                AR = sb("AR", [128, 4, 2, 128], BF16)
                b_AR = Buf()
                BT, b_BT = wt("BT", BF16)
                KTt, b_KTt = wt("KTt", BF16)
                BH, b_BH = wt("BH", BF16)
                KH, b_KH = wt("KH", BF16)
                Vb, b_Vb = wt("Vb", BF16)
                catR, b_catR = wt("catR", BF16)
                TM = [sb("TM%d" % i, [128, 4, 128], BF16) for i in range(4)]
                b_TM = [Buf() for _ in range(4)]
                NMt = [sb("NM%d" % i, [128, 4, 128], BF16) for i in range(4)]
                b_NM = [Buf() for _ in range(4)]
                Dm = [sb("Dm%d" % i, [128, 128], BF16) for i in range(4)]
                b_Dm = [Buf() for _ in range(4)]
                DTm = [sb("DTm%d" % i, [128, 128], BF16) for i in range(4)]
                b_DTm = [Buf() for _ in range(4)]
                Gm = [sb("Gm%d" % i, [128, 128], BF16) for i in range(4)]
                b_Gm = [Buf() for _ in range(4)]
                X2b = [sb("X2b%d" % i, [128, 64], BF16) for i in range(4)]
                b_X2b = [Buf() for _ in range(4)]
                U2s = [sb("U2s%d" % i, [128, 128], F32) for i in range(2)]
                b_U2s = [Buf(), Buf()]
                WTb = [sb("WTb%d" % i, [128, 128], BF16) for i in range(2)]
                b_WTb = [Buf(), Buf()]
                Ub = sb("Ub", [128, 128], BF16)
                b_Ub = Buf()
                dbg_sb = sb("dbg_sbr", [128, SB], F32)
                b_dbg = Buf()
                M1 = [PB[4], PB[5]]
                b_M1 = [b_PB[4], b_PB[5]]
                sA = [PB[i][:, 0:128] for i in range(4)]
                b_sA = [b_PB[i] for i in range(4)]
                sB = [PB[i][:, 128:256] for i in range(4)]
                b_sB = [b_PB[i] for i in range(4)]
                u2_ps, uh_ps = [PB[6][:, i * 128:(i + 1) * 128] for i in range(2)]
                y2_ps = [PB[6][:, (2 + i) * 128:(3 + i) * 128] for i in range(2)]
                b_u2 = b_wt = b_uh = b_yps = b_PB[6]

                def shifted(nm, dst, b_dst):
                    gi, c0, n = groups[nm]

                    def ev(pp, bp):
                        ri = rawc[0] % 2
                        rawc[0] += 1
                        rw_, brw = raw[ri], b_raw[ri]
                        S.op("act", lambda e: e.copy(out=rw_[0:n, 1:SB + 1], in_=pp[0:n, :]), reads=[bp], writes=[brw])
                        S.op("pool", lambda e: e.tensor_copy(out=rw_[0:n, 0:1], in_=carry[0:n, gi:gi + 1]),
                             reads=[b_carry[gi]], writes=[brw])
                        S.op("pool", lambda e: e.tensor_copy(out=carry[0:n, gi:gi + 1], in_=rw_[0:n, SB:SB + 1]),
                             reads=[brw], writes=[b_carry[gi]])
                        S.op("dve", lambda e: e.tensor_tensor(out=dlt[0:n, :], in0=rw_[0:n, 0:SB], in1=rw_[0:n, 1:SB + 1], op=ALU.subtract),
                             reads=[brw], writes=[b_dlt])
                        S.op("dve", lambda e: e.scalar_tensor_tensor(out=dst[0:n, :], in0=dlt[0:n, :], scalar=muT[0:n, gi:gi + 1],
                                                                     in1=rw_[0:n, 1:SB + 1], op0=ALU.mult, op1=ALU.add),
                             reads=[b_dlt, brw, b_mu], writes=[b_dst])
                    proj_fm(wr_bf, b_wr, st, c0, n, ev)

                def v4(ap):
                    return ap.rearrange("p (c t) -> p c t", c=4)

                AR_b = sb("AR_b", [128, 4, 2, 128], BF16)
                BT_b = sb("BT_b", [128, SB], BF16)
                KTt_b = sb("KTt_b", [128, SB], BF16)
                BH_b = sb("BH_b", [128, SB], BF16)
                KH_b = sb("KH_b", [128, SB], BF16)
                Vb_b = sb("Vb_b", [128, SB], BF16)
                E_in_b = sb("E_in_b", [128, SB], F32)
                bonus_b = sb("bonus_b", [128, SB], F32)
                g_t_b = sb("g_t_b", [128, SB], F32)
                AR_2 = [AR, AR_b]
                b_AR_2 = [b_AR, Buf()]
                BT_2 = [BT, BT_b]
                b_BT_2 = [b_BT, Buf()]
                KTt_2 = [KTt, KTt_b]
                b_KTt_2 = [b_KTt, Buf()]
                BH_2 = [BH, BH_b]
                b_BH_2 = [b_BH, Buf()]
                KH_2 = [KH, KH_b]
                b_KH_2 = [b_KH, Buf()]
                Vb_2 = [Vb, Vb_b]
                b_Vb_2 = [b_Vb, Buf()]
                E_in_2 = [E_in, E_in_b]
                b_Ein_2 = [b_Ein, Buf()]
                bonus_2 = [bonus, bonus_b]
                b_bonus_2 = [b_bonus, Buf()]
                g_t_2 = [g_t, g_t_b]
                b_g_2 = [b_g, Buf()]
                tmpB, b_tmpB = wt("tmpB")
                M1 = [PB[2], PB[3], PB[4], PB[5]]
                b_M1 = [b_PB[2], b_PB[3], b_PB[4], b_PB[5]]
                sA = [PB[2 + i][:, 0:128] for i in range(4)]
                b_sA = [b_PB[2 + i] for i in range(4)]
                sB = [PB[2 + i][:, 128:256] for i in range(4)]
                b_sB = [b_PB[2 + i] for i in range(4)]

                def gen_front(s, hp, pb):
                    c_lo, c_hi = s * SB, (s + 1) * SB
                    AR, b_AR = AR_2[pb], b_AR_2[pb]
                    BT, b_BT = BT_2[pb], b_BT_2[pb]
                    KTt, b_KTt = KTt_2[pb], b_KTt_2[pb]
                    BH, b_BH = BH_2[pb], b_BH_2[pb]
                    KH, b_KH = KH_2[pb], b_KH_2[pb]
                    Vb, b_Vb = Vb_2[pb], b_Vb_2[pb]
                    E_in, b_Ein = E_in_2[pb], b_Ein_2[pb]
                    bonus, b_bonus = bonus_2[pb], b_bonus_2[pb]
                    g_t, b_g = g_t_2[pb], b_g_2[pb]
                    if hp == 0:
                        front(st, s)
                        shifted("wa", wa_s, b_was)
                        shifted("g0", g0_s, b_g0s)
                        shifted("g1", g1_s, b_g1s)
                        S.op("act", lambda e: e.activation(out=lat_bf[0:64, :], in_=wa_s[0:64, :], func=AF.Tanh), reads=[b_was], writes=[b_lat])
                        S.op("act", lambda e: e.copy(out=lat_bf[64:128, :], in_=wa_s[64:128, :]), reads=[b_was], writes=[b_lat])
                        S.op("act", lambda e: e.activation(out=sg_bf[:], in_=g0_s[:], func=AF.Sigmoid), reads=[b_g0s], writes=[b_sg])
                        S.op("act", lambda e: e.activation(out=sg1_bf[0:32, :], in_=g1_s[0:32, :], func=AF.Sigmoid), reads=[b_g1s], writes=[b_sg1])
                        yield
                    hc = slice(hp * 128, (hp + 1) * 128)
                    shifted(("r", hp), r_s, b_rs_)
                    shifted(("k", hp), k_s, b_ks)
                    yield
                    shifted(("v", hp), v_s, b_vs)
                    p0, bp0 = PB[0], b_PB[0]
                    S.op("pe", lambda e: e.matmul(out=p0[:], lhsT=lo_bf[0:64, hc], rhs=lat_bf[0:64, :], start=True, stop=True),
                         reads=[b_lo, b_lat], writes=[bp0])
                    yield
                    S.op("act", lambda e: e.activation(out=lw[:], in_=p0[:], func=AF.Sigmoid, bias=w0T[:, hp:hp + 1]),
                         reads=[bp0, b_w0], writes=[b_lw])
                    S.op("pool", lambda e: e.tensor_scalar(out=lw[:], in0=lw[:], scalar1=-0.6065306597126334, scalar2=None, op0=ALU.mult),
                         reads=[b_lw], writes=[b_lw])
                    p1, bp1 = PB[1], b_PB[1]
                    yield
                    S.op("pe", lambda e: e.matmul(out=p1[:], lhsT=lo_bf[64:128, hc], rhs=lat_bf[64:128, :], start=True, stop=True),
                         reads=[b_lo, b_lat], writes=[bp1])
                    S.op("act", lambda e: e.activation(out=a_t[:], in_=p1[:], func=AF.Sigmoid, bias=a0T[:, hp:hp + 1]),
                         reads=[bp1, b_a0], writes=[b_a])
                    S.op("pe", lambda e: e.matmul(out=p0[:], lhsT=gup_bf[:, hc], rhs=sg_bf[:], start=True, stop=False),
                         reads=[b_gup, b_sg], writes=[bp0])
                    yield
                    S.op("pe", lambda e: e.matmul(out=p0[:], lhsT=gup1_bf[0:32, hc], rhs=sg1_bf[0:32, :], start=False, stop=True),
                         reads=[b_gup, b_sg1], writes=[bp0])
                    S.op("act", lambda e: e.copy(out=g_t[:], in_=p0[:]), reads=[bp0], writes=[b_g])
                    S.op("dve", lambda e: e.tensor_scalar(out=kq[:], in0=k_s[:], scalar1=kkT[:, hp:hp + 1], scalar2=None, op0=ALU.mult),
                         reads=[b_ks, b_kkv], writes=[b_kq])
                    yield
                    S.op("pool", lambda e: e.tensor_tensor(out=tmpA[:], in0=kq[:], in1=kq[:], op=ALU.mult), reads=[b_kq], writes=[b_tmpA])
                    S.op("pe", lambda e: e.matmul(out=p1[:], lhsT=cm[:, 12, :], rhs=tmpA[:], start=True, stop=True),
                         reads=[b_cm, b_tmpA], writes=[bp1])
                    S.op("act", lambda e: e.sqrt(out=tmpA[:], in_=p1[:]), reads=[bp1], writes=[b_tmpA])
                    yield
                    S.op("dve", lambda e: e.tensor_scalar(out=tmpA[:], in0=tmpA[:], scalar1=1e-12, scalar2=None, op0=ALU.max),
                         reads=[b_tmpA], writes=[b_tmpA])
                    S.op("dve", lambda e: e.reciprocal(out=tmpA[:], in_=tmpA[:]), reads=[b_tmpA], writes=[b_tmpA])
                    S.op("pool", lambda e: e.tensor_tensor(out=kk[:], in0=kq[:], in1=tmpA[:], op=ALU.mult),
                         reads=[b_kq, b_tmpA], writes=[b_kk])
                    yield
                    S.op("dve", lambda e: e.tensor_scalar(out=kmod[:], in0=a_t[:], scalar1=-1.0, scalar2=kaT[:, hp:hp + 1],
                                                          op0=ALU.add, op1=ALU.mult), reads=[b_a, b_kav], writes=[b_kmod])
                    S.op("dve", lambda e: e.scalar_tensor_tensor(out=kmod[:], in0=kmod[:], scalar=1.0, in1=k_s[:],
                                                                 op0=ALU.add, op1=ALU.mult), reads=[b_kmod, b_ks], writes=[b_kmod])
                    S.op("pool", lambda e: e.tensor_tensor(out=bb[:], in0=kk[:], in1=a_t[:], op=ALU.mult), reads=[b_kk, b_a], writes=[b_bb])
                    yield
                    S.op("dve", lambda e: e.scalar_tensor_tensor(out=tmpA[:], in0=r_s[:], scalar=rkT[:, hp:hp + 1], in1=kmod[:],
                                                                 op0=ALU.mult, op1=ALU.mult), reads=[b_rs_, b_rkv, b_kmod], writes=[b_tmpA])
                    S.op("pe", lambda e: e.matmul(out=p1[:], lhsT=cm[:, 12, :], rhs=tmpA[:], start=True, stop=True),
                         reads=[b_cm, b_tmpA], writes=[bp1])
                    S.op("dve", lambda e: e.tensor_tensor(out=bonus[:], in0=p1[:], in1=v_s[:], op=ALU.mult), reads=[bp1, b_vs], writes=[b_bonus])
                    yield
                    for c in range(4):
                        cc = slice(c * 128, (c + 1) * 128)
                        S.op("dve", lambda e, cc=cc: e.tensor_tensor_scan(out=cum[:, cc], data0=ones128[:], data1=lw[:, cc], initial=0.0,
                                                                          op0=ALU.mult, op1=ALU.add), reads=[b_lw, b_cm], writes=[b_cum])
                    S.op("act", lambda e: e.activation(out=E_in[:], in_=cum[:], func=AF.Exp), reads=[b_cum], writes=[b_Ein])
                    S.op("act", lambda e: e.activation(out=E_neg[:], in_=cum[:], func=AF.Exp, scale=-1.0), reads=[b_cum], writes=[b_Eneg])
                    yield
                    for c in range(4):
                        cc = slice(c * 128, (c + 1) * 128)
                        S.op("act", lambda e, cc=cc, c=c: e.activation(out=E_end[:, cc], in_=cum[:, cc], func=AF.Exp, scale=-1.0,
                                                                       bias=cum[:, c * 128 + 127:c * 128 + 128]),
                             reads=[b_cum], writes=[b_Eend])
                    S.op("pool", lambda e: e.tensor_tensor(out=tmpA[:], in0=cum[:], in1=lw[:], op=ALU.subtract), reads=[b_cum, b_lw], writes=[b_tmpA])
                    S.op("act", lambda e: e.activation(out=E_ex[:], in_=tmpA[:], func=AF.Exp), reads=[b_tmpA], writes=[b_Eex])
                    yield
                    S.op("dve", lambda e: e.scalar_tensor_tensor(out=AR[:, :, 0, :], in0=v4(kk[:]), scalar=-1.0, in1=v4(E_ex[:]),
                                                                 op0=ALU.mult, op1=ALU.mult), reads=[b_kk, b_Eex], writes=[b_AR])
                    S.op("pool", lambda e: e.tensor_tensor(out=AR[:, :, 1, :], in0=v4(r_s[:]), in1=v4(E_in[:]), op=ALU.mult),
                         reads=[b_rs_, b_Ein], writes=[b_AR])
                    S.op("dve", lambda e: e.tensor_tensor(out=BT[:], in0=bb[:], in1=E_neg[:], op=ALU.mult), reads=[b_bb, b_Eneg], writes=[b_BT])
                    yield
                    S.op("pool", lambda e: e.tensor_tensor(out=KTt[:], in0=kmod[:], in1=E_neg[:], op=ALU.mult), reads=[b_kmod, b_Eneg], writes=[b_KTt])
                    S.op("dve", lambda e: e.tensor_tensor(out=BH[:], in0=bb[:], in1=E_end[:], op=ALU.mult), reads=[b_bb, b_Eend], writes=[b_BH])
                    S.op("pool", lambda e: e.tensor_tensor(out=KH[:], in0=kmod[:], in1=E_end[:], op=ALU.mult), reads=[b_kmod, b_Eend], writes=[b_KH])
                    yield
                    S.op("act", lambda e: e.copy(out=Vb[:], in_=v_s[:]), reads=[b_vs], writes=[b_Vb])

                    yield

                def gen_chunks(s, hp, pb):
                    c_lo, c_hi = s * SB, (s + 1) * SB
                    AR, b_AR = AR_2[pb], b_AR_2[pb]
                    BT, b_BT = BT_2[pb], b_BT_2[pb]
                    KTt, b_KTt = KTt_2[pb], b_KTt_2[pb]
                    BH, b_BH = BH_2[pb], b_BH_2[pb]
                    KH, b_KH = KH_2[pb], b_KH_2[pb]
                    Vb, b_Vb = Vb_2[pb], b_Vb_2[pb]
                    E_in, b_Ein = E_in_2[pb], b_Ein_2[pb]
                    bonus, b_bonus = bonus_2[pb], b_bonus_2[pb]
                    g_t, b_g = g_t_2[pb], b_g_2[pb]
                    hc = slice(hp * 128, (hp + 1) * 128)
                    p0, bp0 = PB[0], b_PB[0]
                    p1, bp1 = PB[1], b_PB[1]
                    stop = (dbg or {}).get("stop", "")
                    if stop == "A":
                        return
                    for cp in range(2):
                        chains = [(2 * cp + ci_, e_) for ci_ in range(2) for e_ in range(2)]
                        for ci_ in range(2):
                            c = 2 * cp + ci_
                            cc = slice(c * 128, (c + 1) * 128)
                            srcs = [(AR[:, c, 0, :], b_AR), (Vb[:, cc], b_Vb), (BH[:, cc], b_BH), (KH[:, cc], b_KH)]
                            for k_, (src, bsrc) in enumerate(srcs):
                                S.op("pe", lambda e, k_=k_, src=src: e.transpose(out=tp_ps[:, k_, :], in_=src, identity=ident_bf[:]),
                                     reads=[bsrc, b_ident], writes=[b_tp])
                            S.op("act", lambda e, c=c: e.copy(out=TM[c][:], in_=tp_ps[:, 0:4, :]), reads=[b_tp], writes=[b_TM[c]])
                        yield
                        for ch, (c, e_) in enumerate(chains):
                            pr = slice(e_ * 64, (e_ + 1) * 64)
                            cc = slice(c * 128, (c + 1) * 128)
                            m1, bm1 = M1[ch], b_M1[ch]
                            S.op("pe", lambda e, pr=pr, cc=cc, c=c, m1=m1: e.matmul(out=m1[:, 0:256], lhsT=BT[pr, cc],
                                                                                 rhs=AR[pr, c, :, :], start=True, stop=True),
                                 reads=[b_BT, b_AR], writes=[bm1])
                            S.op("pe", lambda e, pr=pr, cc=cc, c=c, m1=m1: e.matmul(out=m1[:, 256:512], lhsT=KTt[pr, cc],
                                                                                 rhs=AR[pr, c, :, :], start=True, stop=True),
                                 reads=[b_KTt, b_AR], writes=[bm1])
                            S.op("dve", lambda e, ch=ch, m1=m1: e.tensor_tensor(
                                out=NMt[ch][:], in0=m1[:].rearrange("p (a t) -> p a t", a=4),
                                in1=cm[:, 0:4, :],
                                op=ALU.mult), reads=[bm1, b_cm], writes=[b_NM[ch]])
                        for ch, (c, e_) in enumerate(chains):
                            pr = slice(e_ * 64, (e_ + 1) * 64)
                            cc = slice(c * 128, (c + 1) * 128)
                            S.op("pe", lambda e, pr=pr, cc=cc, c=c, ch=ch: e.matmul(out=sA[ch], lhsT=AR[pr, c, 0, :], rhs=BT[pr, cc],
                                                                                 start=True, stop=True),
                                 reads=[b_AR, b_BT], writes=[b_sA[ch]])
                            S.op("dve", lambda e, ch=ch: e.tensor_tensor(out=Dm[ch][:], in0=sA[ch], in1=cm[:, 4, :], op=ALU.mult),
                                 reads=[b_sA[ch], b_cm], writes=[b_Dm[ch]])
                            S.op("pool", lambda e, ch=ch: e.tensor_tensor(out=Dm[ch][:], in0=Dm[ch][:], in1=ident_bf[:], op=ALU.add),
                                 reads=[b_Dm[ch], b_ident], writes=[b_Dm[ch]])
                            S.op("pool", lambda e, ch=ch: e.tensor_tensor(out=DTm[ch][:], in0=NMt[ch][:, 0, :], in1=mk0T_bf[:], op=ALU.mult),
                                 reads=[b_NM[ch], b_cm], writes=[b_DTm[ch]])
                            S.op("pool", lambda e, ch=ch: e.tensor_tensor(out=DTm[ch][:], in0=DTm[ch][:], in1=ident_bf[:], op=ALU.add),
                                 reads=[b_DTm[ch], b_ident], writes=[b_DTm[ch]])
                        if stop == "B":
                            continue
                        yield
                        for li, mm in enumerate(LEVELS):
                            last = (li == len(LEVELS) - 1)
                            yield
                            for ch in range(4):
                                S.op("pe", lambda e, ch=ch: e.matmul(out=sA[ch], lhsT=NMt[ch][:, 0, :], rhs=Dm[ch][:], start=True, stop=True),
                                     reads=[b_NM[ch], b_Dm[ch]], writes=[b_sA[ch]])
                                S.op("dve", lambda e, ch=ch, li=li: e.tensor_tensor(out=Gm[ch][:], in0=sA[ch], in1=cm[:, 6 + li, :], op=ALU.mult),
                                     reads=[b_sA[ch], b_cm], writes=[b_Gm[ch]])
                                S.op("pool", lambda e, ch=ch: e.tensor_tensor(out=Gm[ch][:], in0=Gm[ch][:], in1=ident_bf[:], op=ALU.add),
                                     reads=[b_Gm[ch], b_ident], writes=[b_Gm[ch]])
                            yield
                            for ch in range(4):
                                if not last:
                                    S.op("pe", lambda e, ch=ch: e.matmul(out=sA[ch], lhsT=DTm[ch][:], rhs=Gm[ch][:], start=True, stop=True),
                                         reads=[b_DTm[ch], b_Gm[ch]], writes=[b_sA[ch]])
                                S.op("pe", lambda e, ch=ch: e.matmul(out=sB[ch], lhsT=Gm[ch][:], rhs=DTm[ch][:], start=True, stop=True),
                                     reads=[b_DTm[ch], b_Gm[ch]], writes=[b_sB[ch]])
                                if not last:
                                    S.op("act", lambda e, ch=ch: e.copy(out=Dm[ch][:], in_=sA[ch]), reads=[b_sA[ch]], writes=[b_Dm[ch]])
                                S.op("act", lambda e, ch=ch: e.copy(out=DTm[ch][:], in_=sB[ch]), reads=[b_sB[ch]], writes=[b_DTm[ch]])
                        if stop == "C":
                            continue
                        yield
                        for ch, (c, e_) in enumerate(chains):
                            pr = slice(e_ * 64, (e_ + 1) * 64)
                            S.op("pe", lambda e, ch=ch, c=c, pr=pr: e.matmul(out=sA[ch][:, 0:64], lhsT=NMt[ch][:, 2, :], rhs=TM[c][:, 1, pr],
                                                                          start=True, stop=True),
                                 reads=[b_NM[ch], b_TM[c]], writes=[b_sA[ch]])
                            S.op("act", lambda e, ch=ch: e.copy(out=X2b[ch][:], in_=sA[ch][:, 0:64]), reads=[b_sA[ch]], writes=[b_X2b[ch]])
                        for ci_ in range(2):
                            c = 2 * cp + ci_
                            for e_ in range(2):
                                ch = ci_ * 2 + e_
                                pr = slice(e_ * 64, (e_ + 1) * 64)
                                S.op("pe", lambda e, ch=ch, pr=pr: e.matmul(out=u2_ps[:, pr], lhsT=DTm[ch][:], rhs=X2b[ch][:], start=True, stop=True),
                                     reads=[b_DTm[ch], b_X2b[ch]], writes=[b_u2])
                                S.op("pe", lambda e, ch=ch, c=c: e.matmul(out=sA[ch], lhsT=TM[c][:, 0, :], rhs=DTm[ch][:], start=True, stop=True),
                                     reads=[b_DTm[ch], b_TM[c]], writes=[b_sA[ch]])
                                S.op("dve", lambda e, ci_=ci_, ch=ch, pr=pr: e.tensor_copy(out=WTb[ci_][pr, :], in_=sA[ch][pr, :]),
                                     reads=[b_sA[ch]], writes=[b_WTb[ci_]])
                            S.op("act", lambda e, ci_=ci_: e.copy(out=U2s[ci_][:], in_=u2_ps), reads=[b_u2], writes=[b_U2s[ci_]])
                        if stop == "D":
                            continue
                        yield
                        for ci_ in range(2):
                            c = 2 * cp + ci_
                            cc = slice(c * 128, (c + 1) * 128)
                            yield
                            Ho, bHo = Hbf2[hp][c % 2], b_Hbf2[hp][c % 2]
                            Hn, bHn = Hbf2[hp][(c + 1) % 2], b_Hbf2[hp][(c + 1) % 2]
                            S.op("pe", lambda e, ci_=ci_: e.matmul(out=uh_ps, lhsT=WTb[ci_][:], rhs=Ho[:], start=True, stop=True),
                                 reads=[b_WTb[ci_], bHo], writes=[b_uh])
                            S.op("dve", lambda e, ci_=ci_: e.tensor_tensor(out=Ub[:], in0=uh_ps, in1=U2s[ci_][:], op=ALU.add),
                                 reads=[b_uh, b_U2s[ci_]], writes=[b_Ub])
                            S.op("pe", lambda e, c=c: e.matmul(out=uh_ps, lhsT=TM[c][:, 2, :], rhs=Ub[:], start=True, stop=False),
                                 reads=[b_TM[c], b_Ub], writes=[b_uh])
                            S.op("pe", lambda e, c=c: e.matmul(out=uh_ps, lhsT=TM[c][:, 3, :], rhs=TM[c][:, 1, :], start=False, stop=True),
                                 reads=[b_TM[c]], writes=[b_uh])
                            for e_ in range(2):
                                pr = slice(e_ * 64, (e_ + 1) * 64)
                                S.op("dve", lambda e, pr=pr, c=c: e.scalar_tensor_tensor(
                                    out=H32[hp][pr, pr], in0=H32[hp][pr, pr], scalar=E_in[pr, c * 128 + 127:c * 128 + 128],
                                    in1=uh_ps[pr, pr], op0=ALU.mult, op1=ALU.add),
                                    reads=[b_H32[hp], b_Ein, b_uh], writes=[b_H32[hp]])
                            S.op("dve", lambda e: e.tensor_copy(out=Hn[:], in_=H32[hp][:]), reads=[b_H32[hp]], writes=[bHn])
                            for e_ in range(2):
                                ch = ci_ * 2 + e_
                                pr = slice(e_ * 64, (e_ + 1) * 64)
                                yp = y2_ps[e_]
                                S.op("pe", lambda e, c=c, yp=yp: e.matmul(out=yp, lhsT=Ho[:], rhs=AR[:, c, 1, :], start=True, stop=False),
                                     reads=[bHo, b_AR], writes=[b_yps])
                                S.op("pe", lambda e, ch=ch, yp=yp: e.matmul(out=yp, lhsT=Ub[:], rhs=NMt[ch][:, 1, :], start=False, stop=False),
                                     reads=[b_Ub, b_NM[ch]], writes=[b_yps])
                                S.op("pe", lambda e, ch=ch, c=c, yp=yp: e.matmul(out=yp, lhsT=TM[c][:, 1, :], rhs=NMt[ch][:, 3, :], start=False, stop=True),
                                     reads=[b_TM[c], b_NM[ch]], writes=[b_yps])
                            for e_ in range(2):
                                pr = slice(e_ * 64, (e_ + 1) * 64)
                                S.op("act", lambda e, cc=cc, pr=pr, e_=e_: e.copy(out=y_sb[pr, cc], in_=y2_ps[e_][pr, :]), reads=[b_yps], writes=[b_y])
                    if dbg and "yraw" in dbg:
                        S.op("act", lambda e: e.copy(out=dbg_sb[:], in_=y_sb[:]), reads=[b_y], writes=[b_dbg])
                        S.dma("sp", dbg_aps["yraw"][hp * 128:(hp + 1) * 128, c_lo:c_hi], dbg_sb[:], reads=[b_dbg])
                    yield
                    S.op("pe", lambda e: e.matmul(out=p0[:], lhsT=bd_mean[:], rhs=y_sb[:], start=True, stop=True),
                         reads=[b_cm, b_y], writes=[bp0])
                    S.op("dve", lambda e: e.tensor_tensor(out=y_sb[:], in0=y_sb[:], in1=p0[:], op=ALU.subtract), reads=[b_y, bp0], writes=[b_y])
                    S.op("pool", lambda e: e.tensor_tensor(out=tmpB[:], in0=y_sb[:], in1=y_sb[:], op=ALU.mult), reads=[b_y], writes=[b_tmpB])
                    S.op("pe", lambda e: e.matmul(out=p1[:], lhsT=bd_mean[:], rhs=tmpB[:], start=True, stop=True),
                         reads=[b_cm, b_tmpB], writes=[bp1])
                    S.op("dve", lambda e: e.tensor_scalar(out=tmpB[:], in0=p1[:], scalar1=LNX_EPS, scalar2=None, op0=ALU.add),
                         reads=[bp1], writes=[b_tmpB])
                    S.op("act", lambda e: e.sqrt(out=tmpB[:], in_=tmpB[:]), reads=[b_tmpB], writes=[b_tmpB])
                    S.op("dve", lambda e: e.reciprocal(out=tmpB[:], in_=tmpB[:]), reads=[b_tmpB], writes=[b_tmpB])
                    S.op("pool", lambda e: e.tensor_tensor(out=y_sb[:], in0=y_sb[:], in1=tmpB[:], op=ALU.mult), reads=[b_y, b_tmpB], writes=[b_y])
                    S.op("dve", lambda e: e.tensor_scalar(out=y_sb[:], in0=y_sb[:], scalar1=lnwT[:, hp:hp + 1], scalar2=lnbT[:, hp:hp + 1],
                                                          op0=ALU.mult, op1=ALU.add), reads=[b_y, b_lnw, b_lnb], writes=[b_y])
                    S.op("pool", lambda e: e.tensor_tensor(out=y_sb[:], in0=y_sb[:], in1=bonus[:], op=ALU.add), reads=[b_y, b_bonus], writes=[b_y])
                    S.op("dve", lambda e: e.tensor_tensor(out=catR[:], in0=y_sb[:], in1=g_t[:], op=ALU.mult), reads=[b_y, b_g], writes=[b_catR])
                    S.dma("sp", cat_scr[512 + hp * 128:512 + (hp + 1) * 128, c_lo:c_hi], catR[:], reads=[b_catR])
                    if dbg and "orwkv" in dbg:
                        S.op("act", lambda e: e.copy(out=dbg_sb[:], in_=catR[:]), reads=[b_catR], writes=[b_dbg])
                        S.dma("sp", dbg_aps["orwkv"][hp * 128:(hp + 1) * 128, c_lo:c_hi], dbg_sb[:], reads=[b_dbg])
                    yield

                units = [(s_, hp_) for s_ in range(NSB) for hp_ in range(4)]
                for _ in gen_front(units[0][0], units[0][1], 0):
                    pass
                for ui, (s_, hp_) in enumerate(units):
                    gc = gen_chunks(s_, hp_, ui % 2)
                    gf = gen_front(units[ui + 1][0], units[ui + 1][1], (ui + 1) % 2) if ui + 1 < len(units) else iter(())
                    live = [gc, gf]
                    while live:
                        for g in list(live):
                            try:
                                next(g)
                            except StopIteration:
                                live.remove(g)
                S.barrier()
        esR.close()
        if "O" in phases:
            with ExitStack() as es:
                def sb(name, shape, dt=F32):
                    return es.enter_context(nc.sbuf_tensor(uniq(name), list(shape), dt))
                UT = 256
                NU = T // UT
                stg = [sb("stg%d" % i, [128, D], F32) for i in range(2)]
                b_stg = [Buf(), Buf()]
                st = {"xt": stg, "b_xt": b_stg}
                gfT, b_gfT = load_colvec(es, "gfT", gf_pre, KC)
                wout_bf = sb("wout_bf", [128, KC, D], BF16)
                b_wout = [Buf() for _ in range(KC)]
                wg_bf = sb("wg_bf", [128, KC, DFF], BF16)
                b_wg = [Buf() for _ in range(KC)]
                wu_bf = sb("wu_bf", [128, KC, DFF], BF16)
                b_wu = [Buf() for _ in range(KC)]
                wd_bf = sb("wd_bf", [128, NFF, D], BF16)
                b_wd = [Buf() for _ in range(NFF)]
                load_weight_bf(st, wout_bf, b_wout, w_out, 0, D, None, None)
                load_weight_bf(st, wg_bf, b_wg, w_gate, 0, DFF, gfT, b_gfT)
                load_weight_bf(st, wu_bf, b_wu, w_up, 0, DFF, gfT, b_gfT)
                load_weight_bf(st, wd_bf, b_wd, w_down, 0, D, None, None, nk=NFF)
                gpost_bc = sb("gpost_bc", [128, D], F32)
                gfpost_bc = sb("gfpost_bc", [128, D], F32)
                b_gbc = Buf()
                S.dma("sp", gpost_bc[:], g_post.partition_broadcast(128), writes=[b_gbc])
                S.dma("sp", gfpost_bc[:], gf_post.partition_broadcast(128), writes=[b_gbc])
                catT = sb("catT", [128, KC, UT], BF16)
                b_catT = Buf()
                zn = sb("zn", [128, D], BF16)
                b_zn = Buf()
                zT = sb("zT", [128, KC, UT], BF16)
                b_zT = Buf()
                aT = sb("aT", [128, NFF, UT], BF16)
                b_aT = Buf()
                junk = sb("junkO", [128, D], BF16)
                b_junk = Buf()
                sgt = [sb("sgt%d" % i, [128, UT], F32) for i in range(2)]
                b_sgt = [Buf(), Buf()]
                t1 = sb("t1", [128, D], F32)
                b_t1 = Buf()
                ssO = sb("ssO", [128, 4], F32)
                b_ssO = Buf()
                cat_v = cat_scr.rearrange("(k p) t -> p k t", p=128)

                def rstd_from(srcs, bsrcs):
                    for i, (ap_, b_) in enumerate(zip(srcs, bsrcs)):
                        n = ap_.shape[1]
                        S.op("act", lambda e, ap_=ap_, i=i, n=n: e.activation(out=junk[:, 0:n], in_=ap_, func=AF.Square, accum_out=ssO[:, i:i + 1]),
                             reads=[b_], writes=[b_junk, b_ssO])
                    if len(srcs) == 2:
                        S.op("dve", lambda e: e.tensor_tensor(out=ssO[:, 2:3], in0=ssO[:, 0:1], in1=ssO[:, 1:2], op=ALU.add),
                             reads=[b_ssO], writes=[b_ssO])
                        src = ssO[:, 2:3]
                    else:
                        src = ssO[:, 0:1]
                    S.op("dve", lambda e: e.tensor_scalar(out=ssO[:, 2:3], in0=src, scalar1=1.0 / D, scalar2=EPS, op0=ALU.mult, op1=ALU.add),
                         reads=[b_ssO], writes=[b_ssO])
                    S.op("act", lambda e: e.sqrt(out=ssO[:, 2:3], in_=ssO[:, 2:3]), reads=[b_ssO], writes=[b_ssO])
                    S.op("dve", lambda e: e.reciprocal(out=ssO[:, 2:3], in_=ssO[:, 2:3]), reads=[b_ssO], writes=[b_ssO])

                for u in range(NU):
                    t0 = u * UT
                    S.dma("sp", catT[:], cat_v[:, :, t0:t0 + UT], writes=[b_catT])
                    for j in range(2):
                        tj = t0 + j * 128
                        S.dma("sp", stg[j][:], x[tj:tj + 128, :], writes=[b_stg[j]])
                        for half in range(2):
                            for kc in range(KC):
                                S.op("pe", lambda e, kc=kc, half=half, j=j: e.matmul(
                                    out=PB[half][:], lhsT=catT[:, kc, j * 128:(j + 1) * 128], rhs=wout_bf[:, kc, half * 512:(half + 1) * 512],
                                    start=(kc == 0), stop=(kc == KC - 1)), reads=[b_catT, b_wout[kc]], writes=[b_PB[half]])
                        rstd_from([PB[0][:], PB[1][:]], [b_PB[0], b_PB[1]])
                        for half in range(2):
                            S.op("act", lambda e, half=half: e.activation(out=t1[:, half * 512:(half + 1) * 512], in_=PB[half][:], func=AF.Copy,
                                                                          scale=ssO[:, 2:3]), reads=[b_PB[half], b_ssO], writes=[b_t1])
                        S.op("pool", lambda e: e.tensor_tensor(out=t1[:], in0=t1[:], in1=gpost_bc[:], op=ALU.mult), reads=[b_t1, b_gbc], writes=[b_t1])
                        S.op("dve", lambda e, j=j: e.tensor_tensor(out=stg[j][:], in0=stg[j][:], in1=t1[:], op=ALU.add),
                             reads=[b_stg[j], b_t1], writes=[b_stg[j]])
                        if dbg and "h" in dbg:
                            S.dma("sp", dbg_aps["h"][tj:tj + 128, :], stg[j][:], reads=[b_stg[j]])
                        rstd_from([stg[j][:]], [b_stg[j]])
                        S.op("act", lambda e, j=j: e.activation(out=zn[:], in_=stg[j][:], func=AF.Copy, scale=ssO[:, 2:3]),
                             reads=[b_stg[j], b_ssO], writes=[b_zn])
                        for kc in range(KC):
                            S.op("pe", lambda e, kc=kc: e.transpose(out=tp_ps[:, kc, :], in_=zn[:, kc * 128:(kc + 1) * 128], identity=ident_bf[:]),
                                 reads=[b_zn, b_ident], writes=[b_tp])
                        S.op("dve", lambda e, j=j: e.tensor_copy(out=zT[:, :, j * 128:(j + 1) * 128], in_=tp_ps[:, :, :]), reads=[b_tp], writes=[b_zT])
                    for ffc in range(NFF):
                        gi_ = 2 + 2 * (ffc % 2)
                        ui_ = 3 + 2 * (ffc % 2)
                        fc = slice(ffc * 128, (ffc + 1) * 128)
                        for kc in range(KC):
                            S.op("pe", lambda e, kc=kc, fc=fc, gi_=gi_: e.matmul(out=PB[gi_][:, 0:UT], lhsT=wg_bf[:, kc, fc], rhs=zT[:, kc, :],
                                                                                 start=(kc == 0), stop=(kc == KC - 1)),
                                 reads=[b_wg[kc], b_zT], writes=[b_PB[gi_]])
                        for kc in range(KC):
                            S.op("pe", lambda e, kc=kc, fc=fc, ui_=ui_: e.matmul(out=PB[ui_][:, 0:UT], lhsT=wu_bf[:, kc, fc], rhs=zT[:, kc, :],
                                                                                 start=(kc == 0), stop=(kc == KC - 1)),
                                 reads=[b_wu[kc], b_zT], writes=[b_PB[ui_]])
                        si_ = ffc % 2
                        S.op("act", lambda e, gi_=gi_, si_=si_: e.activation(out=sgt[si_][:], in_=PB[gi_][:, 0:UT], func=AF.Silu),
                             reads=[b_PB[gi_]], writes=[b_sgt[si_]])
                        S.op("dve", lambda e, ui_=ui_, si_=si_, ffc=ffc: e.tensor_tensor(out=aT[:, ffc, :], in0=PB[ui_][:, 0:UT], in1=sgt[si_][:], op=ALU.mult),
                             reads=[b_PB[ui_], b_sgt[si_]], writes=[b_aT])
                    for j in range(2):
                        tj = t0 + j * 128
                        for half in range(2):
                            for ffc in range(NFF):
                                S.op("pe", lambda e, ffc=ffc, half=half, j=j: e.matmul(
                                    out=PB[half][:], lhsT=aT[:, ffc, j * 128:(j + 1) * 128], rhs=wd_bf[:, ffc, half * 512:(half + 1) * 512],
                                    start=(ffc == 0), stop=(ffc == NFF - 1)), reads=[b_aT, b_wd[ffc]], writes=[b_PB[half]])
                        rstd_from([PB[0][:], PB[1][:]], [b_PB[0], b_PB[1]])
                        for half in range(2):
                            S.op("act", lambda e, half=half: e.activation(out=t1[:, half * 512:(half + 1) * 512], in_=PB[half][:], func=AF.Copy,
                                                                          scale=ssO[:, 2:3]), reads=[b_PB[half], b_ssO], writes=[b_t1])
                        S.op("pool", lambda e: e.tensor_tensor(out=t1[:], in0=t1[:], in1=gfpost_bc[:], op=ALU.mult), reads=[b_t1, b_gbc], writes=[b_t1])
                        S.op("dve", lambda e, j=j: e.tensor_tensor(out=t1[:], in0=t1[:], in1=stg[j][:], op=ALU.add),
                             reads=[b_stg[j], b_t1], writes=[b_t1])
                        S.dma("sp", out[tj:tj + 128, :], t1[:], reads=[b_t1])

        S.wait_tokens("sp", [t for e in S.ENGS for t in S.dtoks[e]])
        S.emit()
    return nc


WNAMES = ["attn_norm_pre", "attn_norm_post", "w_in", "fox_forget_bias", "shift_mu", "rwkv_w0", "rwkv_w_up", "rwkv_a0",
          "rwkv_a_up", "rwkv_g_up", "rwkv_k_k", "rwkv_k_a", "rwkv_r_k", "rwkv_ln_w", "rwkv_ln_b", "w_out",
          "ffn_norm_pre", "ffn_norm_post", "ffn_w_gate", "ffn_w_up", "ffn_w_down"]


def make_in_map(inputs, b, T):
    m = {"x": np.ascontiguousarray(np.asarray(inputs["x"], dtype=np.float32)[b, :T])}
    for k in WNAMES:
        a = np.asarray(inputs[k], dtype=np.float32)[0]
        if k == "rwkv_r_k":
            a = a.reshape(-1)
        m[k] = np.ascontiguousarray(a)
    m["cmask"] = make_cmask()
    return m


def kernel(**inputs):
    x = np.asarray(inputs["x"])
    B, T, _ = x.shape
    nc = build_nc(T)
    in_maps = [make_in_map(inputs, b, T) for b in range(B)]
    res = run_bass_kernel_spmd(nc, in_maps, core_ids=list(range(B)))
    return np.stack([np.asarray(r["out"], dtype=np.float32) for r in res.results], axis=0)
```

```python
import numpy as np
from contextlib import ExitStack
import concourse.bass as bass
import concourse.mybir as mybir
from concourse.bass_utils import run_bass_kernel_spmd

F32 = mybir.dt.float32
BF16 = mybir.dt.bfloat16
AF = mybir.ActivationFunctionType
ALU = mybir.AluOpType
AX = mybir.AxisListType

D = 1024
KC = 8
HD = 64
NH = 8
FOXW = 512
RW = 512
DFF = 2816
NFF = 22
WIN = 3368
RBASE = 1544
EPS = 1e-6
LNX_EPS = 64e-5
SB = 512
EPOCH = 12000


class Buf:
    __slots__ = ("w", "r")

    def __init__(self):
        self.w = None
        self.r = []


class _Rec:
    def __getattr__(self, name):
        def f(*a, **k):
            self.call = (name, a, k)
        return f


class Sched:
    ENGS = ("pe", "act", "dve", "pool", "sp")

    def __init__(self, nc, es):
        self.nc = nc
        self.es = es
        self.q = {e: [] for e in self.ENGS}
        self.cnt = {e: 0 for e in self.ENGS}
        self.run = {e: {} for e in self.ENGS}
        self.clk = {}
        self.sems = {}
        self.ndma = {"sp": 16, "act": 6, "pool": 6}
        self.dcnt = {e: 0 for e in self.ENGS}
        self.dtoks = {e: [] for e in self.ENGS}
        self.nwaits = 0

    def sem(self, key):
        s = self.sems.get(key)
        if s is None:
            s = self.es.enter_context(self.nc.semaphore("s_%s_%s" % key))
            self.sems[key] = s
        return s

    def _deps(self, eng, reads, writes, is_dma):
        deps = set()
        for b in reads:
            if b.w is not None:
                deps.add(b.w)
        for b in writes:
            if b.w is not None:
                deps.add(b.w)
            for t in b.r:
                deps.add(t)
        return deps

    def _waits(self, eng, deps):
        run = self.run[eng]
        waits = []
        for t in sorted(deps, key=lambda t: (str(t[0]), t[1])):
            key, val, isd = t
            if eng == "pe" and key[0] == "pe" and not isd:
                continue
            if run.get(key, 0) >= val:
                continue
            waits.append((key, val))
            for k2, v2 in self.clk[(key, val)].items():
                if run.get(k2, 0) < v2:
                    run[k2] = v2
        self.nwaits += len(waits)
        return waits

    def op(self, eng, fn, reads=(), writes=()):
        rec = _Rec()
        fn(rec)
        call = rec.call
        fn = lambda e, call=call: getattr(e, call[0])(*call[1], **call[2])
        deps = self._deps(eng, reads, writes, False)
        waits = self._waits(eng, deps)
        n = self.cnt[eng]
        self.cnt[eng] = n + 1
        key = (eng, n // EPOCH)
        val = n % EPOCH + 1
        tok = (key, val, False)
        c = dict(self.run[eng])
        c[key] = val
        self.clk[(key, val)] = c
        self.q[eng].append((waits, fn, key, 1))
        for b in reads:
            b.r.append(tok)
        for b in writes:
            b.w = tok
            b.r = []
        return tok

    def dma(self, eng, out, in_, reads=(), writes=(), **kw):
        deps = self._deps(eng, reads, writes, True)
        d = self.dcnt[eng]
        self.dcnt[eng] = d + 1
        nd = self.ndma[eng]
        if d >= nd:
            deps.add(self.dtoks[eng][d - nd])
        waits = self._waits(eng, deps)
        key = ("d" + eng, d % nd)
        val = 16 * (d // nd + 1)
        tok = (key, val, True)
        c = dict(self.run[eng])
        c[key] = val
        self.clk[(key, val)] = c
        self.dtoks[eng].append(tok)
        self.q[eng].append((waits, lambda e: e.dma_start(out=out, in_=in_, **kw), key, 16))
        for b in reads:
            b.r.append(tok)
        for b in writes:
            b.w = tok
            b.r = []
        return tok

    def barrier(self):
        best = {}
        for e in self.ENGS:
            n = self.cnt[e]
            if n > 0 and e != "sp":
                best[(e, (n - 1) // EPOCH)] = ((n - 1) % EPOCH + 1, False)
            for (k, v, isd) in self.dtoks[e][-self.ndma.get(e, 1):]:
                if best.get(k, (0, True))[0] < v:
                    best[k] = (v, True)
        toks = [(k, v, isd) for k, (v, isd) in best.items()]
        for e in self.ENGS:
            waits = []
            run = self.run[e]
            for (k, v, isd) in toks:
                if run.get(k, 0) < v:
                    waits.append((k, v))
                    for k2, v2 in self.clk[(k, v)].items():
                        if run.get(k2, 0) < v2:
                            run[k2] = v2
            self.q[e].append((waits, None, None, 0))

    def wait_tokens(self, eng, toks):
        waits = self._waits(eng, set(toks))
        self.q[eng].append((waits, None, None, 0))

    def emit(self):
        nc = self.nc
        for k in set(k for e in self.ENGS for (_, _, k, _) in self.q[e] if k is not None):
            self.sem(k)
        for e in self.ENGS:
            for (waits, _, _, _) in self.q[e]:
                for (k, v) in waits:
                    self.sem(k)
        with nc.Block() as block:
            def run(engname, engobj):
                for (waits, fn, key, inc) in self.q[engname]:
                    for (k, v) in waits:
                        engobj.wait_ge(self.sems[k], v)
                    if fn is not None:
                        ins = fn(engobj)
                        ins.then_inc(self.sems[key], inc)

            @block.tensor
            def _(e):
                run("pe", e)

            @block.scalar
            def _(e):
                run("act", e)

            @block.vector
            def _(e):
                run("dve", e)

            @block.gpsimd
            def _(e):
                run("pool", e)

            @block.sync
            def _(e):
                run("sp", e)


NMASK = 13
LEVELS = (2, 4, 8, 16, 32, 64)


def make_cmask():
    p = np.arange(128)[:, None]
    f = np.arange(128)[None, :]
    m = np.zeros((128, NMASK, 128), np.float32)
    m[:, 0, :] = (f > p)
    m[:, 1, :] = (f >= p)
    m[:, 2, :] = (f > p)
    m[:, 3, :] = (f >= p)
    m[:, 4, :] = ((p % 2 == 1) & (f == p - 1))
    m[:, 5, :] = ((f % 2 == 1) & (p == f - 1))
    for li, mm in enumerate(LEVELS):
        m[:, 6 + li, :] = (((p // mm) % 2 == 1) & ((f // mm) == (p // mm) - 1))
    m[:, 12, :] = ((p // 64) == (f // 64))
    return m


def build_nc(T, dbg=None, phases="FRO"):
    NSB = T // SB
    NBLK = T // 128
    nc = bass.Bass("TRN2", target_bir_lowering=False)
    es0 = ExitStack()
    S = Sched(nc, es0)

    def din(name, shape):
        return nc.dram_tensor(name, list(shape), F32, kind="ExternalInput").ap()

    x = din("x", [T, D])
    g_pre = din("attn_norm_pre", [D])
    g_post = din("attn_norm_post", [D])
    w_in = din("w_in", [D, WIN])
    din_fb = din("fox_forget_bias", [NH])
    shift_mu = din("shift_mu", [1824])
    rwkv_w0 = din("rwkv_w0", [RW])
    rwkv_w_up = din("rwkv_w_up", [64, RW])
    rwkv_a0 = din("rwkv_a0", [RW])
    rwkv_a_up = din("rwkv_a_up", [64, RW])
    rwkv_g_up = din("rwkv_g_up", [160, RW])
    rwkv_k_k = din("rwkv_k_k", [RW])
    rwkv_k_a = din("rwkv_k_a", [RW])
    rwkv_r_k = din("rwkv_r_k", [RW])
    rwkv_ln_w = din("rwkv_ln_w", [RW])
    rwkv_ln_b = din("rwkv_ln_b", [RW])
    w_out = din("w_out", [D, D])
    gf_pre = din("ffn_norm_pre", [D])
    gf_post = din("ffn_norm_post", [D])
    w_gate = din("ffn_w_gate", [D, DFF])
    w_up = din("ffn_w_up", [D, DFF])
    w_down = din("ffn_w_down", [DFF, D])
    cmask = din("cmask", [128, NMASK, 128])
    out = nc.dram_tensor("out", [T, D], F32, kind="ExternalOutput").ap()
    cat_scr = nc.dram_tensor("cat_scr", [D, T], BF16, kind="Internal").ap()
    scr_k = nc.dram_tensor("scr_k", [3, NH, T], BF16, kind="Internal").ap()
    scr_q = nc.dram_tensor("scr_q", [3, NH, T], BF16, kind="Internal").ap()
    dbg_aps = {}
    if dbg:
        for k, shp in dbg.items():
            if k == "stop":
                continue
            dbg_aps[k] = nc.dram_tensor("dbg_" + k, list(shp), F32, kind="ExternalOutput").ap()

    ucnt = [0]

    def uniq(name):
        ucnt[0] += 1
        return "%s_%d" % (name, ucnt[0])

    with es0:
        tp_ps = es0.enter_context(nc.psum_tensor("tp_ps", [128, KC, 128], BF16))
        b_tp = Buf()
        PB = [es0.enter_context(nc.psum_tensor("pb%d" % i, [128, SB], F32)) for i in range(7)]
        b_PB = [Buf() for _ in range(7)]
        ident_bf = es0.enter_context(nc.sbuf_tensor("ident_bf", [128, 128], BF16))
        ident_f = es0.enter_context(nc.sbuf_tensor("ident_f", [128, 128], F32))
        b_ident = Buf()
        S.op("pool", lambda e: e.memset(ident_f[:], 0.0), writes=[b_ident])
        S.op("pool", lambda e: e.affine_select(out=ident_f[:], in_=ident_f[:], pattern=[[-1, 128]],
                                               compare_op=ALU.not_equal, fill=1.0, base=0,
                                               channel_multiplier=1), reads=[b_ident], writes=[b_ident])
        S.op("dve", lambda e: e.tensor_copy(out=ident_bf[:], in_=ident_f[:]), reads=[b_ident], writes=[b_ident])

        def load_colvec(es, name, src, ncol):
            t = es.enter_context(nc.sbuf_tensor(uniq(name), [128, ncol], F32))
            b = Buf()
            S.dma("sp", t[:], src.rearrange("(k p) -> p k", p=128), writes=[b], allow_slow_non_contiguous=True)
            return t, b

        def make_front(es):
            def sbt(name, shape, dt=F32):
                return es.enter_context(nc.sbuf_tensor(uniq(name), list(shape), dt))
            st = {}
            st["xt"] = [sbt("xt%d" % i, [128, D], F32) for i in range(2)]
            st["b_xt"] = [Buf() for _ in range(2)]
            st["junk"] = sbt("junk", [128, D], BF16)
            st["b_junk"] = Buf()
            st["xn"] = [sbt("xn%d" % i, [128, D], BF16) for i in range(2)]
            st["b_xn"] = [Buf(), Buf()]
            st["ss"] = sbt("ss", [128, 8], F32)
            st["b_ss"] = [Buf() for _ in range(8)]
            st["uT"] = sbt("uT", [128, KC, SB], BF16)
            st["b_uT"] = Buf()
            st["tc"] = 0
            return st

        def front(st, s):
            xt, b_xt, xn, b_xn, ss, b_ss = st["xt"], st["b_xt"], st["xn"], st["b_xn"], st["ss"], st["b_ss"]
            junk, b_junk, uT, b_uT = st["junk"], st["b_junk"], st["uT"], st["b_uT"]
            for j in range(4):
                t0 = s * SB + j * 128
                xi = st["tc"] % 2
                ni = st["tc"] % 2
                si = st["tc"] % 8
                st["tc"] += 1
                S.dma("sp", xt[xi][:], x[t0:t0 + 128, :], writes=[b_xt[xi]])
                S.op("act", lambda e, xi=xi, si=si: e.activation(out=junk[:], in_=xt[xi][:], func=AF.Square,
                                                                 accum_out=ss[:, si:si + 1]),
                     reads=[b_xt[xi]], writes=[b_junk, b_ss[si]])
                S.op("dve", lambda e, si=si: e.tensor_scalar(out=ss[:, si:si + 1], in0=ss[:, si:si + 1],
                                                             scalar1=1.0 / D, scalar2=EPS, op0=ALU.mult, op1=ALU.add),
                     reads=[b_ss[si]], writes=[b_ss[si]])
                S.op("act", lambda e, si=si: e.sqrt(out=ss[:, si:si + 1], in_=ss[:, si:si + 1]),
                     reads=[b_ss[si]], writes=[b_ss[si]])
                S.op("dve", lambda e, si=si: e.reciprocal(out=ss[:, si:si + 1], in_=ss[:, si:si + 1]),
                     reads=[b_ss[si]], writes=[b_ss[si]])
                S.op("act", lambda e, xi=xi, ni=ni, si=si: e.activation(out=xn[ni][:], in_=xt[xi][:],
                                                                        func=AF.Copy, scale=ss[:, si:si + 1]),
                     reads=[b_xt[xi], b_ss[si]], writes=[b_xn[ni]])
                for kc in range(KC):
                    S.op("pe", lambda e, kc=kc, ni=ni: e.transpose(out=tp_ps[:, kc, :],
                                                                   in_=xn[ni][:, kc * 128:(kc + 1) * 128],
                                                                   identity=ident_bf[:]),
                         reads=[b_xn[ni], b_ident], writes=[b_tp])
                S.op("dve", lambda e, j=j: e.tensor_copy(out=uT[:, :, j * 128:(j + 1) * 128], in_=tp_ps[:, :, :]),
                     reads=[b_tp], writes=[b_uT])

        def load_weight_bf(st, dst, b_dst, src, c0, ncols, gvec, b_g, nk=KC):
            xt, b_xt = st["xt"], st["b_xt"]
            cnt = 0
            for kc in range(nk):
                for p0 in range(0, ncols, D):
                    n = min(D, ncols - p0)
                    i = cnt % 2
                    cnt += 1
                    S.dma("sp", xt[i][:, 0:n], src[kc * 128:(kc + 1) * 128, c0 + p0:c0 + p0 + n], writes=[b_xt[i]])
                    if cnt % 2 == 0:
                        if gvec is None:
                            S.op("dve", lambda e: e.tensor_copy(out=dst[:, kc, p0:p0 + n], in_=xt[i][:, 0:n]),
                                 reads=[b_xt[i]], writes=[b_dst[kc]])
                        else:
                            S.op("dve", lambda e: e.tensor_scalar(
                                out=dst[:, kc, p0:p0 + n], in0=xt[i][:, 0:n], scalar1=gvec[:, kc:kc + 1], scalar2=None, op0=ALU.mult),
                                 reads=[b_xt[i], b_g], writes=[b_dst[kc]])
                    else:
                        if gvec is None:
                            S.op("act", lambda e: e.copy(out=dst[:, kc, p0:p0 + n], in_=xt[i][:, 0:n]),
                                 reads=[b_xt[i]], writes=[b_dst[kc]])
                        else:
                            S.op("act", lambda e: e.activation(out=dst[:, kc, p0:p0 + n], in_=xt[i][:, 0:n], func=AF.Copy,
                                                               scale=gvec[:, kc:kc + 1]),
                                 reads=[b_xt[i], b_g], writes=[b_dst[kc]])

        pjc = [0]

        def proj_fm(wt, b_w, st, c0, ncols, evac):
            pi = pjc[0] % 2
            pjc[0] += 1
            uT, b_uT = st["uT"], st["b_uT"]
            for kc in range(KC):
                S.op("pe", lambda e, kc=kc: e.matmul(out=PB[pi][0:ncols, :], lhsT=wt[:, kc, c0:c0 + ncols],
                                                     rhs=uT[:, kc, :], start=(kc == 0), stop=(kc == KC - 1)),
                     reads=[b_w[kc], b_uT], writes=[b_PB[pi]])
            evac(PB[pi], b_PB[pi])

        esR = ExitStack()
        NRC = 1824
        wr_bf = esR.enter_context(nc.sbuf_tensor("wr_bf_g", [128, KC, NRC], BF16))
        b_wr = [Buf() for _ in range(KC)]
        PSTW = 256
        pst = [esR.enter_context(nc.sbuf_tensor("pst%d" % i, [128, PSTW], F32)) for i in range(2)]
        b_pst = [Buf(), Buf()]
        gTr, b_gTr = load_colvec(esR, "gTr0", g_pre, KC)

        def prefetch_R():
            cnt = 0
            for kc in range(KC):
                for p0 in range(0, NRC, PSTW):
                    n = min(PSTW, NRC - p0)
                    i = cnt % 2
                    cnt += 1
                    S.dma("pool", pst[i][:, 0:n], w_in[kc * 128:(kc + 1) * 128, RBASE + p0:RBASE + p0 + n], writes=[b_pst[i]])
                    S.op("pool", lambda e: e.tensor_scalar(out=wr_bf[:, kc, p0:p0 + n], in0=pst[i][:, 0:n],
                                                           scalar1=gTr[:, kc:kc + 1], scalar2=None, op0=ALU.mult),
                         reads=[b_pst[i], b_gTr], writes=[b_wr[kc]])

        if "F" not in phases:
            prefetch_R()
        if "F" in phases:
            with ExitStack() as es:
                def sb(name, shape, dt=F32):
                    return es.enter_context(nc.sbuf_tensor(uniq(name), list(shape), dt))
                st = make_front(es)
                uT, b_uT = st["uT"], st["b_uT"]
                gT, b_gT = load_colvec(es, "gT", g_pre, KC)
                w_bf = sb("w_bf", [128, KC, RBASE], BF16)
                b_w = [Buf() for _ in range(KC)]
                load_weight_bf(st, w_bf, b_w, w_in, 0, RBASE, gT, b_gT)

                nfb = sb("nfb", [8, 1], F32)
                b_nfb = Buf()
                S.dma("sp", nfb[:], din_fb.rearrange("(h o) -> h o", o=1), writes=[b_nfb])
                S.op("dve", lambda e: e.tensor_scalar(out=nfb[:], in0=nfb[:], scalar1=-1.0, scalar2=None, op0=ALU.mult),
                     reads=[b_nfb], writes=[b_nfb])
                ones8 = sb("ones8", [8, SB], F32)
                b_ones8 = Buf()
                S.op("pool", lambda e: e.memset(ones8[:], 1.0), writes=[b_ones8])
                ones_f = sb("ones_f", [128, 64], F32)
                b_onesf = Buf()
                S.op("pool", lambda e: e.memset(ones_f[:], 1.0), writes=[b_onesf])
                maskneg_f = sb("maskneg_f", [128, 128], F32)
                maskneg = sb("maskneg", [128, 128], BF16)
                b_mask = Buf()
                S.op("pool", lambda e: e.memset(maskneg_f[:], 0.0), writes=[b_mask])
                S.op("pool", lambda e: e.affine_select(out=maskneg_f[:], in_=maskneg_f[:], pattern=[[1, 128]],
                                                       compare_op=ALU.is_ge, fill=-30000.0, base=0,
                                                       channel_multiplier=-1), reads=[b_mask], writes=[b_mask])
                S.op("dve", lambda e: e.tensor_copy(out=maskneg[:], in_=maskneg_f[:]), reads=[b_mask], writes=[b_mask])

                KT = sb("KT", [70, NH, T], BF16)
                b_KT = [Buf() for _ in range(NSB)]
                QT = sb("QT", [70, NH, SB], BF16)
                b_QT = Buf()
                VT = sb("VT", [128, NBLK, NH, 66], BF16)
                b_VT = [Buf() for _ in range(NSB)]
                S.op("pool", lambda e: e.memset(KT[64:70, :, :], 1.0), writes=b_KT)
                S.op("pool", lambda e: e.memset(QT[64:70, :, :], 1.0), writes=[b_QT])
                S.op("pool", lambda e: e.memset(VT[:, :, :, 64:66], 1.0), writes=b_VT)
                b_scrk = Buf()
                b_scrq = Buf()
                cneg = [sb("cneg%d" % i, [8, SB], F32) for i in range(2)]
                b_cneg = [Buf(), Buf()]
                fl = sb("fl", [8, SB], F32)
                b_fl = Buf()
                res1, b_res1 = fl, b_fl
                ksp = sb("ksp", [8, 3, SB], BF16)
                qsp = sb("qsp", [8, 3, SB], BF16)
                b_ksp = Buf()
                b_qsp = Buf()
                catF = [sb("catF%d" % i, [64, SB], BF16) for i in range(2)]
                b_catF = [Buf(), Buf()]
                PT = [sb("PT%d" % i, [128, SB], BF16) for i in range(4)]
                b_PT = [Buf() for _ in range(4)]
                rs = sb("rs", [66, SB], F32)
                b_rs = Buf()
                bc_sb = sb("bc_sb", [64, SB], F32)
                b_bc = Buf()
                dbg_sb = sb("dbg_sb", [128, SB if dbg else 2], F32)
                b_dbg = Buf()
                st_ps = [PB[0], PB[1], PB[2], PB[3]]
                b_st = [b_PB[0], b_PB[1], b_PB[2], b_PB[3]]
                LA = 3
                o_ps2 = [PB[4], PB[5]]
                b_o2 = [b_PB[4], b_PB[5]]
                bc_ps, b_bcps = PB[6], b_PB[6]

                prefetch_R()
                for s in range(NSB):
                    c_lo, c_hi = s * SB, (s + 1) * SB
                    front(st, s)
                    ci = s % 2

                    def ev_ff(pp, bp):
                        S.op("act", lambda e: e.activation(out=fl[:], in_=pp[0:8, :], func=AF.Exp, bias=nfb[:, 0:1], scale=-1.0),
                             reads=[bp, b_nfb], writes=[b_fl])
                        S.op("act", lambda e: e.activation(out=fl[:], in_=fl[:], func=AF.Ln, bias=1.0, scale=1.0),
                             reads=[b_fl], writes=[b_fl])
                        S.op("dve", lambda e: e.tensor_tensor_scan(out=cneg[ci][:], data0=ones8[:], data1=fl[:], initial=0.0,
                                                                   op0=ALU.mult, op1=ALU.add),
                             reads=[b_fl, b_ones8], writes=[b_cneg[ci]])
                        if s > 0:
                            S.op("dve", lambda e: e.tensor_scalar(out=cneg[ci][:], in0=cneg[ci][:],
                                                                  scalar1=cneg[1 - ci][:, SB - 1:SB], scalar2=None, op0=ALU.add),
                                 reads=[b_cneg[ci], b_cneg[1 - ci]], writes=[b_cneg[ci]])
                        S.op("dve", lambda e: e.tensor_copy(out=ksp[:, 0, :], in_=cneg[ci][:]), reads=[b_cneg[ci]], writes=[b_ksp])
                        S.op("dve", lambda e: e.tensor_tensor(out=res1[:], in0=cneg[ci][:], in1=ksp[:, 0, :], op=ALU.subtract),
                             reads=[b_cneg[ci], b_ksp], writes=[b_res1])
                        S.op("dve", lambda e: e.tensor_copy(out=ksp[:, 1, :], in_=res1[:]), reads=[b_res1], writes=[b_ksp])
                        S.op("dve", lambda e: e.tensor_tensor(out=res1[:], in0=res1[:], in1=ksp[:, 1, :], op=ALU.subtract),
                             reads=[b_res1, b_ksp], writes=[b_res1])
                        S.op("dve", lambda e: e.tensor_copy(out=ksp[:, 2, :], in_=res1[:]), reads=[b_res1], writes=[b_ksp])
                        S.op("dve", lambda e: e.tensor_scalar(out=qsp[:], in0=ksp[:], scalar1=-1.0, scalar2=None, op0=ALU.mult),
                             reads=[b_ksp], writes=[b_qsp])
                        S.dma("sp", scr_k[:, :, c_lo:c_hi].rearrange("r h t -> h r t"), ksp[:], reads=[b_ksp], writes=[b_scrk])
                        S.dma("sp", scr_q[:, :, c_lo:c_hi].rearrange("r h t -> h r t"), qsp[:], reads=[b_qsp], writes=[b_scrq])
                        S.dma("sp", KT[67:70, :, c_lo:c_hi], scr_k[:, :, c_lo:c_hi], reads=[b_scrk], writes=[b_KT[s]])
                        S.dma("sp", QT[64:67, :, :], scr_q[:, :, c_lo:c_hi], reads=[b_scrq], writes=[b_QT])

                    proj_fm(w_bf, b_w, st, 1536, 8, ev_ff)
                    for h in range(NH):
                        def ev_q(pp, bp, h=h):
                            S.op("act", lambda e: e.mul(out=QT[0:64, h, :], in_=pp[0:64, :], mul=0.125), reads=[bp], writes=[b_QT])
                        proj_fm(w_bf, b_w, st, h * 64, 64, ev_q)

                        def ev_k(pp, bp, h=h):
                            S.op("dve", lambda e: e.tensor_copy(out=KT[0:64, h, c_lo:c_hi], in_=pp[0:64, :]), reads=[bp], writes=[b_KT[s]])
                        proj_fm(w_bf, b_w, st, 512 + h * 64, 64, ev_k)
                    for j in range(4):
                        pi = pjc[0] % 2
                        pjc[0] += 1
                        for kc in range(KC):
                            S.op("pe", lambda e, kc=kc, j=j, pi=pi: e.matmul(out=PB[pi][:], lhsT=uT[:, kc, j * 128:(j + 1) * 128],
                                                                             rhs=w_bf[:, kc, 1024:1536], start=(kc == 0), stop=(kc == KC - 1)),
                                 reads=[b_w[kc], b_uT], writes=[b_PB[pi]])
                        S.op("act", lambda e, j=j, pi=pi: e.copy(out=VT[:, s * 4 + j, :, 0:64],
                                                                 in_=PB[pi][:].rearrange("p (h d) -> p h d", h=NH)),
                             reads=[b_PB[pi]], writes=[b_VT[s]])

                    tiles = []
                    nkb = 4 * (s + 1)
                    for h in range(NH):
                        for kb in range(nkb):
                            d = kb - 4 * s
                            tiles.append((h, kb, 0 if d < 0 else d * 128, d >= 0, len(tiles)))

                    def emit_qk(tl):
                        h, kb, q0, diag, idx = tl
                        si_ = idx % 4
                        S.op("pe", lambda e: e.matmul(out=st_ps[si_][:, q0:SB], lhsT=KT[0:70, h, kb * 128:(kb + 1) * 128],
                                                      rhs=QT[0:70, h, q0:SB], start=True, stop=(not diag)),
                             reads=[b_KT[kb // 4], b_QT], writes=[b_st[si_]])
                        if diag:
                            S.op("pe", lambda e: e.matmul(out=st_ps[si_][:, q0:q0 + 128], lhsT=ident_bf[:], rhs=maskneg[:],
                                                          start=False, stop=True),
                                 reads=[b_ident, b_mask], writes=[b_st[si_]])

                    for tl in tiles[0:LA]:
                        emit_qk(tl)
                    for ti, tl in enumerate(tiles):
                        h, kb, q0, diag, idx = tl
                        si_ = idx % 4
                        pi_ = idx % 4
                        oi_ = h % 2
                        if ti + LA < len(tiles):
                            emit_qk(tiles[ti + LA])
                        S.op("act", lambda e: e.activation(out=PT[pi_][:, q0:SB], in_=st_ps[si_][:, q0:SB], func=AF.Exp),
                             reads=[b_st[si_]], writes=[b_PT[pi_]])
                        S.op("pe", lambda e: e.matmul(out=o_ps2[oi_][0:66, q0:SB], lhsT=VT[:, kb, h, :], rhs=PT[pi_][:, q0:SB],
                                                      start=(kb == 0), stop=(kb == nkb - 1)),
                             reads=[b_VT[kb // 4], b_PT[pi_]], writes=[b_o2[oi_]])
                        if kb != nkb - 1:
                            continue
                        o_ps, b_o = o_ps2[oi_], b_o2[oi_]
                        S.op("dve", lambda e: e.reciprocal(out=rs[64:66, :], in_=o_ps[64:66, :]), reads=[b_o], writes=[b_rs])
                        S.op("pe", lambda e: e.matmul(out=bc_ps[0:64, :], lhsT=ones_f[64:65, 0:64], rhs=rs[64:65, :], start=True, stop=True),
                             reads=[b_rs, b_onesf], writes=[b_bcps])
                        S.op("act", lambda e: e.copy(out=bc_sb[:], in_=bc_ps[0:64, :]), reads=[b_bcps], writes=[b_bc])
                        fi = h % 2
                        S.op("dve", lambda e: e.tensor_tensor(out=catF[fi][:], in0=o_ps[0:64, :], in1=bc_sb[:], op=ALU.mult),
                             reads=[b_o, b_bc], writes=[b_catF[fi]])
                        S.dma("sp", cat_scr[h * 64:(h + 1) * 64, c_lo:c_hi], catF[fi][:], reads=[b_catF[fi]])
                        if dbg and "ofox" in dbg:
                            S.op("act", lambda e, fi=fi: e.copy(out=dbg_sb[0:64, :], in_=catF[fi][:]), reads=[b_catF[fi]], writes=[b_dbg])
                            S.dma("sp", dbg_aps["ofox"][h * 64:(h + 1) * 64, c_lo:c_hi], dbg_sb[0:64, :], reads=[b_dbg])
                S.barrier()
        if "R" in phases:
            with ExitStack() as es:
                def sb(name, shape, dt=F32):
                    return es.enter_context(nc.sbuf_tensor(uniq(name), list(shape), dt))
                st = make_front(es)
                uT, b_uT = st["uT"], st["b_uT"]
                lo_bf = sb("lo_bf", [128, RW], BF16)
                b_lo = Buf()
                gup_bf = sb("gup_bf", [128, RW], BF16)
                gup1_bf = sb("gup1_bf", [32, RW], BF16)
                b_gup = Buf()
                xt, b_xt = st["xt"], st["b_xt"]
                S.dma("sp", xt[0][0:64, 0:RW], rwkv_w_up[:, :], writes=[b_xt[0]])
                S.dma("sp", xt[0][64:128, 0:RW], rwkv_a_up[:, :], writes=[b_xt[0]])
                S.op("dve", lambda e: e.tensor_copy(out=lo_bf[:], in_=xt[0][:, 0:RW]), reads=[b_xt[0]], writes=[b_lo])
                S.dma("sp", xt[1][:, 0:RW], rwkv_g_up[0:128, :], writes=[b_xt[1]])
                S.op("dve", lambda e: e.tensor_copy(out=gup_bf[:], in_=xt[1][:, 0:RW]), reads=[b_xt[1]], writes=[b_gup])
                S.dma("sp", xt[0][0:32, 0:RW], rwkv_g_up[128:160, :], reads=[b_lo], writes=[b_xt[0]])
                S.op("dve", lambda e: e.tensor_copy(out=gup1_bf[:], in_=xt[0][0:32, 0:RW]), reads=[b_xt[0]], writes=[b_gup])
                w0T, b_w0 = load_colvec(es, "w0T", rwkv_w0, 4)
                a0T, b_a0 = load_colvec(es, "a0T", rwkv_a0, 4)
                kkT, b_kkv = load_colvec(es, "kkT", rwkv_k_k, 4)
                kaT, b_kav = load_colvec(es, "kaT", rwkv_k_a, 4)
                rkT, b_rkv = load_colvec(es, "rkT", rwkv_r_k, 4)
                lnwT, b_lnw = load_colvec(es, "lnwT", rwkv_ln_w, 4)
                lnbT, b_lnb = load_colvec(es, "lnbT", rwkv_ln_b, 4)
                groups = {}
                gl = []
                for hp in range(4):
                    gl.append((("r", hp), hp * 128, 128))
                    gl.append((("k", hp), 512 + hp * 128, 128))
                    gl.append((("v", hp), 1024 + hp * 128, 128))
                gl.append(("wa", 1536, 128))
                gl.append(("g0", 1664, 128))
                gl.append(("g1", 1792, 32))
                muT = sb("muT", [128, len(gl)], F32)
                b_mu = Buf()
                carry = sb("carry", [128, len(gl)], F32)
                b_carry = [Buf() for _ in gl]
                S.op("pool", lambda e: e.memset(carry[:], 0.0), writes=b_carry)
                for gi, (nm, c0, n) in enumerate(gl):
                    groups[nm] = (gi, c0, n)
                    S.dma("sp", muT[0:n, gi:gi + 1], shift_mu[c0:c0 + n].rearrange("(p o) -> p o", o=1), writes=[b_mu])
                cm = sb("cm", [128, NMASK, 128], F32)
                b_cm = Buf()
                S.dma("sp", cm[:], cmask[:, :, :], writes=[b_cm])
                mk0T_bf = sb("mk0T_bf", [128, 128], BF16)
                S.op("dve", lambda e: e.tensor_copy(out=mk0T_bf[:], in_=cm[:, 5, :]), reads=[b_cm], writes=[b_cm])
                bd_mean = sb("bd_mean", [128, 128], F32)
                S.op("dve", lambda e: e.tensor_scalar(out=bd_mean[:], in0=cm[:, 12, :], scalar1=1.0 / 64, scalar2=None, op0=ALU.mult),
                     reads=[b_cm], writes=[b_cm])
                ones128 = sb("ones128", [128, 128], F32)
                S.op("pool", lambda e: e.memset(ones128[:], 1.0), writes=[b_cm])

                H32 = [sb("H32_%d" % i, [128, 128], F32) for i in range(4)]
                Hbf = [sb("Hbf_%d" % i, [128, 128], BF16) for i in range(4)]
                b_H32 = [Buf() for _ in range(4)]
                b_Hbf = [Buf() for _ in range(4)]
                HbfB = [sb("HbfB_%d" % i, [128, 128], BF16) for i in range(4)]
                b_HbfB = [Buf() for _ in range(4)]
                Hbf2 = [[Hbf[i], HbfB[i]] for i in range(4)]
                b_Hbf2 = [[b_Hbf[i], b_HbfB[i]] for i in range(4)]
                for i in range(4):
                    S.op("pool", lambda e, i=i: e.memset(H32[i][:], 0.0), writes=[b_H32[i]])
                    S.op("pool", lambda e, i=i: e.memset(Hbf[i][:], 0.0), writes=[b_Hbf[i]])
                    S.op("pool", lambda e, i=i: e.memset(HbfB[i][:], 0.0), writes=[b_HbfB[i]])

                def wt(name, dt=F32, n=SB):
                    return sb(name, [128, n], dt), Buf()
                raw = [sb("raw%d" % i, [128, SB + 1], F32) for i in range(2)]
                b_raw = [Buf(), Buf()]
                rawc = [0]
                dlt, b_dlt = wt("dlt")
                wa_s, b_was = wt("wa_s")
                g0_s, b_g0s = wt("g0_s")
                g1_s, b_g1s = wt("g1_s")
                lat_bf, b_lat = wt("lat_bf", BF16)
                sg_bf, b_sg = wt("sg_bf", BF16)
                sg1_bf, b_sg1 = wt("sg1_bf", BF16)
                r_s, b_rs_ = wt("r_s")
                k_s, b_ks = wt("k_s")
                v_s, b_vs = wt("v_s")
                lw, b_lw = wt("lw")
                a_t, b_a = wt("a_t")
                g_t, b_g = wt("g_t")
                kq, b_kq = wt("kq")
                tmpA, b_tmpA = wt("tmpA")
                kk, b_kk = wt("kk")
                kmod, b_kmod = wt("kmod")
                bb, b_bb = wt("bb")
                bonus, b_bonus = wt("bonus")
                cum, b_cum = wt("cum")
                E_in, b_Ein = wt("E_in")
                E_neg, b_Eneg = wt("E_neg")
                E_ex, b_Eex = wt("E_ex")
                E_end, b_Eend = wt("E_end")
                y_sb, b_y = wt("y_sb")
                AR = sb("AR", [128, 4, 2, 128], BF16)
                b_AR = Buf()
                BT, b_BT = wt("BT", BF16)
                KTt, b_KTt = wt("KTt", BF16)
                BH, b_BH = wt("BH", BF16)
                KH, b_KH = wt("KH", BF16)
                Vb, b_Vb = wt("Vb", BF16)
                catR, b_catR = wt("catR", BF16)
                TM = [sb("TM%d" % i, [128, 4, 128], BF16) for i in range(4)]
                b_TM = [Buf() for _ in range(4)]
                NMt = [sb("NM%d" % i, [128, 4, 128], BF16) for i in range(4)]
                b_NM = [Buf() for _ in range(4)]
                Dm = [sb("Dm%d" % i, [128, 128], BF16) for i in range(4)]
                b_Dm = [Buf() for _ in range(4)]
                DTm = [sb("DTm%d" % i, [128, 128], BF16) for i in range(4)]
                b_DTm = [Buf() for _ in range(4)]
                Gm = [sb("Gm%d" % i, [128, 128], BF16) for i in range(4)]
                b_Gm = [Buf() for _ in range(4)]
                X2b = [sb("X2b%d" % i, [128, 64], BF16) for i in range(4)]
                b_X2b = [Buf() for _ in range(4)]
                U2s = [sb("U2s%d" % i, [128, 128], F32) for i in range(2)]
                b_U2s = [Buf(), Buf()]
                WTb = [sb("WTb%d" % i, [128, 128], BF16) for i in range(2)]
                b_WTb = [Buf(), Buf()]
                Ub = sb("Ub", [128, 128], BF16)
                b_Ub = Buf()
                dbg_sb = sb("dbg_sbr", [128, SB], F32)
                b_dbg = Buf()
                M1 = [PB[4], PB[5]]
                b_M1 = [b_PB[4], b_PB[5]]
                sA = [PB[i][:, 0:128] for i in range(4)]
                b_sA = [b_PB[i] for i in range(4)]
                sB = [PB[i][:, 128:256] for i in range(4)]
                b_sB = [b_PB[i] for i in range(4)]
                u2_ps, uh_ps = [PB[6][:, i * 128:(i + 1) * 128] for i in range(2)]
                y2_ps = [PB[6][:, (2 + i) * 128:(3 + i) * 128] for i in range(2)]
                b_u2 = b_wt = b_uh = b_yps = b_PB[6]

                def shifted(nm, dst, b_dst):
                    gi, c0, n = groups[nm]

                    def ev(pp, bp):
                        ri = rawc[0] % 2
                        rawc[0] += 1
                        rw_, brw = raw[ri], b_raw[ri]
                        S.op("act", lambda e: e.copy(out=rw_[0:n, 1:SB + 1], in_=pp[0:n, :]), reads=[bp], writes=[brw])
                        S.op("pool", lambda e: e.tensor_copy(out=rw_[0:n, 0:1], in_=carry[0:n, gi:gi + 1]),
                             reads=[b_carry[gi]], writes=[brw])
                        S.op("pool", lambda e: e.tensor_copy(out=carry[0:n, gi:gi + 1], in_=rw_[0:n, SB:SB + 1]),
                             reads=[brw], writes=[b_carry[gi]])
                        S.op("dve", lambda e: e.tensor_tensor(out=dlt[0:n, :], in0=rw_[0:n, 0:SB], in1=rw_[0:n, 1:SB + 1], op=ALU.subtract),
                             reads=[brw], writes=[b_dlt])
                        S.op("dve", lambda e: e.scalar_tensor_tensor(out=dst[0:n, :], in0=dlt[0:n, :], scalar=muT[0:n, gi:gi + 1],
                                                                     in1=rw_[0:n, 1:SB + 1], op0=ALU.mult, op1=ALU.add),
                             reads=[b_dlt, brw, b_mu], writes=[b_dst])
                    proj_fm(wr_bf, b_wr, st, c0, n, ev)

                def v4(ap):
                    return ap.rearrange("p (c t) -> p c t", c=4)

                AR_b = sb("AR_b", [128, 4, 2, 128], BF16)
                BT_b = sb("BT_b", [128, SB], BF16)
                KTt_b = sb("KTt_b", [128, SB], BF16)
                BH_b = sb("BH_b", [128, SB], BF16)
                KH_b = sb("KH_b", [128, SB], BF16)
                Vb_b = sb("Vb_b", [128, SB], BF16)
                E_in_b = sb("E_in_b", [128, SB], F32)
                bonus_b = sb("bonus_b", [128, SB], F32)
                g_t_b = sb("g_t_b", [128, SB], F32)
                AR_2 = [AR, AR_b]
                b_AR_2 = [b_AR, Buf()]
                BT_2 = [BT, BT_b]
                b_BT_2 = [b_BT, Buf()]
                KTt_2 = [KTt, KTt_b]
                b_KTt_2 = [b_KTt, Buf()]
                BH_2 = [BH, BH_b]
                b_BH_2 = [b_BH, Buf()]
                KH_2 = [KH, KH_b]
                b_KH_2 = [b_KH, Buf()]
                Vb_2 = [Vb, Vb_b]
                b_Vb_2 = [b_Vb, Buf()]
                E_in_2 = [E_in, E_in_b]
                b_Ein_2 = [b_Ein, Buf()]
                bonus_2 = [bonus, bonus_b]
                b_bonus_2 = [b_bonus, Buf()]
                g_t_2 = [g_t, g_t_b]
                b_g_2 = [b_g, Buf()]
                tmpB, b_tmpB = wt("tmpB")
                M1 = [PB[2], PB[3], PB[4], PB[5]]
                b_M1 = [b_PB[2], b_PB[3], b_PB[4], b_PB[5]]
                sA = [PB[2 + i][:, 0:128] for i in range(4)]
                b_sA = [b_PB[2 + i] for i in range(4)]
                sB = [PB[2 + i][:, 128:256] for i in range(4)]
                b_sB = [b_PB[2 + i] for i in range(4)]

                def gen_front(s, hp, pb):
                    c_lo, c_hi = s * SB, (s + 1) * SB
                    AR, b_AR = AR_2[pb], b_AR_2[pb]
                    BT, b_BT = BT_2[pb], b_BT_2[pb]
                    KTt, b_KTt = KTt_2[pb], b_KTt_2[pb]
                    BH, b_BH = BH_2[pb], b_BH_2[pb]
                    KH, b_KH = KH_2[pb], b_KH_2[pb]
                    Vb, b_Vb = Vb_2[pb], b_Vb_2[pb]
                    E_in, b_Ein = E_in_2[pb], b_Ein_2[pb]
                    bonus, b_bonus = bonus_2[pb], b_bonus_2[pb]
                    g_t, b_g = g_t_2[pb], b_g_2[pb]
                    if hp == 0:
                        front(st, s)
                        shifted("wa", wa_s, b_was)
                        shifted("g0", g0_s, b_g0s)
                        shifted("g1", g1_s, b_g1s)
                        S.op("act", lambda e: e.activation(out=lat_bf[0:64, :], in_=wa_s[0:64, :], func=AF.Tanh), reads=[b_was], writes=[b_lat])
                        S.op("act", lambda e: e.copy(out=lat_bf[64:128, :], in_=wa_s[64:128, :]), reads=[b_was], writes=[b_lat])
                        S.op("act", lambda e: e.activation(out=sg_bf[:], in_=g0_s[:], func=AF.Sigmoid), reads=[b_g0s], writes=[b_sg])
                        S.op("act", lambda e: e.activation(out=sg1_bf[0:32, :], in_=g1_s[0:32, :], func=AF.Sigmoid), reads=[b_g1s], writes=[b_sg1])
                        yield
                    hc = slice(hp * 128, (hp + 1) * 128)
                    shifted(("r", hp), r_s, b_rs_)
                    shifted(("k", hp), k_s, b_ks)
                    yield
                    shifted(("v", hp), v_s, b_vs)
                    p0, bp0 = PB[0], b_PB[0]
                    S.op("pe", lambda e: e.matmul(out=p0[:], lhsT=lo_bf[0:64, hc], rhs=lat_bf[0:64, :], start=True, stop=True),
                         reads=[b_lo, b_lat], writes=[bp0])
                    yield
                    S.op("act", lambda e: e.activation(out=lw[:], in_=p0[:], func=AF.Sigmoid, bias=w0T[:, hp:hp + 1]),
                         reads=[bp0, b_w0], writes=[b_lw])
                    S.op("pool", lambda e: e.tensor_scalar(out=lw[:], in0=lw[:], scalar1=-0.6065306597126334, scalar2=None, op0=ALU.mult),
                         reads=[b_lw], writes=[b_lw])
                    p1, bp1 = PB[1], b_PB[1]
                    yield
                    S.op("pe", lambda e: e.matmul(out=p1[:], lhsT=lo_bf[64:128, hc], rhs=lat_bf[64:128, :], start=True, stop=True),
                         reads=[b_lo, b_lat], writes=[bp1])
                    S.op("act", lambda e: e.activation(out=a_t[:], in_=p1[:], func=AF.Sigmoid, bias=a0T[:, hp:hp + 1]),
                         reads=[bp1, b_a0], writes=[b_a])
                    S.op("pe", lambda e: e.matmul(out=p0[:], lhsT=gup_bf[:, hc], rhs=sg_bf[:], start=True, stop=False),
                         reads=[b_gup, b_sg], writes=[bp0])
                    yield
                    S.op("pe", lambda e: e.matmul(out=p0[:], lhsT=gup1_bf[0:32, hc], rhs=sg1_bf[0:32, :], start=False, stop=True),
                         reads=[b_gup, b_sg1], writes=[bp0])
                    S.op("act", lambda e: e.copy(out=g_t[:], in_=p0[:]), reads=[bp0], writes=[b_g])
                    S.op("dve", lambda e: e.tensor_scalar(out=kq[:], in0=k_s[:], scalar1=kkT[:, hp:hp + 1], scalar2=None, op0=ALU.mult),
                         reads=[b_ks, b_kkv], writes=[b_kq])
                    yield
                    S.op("pool", lambda e: e.tensor_tensor(out=tmpA[:], in0=kq[:], in1=kq[:], op=ALU.mult), reads=[b_kq], writes=[b_tmpA])
                    S.op("pe", lambda e: e.matmul(out=p1[:], lhsT=cm[:, 12, :], rhs=tmpA[:], start=True, stop=True),
                         reads=[b_cm, b_tmpA], writes=[bp1])
                    S.op("act", lambda e: e.sqrt(out=tmpA[:], in_=p1[:]), reads=[bp1], writes=[b_tmpA])
                    yield
                    S.op("dve", lambda e: e.tensor_scalar(out=tmpA[:], in0=tmpA[:], scalar1=1e-12, scalar2=None, op0=ALU.max),
                         reads=[b_tmpA], writes=[b_tmpA])
                    S.op("dve", lambda e: e.reciprocal(out=tmpA[:], in_=tmpA[:]), reads=[b_tmpA], writes=[b_tmpA])
                    S.op("pool", lambda e: e.tensor_tensor(out=kk[:], in0=kq[:], in1=tmpA[:], op=ALU.mult),
                         reads=[b_kq, b_tmpA], writes=[b_kk])
                    yield
                    S.op("dve", lambda e: e.tensor_scalar(out=kmod[:], in0=a_t[:], scalar1=-1.0, scalar2=kaT[:, hp:hp + 1],
                                                          op0=ALU.add, op1=ALU.mult), reads=[b_a, b_kav], writes=[b_kmod])
                    S.op("dve", lambda e: e.scalar_tensor_tensor(out=kmod[:], in0=kmod[:], scalar=1.0, in1=k_s[:],
                                                                 op0=ALU.add, op1=ALU.mult), reads=[b_kmod, b_ks], writes=[b_kmod])
                    S.op("pool", lambda e: e.tensor_tensor(out=bb[:], in0=kk[:], in1=a_t[:], op=ALU.mult), reads=[b_kk, b_a], writes=[b_bb])
                    yield
                    S.op("dve", lambda e: e.scalar_tensor_tensor(out=tmpA[:], in0=r_s[:], scalar=rkT[:, hp:hp + 1], in1=kmod[:],
                                                                 op0=ALU.mult, op1=ALU.mult), reads=[b_rs_, b_rkv, b_kmod], writes=[b_tmpA])
                    S.op("pe", lambda e: e.matmul(out=p1[:], lhsT=cm[:, 12, :], rhs=tmpA[:], start=True, stop=True),
                         reads=[b_cm, b_tmpA], writes=[bp1])
                    S.op("dve", lambda e: e.tensor_tensor(out=bonus[:], in0=p1[:], in1=v_s[:], op=ALU.mult), reads=[bp1, b_vs], writes=[b_bonus])
                    yield
                    for c in range(4):
                        cc = slice(c * 128, (c + 1) * 128)
                        S.op("dve", lambda e, cc=cc: e.tensor_tensor_scan(out=cum[:, cc], data0=ones128[:], data1=lw[:, cc], initial=0.0,
                                                                          op0=ALU.mult, op1=ALU.add), reads=[b_lw, b_cm], writes=[b_cum])
                    S.op("act", lambda e: e.activation(out=E_in[:], in_=cum[:], func=AF.Exp), reads=[b_cum], writes=[b_Ein])
                    S.op("act", lambda e: e.activation(out=E_neg[:], in_=cum[:], func=AF.Exp, scale=-1.0), reads=[b_cum], writes=[b_Eneg])
                    yield
                    for c in range(4):
                        cc = slice(c * 128, (c + 1) * 128)
                        S.op("act", lambda e, cc=cc, c=c: e.activation(out=E_end[:, cc], in_=cum[:, cc], func=AF.Exp, scale=-1.0,
                                                                       bias=cum[:, c * 128 + 127:c * 128 + 128]),
                             reads=[b_cum], writes=[b_Eend])
                    S.op("pool", lambda e: e.tensor_tensor(out=tmpA[:], in0=cum[:], in1=lw[:], op=ALU.subtract), reads=[b_cum, b_lw], writes=[b_tmpA])
                    S.op("act", lambda e: e.activation(out=E_ex[:], in_=tmpA[:], func=AF.Exp), reads=[b_tmpA], writes=[b_Eex])
                    yield
                    S.op("dve", lambda e: e.scalar_tensor_tensor(out=AR[:, :, 0, :], in0=v4(kk[:]), scalar=-1.0, in1=v4(E_ex[:]),
                                                                 op0=ALU.mult, op1=ALU.mult), reads=[b_kk, b_Eex], writes=[b_AR])
                    S.op("pool", lambda e: e.tensor_tensor(out=AR[:, :, 1, :], in0=v4(r_s[:]), in1=v4(E_in[:]), op=ALU.mult),
                         reads=[b_rs_, b_Ein], writes=[b_AR])
                    S.op("dve", lambda e: e.tensor_tensor(out=BT[:], in0=bb[:], in1=E_neg[:], op=ALU.mult), reads=[b_bb, b_Eneg], writes=[b_BT])
                    yield
                    S.op("pool", lambda e: e.tensor_tensor(out=KTt[:], in0=kmod[:], in1=E_neg[:], op=ALU.mult), reads=[b_kmod, b_Eneg], writes=[b_KTt])
                    S.op("dve", lambda e: e.tensor_tensor(out=BH[:], in0=bb[:], in1=E_end[:], op=ALU.mult), reads=[b_bb, b_Eend], writes=[b_BH])
                    S.op("pool", lambda e: e.tensor_tensor(out=KH[:], in0=kmod[:], in1=E_end[:], op=ALU.mult), reads=[b_kmod, b_Eend], writes=[b_KH])
                    yield
                    S.op("act", lambda e: e.copy(out=Vb[:], in_=v_s[:]), reads=[b_vs], writes=[b_Vb])

                    yield

                def gen_chunks(s, hp, pb):
                    c_lo, c_hi = s * SB, (s + 1) * SB
                    AR, b_AR = AR_2[pb], b_AR_2[pb]
                    BT, b_BT = BT_2[pb], b_BT_2[pb]
                    KTt, b_KTt = KTt_2[pb], b_KTt_2[pb]
                    BH, b_BH = BH_2[pb], b_BH_2[pb]
                    KH, b_KH = KH_2[pb], b_KH_2[pb]
                    Vb, b_Vb = Vb_2[pb], b_Vb_2[pb]
                    E_in, b_Ein = E_in_2[pb], b_Ein_2[pb]
                    bonus, b_bonus = bonus_2[pb], b_bonus_2[pb]
                    g_t, b_g = g_t_2[pb], b_g_2[pb]
                    hc = slice(hp * 128, (hp + 1) * 128)
                    p0, bp0 = PB[0], b_PB[0]
                    p1, bp1 = PB[1], b_PB[1]
                    stop = (dbg or {}).get("stop", "")
                    if stop == "A":
                        return
                    for cp in range(2):
                        chains = [(2 * cp + ci_, e_) for ci_ in range(2) for e_ in range(2)]
                        for ci_ in range(2):
                            c = 2 * cp + ci_
                            cc = slice(c * 128, (c + 1) * 128)
                            srcs = [(AR[:, c, 0, :], b_AR), (Vb[:, cc], b_Vb), (BH[:, cc], b_BH), (KH[:, cc], b_KH)]
                            for k_, (src, bsrc) in enumerate(srcs):
                                S.op("pe", lambda e, k_=k_, src=src: e.transpose(out=tp_ps[:, k_, :], in_=src, identity=ident_bf[:]),
                                     reads=[bsrc, b_ident], writes=[b_tp])
                            S.op("act", lambda e, c=c: e.copy(out=TM[c][:], in_=tp_ps[:, 0:4, :]), reads=[b_tp], writes=[b_TM[c]])
                        yield
                        for ch, (c, e_) in enumerate(chains):
                            pr = slice(e_ * 64, (e_ + 1) * 64)
                            cc = slice(c * 128, (c + 1) * 128)
                            m1, bm1 = M1[ch], b_M1[ch]
                            S.op("pe", lambda e, pr=pr, cc=cc, c=c, m1=m1: e.matmul(out=m1[:, 0:256], lhsT=BT[pr, cc],
                                                                                 rhs=AR[pr, c, :, :], start=True, stop=True),
                                 reads=[b_BT, b_AR], writes=[bm1])
                            S.op("pe", lambda e, pr=pr, cc=cc, c=c, m1=m1: e.matmul(out=m1[:, 256:512], lhsT=KTt[pr, cc],
                                                                                 rhs=AR[pr, c, :, :], start=True, stop=True),
                                 reads=[b_KTt, b_AR], writes=[bm1])
                            S.op("dve", lambda e, ch=ch, m1=m1: e.tensor_tensor(
                                out=NMt[ch][:], in0=m1[:].rearrange("p (a t) -> p a t", a=4),
                                in1=cm[:, 0:4, :],
                                op=ALU.mult), reads=[bm1, b_cm], writes=[b_NM[ch]])
                        for ch, (c, e_) in enumerate(chains):
                            pr = slice(e_ * 64, (e_ + 1) * 64)
                            cc = slice(c * 128, (c + 1) * 128)
                            S.op("pe", lambda e, pr=pr, cc=cc, c=c, ch=ch: e.matmul(out=sA[ch], lhsT=AR[pr, c, 0, :], rhs=BT[pr, cc],
                                                                                 start=True, stop=True),
                                 reads=[b_AR, b_BT], writes=[b_sA[ch]])
                            S.op("dve", lambda e, ch=ch: e.tensor_tensor(out=Dm[ch][:], in0=sA[ch], in1=cm[:, 4, :], op=ALU.mult),
                                 reads=[b_sA[ch], b_cm], writes=[b_Dm[ch]])
                            S.op("pool", lambda e, ch=ch: e.tensor_tensor(out=Dm[ch][:], in0=Dm[ch][:], in1=ident_bf[:], op=ALU.add),
                                 reads=[b_Dm[ch], b_ident], writes=[b_Dm[ch]])
                            S.op("pool", lambda e, ch=ch: e.tensor_tensor(out=DTm[ch][:], in0=NMt[ch][:, 0, :], in1=mk0T_bf[:], op=ALU.mult),
                                 reads=[b_NM[ch], b_cm], writes=[b_DTm[ch]])
                            S.op("pool", lambda e, ch=ch: e.tensor_tensor(out=DTm[ch][:], in0=DTm[ch][:], in1=ident_bf[:], op=ALU.add),
                                 reads=[b_DTm[ch], b_ident], writes=[b_DTm[ch]])
                        if stop == "B":
                            continue
                        yield
                        for li, mm in enumerate(LEVELS):
                            last = (li == len(LEVELS) - 1)
                            yield
                            for ch in range(4):
                                S.op("pe", lambda e, ch=ch: e.matmul(out=sA[ch], lhsT=NMt[ch][:, 0, :], rhs=Dm[ch][:], start=True, stop=True),
                                     reads=[b_NM[ch], b_Dm[ch]], writes=[b_sA[ch]])
                                S.op("dve", lambda e, ch=ch, li=li: e.tensor_tensor(out=Gm[ch][:], in0=sA[ch], in1=cm[:, 6 + li, :], op=ALU.mult),
                                     reads=[b_sA[ch], b_cm], writes=[b_Gm[ch]])
                                S.op("pool", lambda e, ch=ch: e.tensor_tensor(out=Gm[ch][:], in0=Gm[ch][:], in1=ident_bf[:], op=ALU.add),
                                     reads=[b_Gm[ch], b_ident], writes=[b_Gm[ch]])
                            yield
                            for ch in range(4):
                                if not last:
                                    S.op("pe", lambda e, ch=ch: e.matmul(out=sA[ch], lhsT=DTm[ch][:], rhs=Gm[ch][:], start=True, stop=True),
                                         reads=[b_DTm[ch], b_Gm[ch]], writes=[b_sA[ch]])
                                S.op("pe", lambda e, ch=ch: e.matmul(out=sB[ch], lhsT=Gm[ch][:], rhs=DTm[ch][:], start=True, stop=True),
                                     reads=[b_DTm[ch], b_Gm[ch]], writes=[b_sB[ch]])
                                if not last:
                                    S.op("act", lambda e, ch=ch: e.copy(out=Dm[ch][:], in_=sA[ch]), reads=[b_sA[ch]], writes=[b_Dm[ch]])
                                S.op("act", lambda e, ch=ch: e.copy(out=DTm[ch][:], in_=sB[ch]), reads=[b_sB[ch]], writes=[b_DTm[ch]])
                        if stop == "C":
                            continue
                        yield
                        for ch, (c, e_) in enumerate(chains):
                            pr = slice(e_ * 64, (e_ + 1) * 64)
                            S.op("pe", lambda e, ch=ch, c=c, pr=pr: e.matmul(out=sA[ch][:, 0:64], lhsT=NMt[ch][:, 2, :], rhs=TM[c][:, 1, pr],
                                                                          start=True, stop=True),
                                 reads=[b_NM[ch], b_TM[c]], writes=[b_sA[ch]])
                            S.op("act", lambda e, ch=ch: e.copy(out=X2b[ch][:], in_=sA[ch][:, 0:64]), reads=[b_sA[ch]], writes=[b_X2b[ch]])
                        for ci_ in range(2):
                            c = 2 * cp + ci_
                            for e_ in range(2):
                                ch = ci_ * 2 + e_
                                pr = slice(e_ * 64, (e_ + 1) * 64)
                                S.op("pe", lambda e, ch=ch, pr=pr: e.matmul(out=u2_ps[:, pr], lhsT=DTm[ch][:], rhs=X2b[ch][:], start=True, stop=True),
                                     reads=[b_DTm[ch], b_X2b[ch]], writes=[b_u2])
                                S.op("pe", lambda e, ch=ch, c=c: e.matmul(out=sA[ch], lhsT=TM[c][:, 0, :], rhs=DTm[ch][:], start=True, stop=True),
                                     reads=[b_DTm[ch], b_TM[c]], writes=[b_sA[ch]])
                                S.op("dve", lambda e, ci_=ci_, ch=ch, pr=pr: e.tensor_copy(out=WTb[ci_][pr, :], in_=sA[ch][pr, :]),
                                     reads=[b_sA[ch]], writes=[b_WTb[ci_]])
                            S.op("act", lambda e, ci_=ci_: e.copy(out=U2s[ci_][:], in_=u2_ps), reads=[b_u2], writes=[b_U2s[ci_]])
                        if stop == "D":
                            continue
                        yield
                        for ci_ in range(2):
                            c = 2 * cp + ci_
                            cc = slice(c * 128, (c + 1) * 128)
                            yield
                            Ho, bHo = Hbf2[hp][c % 2], b_Hbf2[hp][c % 2]
                            Hn, bHn = Hbf2[hp][(c + 1) % 2], b_Hbf2[hp][(c + 1) % 2]
                            S.op("pe", lambda e, ci_=ci_: e.matmul(out=uh_ps, lhsT=WTb[ci_][:], rhs=Ho[:], start=True, stop=True),
                                 reads=[b_WTb[ci_], bHo], writes=[b_uh])
                            S.op("dve", lambda e, ci_=ci_: e.tensor_tensor(out=Ub[:], in0=uh_ps, in1=U2s[ci_][:], op=ALU.add),
                                 reads=[b_uh, b_U2s[ci_]], writes=[b_Ub])
                            S.op("pe", lambda e, c=c: e.matmul(out=uh_ps, lhsT=TM[c][:, 2, :], rhs=Ub[:], start=True, stop=False),
                                 reads=[b_TM[c], b_Ub], writes=[b_uh])
                            S.op("pe", lambda e, c=c: e.matmul(out=uh_ps, lhsT=TM[c][:, 3, :], rhs=TM[c][:, 1, :], start=False, stop=True),
                                 reads=[b_TM[c]], writes=[b_uh])
                            for e_ in range(2):
                                pr = slice(e_ * 64, (e_ + 1) * 64)
                                S.op("dve", lambda e, pr=pr, c=c: e.scalar_tensor_tensor(
                                    out=H32[hp][pr, pr], in0=H32[hp][pr, pr], scalar=E_in[pr, c * 128 + 127:c * 128 + 128],
                                    in1=uh_ps[pr, pr], op0=ALU.mult, op1=ALU.add),
                                    reads=[b_H32[hp], b_Ein, b_uh], writes=[b_H32[hp]])
                            S.op("dve", lambda e: e.tensor_copy(out=Hn[:], in_=H32[hp][:]), reads=[b_H32[hp]], writes=[bHn])
                            for e_ in range(2):
                                ch = ci_ * 2 + e_
                                pr = slice(e_ * 64, (e_ + 1) * 64)
                                yp = y2_ps[e_]
                                S.op("pe", lambda e, c=c, yp=yp: e.matmul(out=yp, lhsT=Ho[:], rhs=AR[:, c, 1, :], start=True, stop=False),
                                     reads=[bHo, b_AR], writes=[b_yps])
                                S.op("pe", lambda e, ch=ch, yp=yp: e.matmul(out=yp, lhsT=Ub[:], rhs=NMt[ch][:, 1, :], start=False, stop=False),
                                     reads=[b_Ub, b_NM[ch]], writes=[b_yps])
                                S.op("pe", lambda e, ch=ch, c=c, yp=yp: e.matmul(out=yp, lhsT=TM[c][:, 1, :], rhs=NMt[ch][:, 3, :], start=False, stop=True),
                                     reads=[b_TM[c], b_NM[ch]], writes=[b_yps])
                            for e_ in range(2):
                                pr = slice(e_ * 64, (e_ + 1) * 64)
                                S.op("act", lambda e, cc=cc, pr=pr, e_=e_: e.copy(out=y_sb[pr, cc], in_=y2_ps[e_][pr, :]), reads=[b_yps], writes=[b_y])
                    if dbg and "yraw" in dbg:
                        S.op("act", lambda e: e.copy(out=dbg_sb[:], in_=y_sb[:]), reads=[b_y], writes=[b_dbg])
                        S.dma("sp", dbg_aps["yraw"][hp * 128:(hp + 1) * 128, c_lo:c_hi], dbg_sb[:], reads=[b_dbg])
                    yield
                    S.op("pe", lambda e: e.matmul(out=p0[:], lhsT=bd_mean[:], rhs=y_sb[:], start=True, stop=True),
                         reads=[b_cm, b_y], writes=[bp0])
                    S.op("dve", lambda e: e.tensor_tensor(out=y_sb[:], in0=y_sb[:], in1=p0[:], op=ALU.subtract), reads=[b_y, bp0], writes=[b_y])
                    S.op("pool", lambda e: e.tensor_tensor(out=tmpB[:], in0=y_sb[:], in1=y_sb[:], op=ALU.mult), reads=[b_y], writes=[b_tmpB])
                    S.op("pe", lambda e: e.matmul(out=p1[:], lhsT=bd_mean[:], rhs=tmpB[:], start=True, stop=True),
                         reads=[b_cm, b_tmpB], writes=[bp1])
                    S.op("dve", lambda e: e.tensor_scalar(out=tmpB[:], in0=p1[:], scalar1=LNX_EPS, scalar2=None, op0=ALU.add),
                         reads=[bp1], writes=[b_tmpB])
                    S.op("act", lambda e: e.sqrt(out=tmpB[:], in_=tmpB[:]), reads=[b_tmpB], writes=[b_tmpB])
                    S.op("dve", lambda e: e.reciprocal(out=tmpB[:], in_=tmpB[:]), reads=[b_tmpB], writes=[b_tmpB])
                    S.op("pool", lambda e: e.tensor_tensor(out=y_sb[:], in0=y_sb[:], in1=tmpB[:], op=ALU.mult), reads=[b_y, b_tmpB], writes=[b_y])
                    S.op("dve", lambda e: e.tensor_scalar(out=y_sb[:], in0=y_sb[:], scalar1=lnwT[:, hp:hp + 1], scalar2=lnbT[:, hp:hp + 1],
                                                          op0=ALU.mult, op1=ALU.add), reads=[b_y, b_lnw, b_lnb], writes=[b_y])
                    S.op("pool", lambda e: e.tensor_tensor(out=y_sb[:], in0=y_sb[:], in1=bonus[:], op=ALU.add), reads=[b_y, b_bonus], writes=[b_y])
                    S.op("dve", lambda e: e.tensor_tensor(out=catR[:], in0=y_sb[:], in1=g_t[:], op=ALU.mult), reads=[b_y, b_g], writes=[b_catR])
                    S.dma("sp", cat_scr[512 + hp * 128:512 + (hp + 1) * 128, c_lo:c_hi], catR[:], reads=[b_catR])
                    if dbg and "orwkv" in dbg:
                        S.op("act", lambda e: e.copy(out=dbg_sb[:], in_=catR[:]), reads=[b_catR], writes=[b_dbg])
                        S.dma("sp", dbg_aps["orwkv"][hp * 128:(hp + 1) * 128, c_lo:c_hi], dbg_sb[:], reads=[b_dbg])
                    yield

                units = [(s_, hp_) for s_ in range(NSB) for hp_ in range(4)]
                for _ in gen_front(units[0][0], units[0][1], 0):
                    pass
                for ui, (s_, hp_) in enumerate(units):
                    gc = gen_chunks(s_, hp_, ui % 2)
                    gf = gen_front(units[ui + 1][0], units[ui + 1][1], (ui + 1) % 2) if ui + 1 < len(units) else iter(())
                    live = [gc, gf]
                    while live:
                        for g in list(live):
                            try:
                                next(g)
                            except StopIteration:
                                live.remove(g)
                S.barrier()
        esR.close()
        if "O" in phases:
            with ExitStack() as es:
                def sb(name, shape, dt=F32):
                    return es.enter_context(nc.sbuf_tensor(uniq(name), list(shape), dt))
                UT = 256
                NU = T // UT
                stg = [sb("stg%d" % i, [128, D], F32) for i in range(2)]
                b_stg = [Buf(), Buf()]
                st = {"xt": stg, "b_xt": b_stg}
                gfT, b_gfT = load_colvec(es, "gfT", gf_pre, KC)
                wout_bf = sb("wout_bf", [128, KC, D], BF16)
                b_wout = [Buf() for _ in range(KC)]
                wg_bf = sb("wg_bf", [128, KC, DFF], BF16)
                b_wg = [Buf() for _ in range(KC)]
                wu_bf = sb("wu_bf", [128, KC, DFF], BF16)
                b_wu = [Buf() for _ in range(KC)]
                wd_bf = sb("wd_bf", [128, NFF, D], BF16)
                b_wd = [Buf() for _ in range(NFF)]
                load_weight_bf(st, wout_bf, b_wout, w_out, 0, D, None, None)
                load_weight_bf(st, wg_bf, b_wg, w_gate, 0, DFF, gfT, b_gfT)
                load_weight_bf(st, wu_bf, b_wu, w_up, 0, DFF, gfT, b_gfT)
                load_weight_bf(st, wd_bf, b_wd, w_down, 0, D, None, None, nk=NFF)
                gpost_bc = sb("gpost_bc", [128, D], F32)
                gfpost_bc = sb("gfpost_bc", [128, D], F32)
                b_gbc = Buf()
                S.dma("sp", gpost_bc[:], g_post.partition_broadcast(128), writes=[b_gbc])
                S.dma("sp", gfpost_bc[:], gf_post.partition_broadcast(128), writes=[b_gbc])
                catT = sb("catT", [128, KC, UT], BF16)
                b_catT = Buf()
                zn = sb("zn", [128, D], BF16)
                b_zn = Buf()
                zT = sb("zT", [128, KC, UT], BF16)
                b_zT = Buf()
                aT = sb("aT", [128, NFF, UT], BF16)
                b_aT = Buf()
                junk = sb("junkO", [128, D], BF16)
                b_junk = Buf()
                sgt = [sb("sgt%d" % i, [128, UT], F32) for i in range(2)]
                b_sgt = [Buf(), Buf()]
                t1 = sb("t1", [128, D], F32)
                b_t1 = Buf()
                ssO = sb("ssO", [128, 4], F32)
                b_ssO = Buf()
                cat_v = cat_scr.rearrange("(k p) t -> p k t", p=128)

                def rstd_from(srcs, bsrcs):
                    for i, (ap_, b_) in enumerate(zip(srcs, bsrcs)):
                        n = ap_.shape[1]
                        S.op("act", lambda e, ap_=ap_, i=i, n=n: e.activation(out=junk[:, 0:n], in_=ap_, func=AF.Square, accum_out=ssO[:, i:i + 1]),
                             reads=[b_], writes=[b_junk, b_ssO])
                    if len(srcs) == 2:
                        S.op("dve", lambda e: e.tensor_tensor(out=ssO[:, 2:3], in0=ssO[:, 0:1], in1=ssO[:, 1:2], op=ALU.add),
                             reads=[b_ssO], writes=[b_ssO])
                        src = ssO[:, 2:3]
                    else:
                        src = ssO[:, 0:1]
                    S.op("dve", lambda e: e.tensor_scalar(out=ssO[:, 2:3], in0=src, scalar1=1.0 / D, scalar2=EPS, op0=ALU.mult, op1=ALU.add),
                         reads=[b_ssO], writes=[b_ssO])
                    S.op("act", lambda e: e.sqrt(out=ssO[:, 2:3], in_=ssO[:, 2:3]), reads=[b_ssO], writes=[b_ssO])
                    S.op("dve", lambda e: e.reciprocal(out=ssO[:, 2:3], in_=ssO[:, 2:3]), reads=[b_ssO], writes=[b_ssO])

                for u in range(NU):
                    t0 = u * UT
                    S.dma("sp", catT[:], cat_v[:, :, t0:t0 + UT], writes=[b_catT])
                    for j in range(2):
                        tj = t0 + j * 128
                        S.dma("sp", stg[j][:], x[tj:tj + 128, :], writes=[b_stg[j]])
                        for half in range(2):
                            for kc in range(KC):
                                S.op("pe", lambda e, kc=kc, half=half, j=j: e.matmul(
                                    out=PB[half][:], lhsT=catT[:, kc, j * 128:(j + 1) * 128], rhs=wout_bf[:, kc, half * 512:(half + 1) * 512],
                                    start=(kc == 0), stop=(kc == KC - 1)), reads=[b_catT, b_wout[kc]], writes=[b_PB[half]])
                        rstd_from([PB[0][:], PB[1][:]], [b_PB[0], b_PB[1]])
                        for half in range(2):
                            S.op("act", lambda e, half=half: e.activation(out=t1[:, half * 512:(half + 1) * 512], in_=PB[half][:], func=AF.Copy,
                                                                          scale=ssO[:, 2:3]), reads=[b_PB[half], b_ssO], writes=[b_t1])
                        S.op("pool", lambda e: e.tensor_tensor(out=t1[:], in0=t1[:], in1=gpost_bc[:], op=ALU.mult), reads=[b_t1, b_gbc], writes=[b_t1])
                        S.op("dve", lambda e, j=j: e.tensor_tensor(out=stg[j][:], in0=stg[j][:], in1=t1[:], op=ALU.add),
                             reads=[b_stg[j], b_t1], writes=[b_stg[j]])
                        if dbg and "h" in dbg:
                            S.dma("sp", dbg_aps["h"][tj:tj + 128, :], stg[j][:], reads=[b_stg[j]])
                        rstd_from([stg[j][:]], [b_stg[j]])
                        S.op("act", lambda e, j=j: e.activation(out=zn[:], in_=stg[j][:], func=AF.Copy, scale=ssO[:, 2:3]),
                             reads=[b_stg[j], b_ssO], writes=[b_zn])
                        for kc in range(KC):
                            S.op("pe", lambda e, kc=kc: e.transpose(out=tp_ps[:, kc, :], in_=zn[:, kc * 128:(kc + 1) * 128], identity=ident_bf[:]),
                                 reads=[b_zn, b_ident], writes=[b_tp])
                        S.op("dve", lambda e, j=j: e.tensor_copy(out=zT[:, :, j * 128:(j + 1) * 128], in_=tp_ps[:, :, :]), reads=[b_tp], writes=[b_zT])
                    for ffc in range(NFF):
                        gi_ = 2 + 2 * (ffc % 2)
                        ui_ = 3 + 2 * (ffc % 2)
                        fc = slice(ffc * 128, (ffc + 1) * 128)
                        for kc in range(KC):
                            S.op("pe", lambda e, kc=kc, fc=fc, gi_=gi_: e.matmul(out=PB[gi_][:, 0:UT], lhsT=wg_bf[:, kc, fc], rhs=zT[:, kc, :],
                                                                                 start=(kc == 0), stop=(kc == KC - 1)),
                                 reads=[b_wg[kc], b_zT], writes=[b_PB[gi_]])
                        for kc in range(KC):
                            S.op("pe", lambda e, kc=kc, fc=fc, ui_=ui_: e.matmul(out=PB[ui_][:, 0:UT], lhsT=wu_bf[:, kc, fc], rhs=zT[:, kc, :],
                                                                                 start=(kc == 0), stop=(kc == KC - 1)),
                                 reads=[b_wu[kc], b_zT], writes=[b_PB[ui_]])
                        si_ = ffc % 2
                        S.op("act", lambda e, gi_=gi_, si_=si_: e.activation(out=sgt[si_][:], in_=PB[gi_][:, 0:UT], func=AF.Silu),
                             reads=[b_PB[gi_]], writes=[b_sgt[si_]])
                        S.op("dve", lambda e, ui_=ui_, si_=si_, ffc=ffc: e.tensor_tensor(out=aT[:, ffc, :], in0=PB[ui_][:, 0:UT], in1=sgt[si_][:], op=ALU.mult),
                             reads=[b_PB[ui_], b_sgt[si_]], writes=[b_aT])
                    for j in range(2):
                        tj = t0 + j * 128
                        for half in range(2):
                            for ffc in range(NFF):
                                S.op("pe", lambda e, ffc=ffc, half=half, j=j: e.matmul(
                                    out=PB[half][:], lhsT=aT[:, ffc, j * 128:(j + 1) * 128], rhs=wd_bf[:, ffc, half * 512:(half + 1) * 512],
                                    start=(ffc == 0), stop=(ffc == NFF - 1)), reads=[b_aT, b_wd[ffc]], writes=[b_PB[half]])
                        rstd_from([PB[0][:], PB[1][:]], [b_PB[0], b_PB[1]])
                        for half in range(2):
                            S.op("act", lambda e, half=half: e.activation(out=t1[:, half * 512:(half + 1) * 512], in_=PB[half][:], func=AF.Copy,
                                                                          scale=ssO[:, 2:3]), reads=[b_PB[half], b_ssO], writes=[b_t1])
                        S.op("pool", lambda e: e.tensor_tensor(out=t1[:], in0=t1[:], in1=gfpost_bc[:], op=ALU.mult), reads=[b_t1, b_gbc], writes=[b_t1])
                        S.op("dve", lambda e, j=j: e.tensor_tensor(out=t1[:], in0=t1[:], in1=stg[j][:], op=ALU.add),
                             reads=[b_stg[j], b_t1], writes=[b_t1])
                        S.dma("sp", out[tj:tj + 128, :], t1[:], reads=[b_t1])

        S.wait_tokens("sp", [t for e in S.ENGS for t in S.dtoks[e]])
        S.emit()
    return nc


WNAMES = ["attn_norm_pre", "attn_norm_post", "w_in", "fox_forget_bias", "shift_mu", "rwkv_w0", "rwkv_w_up", "rwkv_a0",
          "rwkv_a_up", "rwkv_g_up", "rwkv_k_k", "rwkv_k_a", "rwkv_r_k", "rwkv_ln_w", "rwkv_ln_b", "w_out",
          "ffn_norm_pre", "ffn_norm_post", "ffn_w_gate", "ffn_w_up", "ffn_w_down"]


def make_in_map(inputs, b, T):
    m = {"x": np.ascontiguousarray(np.asarray(inputs["x"], dtype=np.float32)[b, :T])}
    for k in WNAMES:
        a = np.asarray(inputs[k], dtype=np.float32)[0]
        if k == "rwkv_r_k":
            a = a.reshape(-1)
        m[k] = np.ascontiguousarray(a)
    m["cmask"] = make_cmask()
    return m


def kernel(**inputs):
    x = np.asarray(inputs["x"])
    B, T, _ = x.shape
    nc = build_nc(T)
    in_maps = [make_in_map(inputs, b, T) for b in range(B)]
    res = run_bass_kernel_spmd(nc, in_maps, core_ids=list(range(B)))
    return np.stack([np.asarray(r["out"], dtype=np.float32) for r in res.results], axis=0)
```
